# Optimizing a Trainium2 kernel written in Bass

```python
import math
import jax
import jax.numpy as jnp
from jax import lax
import numpy as np

D_MODEL = 1024
BATCH = 4
SEQ = 8192
DEPTH = 1

GRID_W = 64
CTX_LEN = 256
EPS = 1e-6

SSD_WIDTH = 512
SSD_HEADDIM = 64
SSD_HEADS = 8
SSD_GROUPS = 2
SSD_STATE = 128
SSD_CONV = 5
SSD_CHUNK = 128
SSD_CONV_CH = SSD_WIDTH + 2 * SSD_GROUPS * SSD_STATE

HGRN_WIDTH = 512
HGRN_HEADS = 4
HGRN_HEADDIM = 128
HGRN_CHUNK = 64

N_EXPERTS = 16
EXPERT_FF = 1024
CAPACITY_FACTOR = 2

COL_SIZES = (SSD_WIDTH, SSD_CONV_CH, 2 * SSD_HEADS, HGRN_WIDTH, HGRN_WIDTH, HGRN_WIDTH, HGRN_WIDTH, HGRN_WIDTH)
IN_COLS = SSD_WIDTH + SSD_CONV_CH + 2 * SSD_HEADS + 5 * HGRN_WIDTH

kernel_name = 'hymba_ssd_hgrn2_ecmoe_prefix_dit_layer'


def rmsnorm(x, w):
    xf = x.astype(jnp.float32)
    y = xf * lax.rsqrt(jnp.mean(xf * xf, axis=-1, keepdims=True) + EPS)
    return (y * w.astype(jnp.float32)).astype(x.dtype)


def modulate(h, shift, scale):
    return h * (1 + scale) + shift


def flip_seq(t):
    return jnp.flip(t, axis=1)


def split_cols(u):
    points, acc = [], 0
    for s in COL_SIZES[:-1]:
        acc += s
        points.append(acc)
    return jnp.split(u, points, axis=-1)


def dwconv_centred(u, w, b):
    pad = w.shape[0] // 2
    y = lax.conv_general_dilated(u, w[:, None, :], (1,), [(pad, pad)],
                                 dimension_numbers=('NWC', 'WIO', 'NWC'),
                                 feature_group_count=u.shape[-1])
    return y + b


def latent_conv(u, w, b):
    bsz, n, ch = u.shape
    rows = n // GRID_W
    return dwconv_centred(u.reshape(bsz * rows, GRID_W, ch), w, b).reshape(bsz, n, ch)


def ssd_scan(xdt, log_a, bmat, cmat, s0, return_y=True):
    f32 = jnp.float32
    b, l, h, p = xdt.shape
    g, n = bmat.shape[2], bmat.shape[3]
    hg = h // g
    q = SSD_CHUNK
    c = l // q
    xc = xdt.reshape(b, c, q, g, hg, p).astype(f32)
    ac = log_a.reshape(b, c, q, g, hg).astype(f32)
    bc = bmat.reshape(b, c, q, g, n).astype(f32)
    cc = cmat.reshape(b, c, q, g, n).astype(f32)
    a_cs = jnp.cumsum(ac, axis=2)
    a_tot = a_cs[:, :, -1]
    decay_end = jnp.exp(a_tot[:, :, None] - a_cs)
    states = jnp.einsum('bcsgn,bcsgj,bcsgjp->bcgjpn', bc, decay_end, xc)

    def step(s, inp):
        st, at = inp
        return jnp.exp(at)[..., None, None] * s + st, s

    s_final, s_in = lax.scan(step, s0.reshape(b, g, hg, p, n).astype(f32),
                             (jnp.moveaxis(states, 1, 0), jnp.moveaxis(a_tot, 1, 0)))
    s_final = s_final.reshape(b, h, p, n)
    if not return_y:
        return None, s_final
    s_in = jnp.moveaxis(s_in, 0, 1)
    seg = a_cs[:, :, :, None] - a_cs[:, :, None, :]
    mask = jnp.tril(jnp.ones((q, q), bool))[:, :, None, None]
    lmat = jnp.exp(jnp.where(mask, seg, -jnp.inf))
    cb = jnp.einsum('bclgn,bcsgn->bclsg', cc, bc)
    y_diag = jnp.einsum('bclsg,bclsgj,bcsgjp->bclgjp', cb, lmat, xc)
    y_off = jnp.einsum('bclgn,bcgjpn,bclgj->bclgjp', cc, s_in, jnp.exp(a_cs))
    return (y_diag + y_off).reshape(b, l, h, p), s_final


def hgrn2_scan(q, f, v, s0, return_y=True):
    f32 = jnp.float32
    b, l, h, dk = q.shape
    dv = v.shape[-1]
    cq = HGRN_CHUNK
    c = l // cq
    qc = q.reshape(b, c, cq, h, dk).astype(f32)
    fc = f.reshape(b, c, cq, h, dk).astype(f32)
    vc = v.reshape(b, c, cq, h, dv).astype(f32)
    kc = 1 - fc
    bcs = jnp.cumsum(jnp.log(fc), axis=2)
    btot = bcs[:, :, -1]
    states = jnp.einsum('bcshk,bcshv->bchkv', kc * jnp.exp(btot[:, :, None] - bcs), vc)

    def step(s, inp):
        st, dt = inp
        return jnp.exp(dt)[..., None] * s + st, s

    s_final, s_in = lax.scan(step, s0.astype(f32),
                             (jnp.moveaxis(states, 1, 0), jnp.moveaxis(btot, 1, 0)))
    if not return_y:
        return None, s_final
    s_in = jnp.moveaxis(s_in, 0, 1)
    q_dec = qc * jnp.exp(bcs)
    k_inv = kc * jnp.exp(-bcs)
    att = jnp.einsum('bclhk,bcshk->bchls', q_dec, k_inv)
    att = jnp.where(jnp.tril(jnp.ones((cq, cq), bool)), att, 0.0)
    o = jnp.einsum('bchls,bcshv->bclhv', att, vc) + jnp.einsum('bclhk,bchkv->bclhv', q_dec, s_in)
    return o.reshape(b, l, h, dv), s_final


def token_mixer(u_lat, u_ctx, conv_w, conv_b, dt_bias, a_log, d_skip, ssd_nw, lb, hgrn_nw, ctx_out):
    f32 = jnp.float32
    z_l, xbc_l, dt_l, q_l, ff_l, fb_l, i_l, g_l = split_cols(u_lat)
    z_c, xbc_c, dt_c, q_c, ff_c, fb_c, i_c, g_c = split_cols(u_ctx)
    bsz = u_lat.shape[0]
    a_neg = -jnp.exp(a_log.astype(f32))
    d32 = d_skip.astype(f32)

    def ssd_inputs(xbc, dt_raw):
        b, l = xbc.shape[:2]
        xs, bm, cm = jnp.split(jax.nn.silu(xbc.astype(f32)),
                               [SSD_WIDTH, SSD_WIDTH + SSD_GROUPS * SSD_STATE], axis=-1)
        xs = xs.reshape(b, l, SSD_HEADS, SSD_HEADDIM)
        bm = bm.reshape(b, l, SSD_GROUPS, SSD_STATE)
        cm = cm.reshape(b, l, SSD_GROUPS, SSD_STATE)
        dt = jax.nn.softplus(dt_raw.astype(f32).reshape(b, l, 2, SSD_HEADS) + dt_bias.astype(f32))
        xdt = xs[:, :, None] * dt[..., None]
        return xs, bm, cm, xdt, dt * a_neg

    def ssd_output(y_f, y_b, xs, z):
        b, l = xs.shape[:2]
        y = y_f + flip_seq(y_b) + d32[:, None] * xs
        y = y.reshape(b, l, SSD_WIDTH) * jax.nn.silu(z.astype(f32))
        y = rmsnorm(y.reshape(b, l, SSD_GROUPS, -1), ssd_nw.reshape(SSD_GROUPS, -1))
        return y.reshape(b, l, SSD_WIDTH)

    def hgrn_inputs(q_raw, f_raw_f, f_raw_b, i_raw):
        b, l = q_raw.shape[:2]
        shp = (b, l, HGRN_HEADS, HGRN_HEADDIM)
        qq = jax.nn.silu(q_raw.astype(f32)).reshape(shp)
        f_f = (lb[0] + (1 - lb[0]) * jax.nn.sigmoid(f_raw_f.astype(f32))).reshape(shp)
        f_b = (lb[1] + (1 - lb[1]) * jax.nn.sigmoid(f_raw_b.astype(f32))).reshape(shp)
        return qq, f_f, f_b, i_raw.astype(f32).reshape(shp)

    def hgrn_output(o_f, o_b, g):
        b, l = g.shape[:2]
        o = rmsnorm(o_f + flip_seq(o_b), hgrn_nw.reshape(HGRN_HEADS, HGRN_HEADDIM))
        return o.reshape(b, l, HGRN_WIDTH) * jax.nn.silu(g.astype(f32))

    xs_c, b_c, c_c, xdt_c, la_c = ssd_inputs(dwconv_centred(xbc_c, conv_w, conv_b), dt_c)
    xs_l, b_l, c_l, xdt_l, la_l = ssd_inputs(latent_conv(xbc_l, conv_w, conv_b), dt_l)
    s0 = jnp.zeros((bsz, SSD_HEADS, SSD_HEADDIM, SSD_STATE), f32)
    yc_f, sc_f = ssd_scan(xdt_c[:, :, 0], la_c[:, :, 0], b_c, c_c, s0, ctx_out)
    yc_b, sc_b = ssd_scan(flip_seq(xdt_c[:, :, 1]), flip_seq(la_c[:, :, 1]),
                          flip_seq(b_c), flip_seq(c_c), s0, ctx_out)
    yl_f, _ = ssd_scan(xdt_l[:, :, 0], la_l[:, :, 0], b_l, c_l, sc_f)
    yl_b, _ = ssd_scan(flip_seq(xdt_l[:, :, 1]), flip_seq(la_l[:, :, 1]),
                       flip_seq(b_l), flip_seq(c_l), sc_b)
    ssd_lat = ssd_output(yl_f, yl_b, xs_l, z_l)

    qh_c, f_fc, f_bc, v_c = hgrn_inputs(q_c, ff_c, fb_c, i_c)
    qh_l, f_fl, f_bl, v_l = hgrn_inputs(q_l, ff_l, fb_l, i_l)
    r0 = jnp.zeros((bsz, HGRN_HEADS, HGRN_HEADDIM, HGRN_HEADDIM), f32)
    oc_f, rc_f = hgrn2_scan(qh_c, f_fc, v_c, r0, ctx_out)
    oc_b, rc_b = hgrn2_scan(flip_seq(qh_c), flip_seq(f_bc), flip_seq(v_c), r0, ctx_out)
    ol_f, _ = hgrn2_scan(qh_l, f_fl, v_l, rc_f)
    ol_b, _ = hgrn2_scan(flip_seq(qh_l), flip_seq(f_bl), flip_seq(v_l), rc_b)
    hgrn_lat = hgrn_output(ol_f, ol_b, g_l)

    y_lat = jnp.concatenate([ssd_lat, hgrn_lat], axis=-1).astype(u_lat.dtype)
    y_ctx = None
    if ctx_out:
        y_ctx = jnp.concatenate([ssd_output(yc_f, yc_b, xs_c, z_c),
                                 hgrn_output(oc_f, oc_b, g_c)], axis=-1).astype(u_ctx.dtype)
    return y_lat, y_ctx


def expert_choice_moe(h, w_router, w_gate, w_up, w_down):
    n, d = h.shape[1], h.shape[2]
    cap = CAPACITY_FACTOR * n // N_EXPERTS
    aff = jax.nn.softmax(jnp.einsum('bnd,de->ben', h, w_router).astype(jnp.float32), axis=1)
    gate, idx = lax.top_k(aff, cap)
    xin = jax.vmap(lambda hb, ib: hb[ib])(h, idx)
    hg = jnp.einsum('becd,edf->becf', xin, w_gate)
    hu = jnp.einsum('becd,edf->becf', xin, w_up)
    y = jnp.einsum('becf,efd->becd', jax.nn.silu(hg) * hu, w_down)
    y = y * gate[..., None].astype(y.dtype)
    return jax.vmap(lambda yb, ib: jnp.zeros((n, d), y.dtype).at[ib.reshape(-1)].add(yb.reshape(-1, d)))(y, idx)


def setup_inputs(seed: int = 0) -> dict:
    key = jax.random.key(seed)
    ks = jax.random.split(key, 24)
    f32 = jnp.float32

    def nrm(k, shape, scale):
        return jax.random.normal(k, shape, f32) * scale

    dt0 = jnp.exp(jax.random.uniform(ks[10], (DEPTH, 2, SSD_HEADS), f32, math.log(1e-3), math.log(1e-1)))
    return {
        'x': nrm(ks[0], (BATCH, SEQ, D_MODEL), 1.0),
        'c': nrm(ks[1], (BATCH, D_MODEL), 1.0),
        'ctx': nrm(ks[2], (BATCH, CTX_LEN, D_MODEL), 1.0),
        'c_ctx': nrm(ks[3], (D_MODEL,), 1.0),
        'ada_w': nrm(ks[4], (DEPTH, D_MODEL, 6 * D_MODEL), 0.5 * D_MODEL ** -0.5),
        'ada_b': nrm(ks[5], (DEPTH, 6 * D_MODEL), 0.02),
        'norm_w': 1.0 + nrm(ks[6], (DEPTH, 4, D_MODEL), 0.02),
        'w_in': nrm(ks[7], (DEPTH, D_MODEL, IN_COLS), D_MODEL ** -0.5),
        'ssd_conv_w': nrm(ks[8], (DEPTH, SSD_CONV, SSD_CONV_CH), SSD_CONV ** -0.5),
        'ssd_conv_b': nrm(ks[9], (DEPTH, SSD_CONV_CH), 0.02),
        'ssd_dt_bias': dt0 + jnp.log(-jnp.expm1(-dt0)),
        'ssd_a_log': jnp.log(jax.random.uniform(ks[11], (DEPTH, 2, SSD_HEADS), f32, 1.0, 16.0)),
        'ssd_d': 1.0 + nrm(ks[12], (DEPTH, SSD_HEADS), 0.02),
        'ssd_norm_w': 1.0 + nrm(ks[13], (DEPTH, SSD_WIDTH), 0.02),
        'hgrn_lb': nrm(ks[14], (DEPTH + 1, 2, HGRN_WIDTH), 0.1),
        'hgrn_norm_w': 1.0 + nrm(ks[15], (DEPTH, HGRN_WIDTH), 0.02),
        'w_out': nrm(ks[16], (DEPTH, D_MODEL, D_MODEL), D_MODEL ** -0.5),
        'w_router': nrm(ks[17], (DEPTH, D_MODEL, N_EXPERTS), D_MODEL ** -0.5),
        'w_gate': nrm(ks[18], (DEPTH, N_EXPERTS, D_MODEL, EXPERT_FF), D_MODEL ** -0.5),
        'w_up': nrm(ks[19], (DEPTH, N_EXPERTS, D_MODEL, EXPERT_FF), D_MODEL ** -0.5),
        'w_down': nrm(ks[20], (DEPTH, N_EXPERTS, EXPERT_FF, D_MODEL), EXPERT_FF ** -0.5),
    }


def reference(x, c, ctx, c_ctx, ada_w, ada_b, norm_w, w_in, ssd_conv_w, ssd_conv_b, ssd_dt_bias,
              ssd_a_log, ssd_d, ssd_norm_w, hgrn_lb, hgrn_norm_w, w_out, w_router, w_gate, w_up, w_down):
    lower_bounds = jnp.cumsum(jax.nn.softmax(hgrn_lb.astype(jnp.float32), axis=0), axis=0)
    x_lat, x_ctx = x, ctx
    for layer in range(DEPTH):
        ctx_out = layer < DEPTH - 1
        nw = norm_w[layer]
        mod_lat = jnp.split((jax.nn.silu(c) @ ada_w[layer] + ada_b[layer])[:, None, :], 6, axis=-1)
        mod_ctx = jnp.split(jax.nn.silu(c_ctx) @ ada_w[layer] + ada_b[layer], 6, axis=-1)

        h_lat = modulate(rmsnorm(x_lat, nw[0]), mod_lat[0], mod_lat[1])
        h_ctx = modulate(rmsnorm(x_ctx, nw[0]), mod_ctx[0], mod_ctx[1])
        y_lat, y_ctx = token_mixer(h_lat @ w_in[layer], h_ctx @ w_in[layer], ssd_conv_w[layer],
                                   ssd_conv_b[layer], ssd_dt_bias[layer], ssd_a_log[layer], ssd_d[layer],
                                   ssd_norm_w[layer], lower_bounds[layer], hgrn_norm_w[layer], ctx_out)
        x_lat = x_lat + mod_lat[2] * rmsnorm(y_lat @ w_out[layer], nw[1])

        h_lat = modulate(rmsnorm(x_lat, nw[2]), mod_lat[3], mod_lat[4])
        moe_lat = expert_choice_moe(h_lat, w_router[layer], w_gate[layer], w_up[layer], w_down[layer])
        x_lat = x_lat + mod_lat[5] * rmsnorm(moe_lat, nw[3])

        if ctx_out:
            x_ctx = x_ctx + mod_ctx[2] * rmsnorm(y_ctx @ w_out[layer], nw[1])
            h_ctx = modulate(rmsnorm(x_ctx, nw[2]), mod_ctx[3], mod_ctx[4])
            moe_ctx = expert_choice_moe(h_ctx, w_router[layer], w_gate[layer], w_up[layer], w_down[layer])
            x_ctx = x_ctx + mod_ctx[5] * rmsnorm(moe_ctx, nw[3])
    return x_lat
```

```python
import contextlib
import os
import numpy as np
import concourse.bass as bass
import concourse.mybir as mybir
from concourse.bass_utils import run_bass_kernel_spmd

F32 = mybir.dt.float32
BF16 = mybir.dt.bfloat16
I32 = mybir.dt.int32
AF = mybir.ActivationFunctionType
ALU = mybir.AluOpType
AX = mybir.AxisListType

D = 1024
SEQ = 8192
CTX = 256
NT = SEQ // 128
NH = NT // 2
HALF = SEQ // 2
NE = 16
CAP = 1024
EPS = 1e-6
WCOLS = 3592
C_XBC, C_Q, C_F, C_I, C_DT, C_Z, C_G = 0, 1024, 1536, 2048, 2560, 2568, 3080
NCONST = 10
ROWW = 1028
TRASH = HALF


class Buf:
    __slots__ = ("name", "t", "last_w", "readers")

    def __init__(self, name, t=None):
        self.name = name
        self.t = t
        self.last_w = None
        self.readers = []

    def __getitem__(self, k):
        return self.t[k]


class Sched:
    COMPUTE = ("pe", "dve", "act", "pool")
    NDSEM = 6

    def __init__(self, nc, es, same_engine_sync=True):
        self.nc = nc
        self.es = es
        self.same_engine_sync = same_engine_sync
        self.prog = {k: [] for k in ("pe", "dve", "act", "pool", "sp")}
        self.ninst = {k: 0 for k in self.COMPUTE}
        self.waited_idx = {}
        self.milestones = {k: set() for k in self.COMPUTE}
        self.csem = {k: es.enter_context(nc.semaphore("cs_" + k)) for k in self.COMPUTE}
        self.dsem, self.dcnt, self.drot = {}, {}, {}
        for q in ("sp", "act", "pool"):
            self.dsem[q] = [es.enter_context(nc.semaphore(f"ds_{q}{j}")) for j in range(self.NDSEM)]
            self.dcnt[q] = [0] * self.NDSEM
            self.drot[q] = 0
        self.ccsem = es.enter_context(nc.semaphore("cc_sem"))
        self.cccnt = 0

    def buf(self, name, shape, dtype, psum=False, es=None):
        es = es or self.es
        name = "s_" + name
        if psum:
            t = es.enter_context(self.nc.psum_tensor(name, list(shape), dtype))
        else:
            t = es.enter_context(self.nc.sbuf_tensor(name, list(shape), dtype))
        return Buf(name, t)

    def _need(self, eng, tok):
        if tok is None:
            return
        semkey, v = tok
        key = (eng, semkey)
        if semkey[0] == "c":
            src = semkey[1]
            if src == eng and (eng == "pe" or not self.same_engine_sync):
                return
            if self.waited_idx.get(key, -1) >= v:
                return
            self.waited_idx[key] = v
            self.milestones[src].add(v)
            self.prog[eng].append(("cwait", src, v))
        elif semkey[0] == "x":
            if self.waited_idx.get(key, -1) >= v:
                return
            self.waited_idx[key] = v
            self.prog[eng].append(("xwait", None, v))
        else:
            if self.waited_idx.get(key, -1) >= v:
                return
            self.waited_idx[key] = v
            _, q, j = semkey
            self.prog[eng].append(("dwait", (q, j), v))

    def _deps(self, eng, r, w):
        for b in r:
            self._need(eng, b.last_w)
        for b in w:
            self._need(eng, b.last_w)
            for t in b.readers:
                self._need(eng, t)

    def _commit(self, tok, r, w):
        for b in w:
            b.last_w = tok
            b.readers = []
        for b in r:
            if b not in w:
                b.readers.append(tok)

    def op(self, eng, fn, r=(), w=()):
        self._deps(eng, r, w)
        idx = self.ninst[eng]
        self.ninst[eng] += 1
        self.prog[eng].append(("op", fn, idx))
        tok = (("c", eng), idx)
        self._commit(tok, r, w)
        return tok

    def dma(self, q, fn, r=(), w=()):
        self._deps(q, r, w)
        j = self.drot[q]
        self.drot[q] = (j + 1) % self.NDSEM
        semkey = ("d", q, j)
        prev = self.dcnt[q][j]
        if prev > 0:
            self._need(q, (semkey, prev))
        self.dcnt[q][j] += 16
        v = self.dcnt[q][j]
        self.prog[q].append(("dma", fn, (q, j)))
        tok = (semkey, v)
        self._commit(tok, r, w)
        return tok

    def coll(self, fn, r=(), w=()):
        q = "pool"
        self._deps(q, r, w)
        self.cccnt += 1
        v = self.cccnt
        self.prog[q].append(("coll", fn, v))
        tok = (("x", "cc"), v)
        self._commit(tok, r, w)
        return tok

    def barrier(self):
        for eng in ("pe", "dve", "act", "pool", "sp"):
            for x in self.COMPUTE:
                if x != eng and self.ninst[x] > 0:
                    self._need(eng, (("c", x), self.ninst[x] - 1))
            self.wait_all_dma(eng)

    def wait_all_dma(self, eng="sp"):
        for q in ("sp", "act", "pool"):
            for j in range(self.NDSEM):
                if self.dcnt[q][j]:
                    self._need(eng, (("d", q, j), self.dcnt[q][j]))
        if self.cccnt:
            self._need(eng, (("x", "cc"), self.cccnt))

    def emit(self):
        nc = self.nc
        rank = {}
        for e in self.COMPUTE:
            ms = sorted(self.milestones[e])
            rank[e] = {idx: i + 1 for i, idx in enumerate(ms)}
        sched = self

        def run(engname, e):
            for ent in sched.prog[engname]:
                kind = ent[0]
                if kind == "op":
                    ins = ent[1](e)
                    if ent[2] in rank[engname]:
                        ins.then_inc(sched.csem[engname], 1)
                elif kind == "cwait":
                    e.wait_ge(sched.csem[ent[1]], rank[ent[1]][ent[2]])
                elif kind == "dwait":
                    q, j = ent[1]
                    e.wait_ge(sched.dsem[q][j], ent[2])
                elif kind == "xwait":
                    e.wait_ge(sched.ccsem, ent[2])
                elif kind == "coll":
                    ent[1](e).then_inc(sched.ccsem, 1)
                elif kind == "dma":
                    q, j = ent[2]
                    ent[1](e).then_inc(sched.dsem[q][j], 16)

        with nc.Block() as block:
            @block.sync
            def _(e):
                run("sp", e)

            @block.scalar
            def _(e):
                run("act", e)

            @block.vector
            def _(e):
                run("dve", e)

            @block.gpsimd
            def _(e):
                run("pool", e)

            @block.tensor
            def _(e):
                run("pe", e)


class K:
    def __init__(self, nc, es):
        self.nc = nc
        self.S = Sched(nc, es)

    def MM(self, out, lhsT, rhs, start, stop, r, w):
        self.S.op("pe", lambda e: e.matmul(out, lhsT=lhsT, rhs=rhs, start=start, stop=stop), r=r, w=w)

    def TR(self, out, in_, ident, r, w):
        self.S.op("pe", lambda e: e.transpose(out=out, in_=in_, identity=ident), r=r, w=w)

    def ACT(self, out, in_, func, r, w, bias=None, scale=None, accum=None):
        kw = {}
        if bias is not None:
            kw["bias"] = bias
        if scale is not None:
            kw["scale"] = scale
        if accum is not None:
            kw["accum_out"] = accum
        self.S.op("act", lambda e: e.activation(out=out, in_=in_, func=func, **kw), r=r, w=w)

    def TT(self, eng, out, in0, in1, op, r, w):
        self.S.op(eng, lambda e: e.tensor_tensor(out=out, in0=in0, in1=in1, op=op), r=r, w=w)

    def TS(self, eng, out, in0, s1, s2, op0, op1, r, w, accum=None):
        if op1 is None:
            self.S.op(eng, lambda e: e.tensor_scalar(out=out, in0=in0, scalar1=s1, scalar2=None, op0=op0), r=r, w=w)
        elif accum is not None:
            self.S.op(eng, lambda e: e.tensor_scalar(out=out, in0=in0, scalar1=s1, scalar2=s2, op0=op0, op1=op1,
                                                     accum_out=accum), r=r, w=w)
        else:
            self.S.op(eng, lambda e: e.tensor_scalar(out=out, in0=in0, scalar1=s1, scalar2=s2, op0=op0, op1=op1),
                      r=r, w=w)

    def STT(self, eng, out, in0, scalar, in1, op0, op1, r, w):
        self.S.op(eng, lambda e: e.scalar_tensor_tensor(out=out, in0=in0, scalar=scalar, in1=in1, op0=op0, op1=op1),
                  r=r, w=w)

    def CP(self, eng, out, in_, r, w):
        if eng == "act":
            self.S.op("act", lambda e: e.copy(out=out, in_=in_), r=r, w=w)
        else:
            self.S.op(eng, lambda e: e.tensor_copy(out=out, in_=in_), r=r, w=w)

    def MEMSET(self, eng, ap, val, w):
        self.S.op(eng, lambda e: e.memset(ap, val), w=w)

    def DMA(self, q, out, in_, r, w):
        return self.S.dma(q, lambda e: e.dma_start(out=out, in_=in_), r=r, w=w)


def build_program(stage):
    nc = bass.Bass("TRN2", target_bir_lowering=False)
    dt_in = lambda name, shape, dt=F32: nc.dram_tensor(name, list(shape), dt, kind="ExternalInput").ap()
    x_d = dt_in("xs", [1024 if LITE else SEQ, D])
    ctx_d = dt_in("ctxs", [CTX, D])
    cvec_d = dt_in("cvec", [128, 8, 2])
    adaw_d = dt_in("ada_w", [8 if LITE else D, 6 * D])
    adabT_d = dt_in("ada_bT", [128, 16])
    adabrep_d = dt_in("ada_brep", [128, 4 * D])
    nw0T_d = dt_in("nw0T", [128, 8])
    nwrep_d = dt_in("nwrep", [128, 3, D])
    wmain_d = dt_in("wmain", [D, WCOLS])
    cw_d = dt_in("cw", [128, 8, 5])
    cb_d = dt_in("cb", [128, 8])
    sp8_d = dt_in("sp8", [128, 3, 8])
    lbrep_d = dt_in("lbrep", [128, 2, 512])
    snw_d = dt_in("snw", [128, 512])
    hnw_d = dt_in("hnw", [128, 512])
    wout_d = dt_in("w_out", [D, D])
    wr_d = dt_in("w_router", [D, NE])
    if stage >= 7:
        wg_d = dt_in("w_gate", [NE, D, D])
        wu_d = dt_in("w_up", [NE, D, D])
        wd_d = dt_in("w_down", [NE, D, D])
    consts_d = dt_in("consts", [128, NCONST, 128])
    pidx_d = dt_in("pidx", [128, 2 * NH], I32)
    out_d = nc.dram_tensor("out", [HALF, D], F32, kind="ExternalOutput").ap()
    dbg_d = None
    if stage < 9:
        dbg_d = nc.dram_tensor("dbg", [SEQ, D], F32, kind="ExternalOutput").ap()
    ybuf_d = nc.dram_tensor("ybuf", [HALF, D], BF16).ap()
    ysend_t = [nc.dram_tensor(f"ysend{c}", [1024, D], BF16) for c in range(4)]
    ygath_t = [nc.dram_tensor(f"ygath{c}", [2048, D], BF16) for c in range(4)]
    zgbuf_d = nc.dram_tensor("zgbuf", [HALF, D], BF16).ap()
    x1buf_d = nc.dram_tensor("x1buf", [HALF, D], F32).ap()
    h2buf_d = nc.dram_tensor("h2buf", [HALF, ROWW], BF16).ap()
    affs_t = nc.dram_tensor("affsend", [HALF, NE], F32)
    affg_t = nc.dram_tensor("affgath", [SEQ, NE], F32)
    xin_flat = nc.dram_tensor("xin", [NE * CAP + 128, ROWW], BF16).ap()
    xin_d = xin_flat[0:NE * CAP, :].rearrange("(e c) r -> e c r", e=NE)
    acc_d = nc.dram_tensor("moeacc", [HALF + 128, D], F32).ap()
    ybuf_b, ysend_b, ygath_b, zgbuf_b, x1buf_b, h2buf_b = (Buf(n) for n in ("ybuf", "ysend", "ygath", "zgbuf", "x1buf", "h2buf"))
    affs_b, affg_b, xin_b, acc_b = (Buf(n) for n in ("affs", "affg", "xin", "acc"))
    PAIRS = [[0, 1], [2, 3], [4, 5], [6, 7]]

    with contextlib.ExitStack() as es:
        k = K(nc, es)
        S = k.S
        P = [S.buf(f"P{i}", [128, 512], F32, psum=True) for i in range(8)]
        P0b = P[0].t[:, :].bitcast(BF16)

        cst = S.buf("cst", [128, NCONST, 128], F32)
        cstb = S.buf("cstb", [128, NCONST, 128], BF16)
        k.DMA("sp", cst[:, :, :], consts_d, r=[], w=[cst])
        k.CP("dve", cstb[:, :, :], cst[:, :, :], r=[cst], w=[cstb])
        ident, identb = cst[:, 0, :], cstb[:, 0, :]
        trib, negmb = cstb[:, 1, :], cstb[:, 2, :]
        mask01 = cst[:, 3, :]
        tri2i, tri2r, mask2 = cst[:, 4, :], cst[:, 5, :], cst[:, 6, :]
        onesb = cstb[:, 7, :]
        chunkind = cst[:, 9, 0:2]


        cvec = S.buf("cvec", [128, 8, 2], F32)
        k.DMA("sp", cvec[:, :, :], cvec_d, r=[], w=[cvec])
        adabT = S.buf("adabT", [128, 16], F32)
        k.DMA("sp", adabT[:, :], adabT_d, r=[], w=[adabT])
        nw0T = S.buf("nw0T", [128, 8], F32)
        k.DMA("sp", nw0T[:, :], nw0T_d, r=[], w=[nw0T])
        cw = S.buf("cw", [128, 8, 5], F32)
        k.DMA("sp", cw[:, :, :], cw_d, r=[], w=[cw])
        cb = S.buf("cb", [128, 8], F32)
        k.DMA("sp", cb[:, :], cb_d, r=[], w=[cb])
        sp8 = S.buf("sp8", [128, 3, 8], F32)
        k.DMA("sp", sp8[:, :, :], sp8_d, r=[], w=[sp8])
        lbrep = S.buf("lbrep", [128, 2, 512], F32)
        k.DMA("sp", lbrep[:, :, :], lbrep_d, r=[], w=[lbrep])

        sc = S.buf("sc", [128, 8, 2], F32)
        k.ACT(sc[:, :, :], cvec[:, :, :], AF.Silu, r=[cvec], w=[sc])
        modT = S.buf("modT", [128, 16, 2], F32)
        modrep = S.buf("modrep", [128, 4 * D], F32)
        with contextlib.ExitStack() as es0:
            adap = [S.buf(f"adap{i}", [128, 8, 512], F32, es=es0) for i in range(2)]
            adabrep = S.buf("adabrep", [128, 4 * D], F32, es=es0)
            nwrep = S.buf("nwrep", [128, 3, D], F32, es=es0)
            k.DMA("sp", adabrep[:, :], adabrep_d, r=[], w=[adabrep])
            k.DMA("sp", nwrep[:, :, :], nwrep_d, r=[], w=[nwrep])
            if LITE:
                k.MEMSET("pool", modT[:, :, :], 0.1, w=[modT])
                k.MEMSET("pool", modrep[:, :], 0.1, w=[modrep])
            for j in range(0 if LITE else 12):
                ap_ = adap[j % 2]
                k.DMA("sp", ap_[:, :, :], adaw_d[:, j * 512:(j + 1) * 512].rearrange("(k p) n -> p k n", p=128),
                      r=[], w=[ap_])
                if j < 4:
                    for m in range(4):
                        cc = j * 4 + m
                        for kc in range(8):
                            k.MM(P[6][:, 0:2], ap_[:, kc, m * 128:(m + 1) * 128], sc[:, kc, :], kc == 0, kc == 7,
                                 r=[ap_, sc], w=[P[6]])
                        k.TS("dve", modT[:, cc, :], P[6][:, 0:2], adabT[:, cc:cc + 1], None, ALU.add, None,
                             r=[P[6], adabT], w=[modT])
                else:
                    pb = P[j % 2 + 1]
                    for kc in range(8):
                        k.MM(pb[:, :], sc[:, kc, 0:1].to_broadcast([128, 128]), ap_[:, kc, :], kc == 0, kc == 7,
                             r=[ap_, sc], w=[pb])
                    o = (j - 4) * 512
                    k.TT("dve", modrep[:, o:o + 512], pb[:, :], adabrep[:, o:o + 512], ALU.add,
                         r=[pb, adabrep], w=[modrep])
            k.TT("dve", modrep[:, 0:D], modrep[:, 0:D], nwrep[:, 0, :], ALU.mult, r=[modrep, nwrep], w=[modrep])
            k.STT("dve", modrep[:, 2 * D:3 * D], modrep[:, 2 * D:3 * D], 1.0, nwrep[:, 1, :], ALU.add, ALU.mult,
                  r=[modrep, nwrep], w=[modrep])
            k.TT("dve", modrep[:, 3 * D:4 * D], modrep[:, 3 * D:4 * D], nwrep[:, 2, :], ALU.mult,
                 r=[modrep, nwrep], w=[modrep])
            S.barrier()
        G1, B2, A2, G2 = (modrep[:, i * D:(i + 1) * D] for i in range(4))
        A0 = S.buf("A0", [128, 2, 8], F32)
        B0 = S.buf("B0", [128, 2, 8], F32)
        for i in range(2):
            k.STT("dve", A0[:, i, :], modT[:, 8:16, i], 1.0, nw0T[:, :], ALU.add, ALU.mult, r=[modT, nw0T], w=[A0])
            k.CP("dve", B0[:, i, :], modT[:, 0:8, i], r=[modT], w=[B0])
        aneg = S.buf("aneg", [128, 8], F32)
        k.ACT(aneg[:, :], sp8[:, 1, :], AF.Exp, r=[sp8], w=[aneg])
        k.TS("dve", aneg[:, :], aneg[:, :], -1.0, None, ALU.mult, None, r=[aneg], w=[aneg])
        dsk = S.buf("dsk", [128, 8], F32)
        k.TS("dve", dsk[:, :], sp8[:, 2, :], 0.5, None, ALU.mult, None, r=[sp8], w=[dsk])
        Dm = S.buf("Dm", [128, 8, 128], BF16)
        for j in range(8):
            k.TS("dve", Dm[:, j, :], ident, dsk[:, j:j + 1], None, ALU.mult, None, r=[cst, dsk], w=[Dm])
        c01 = S.buf("c01", [128, 2, 512], F32)
        k.TT("dve", c01[:, 0, :], lbrep[:, 0, :], lbrep[:, 1, :], ALU.subtract, r=[lbrep], w=[c01])
        k.ACT(c01[:, 1, :], c01[:, 0, :], AF.Tanh, r=[c01], w=[c01], scale=0.5)
        k.TS("dve", c01[:, 0, :], c01[:, 1, :], 0.25, 0.75, ALU.mult, ALU.add, r=[c01], w=[c01])
        k.TS("dve", c01[:, 1, :], c01[:, 1, :], -0.25, 0.25, ALU.mult, ALU.add, r=[c01], w=[c01])
        ST = S.buf("ST", [128, 512], F32)
        STb = S.buf("STb", [128, 512], BF16)
        SH = S.buf("SH", [128, 512], F32)
        SHb = S.buf("SHb", [128, 512], BF16)
        k.MEMSET("pool", ST[:, :], 0.0, w=[ST])
        k.MEMSET("pool", STb[:, :], 0.0, w=[STb])
        k.MEMSET("pool", SH[:, :], 0.0, w=[SH])
        k.MEMSET("pool", SHb[:, :], 0.0, w=[SHb])

        with contextlib.ExitStack() as es2:
            def sb(name, shape, dtype):
                return S.buf(name, shape, dtype, es=es2)
            wm = sb("wm", [128, 8, WCOLS], BF16)
            for kc in range(8):
                for (c0, c1) in ((0, 1796), (1796, WCOLS)):
                    k.DMA("pool", wm[:, kc, c0:c1], wmain_d[kc * 128:(kc + 1) * 128, c0:c1], r=[], w=[wm])
            xt = [sb(f"xt{i}", [128, D], F32) for i in range(3)]
            junk = sb("junk", [128, D], BF16)
            ssq = sb("ssq", [128, 2], F32)
            xn = sb("xn", [128, D], BF16)
            hT = [sb(f"hT{i}", [128, 8, 256], BF16) for i in range(2)]
            cacc = sb("cacc", [128, 8, 256], F32)
            xcT = sb("xcT", [128, 8, 256], BF16)
            xs_tm = sb("xs_tm", [128, 512], BF16)
            B_tm = sb("B_tm", [128, 256], BF16)
            dts = sb("dts", [128, 8, 8], F32)
            eatot = sb("eatot", [128, 8], F32)
            a_hi = sb("a_hi", [128, 8], BF16)
            a_lo = sb("a_lo", [128, 8], BF16)
            xdt = sb("xdt", [128, 512], BF16)
            xdtd = sb("xdtd", [128, 512], BF16)
            LT = sb("LT", [128, 8, 128], BF16)
            MT = sb("MT", [128, 8, 128], BF16)
            CBm = sb("CBm", [128, 2, 128], BF16)
            yoff = sb("yoff", [128, 512], F32)
            yo = sb("yo", [128, 512], BF16)
            qs = sb("qs", [128, 512], F32)
            vb = sb("vb", [128, 512], BF16)
            vm = sb("vm", [128, 2, 512], BF16)
            ff = sb("ff", [128, 512], F32)
            lf = sb("lf", [128, 512], F32)
            kk = sb("kk", [128, 512], F32)
            et = [sb(f"et{i}", [128, 512], F32) for i in range(2)]
            qdec = sb("qdec", [128, 512], BF16)
            kinv = sb("kinv", [128, 512], BF16)
            kdec = sb("kdec", [128, 512], BF16)
            qdT = sb("qdT", [128, 4, 128], BF16)
            kiT = sb("kiT", [128, 4, 128], BF16)
            attm = sb("attm", [128, 4, 128], BF16)
            oc = sb("oc", [64, 2, 512], BF16)
            ebt = sb("ebt", [128, 4, 2], F32)
            zg = sb("zg", [128, D], BF16)

            def load_x(ti_all):
                b = xt[ti_all % 3]
                src = ctx_d[ti_all * 128:(ti_all + 1) * 128, :] if ti_all < 2 else \
                    x_d[(ti_all - 2) * 128:(ti_all - 1) * 128, :]
                k.DMA("sp", b[:, :], src, r=[], w=[b])

            def norm_T(ti_all, hbuf, col0, which):
                b = xt[ti_all % 3]
                k.MEMSET("pool", ssq[:, 0:1], 0.0, w=[ssq])
                k.ACT(junk[:, :], b[:, :], AF.Square, r=[b], w=[junk, ssq], accum=ssq[:, 0:1])
                k.ACT(ssq[:, 1:2], ssq[:, 0:1], AF.Ln, r=[ssq], w=[ssq], bias=EPS, scale=1.0 / D)
                k.ACT(ssq[:, 1:2], ssq[:, 1:2], AF.Exp, r=[ssq], w=[ssq], scale=-0.5)
                k.TS("pool", xn[:, :], b[:, :], ssq[:, 1:2], None, ALU.mult, None, r=[b, ssq], w=[xn])
                for kc in range(8):
                    k.TR(P0b[:, kc * 128:(kc + 1) * 128], xn[:, kc * 128:(kc + 1) * 128], identb,
                         r=[xn, cstb], w=[P[0]])
                for kc in range(8):
                    o = hbuf[:, kc, col0:col0 + 128]
                    i_ = P0b[:, kc * 128:(kc + 1) * 128]
                    if kc % 2 == 0:
                        k.ACT(o, i_, AF.Identity, r=[P[0], A0, B0], w=[hbuf],
                              bias=B0[:, which, kc:kc + 1], scale=A0[:, which, kc:kc + 1])
                    else:
                        k.TS("dve", o, i_, A0[:, which, kc:kc + 1], B0[:, which, kc:kc + 1], ALU.mult, ALU.add,
                             r=[P[0], A0, B0], w=[hbuf])

            def conv_stage(hbuf, T, roww, chunks):
                per_bank = 512 // T
                for ci, c in enumerate(chunks):
                    pb = P[1 + ci // per_bank]
                    po = (ci % per_bank) * T
                    for kc in range(8):
                        k.MM(pb[:, po:po + T], wm[:, kc, C_XBC + c * 128:C_XBC + (c + 1) * 128], hbuf[:, kc, 0:T],
                             kc == 0, kc == 7, r=[wm, hbuf], w=[pb])
                for ci, c in enumerate(chunks):
                    pb = P[1 + ci // per_bank]
                    po = (ci % per_bank) * T
                    src = pb[:, po:po + T]
                    acc = cacc[:, c, 0:T]
                    k.ACT(acc, src, AF.Identity, r=[pb, cw, cb], w=[cacc], bias=cb[:, c:c + 1], scale=cw[:, c, 2:3])
                    srcv = src.rearrange("p (r w) -> p r w", w=roww)
                    accv = acc.rearrange("p (r w) -> p r w", w=roww)
                    for kt in (0, 1, 3, 4):
                        s = kt - 2
                        if s > 0:
                            o_, i_ = accv[:, :, 0:roww - s], srcv[:, :, s:roww]
                        else:
                            o_, i_ = accv[:, :, -s:roww], srcv[:, :, 0:roww + s]
                        k.STT("dve", o_, i_, cw[:, c, kt:kt + 1], o_, ALU.mult, ALU.add, r=[pb, cw, cacc], w=[cacc])
                    k.ACT(xcT[:, c, 0:T], acc, AF.Silu, r=[cacc], w=[xcT])

            def scan_tile(hbuf, col0, tcol, lat, ti, zgproj):
                hsl = lambda kc: hbuf[:, kc, col0:col0 + 128]
                if zgproj:
                    for (pb, c0) in ((P[1], C_Z), (P[2], C_G)):
                        for kc in range(8):
                            k.MM(pb[:, :], hsl(kc), wm[:, kc, c0:c0 + 512], kc == 0, kc == 7, r=[hbuf, wm], w=[pb])
                    k.ACT(zg[:, 0:512], P[1][:, :], AF.Silu, r=[P[1]], w=[zg])
                    k.ACT(zg[:, 512:1024], P[2][:, :], AF.Silu, r=[P[2]], w=[zg])
                    k.DMA("sp", zgbuf_d[ti * 128:(ti + 1) * 128, :], zg[:, :], r=[zg], w=[zgbuf_b])
                for (pb, c0) in ((P[3], C_Q), (P[4], C_F), (P[5], C_I)):
                    for kc in range(8):
                        k.MM(pb[:, :], hsl(kc), wm[:, kc, c0:c0 + 512], kc == 0, kc == 7, r=[hbuf, wm], w=[pb])
                for kc in range(8):
                    k.MM(P[6][:, 0:8], hsl(kc), wm[:, kc, C_DT:C_DT + 8], kc == 0, kc == 7, r=[hbuf, wm], w=[P[6]])
                if lat:
                    k.ACT(qs[:, :], P[3][:, :], AF.Silu, r=[P[3]], w=[qs])
                k.ACT(ff[:, :], P[4][:, :], AF.Tanh, r=[P[4]], w=[ff], scale=0.5)
                k.CP("dve", vb[:, :], P[5][:, :], r=[P[5]], w=[vb])
                for c in range(2):
                    k.TS("pool", vm[:, c, :], vb[:, :], chunkind[:, c:c + 1], None, ALU.mult, None, r=[vb, cst], w=[vm])
                if SUBCUT == 'A':
                    return
                for c in range(6):
                    k.TR(P0b[:, c * 128:(c + 1) * 128], xcT[:, c, tcol:tcol + 128], identb, r=[xcT, cstb], w=[P[0]])
                if SUBCUT == 'B1':
                    return
                k.CP("act", xs_tm[:, :], P0b[:, 0:512], r=[P[0]], w=[xs_tm])
                if SUBCUT == 'B2':
                    return
                k.CP("act", B_tm[:, :], P0b[:, 512:768], r=[P[0]], w=[B_tm])
                if SUBCUT == 'B':
                    return
                v_, av_, l_, dt_, a_, nacs, eacs, w2 = (dts[:, i, :] for i in range(8))
                k.TT("dve", v_, P[6][:, 0:8], sp8[:, 0, :], ALU.add, r=[P[6], sp8], w=[dts])
                k.TS("dve", av_, v_, 30.0, None, ALU.min, None, r=[dts], w=[dts])
                k.ACT(av_, av_, AF.Exp, r=[dts], w=[dts])
                k.ACT(l_, av_, AF.Ln, r=[dts], w=[dts], bias=1.0)
                k.TT("dve", dt_, l_, v_, ALU.max, r=[dts], w=[dts])
                k.TT("dve", a_, dt_, aneg[:, :], ALU.mult, r=[dts, aneg], w=[dts])
                k.CP("dve", a_hi[:, :], a_, r=[dts], w=[a_hi])
                k.TT("dve", a_lo[:, :], a_, a_hi[:, :], ALU.subtract, r=[dts, a_hi], w=[a_lo])
                if SUBCUT == 'C1':
                    return
                k.MM(P[6][:, 64:72], trib, a_hi[:, :], True, False, r=[cstb, a_hi], w=[P[6]])
                k.MM(P[6][:, 64:72], trib, a_lo[:, :], False, True, r=[cstb, a_lo], w=[P[6]])
                k.MM(P[6][:, 128:136], onesb, a_hi[:, :], True, False, r=[cstb, a_hi], w=[P[6]])
                k.MM(P[6][:, 128:136], onesb, a_lo[:, :], False, True, r=[cstb, a_lo], w=[P[6]])
                k.TS("dve", nacs, P[6][:, 64:72], -1.0, None, ALU.mult, None, r=[P[6]], w=[dts])
                k.ACT(eacs, P[6][:, 64:72], AF.Exp, r=[P[6]], w=[dts])
                k.TT("dve", w2, P[6][:, 128:136], nacs, ALU.add, r=[P[6], dts], w=[dts])
                k.ACT(w2, w2, AF.Exp, r=[dts], w=[dts])
                k.ACT(eatot[:, :], P[6][:, 128:136], AF.Exp, r=[P[6]], w=[eatot])
                k.TT("dve", w2, w2, dt_, ALU.mult, r=[dts], w=[dts])
                if SUBCUT == 'C2':
                    return
                xs3 = xs_tm[:, :].rearrange("p (j q) -> p j q", q=64)
                k.TT("dve", xdtd[:, :].rearrange("p (j q) -> p j q", q=64), xs3, w2.unsqueeze(2).to_broadcast([128, 8, 64]), ALU.mult,
                     r=[xs_tm, dts], w=[xdtd])
                if lat:
                    k.TT("pool", xdt[:, :].rearrange("p (j q) -> p j q", q=64), xs3, dt_.unsqueeze(2).to_broadcast([128, 8, 64]), ALU.mult,
                         r=[xs_tm, dts], w=[xdt])
                    for j in range(8):
                        pa = P[1 + j // 4]
                        o = pa[:, (j % 4) * 128:(j % 4 + 1) * 128]
                        k.MM(o, a_hi[:, j:j + 1].to_broadcast([128, 128]), trib, True, False, r=[a_hi, cstb], w=[pa])
                        k.MM(o, a_lo[:, j:j + 1].to_broadcast([128, 128]), trib, False, False, r=[a_lo, cstb], w=[pa])
                        k.MM(o, identb, negmb, False, True, r=[cstb], w=[pa])
                        k.ACT(LT[:, j, :], o, AF.Exp, r=[pa, dts], w=[LT], bias=nacs[:, j:j + 1])
                    for g in range(2):
                        k.MM(P[6][:, 128 + g * 128:256 + g * 128], xcT[:, 4 + g, tcol:tcol + 128],
                             xcT[:, 6 + g, tcol:tcol + 128], True, True, r=[xcT], w=[P[6]])
                    k.TT("dve", CBm[:, :, :], P[6][:, 128:384].rearrange("p (g l) -> p g l", g=2),
                         mask01.unsqueeze(1).to_broadcast([128, 2, 128]), ALU.mult, r=[P[6], cst], w=[CBm])
                    for g in range(2):
                        k.TT("pool", MT[:, 4 * g:4 * g + 4, :], LT[:, 4 * g:4 * g + 4, :],
                             CBm[:, g:g + 1, :].to_broadcast([128, 4, 128]), ALU.mult, r=[LT, CBm], w=[MT])
                    for j in range(8):
                        o = P[3][:, j * 64:(j + 1) * 64]
                        k.MM(o, MT[:, j, :], xdt[:, j * 64:(j + 1) * 64], True, False, r=[MT, xdt], w=[P[3]])
                        k.MM(o, Dm[:, j, :], xs_tm[:, j * 64:(j + 1) * 64], False, True, r=[Dm, xs_tm], w=[P[3]])
                    for g in range(2):
                        k.MM(P[7][:, g * 256:(g + 1) * 256], xcT[:, 6 + g, tcol:tcol + 128],
                             STb[:, g * 256:(g + 1) * 256], True, True, r=[xcT, STb], w=[P[7]])
                    k.TT("dve", yoff[:, :].rearrange("p (j q) -> p j q", q=64),
                         P[7][:, :].rearrange("p (j q) -> p j q", q=64),
                         eacs.unsqueeze(2).to_broadcast([128, 8, 64]), ALU.mult, r=[P[7], dts], w=[yoff])
                    k.TT("dve", yo[:, :], P[3][:, :], yoff[:, :], ALU.add, r=[P[3], yoff], w=[yo])
                if SUBCUT == 'C':
                    return
                for g in range(2):
                    k.MM(P[7][:, g * 256:(g + 1) * 256], B_tm[:, g * 128:(g + 1) * 128],
                         xdtd[:, g * 256:(g + 1) * 256], True, True, r=[B_tm, xdtd], w=[P[7]])
                k.TT("dve", ST[:, :].rearrange("p (j q) -> p j q", q=64), ST[:, :].rearrange("p (j q) -> p j q", q=64),
                     eatot[:, :].unsqueeze(2).to_broadcast([128, 8, 64]), ALU.mult, r=[ST, eatot], w=[ST])
                k.TT("dve", ST[:, :], ST[:, :], P[7][:, :], ALU.add, r=[ST, P[7]], w=[ST])
                k.CP("act", STb[:, :], ST[:, :], r=[ST], w=[STb])
                if SUBCUT == 'D':
                    return
                k.TT("dve", ff[:, :], ff[:, :], c01[:, 1, :], ALU.mult, r=[ff, c01], w=[ff])
                k.TT("dve", ff[:, :], ff[:, :], c01[:, 0, :], ALU.add, r=[ff, c01], w=[ff])
                k.ACT(lf[:, :], ff[:, :], AF.Ln, r=[ff], w=[lf])
                k.TS("pool", kk[:, :], ff[:, :], -1.0, 1.0, ALU.mult, ALU.add, r=[ff], w=[kk])
                k.MM(P[4][:, :], tri2i, lf[:, :], True, True, r=[cst, lf], w=[P[4]])
                k.MM(P[5][:, :], tri2r, lf[:, :], True, True, r=[cst, lf], w=[P[5]])
                for h in range(4):
                    k.MM(P[7][:, h * 64:h * 64 + 2], lf[:, h * 128:(h + 1) * 128], chunkind, True, True,
                         r=[lf, cst], w=[P[7]])
                k.ACT(ebt[:, :, :], P[7][:, 0:256].rearrange("p (h c) -> p h c", c=64)[:, :, 0:2], AF.Exp,
                      r=[P[7]], w=[ebt])
                k.ACT(et[0][:, :], P[5][:, :], AF.Exp, r=[P[5]], w=[et[0]])
                k.TT("dve", kdec[:, :], kk[:, :], et[0][:, :], ALU.mult, r=[kk, et[0]], w=[kdec])
                if lat:
                    k.ACT(et[1][:, :], P[4][:, :], AF.Exp, r=[P[4]], w=[et[1]])
                    k.TT("dve", qdec[:, :], qs[:, :], et[1][:, :], ALU.mult, r=[qs, et[1]], w=[qdec])
                    k.ACT(et[0][:, :], P[4][:, :], AF.Exp, r=[P[4]], w=[et[0]], scale=-1.0)
                    k.TT("pool", kinv[:, :], kk[:, :], et[0][:, :], ALU.mult, r=[kk, et[0]], w=[kinv])
                    for h in range(4):
                        k.TR(P0b[:, h * 128:(h + 1) * 128], qdec[:, h * 128:(h + 1) * 128], identb,
                             r=[qdec, cstb], w=[P[0]])
                        k.TR(P0b[:, 512 + h * 128:512 + (h + 1) * 128], kinv[:, h * 128:(h + 1) * 128], identb,
                             r=[kinv, cstb], w=[P[0]])
                    k.CP("act", qdT[:, :, :], P0b[:, 0:512].rearrange("p (h l) -> p h l", h=4), r=[P[0]], w=[qdT])
                    k.CP("act", kiT[:, :, :], P0b[:, 512:1024].rearrange("p (h l) -> p h l", h=4), r=[P[0]], w=[kiT])
                    for h in range(4):
                        k.MM(P[4][:, h * 128:(h + 1) * 128], kiT[:, h, :], qdT[:, h, :], True, True,
                             r=[kiT, qdT], w=[P[4]])
                    k.TT("dve", attm[:, :, :], P[4][:, :].rearrange("p (h l) -> p h l", h=4),
                         mask2.unsqueeze(1).to_broadcast([128, 4, 128]), ALU.mult, r=[P[4], cst], w=[attm])
                if SUBCUT == 'E':
                    return
                for c in range(2):
                    if lat:
                        for h in range(4):
                            o = P[5][0:64, h * 128:(h + 1) * 128]
                            k.MM(o, attm[:, h, c * 64:(c + 1) * 64], vb[:, h * 128:(h + 1) * 128], True, False,
                                 r=[attm, vb], w=[P[5]])
                            k.MM(o, qdT[:, h, c * 64:(c + 1) * 64], SHb[:, h * 128:(h + 1) * 128], False, True,
                                 r=[qdT, SHb], w=[P[5]])
                        k.CP("act", oc[:, c, :], P[5][0:64, :], r=[P[5]], w=[oc])
                    for h in range(4):
                        k.MM(P[7][:, h * 128:(h + 1) * 128], kdec[:, h * 128:(h + 1) * 128],
                             vm[:, c, h * 128:(h + 1) * 128], True, True, r=[kdec, vm], w=[P[7]])
                    for h in range(4):
                        sl = slice(h * 128, (h + 1) * 128)
                        k.STT("dve", SH[:, sl], SH[:, sl], ebt[:, h, c:c + 1], P[7][:, sl], ALU.mult, ALU.add,
                              r=[SH, ebt, P[7]], w=[SH])
                    k.CP("act", SHb[:, :], SH[:, :], r=[SH], w=[SHb])
                if lat:
                    if ti < NH:
                        dst, dstb, rows = ybuf_d, ybuf_b, slice(ti * 128, (ti + 1) * 128)
                    else:
                        dst, dstb = ysend_t[(ti - NH) // 8].ap(), ysend_b
                        rows = slice(((ti - NH) % 8) * 128, ((ti - NH) % 8 + 1) * 128)
                    k.DMA("sp", dst[rows, 0:512], yo[:, :], r=[yo], w=[dstb])
                    k.DMA("sp", dst[rows, 512:1024].rearrange("(c p) v -> p c v", p=64), oc[:, :, :],
                          r=[oc], w=[dstb])

            cut = int(os.environ.get("KCUT", "99"))
            load_x(0)
            load_x(1)
            load_x(2)
            if cut >= 1:
                norm_T(0, hT[0], 0, 1)
                norm_T(1, hT[0], 128, 1)
            if cut >= 2:
                conv_stage(hT[0], 256, 256, [0, 1, 2, 3])
                conv_stage(hT[0], 256, 256, [4, 5, 6, 7])
            if cut >= 3:
                for sub in range(2):
                    scan_tile(hT[0], sub * 128, sub * 128, False, -1, False)
            ntl = NT if stage >= 2 else 4
            ntl = int(os.environ.get("KNT", ntl))
            if cut < 4:
                ntl = 0
            for ti in range(ntl):
                if ti + 1 < ntl:
                    load_x(ti + 3)
                hb = hT[(ti + 1) % 2]
                norm_T(ti + 2, hb, 0, 0)
                conv_stage(hb, 128, 64, list(range(8)))
                scan_tile(hb, 0, 0, True, ti, ti < NH)

            S.barrier()
        if stage < 3:
            with contextlib.ExitStack() as esd:
                tb = S.buf("dbg_b", [128, D], BF16, es=esd)
                tf = S.buf("dbg_f", [128, D], F32, es=esd)
                for ti in range(min(ntl, NH)):
                    rows = slice(ti * 128, (ti + 1) * 128)
                    k.DMA("sp", tb[:, :], ybuf_d[rows, :], r=[ybuf_b], w=[tb])
                    k.CP("dve", tf[:, :], tb[:, :], r=[tb], w=[tf])
                    k.DMA("sp", dbg_d[rows, :], tf[:, :], r=[tf], w=[])
        else:
            for c in range(4):
                if SIM:
                    for hh in range(2):
                        k.DMA("sp", ygath_t[c].ap()[hh * 1024:(hh + 1) * 1024, :], ysend_t[c].ap(), r=[ysend_b], w=[ygath_b])
                else:
                    S.coll((lambda c: lambda e: e.collective_compute(
                        "AllGather", ALU.bypass, replica_groups=PAIRS, ins=[ysend_t[c].ap().opt()],
                        outs=[ygath_t[c].ap().opt()]))(c), r=[ysend_b], w=[ygath_b])
            affall = S.buf("affall", [128, NH, NE], F32)
            pidx = S.buf("pidx", [128, 2 * NH], I32)
            k.DMA("sp", pidx[:, :], pidx_d, r=[], w=[pidx])
            with contextlib.ExitStack() as es4:
                def sb(name, shape, dtype):
                    return S.buf(name, shape, dtype, es=es4)
                wout = sb("wout", [128, 8, D], BF16)
                for kc in range(8):
                    k.DMA("pool", wout[:, kc, :], wout_d[kc * 128:(kc + 1) * 128, :], r=[], w=[wout])
                wr = sb("wr", [128, 8, NE], BF16)
                k.DMA("pool", wr[:, :, :], wr_d.rearrange("(k p) e -> p k e", p=128), r=[], w=[wr])
                snw = sb("snw", [128, 512], F32)
                hnw = sb("hnw", [128, 512], F32)
                k.DMA("sp", snw[:, :], snw_d, r=[], w=[snw])
                k.DMA("sp", hnw[:, :], hnw_d, r=[], w=[hnw])
                yown = [sb(f"yown{i}", [128, D], BF16) for i in range(2)]
                ypar = [sb(f"ypar{i}", [128, D], BF16) for i in range(2)]
                zgt = [sb(f"zgt{i}", [128, D], BF16) for i in range(2)]
                xt2 = [sb(f"xt2{i}", [128, D], F32) for i in range(2)]
                ysum = sb("ysum", [128, D], F32)
                junk4 = sb("junk4", [128, D], BF16)
                ssq6 = sb("ssq6", [128, 8], F32)
                rs6 = sb("rs6", [128, 8], F32)
                tmpo = sb("tmpo", [128, 512], F32)
                ylat = sb("ylat", [128, D], BF16)
                ylT = sb("ylT", [128, 8, 128], BF16)
                ssq2 = sb("ssq2", [128, 4], F32)
                x1 = sb("x1", [128, D], F32)
                h2f = sb("h2f", [128, D], F32)
                h2b = sb("h2b", [128, ROWW], BF16)
                h2T = sb("h2T", [128, 8, 128], BF16)
                smx = sb("smx", [128, 4], F32)
                ex = sb("ex", [128, NE], F32)
                k.MEMSET("pool", h2b[:, 1024:ROWW], 0.0, w=[h2b])

                def p4_load(t):
                    i = t % 2
                    rows = slice(t * 128, (t + 1) * 128)
                    k.DMA("sp", yown[i][:, :], ybuf_d[rows, :], r=[ybuf_b], w=[yown[i]])
                    k.DMA("sp", zgt[i][:, :], zgbuf_d[rows, :], r=[zgbuf_b], w=[zgt[i]])
                    k.DMA("sp", xt2[i][:, :], x_d[rows, :], r=[], w=[xt2[i]])
                    S.dma("pool", lambda e: e.indirect_dma_start(
                        out=ypar[i][:, :], out_offset=None, in_=ygath_t[3 - t // 8].ap(),
                        in_offset=bass.IndirectOffsetOnAxis(ap=pidx[:, t:t + 1], axis=0)),
                        r=[ygath_b, pidx], w=[ypar[i]])

                def rstd_from(ssq_ap, out_ap, n, rbufs):
                    k.ACT(out_ap, ssq_ap, AF.Ln, r=rbufs, w=rbufs, bias=EPS, scale=1.0 / n)
                    k.ACT(out_ap, out_ap, AF.Exp, r=rbufs, w=rbufs, scale=-0.5)

                nt4 = NH if stage >= 4 else 2
                nt4 = int(os.environ.get("KNT4", nt4))
                p4_load(0)
                for t in range(nt4):
                    i = t % 2
                    if t + 1 < nt4:
                        p4_load(t + 1)
                    rows = slice(t * 128, (t + 1) * 128)
                    k.TT("dve", ysum[:, :], yown[i][:, :], ypar[i][:, :], ALU.add, r=[yown[i], ypar[i]], w=[ysum])
                    k.TT("pool", ysum[:, 0:512], ysum[:, 0:512], zgt[i][:, 0:512], ALU.mult, r=[ysum, zgt[i]], w=[ysum])
                    k.MEMSET("pool", ssq6[:, :], 0.0, w=[ssq6])
                    for g in range(2):
                        k.ACT(junk4[:, g * 256:(g + 1) * 256], ysum[:, g * 256:(g + 1) * 256], AF.Square,
                              r=[ysum], w=[junk4, ssq6], accum=ssq6[:, g:g + 1])
                    for h in range(4):
                        sl = slice(512 + h * 128, 512 + (h + 1) * 128)
                        k.ACT(junk4[:, sl], ysum[:, sl], AF.Square, r=[ysum], w=[junk4, ssq6],
                              accum=ssq6[:, 2 + h:3 + h])
                    rstd_from(ssq6[:, 0:2], rs6[:, 0:2], 256, [ssq6, rs6])
                    rstd_from(ssq6[:, 2:6], rs6[:, 2:6], 128, [ssq6, rs6])
                    for g in range(2):
                        sl = slice(g * 256, (g + 1) * 256)
                        k.STT("dve", ylat[:, sl], ysum[:, sl], rs6[:, g:g + 1], snw[:, sl], ALU.mult, ALU.mult,
                              r=[ysum, rs6, snw], w=[ylat])
                    for h in range(4):
                        sl = slice(h * 128, (h + 1) * 128)
                        sl2 = slice(512 + h * 128, 512 + (h + 1) * 128)
                        k.STT("dve", tmpo[:, sl], ysum[:, sl2], rs6[:, 2 + h:3 + h], hnw[:, sl], ALU.mult, ALU.mult,
                              r=[ysum, rs6, hnw], w=[tmpo])
                    k.TT("pool", ylat[:, 512:1024], tmpo[:, :], zgt[i][:, 512:1024], ALU.mult,
                         r=[tmpo, zgt[i]], w=[ylat])
                    for kc in range(8):
                        k.TR(P0b[:, kc * 128:(kc + 1) * 128], ylat[:, kc * 128:(kc + 1) * 128], identb,
                             r=[ylat, cstb], w=[P[0]])
                    k.CP("act", ylT[:, 0:4, :], P0b[:, 0:512].rearrange("p (k l) -> p k l", k=4), r=[P[0]], w=[ylT])
                    k.CP("act", ylT[:, 4:8, :], P0b[:, 512:1024].rearrange("p (k l) -> p k l", k=4), r=[P[0]], w=[ylT])
                    for n in range(2):
                        for kc in range(8):
                            k.MM(P[1 + n][:, :], ylT[:, kc, :], wout[:, kc, n * 512:(n + 1) * 512], kc == 0, kc == 7,
                                 r=[ylT, wout], w=[P[1 + n]])
                    k.MEMSET("pool", ssq2[:, :], 0.0, w=[ssq2])
                    for n in range(2):
                        k.ACT(junk4[:, n * 512:(n + 1) * 512], P[1 + n][:, :], AF.Square, r=[P[1 + n]],
                              w=[junk4, ssq2], accum=ssq2[:, n:n + 1])
                    k.TT("dve", ssq2[:, 2:3], ssq2[:, 0:1], ssq2[:, 1:2], ALU.add, r=[ssq2], w=[ssq2])
                    rstd_from(ssq2[:, 2:3], ssq2[:, 3:4], D, [ssq2])
                    for n in range(2):
                        sl = slice(n * 512, (n + 1) * 512)
                        k.STT("dve", x1[:, sl], P[1 + n][:, :], ssq2[:, 3:4], G1[:, sl], ALU.mult, ALU.mult,
                              r=[P[1 + n], ssq2, modrep], w=[x1])
                    k.TT("pool", x1[:, :], x1[:, :], xt2[i][:, :], ALU.add, r=[x1, xt2[i]], w=[x1])
                    k.DMA("sp", x1buf_d[rows, :], x1[:, :], r=[x1], w=[x1buf_b])
                    k.MEMSET("pool", ssq2[:, 0:1], 0.0, w=[ssq2])
                    k.ACT(junk4[:, :], x1[:, :], AF.Square, r=[x1], w=[junk4, ssq2], accum=ssq2[:, 0:1])
                    rstd_from(ssq2[:, 0:1], ssq2[:, 1:2], D, [ssq2])
                    k.STT("dve", h2f[:, :], x1[:, :], ssq2[:, 1:2], A2, ALU.mult, ALU.mult, r=[x1, ssq2, modrep], w=[h2f])
                    k.TT("dve", h2b[:, 0:D], h2f[:, :], B2, ALU.add, r=[h2f, modrep], w=[h2b])
                    k.CP("dve", h2b[:, 1026:1028].bitcast(I32), pidx[:, NH + t:NH + t + 1], r=[pidx], w=[h2b])
                    k.DMA("sp", h2buf_d[rows, :], h2b[:, :], r=[h2b], w=[h2buf_b])
                    for kc in range(8):
                        k.TR(P0b[:, kc * 128:(kc + 1) * 128], h2b[:, kc * 128:(kc + 1) * 128], identb,
                             r=[h2b, cstb], w=[P[0]])
                    k.CP("act", h2T[:, :, :], P0b[:, :].rearrange("p (k l) -> p k l", k=8), r=[P[0]], w=[h2T])
                    for kc in range(8):
                        k.MM(P[6][:, 0:NE], h2T[:, kc, :], wr[:, kc, :], kc == 0, kc == 7, r=[h2T, wr], w=[P[6]])
                    S.op("dve", lambda e: e.tensor_reduce(out=smx[:, 0:1], in_=P[6][:, 0:NE], axis=AX.X, op=ALU.max),
                         r=[P[6]], w=[smx])
                    k.TS("dve", smx[:, 1:2], smx[:, 0:1], -1.0, None, ALU.mult, None, r=[smx], w=[smx])
                    k.MEMSET("pool", smx[:, 2:3], 0.0, w=[smx])
                    k.ACT(ex[:, :], P[6][:, 0:NE], AF.Exp, r=[P[6], smx], w=[ex, smx], bias=smx[:, 1:2],
                          accum=smx[:, 2:3])
                    S.op("dve", lambda e: e.reciprocal(out=smx[:, 3:4], in_=smx[:, 2:3]), r=[smx], w=[smx])
                    k.TS("dve", affall[:, t, :], ex[:, :], smx[:, 3:4], None, ALU.mult, None, r=[ex, smx], w=[affall])
                S.barrier()
            if stage < 5:
                with contextlib.ExitStack() as esd:
                    tf = S.buf("dbg_f", [128, D], F32, es=esd)
                    for t in range(nt4):
                        rows = slice(t * 128, (t + 1) * 128)
                        k.DMA("sp", tf[:, :], x1buf_d[rows, :], r=[x1buf_b], w=[tf])
                        k.DMA("sp", dbg_d[rows, :], tf[:, :], r=[tf], w=[])
                    k.DMA("sp", dbg_d[HALF:HALF + 128, 0:NH * NE], affall[:, :, :].rearrange("p t e -> p (t e)"),
                          r=[affall], w=[])
        if stage >= 5:
            k.DMA("sp", affs_t.ap().rearrange("(t p) e -> p t e", p=128), affall[:, :, :], r=[affall], w=[affs_b])
            if SIM:
                for hh in range(2):
                    k.DMA("sp", affg_t.ap()[hh * HALF:(hh + 1) * HALF, :], affs_t.ap(), r=[affs_b], w=[affg_b])
            else:
                S.coll(lambda e: e.collective_compute("AllGather", ALU.bypass, replica_groups=PAIRS,
                                                      ins=[affs_t.ap().opt()], outs=[affg_t.ap().opt()]),
                       r=[affs_b], w=[affg_b])
            tau = S.buf("tau", [128, NE], F32)
            gate = S.buf("gate", [128, NH, NE], F32)
            sloti = S.buf("sloti", [128, NH, NE], I32)
            with contextlib.ExitStack() as es6:
                def sb(name, shape, dtype):
                    return S.buf(name, shape, dtype, es=es6)
                affb = sb("affb", [128, NT, NE], F32)
                k.DMA("sp", affb[:, :, :], affg_t.ap().rearrange("(t p) e -> p t e", p=128), r=[affg_b], w=[affb])
                cmpb = sb("cmpb", [128, NT, NE], F32)
                hi = sb("hi", [128, NE], F32)
                d2 = sb("d2", [128, NE], F32)
                mid = sb("mid", [128, NE], F32)
                cntp = sb("cntp", [128, NE], F32)
                ge = sb("ge", [128, NE], F32)
                k.MEMSET("pool", tau[:, :], 0.0, w=[tau])
                k.MEMSET("pool", hi[:, :], 1.0001, w=[hi])
                ones_f = cst[:, 7, :]
                for it in range(32):
                    k.TT("dve", d2[:, :], hi[:, :], tau[:, :], ALU.subtract, r=[hi, tau], w=[d2])
                    k.STT("dve", mid[:, :], d2[:, :], 0.5, tau[:, :], ALU.mult, ALU.add, r=[d2, tau], w=[mid])
                    k.TT("dve", cmpb[:, :, :], affb[:, :, :], mid[:, :].unsqueeze(1).to_broadcast([128, NT, NE]),
                         ALU.is_ge, r=[affb, mid], w=[cmpb])
                    S.op("dve", lambda e: e.tensor_reduce(out=cntp[:, :], in_=cmpb[:, :, :].rearrange("p t e -> p e t"),
                                                          axis=AX.X, op=ALU.add), r=[cmpb], w=[cntp])
                    k.MM(P[6][:, 0:NE], ones_f, cntp[:, :], True, True, r=[cst, cntp], w=[P[6]])
                    k.TS("dve", ge[:, :], P[6][:, 0:NE], float(CAP), None, ALU.is_ge, None, r=[P[6]], w=[ge])
                    k.TT("dve", ge[:, :], ge[:, :], d2[:, :], ALU.mult, r=[ge, d2], w=[ge])
                    k.STT("dve", tau[:, :], ge[:, :], 0.5, tau[:, :], ALU.mult, ALU.add, r=[ge, tau], w=[tau])
                    k.STT("dve", hi[:, :], d2[:, :], -0.5, hi[:, :], ALU.mult, ALU.add, r=[d2, hi], w=[hi])
                    k.STT("dve", hi[:, :], ge[:, :], 0.5, hi[:, :], ALU.mult, ALU.add, r=[ge, hi], w=[hi])
                maskf = sb("maskf", [128, NH, NE], F32)
                maskb = sb("maskb", [128, NH * NE], BF16)
                totE = sb("totE", [128, NE, NH], F32)
                cumE = sb("cumE", [128, NE, NH], F32)
                ones32 = sb("ones32", [128, NH], F32)
                posf = sb("posf", [128, NH, NE], F32)
                k.TT("dve", maskf[:, :, :], affall[:, :, :], tau[:, :].unsqueeze(1).to_broadcast([128, NH, NE]),
                     ALU.is_ge, r=[affall, tau], w=[maskf])
                k.TT("dve", gate[:, :, :], maskf[:, :, :], affall[:, :, :], ALU.mult, r=[maskf, affall], w=[gate])
                k.CP("dve", maskb[:, :], maskf[:, :, :].rearrange("p t e -> p (t e)"), r=[maskf], w=[maskb])
                k.MM(P[1][:, :], cstb[:, 8, :], maskb[:, :], True, True, r=[cstb, maskb], w=[P[1]])
                k.MM(P[2][:, :], onesb, maskb[:, :], True, True, r=[cstb, maskb], w=[P[2]])
                k.CP("dve", totE[:, :, :], P[2][:, :].rearrange("p (t e) -> p e t", e=NE), r=[P[2]], w=[totE])
                k.MEMSET("pool", ones32[:, :], 1.0, w=[ones32])
                for e_ in range(NE):
                    S.op("dve", (lambda e_: lambda eng: eng.tensor_tensor_scan(
                        out=cumE[:, e_, :], data0=ones32[:, :], data1=totE[:, e_, :], initial=0.0,
                        op0=ALU.mult, op1=ALU.add))(e_), r=[ones32, totE], w=[cumE])
                k.TT("dve", cumE[:, :, :], cumE[:, :, :], totE[:, :, :], ALU.subtract, r=[cumE, totE], w=[cumE])
                k.TT("dve", posf[:, :, :], P[1][:, :].rearrange("p (t e) -> p t e", e=NE),
                     cumE[:, :, :].rearrange("p e t -> p t e"), ALU.add, r=[P[1], cumE], w=[posf])
                ltm = sb("ltm", [128, NH, NE], F32)
                k.TS("dve", ltm[:, :, :], posf[:, :, :], float(CAP), None, ALU.is_lt, None, r=[posf], w=[ltm])
                k.TT("dve", maskf[:, :, :], maskf[:, :, :], ltm[:, :, :], ALU.mult, r=[maskf, ltm], w=[maskf])
                k.TT("dve", posf[:, :, :], posf[:, :, :], cst[:, 9, 2:2 + NE].unsqueeze(1).to_broadcast([128, NH, NE]),
                     ALU.add, r=[posf, cst], w=[posf])
                k.TT("dve", posf[:, :, :], posf[:, :, :], maskf[:, :, :], ALU.mult, r=[posf, maskf], w=[posf])
                k.TS("dve", maskf[:, :, :], maskf[:, :, :], -1.0, 1.0, ALU.mult, ALU.add, r=[maskf], w=[maskf])
                k.TS("dve", maskf[:, :, :], maskf[:, :, :], cst[:, 9, 18:19], None, ALU.mult, None, r=[maskf, cst], w=[maskf])
                k.TT("dve", posf[:, :, :], posf[:, :, :], maskf[:, :, :], ALU.add, r=[posf, maskf], w=[posf])
                k.CP("dve", sloti[:, :, :], posf[:, :, :], r=[posf], w=[sloti])
                S.barrier()
            if stage < 7:
                with contextlib.ExitStack() as esd:
                    tf = S.buf("dbg_g", [128, NH * NE], F32, es=esd)
                    k.DMA("sp", dbg_d[HALF + 128:HALF + 256, 0:NE], tau[:, :], r=[tau], w=[])
                    k.DMA("sp", dbg_d[HALF + 256:HALF + 384, 0:NH * NE], gate[:, :, :].rearrange("p t e -> p (t e)"),
                          r=[gate], w=[])
                    k.CP("dve", tf[:, :], sloti[:, :, :].rearrange("p t e -> p (t e)"), r=[sloti], w=[tf])
                    k.DMA("sp", dbg_d[HALF + 384:HALF + 512, 0:NH * NE], tf[:, :], r=[tf], w=[])
        if stage >= 7:
            with contextlib.ExitStack() as es7:
                def sb(name, shape, dtype):
                    return S.buf(name, shape, dtype)
                initr = sb("initr", [128, ROWW], BF16)
                k.MEMSET("pool", initr[:, :], 0.0, w=[initr])
                k.MEMSET("pool", initr[:, 1026:1028].bitcast(I32), TRASH, w=[initr])
                zt = sb("zt", [128, D], F32)
                k.MEMSET("pool", zt[:, :], 0.0, w=[zt])
                for e_ in range(NE):
                    for sc in range(8):
                        k.DMA("sp", xin_d[e_, sc * 128:(sc + 1) * 128, :], initr[:, :], r=[initr], w=[xin_b])
                for t in range(NH + 1):
                    k.DMA("sp", acc_d[t * 128:(t + 1) * 128, :], zt[:, :], r=[zt], w=[acc_b])
                wgb = sb("wgb", [128, 8, D], BF16)
                wub = sb("wub", [128, 8, D], BF16)
                wdb = sb("wdb", [128, 8, D], BF16)
                hd = [sb(f"hd{i}", [128, ROWW], BF16) for i in range(2)]
                xs_in = [sb(f"xs_in{i}", [128, ROWW], BF16) for i in range(2)]
                xinT = sb("xinT", [128, 8, CAP], BF16)
                hid = sb("hid", [128, 8, CAP], BF16)
                gall = sb("gall", [128, 8, 8], F32)
                iall = sb("iall", [128, 8, 8], I32)
                sg = [sb(f"sg{i}", [128, 512], F32) for i in range(2)]
                yt = [sb(f"yt{i}", [128, D], F32) for i in range(2)]

                def load_w(e_):
                    for (wb, wd_) in ((wgb, wg_d), (wub, wu_d), (wdb, wd_d)):
                        for kc in range(8):
                            k.DMA("pool", wb[:, kc, :], wd_[e_, kc * 128:(kc + 1) * 128, :], r=[], w=[wb])

                def dispatch(e_):
                    for t in range(NH):
                        hb_ = hd[(e_ * NH + t) % 2]
                        k.DMA("sp", hb_[:, :], h2buf_d[t * 128:(t + 1) * 128, :], r=[h2buf_b], w=[hb_])
                        k.CP("dve", hb_[:, 1024:1026].bitcast(F32), gate[:, t, e_:e_ + 1], r=[gate], w=[hb_])
                        S.dma("pool", (lambda hb_, t, e_: lambda eng: eng.indirect_dma_start(
                            out=xin_flat, out_offset=bass.IndirectOffsetOnAxis(ap=sloti[:, t, e_:e_ + 1], axis=0),
                            in_=hb_[:, :], in_offset=None))(hb_, t, e_),
                            r=[hb_, sloti], w=[xin_b])

                nexp = NE if stage >= 8 else 2
                load_w(0)
                dispatch(0)
                pbi = 0
                ybi = 0
                for e_ in range(nexp):
                    for sc in range(8):
                        xb_ = xs_in[sc % 2]
                        k.DMA("sp", xb_[:, :], xin_d[e_, sc * 128:(sc + 1) * 128, :], r=[xin_b], w=[xb_])
                        k.CP("dve", gall[:, sc, 0:1], xb_[:, 1024:1026].bitcast(F32), r=[xb_], w=[gall])
                        k.CP("dve", iall[:, sc, 0:1], xb_[:, 1026:1028].bitcast(I32), r=[xb_], w=[iall])
                        for kc in range(8):
                            k.TR(P0b[:, kc * 128:(kc + 1) * 128], xb_[:, kc * 128:(kc + 1) * 128], identb,
                                 r=[xb_, cstb], w=[P[0]])
                        k.CP("act", xinT[:, :, sc * 128:(sc + 1) * 128], P0b[:, :].rearrange("p (k l) -> p k l", k=8),
                             r=[P[0]], w=[xinT])
                    if e_ + 1 < nexp:
                        dispatch(e_ + 1)
                    for fc in range(8):
                        for half in range(2):
                            pg, pu = P[1 + 2 * (pbi % 3)], P[2 + 2 * (pbi % 3)]
                            pbi += 1
                            for kc in range(8):
                                k.MM(pg[:, :], wgb[:, kc, fc * 128:(fc + 1) * 128], xinT[:, kc, half * 512:(half + 1) * 512],
                                     kc == 0, kc == 7, r=[wgb, xinT], w=[pg])
                            for kc in range(8):
                                k.MM(pu[:, :], wub[:, kc, fc * 128:(fc + 1) * 128], xinT[:, kc, half * 512:(half + 1) * 512],
                                     kc == 0, kc == 7, r=[wub, xinT], w=[pu])
                            sgb = sg[(fc * 2 + half) % 2]
                            k.ACT(sgb[:, :], pg[:, :], AF.Silu, r=[pg], w=[sgb])
                            k.TT("dve", hid[:, fc, half * 512:(half + 1) * 512], sgb[:, :], pu[:, :], ALU.mult,
                                 r=[sgb, pu], w=[hid])
                    if e_ + 1 < nexp:
                        for (wb, wd_) in ((wgb, wg_d), (wub, wu_d)):
                            for kc in range(8):
                                k.DMA("pool", wb[:, kc, :], wd_[e_ + 1, kc * 128:(kc + 1) * 128, :], r=[], w=[wb])
                    for sc in range(8):
                        ytb = yt[sc % 2]
                        for n in range(2):
                            py = P[7] if (ybi % 2 == 0) else P[0]
                            ybi += 1
                            for fc in range(8):
                                k.MM(py[:, :], hid[:, fc, sc * 128:(sc + 1) * 128], wdb[:, fc, n * 512:(n + 1) * 512],
                                     fc == 0, fc == 7, r=[hid, wdb], w=[py])
                            k.ACT(ytb[:, n * 512:(n + 1) * 512], py[:, :], AF.Identity, r=[py, gall], w=[ytb],
                                  scale=gall[:, sc, 0:1])
                        S.dma("pool", (lambda ytb, sc: lambda eng: eng.indirect_dma_start(
                            out=acc_d, out_offset=bass.IndirectOffsetOnAxis(ap=iall[:, sc, 0:1], axis=0),
                            in_=ytb[:, :], in_offset=None, compute_op=ALU.add))(ytb, sc),
                            r=[ytb, iall], w=[acc_b])
                    if e_ + 1 < nexp:
                        for kc in range(8):
                            k.DMA("pool", wdb[:, kc, :], wd_d[e_ + 1, kc * 128:(kc + 1) * 128, :], r=[], w=[wdb])
                S.barrier()
            with contextlib.ExitStack() as es8:
                def sb(name, shape, dtype):
                    return S.buf(name, shape, dtype)
                at = [sb(f"at{i}", [128, D], F32) for i in range(2)]
                x1t = [sb(f"x1t{i}", [128, D], F32) for i in range(2)]
                ot = [sb(f"ot{i}", [128, D], F32) for i in range(2)]
                junk8 = sb("junk8", [128, D], BF16)
                s8 = sb("s8", [128, 8, 8], F32)
                for t in range(NH):
                    i = t % 2
                    rows = slice(t * 128, (t + 1) * 128)
                    k.DMA("sp", at[i][:, :], acc_d[rows, :], r=[acc_b], w=[at[i]])
                    k.DMA("sp", x1t[i][:, :], x1buf_d[rows, :], r=[x1buf_b], w=[x1t[i]])
                    k.MEMSET("pool", s8[:, 0, 0:1], 0.0, w=[s8])
                    k.ACT(junk8[:, :], at[i][:, :], AF.Square, r=[at[i]], w=[junk8, s8], accum=s8[:, 0, 0:1])
                    k.ACT(s8[:, 1, 0:1], s8[:, 0, 0:1], AF.Ln, r=[s8], w=[s8], bias=EPS, scale=1.0 / D)
                    k.ACT(s8[:, 2, 0:1], s8[:, 1, 0:1], AF.Exp, r=[s8], w=[s8], scale=-0.5)
                    k.STT("dve", ot[i][:, :], at[i][:, :], s8[:, 2, 0:1], G2, ALU.mult, ALU.mult,
                          r=[at[i], s8, modrep], w=[ot[i]])
                    k.TT("pool", ot[i][:, :], ot[i][:, :], x1t[i][:, :], ALU.add, r=[ot[i], x1t[i]], w=[ot[i]])
                    k.DMA("sp", out_d[rows, :], ot[i][:, :], r=[ot[i]], w=[])
        S.wait_all_dma("sp")
        S.emit()
    return nc


def make_consts():
    c = np.zeros((128, NCONST, 128), np.float32)
    i = np.arange(128)
    r, cc = i[:, None], i[None, :]
    same = (r // 64) == (cc // 64)
    c[:, 0] = (r == cc)
    c[:, 1] = (r <= cc)
    c[:, 2] = np.where(r > cc, -30000.0, 0.0)
    c[:, 3] = (r <= cc)
    c[:, 4] = same & (r <= cc)
    c[:, 5] = same & (r > cc)
    c[:, 6] = same & (r <= cc)
    c[:, 7] = 1.0
    c[:, 8] = (r < cc)
    c[:, 9, 0] = (i < 64)
    c[:, 9, 1] = (i >= 64)
    c[:, 9, 2:2 + NE] = np.arange(NE)[None, :] * CAP
    c[:, 9, 18] = NE * CAP + i
    return c


def rep(v, n=128):
    return np.ascontiguousarray(np.broadcast_to(np.asarray(v, np.float32)[None], (n,) + tuple(np.shape(v))))


def fm(v):
    return np.ascontiguousarray(np.asarray(v, np.float32).reshape(-1, 128).T)


def prep_inputs(inp):
    x, c, ctx, c_ctx = inp["x"], inp["c"], inp["ctx"], inp["c_ctx"]
    w_in = inp["w_in"][0]
    consts = make_consts()
    shared = {
        "ada_w": np.ascontiguousarray(inp["ada_w"][0]),
        "ada_bT": np.ascontiguousarray(inp["ada_b"][0][:2048].reshape(16, 128).T),
        "ada_brep": rep(inp["ada_b"][0][2048:]),
        "nw0T": fm(inp["norm_w"][0, 0]),
        "nwrep": rep(inp["norm_w"][0, 1:4]),
        "cb": fm(inp["ssd_conv_b"][0]),
        "snw": rep(inp["ssd_norm_w"][0]),
        "hnw": rep(inp["hgrn_norm_w"][0]),
        "w_out": np.ascontiguousarray(inp["w_out"][0]),
        "w_router": np.ascontiguousarray(inp["w_router"][0]),
        "w_gate": np.ascontiguousarray(inp["w_gate"][0]),
        "w_up": np.ascontiguousarray(inp["w_up"][0]),
        "w_down": np.ascontiguousarray(inp["w_down"][0]),
        "consts": consts,
    }
    maps = []
    for core in range(8):
        b, d = core // 2, core % 2
        m = dict(shared)
        m["xs"] = np.ascontiguousarray(x[b][::-1] if d else x[b])
        m["ctxs"] = np.ascontiguousarray(ctx[b][::-1] if d else ctx[b])
        cv = np.stack([fm(c[b]), fm(c_ctx)], axis=-1)
        m["cvec"] = np.ascontiguousarray(cv)
        cols = [w_in[:, 512:1536], w_in[:, 1552:2064], w_in[:, 2064 + 512 * d:2576 + 512 * d], w_in[:, 3088:3600],
                w_in[:, 1536 + 8 * d:1544 + 8 * d], w_in[:, 0:512], w_in[:, 3600:4112]]
        m["wmain"] = np.ascontiguousarray(np.concatenate(cols, axis=1))
        cwk = inp["ssd_conv_w"][0]
        if d:
            cwk = cwk[::-1]
        m["cw"] = np.ascontiguousarray(cwk.T.reshape(8, 128, 5).transpose(1, 0, 2))
        m["sp8"] = rep(np.stack([inp["ssd_dt_bias"][0, d], inp["ssd_a_log"][0, d], inp["ssd_d"][0]]))
        m["lbrep"] = rep(np.stack([inp["hgrn_lb"][0, d], inp["hgrn_lb"][1, d]]))
        p = np.arange(128)[:, None]
        t = np.arange(NH)[None, :]
        m["pidx"] = np.ascontiguousarray(np.concatenate(
            [(1 - d) * 1024 + ((HALF - 1) - (t * 128 + p)) % 1024, t * 128 + p], axis=1).astype(np.int32))
        maps.append(m)
    return maps


STAGE = int(os.environ.get("KSTAGE", "9"))
LITE = int(os.environ.get("KLITE", "0"))
SIM = int(os.environ.get("KSIM", "0"))
SUBCUT = os.environ.get("KSUB", "")
_CACHE = {}


def kernel(**inputs):
    inp = {k_: np.asarray(v) for k_, v in inputs.items()}
    maps = prep_inputs(inp)
    if LITE:
        for m in maps:
            m["ada_w"] = m["ada_w"][:8]
            m["xs"] = m["xs"][:1024]
    if STAGE < 7:
        for m in maps:
            for kk_ in ("w_gate", "w_up", "w_down"):
                m.pop(kk_)
    if STAGE not in _CACHE:
        _CACHE[STAGE] = build_program(STAGE)
    nc = _CACHE[STAGE]
    res = run_bass_kernel_spmd(nc, maps, core_ids=list(range(8)))
    if STAGE < 9:
        return [r["dbg"] for r in res.results]
    out = np.empty((4, SEQ, D), np.float32)
    for core in range(8):
        b, d = core // 2, core % 2
        o = res.results[core]["out"]
        if d:
            out[b, HALF:] = o[::-1]
        else:
            out[b, :HALF] = o
    return out
```

```python
import contextlib
import os
import numpy as np
import concourse.bass as bass
import concourse.mybir as mybir
from concourse.bass_utils import run_bass_kernel_spmd

F32 = mybir.dt.float32
BF16 = mybir.dt.bfloat16
I32 = mybir.dt.int32
AF = mybir.ActivationFunctionType
ALU = mybir.AluOpType
AX = mybir.AxisListType

D = 1024
SEQ = 8192
CTX = 256
NT = SEQ // 128
NH = NT // 2
HALF = SEQ // 2
NE = 16
CAP = 1024
EPS = 1e-6
WCOLS = 3592
C_XBC, C_Q, C_F, C_I, C_DT, C_Z, C_G = 0, 1024, 1536, 2048, 2560, 2568, 3080
NCONST = 10
ROWW = 1028
TRASH = HALF


class Buf:
    __slots__ = ("name", "t", "last_w", "readers")

    def __init__(self, name, t=None):
        self.name = name
        self.t = t
        self.last_w = None
        self.readers = []

    def __getitem__(self, k):
        return self.t[k]


class Sched:
    COMPUTE = ("pe", "dve", "act", "pool")
    NDSEM = 6

    def __init__(self, nc, es, same_engine_sync=True):
        self.nc = nc
        self.es = es
        self.same_engine_sync = same_engine_sync
        self.prog = {k: [] for k in ("pe", "dve", "act", "pool", "sp")}
        self.ninst = {k: 0 for k in self.COMPUTE}
        self.waited_idx = {}
        self.milestones = {k: set() for k in self.COMPUTE}
        self.csem = {k: es.enter_context(nc.semaphore("cs_" + k)) for k in self.COMPUTE}
        self.dsem, self.dcnt, self.drot = {}, {}, {}
        for q in ("sp", "act", "pool"):
            self.dsem[q] = [es.enter_context(nc.semaphore(f"ds_{q}{j}")) for j in range(self.NDSEM)]
            self.dcnt[q] = [0] * self.NDSEM
            self.drot[q] = 0
        self.ccsem = es.enter_context(nc.semaphore("cc_sem"))
        self.cccnt = 0

    def buf(self, name, shape, dtype, psum=False, es=None):
        es = es or self.es
        name = "s_" + name
        if psum:
            t = es.enter_context(self.nc.psum_tensor(name, list(shape), dtype))
        else:
            t = es.enter_context(self.nc.sbuf_tensor(name, list(shape), dtype))
        return Buf(name, t)

    def _need(self, eng, tok):
        if tok is None:
            return
        semkey, v = tok
        key = (eng, semkey)
        if semkey[0] == "c":
            src = semkey[1]
            if src == eng and (eng == "pe" or not self.same_engine_sync):
                return
            if self.waited_idx.get(key, -1) >= v:
                return
            self.waited_idx[key] = v
            self.milestones[src].add(v)
            self.prog[eng].append(("cwait", src, v))
        elif semkey[0] == "x":
            if self.waited_idx.get(key, -1) >= v:
                return
            self.waited_idx[key] = v
            self.prog[eng].append(("xwait", None, v))
        else:
            if self.waited_idx.get(key, -1) >= v:
                return
            self.waited_idx[key] = v
            _, q, j = semkey
            self.prog[eng].append(("dwait", (q, j), v))

    def _deps(self, eng, r, w):
        for b in r:
            self._need(eng, b.last_w)
        for b in w:
            self._need(eng, b.last_w)
            for t in b.readers:
                self._need(eng, t)

    def _commit(self, tok, r, w):
        for b in w:
            b.last_w = tok
            b.readers = []
        for b in r:
            if b not in w:
                b.readers.append(tok)

    def op(self, eng, fn, r=(), w=()):
        self._deps(eng, r, w)
        idx = self.ninst[eng]
        self.ninst[eng] += 1
        self.prog[eng].append(("op", fn, idx))
        tok = (("c", eng), idx)
        self._commit(tok, r, w)
        return tok

    def dma(self, q, fn, r=(), w=()):
        self._deps(q, r, w)
        j = self.drot[q]
        self.drot[q] = (j + 1) % self.NDSEM
        semkey = ("d", q, j)
        prev = self.dcnt[q][j]
        if prev > 0:
            self._need(q, (semkey, prev))
        self.dcnt[q][j] += 16
        v = self.dcnt[q][j]
        self.prog[q].append(("dma", fn, (q, j)))
        tok = (semkey, v)
        self._commit(tok, r, w)
        return tok

    def coll(self, fn, r=(), w=()):
        q = "pool"
        self._deps(q, r, w)
        self.cccnt += 1
        v = self.cccnt
        self.prog[q].append(("coll", fn, v))
        tok = (("x", "cc"), v)
        self._commit(tok, r, w)
        return tok

    def barrier(self):
        for eng in ("pe", "dve", "act", "pool", "sp"):
            for x in self.COMPUTE:
                if x != eng and self.ninst[x] > 0:
                    self._need(eng, (("c", x), self.ninst[x] - 1))
            self.wait_all_dma(eng)

    def wait_all_dma(self, eng="sp"):
        for q in ("sp", "act", "pool"):
            for j in range(self.NDSEM):
                if self.dcnt[q][j]:
                    self._need(eng, (("d", q, j), self.dcnt[q][j]))
        if self.cccnt:
            self._need(eng, (("x", "cc"), self.cccnt))

    def emit(self):
        nc = self.nc
        rank = {}
        for e in self.COMPUTE:
            ms = sorted(self.milestones[e])
            rank[e] = {idx: i + 1 for i, idx in enumerate(ms)}
        sched = self

        def run(engname, e):
            for ent in sched.prog[engname]:
                kind = ent[0]
                if kind == "op":
                    ins = ent[1](e)
                    if ent[2] in rank[engname]:
                        ins.then_inc(sched.csem[engname], 1)
                elif kind == "cwait":
                    e.wait_ge(sched.csem[ent[1]], rank[ent[1]][ent[2]])
                elif kind == "dwait":
                    q, j = ent[1]
                    e.wait_ge(sched.dsem[q][j], ent[2])
                elif kind == "xwait":
                    e.wait_ge(sched.ccsem, ent[2])
                elif kind == "coll":
                    ent[1](e).then_inc(sched.ccsem, 1)
                elif kind == "dma":
                    q, j = ent[2]
                    ent[1](e).then_inc(sched.dsem[q][j], 16)

        with nc.Block() as block:
            @block.sync
            def _(e):
                run("sp", e)

            @block.scalar
            def _(e):
                run("act", e)

            @block.vector
            def _(e):
                run("dve", e)

            @block.gpsimd
            def _(e):
                run("pool", e)

            @block.tensor
            def _(e):
                run("pe", e)


class K:
    def __init__(self, nc, es):
        self.nc = nc
        self.S = Sched(nc, es, same_engine_sync=SAME_ENGINE_SYNC)

    def MM(self, out, lhsT, rhs, start, stop, r, w):
        self.S.op("pe", lambda e: e.matmul(out, lhsT=lhsT, rhs=rhs, start=start, stop=stop), r=r, w=w)

    def TR(self, out, in_, ident, r, w):
        self.S.op("pe", lambda e: e.transpose(out=out, in_=in_, identity=ident), r=r, w=w)

    def ACT(self, out, in_, func, r, w, bias=None, scale=None, accum=None):
        kw = {}
        if bias is not None:
            kw["bias"] = bias
        if scale is not None:
            kw["scale"] = scale
        if accum is not None:
            kw["accum_out"] = accum
        self.S.op("act", lambda e: e.activation(out=out, in_=in_, func=func, **kw), r=r, w=w)

    def TT(self, eng, out, in0, in1, op, r, w):
        self.S.op(eng, lambda e: e.tensor_tensor(out=out, in0=in0, in1=in1, op=op), r=r, w=w)

    def TS(self, eng, out, in0, s1, s2, op0, op1, r, w, accum=None):
        if op1 is None:
            self.S.op(eng, lambda e: e.tensor_scalar(out=out, in0=in0, scalar1=s1, scalar2=None, op0=op0), r=r, w=w)
        elif accum is not None:
            self.S.op(eng, lambda e: e.tensor_scalar(out=out, in0=in0, scalar1=s1, scalar2=s2, op0=op0, op1=op1,
                                                     accum_out=accum), r=r, w=w)
        else:
            self.S.op(eng, lambda e: e.tensor_scalar(out=out, in0=in0, scalar1=s1, scalar2=s2, op0=op0, op1=op1),
                      r=r, w=w)

    def STT(self, eng, out, in0, scalar, in1, op0, op1, r, w):
        self.S.op(eng, lambda e: e.scalar_tensor_tensor(out=out, in0=in0, scalar=scalar, in1=in1, op0=op0, op1=op1),
                  r=r, w=w)

    def CP(self, eng, out, in_, r, w):
        if eng == "act":
            self.S.op("act", lambda e: e.copy(out=out, in_=in_), r=r, w=w)
        else:
            self.S.op(eng, lambda e: e.tensor_copy(out=out, in_=in_), r=r, w=w)

    def MEMSET(self, eng, ap, val, w):
        self.S.op(eng, lambda e: e.memset(ap, val), w=w)

    def DMA(self, q, out, in_, r, w):
        return self.S.dma(q, lambda e: e.dma_start(out=out, in_=in_), r=r, w=w)


def build_program(stage):
    nc = bass.Bass("TRN2", target_bir_lowering=False)
    dt_in = lambda name, shape, dt=F32: nc.dram_tensor(name, list(shape), dt, kind="ExternalInput").ap()
    x_d = dt_in("xs", [1024 if LITE else SEQ, D])
    ctx_d = dt_in("ctxs", [CTX, D])
    cvec_d = dt_in("cvec", [128, 8, 2])
    adaw_d = dt_in("ada_w", [8 if LITE else D, 6 * D])
    adabT_d = dt_in("ada_bT", [128, 16])
    adabrep_d = dt_in("ada_brep", [128, 4 * D])
    nw0T_d = dt_in("nw0T", [128, 8])
    nwrep_d = dt_in("nwrep", [128, 3, D])
    wmain_d = dt_in("wmain", [D, WCOLS])
    cw_d = dt_in("cw", [128, 8, 5])
    cb_d = dt_in("cb", [128, 8])
    sp8_d = dt_in("sp8", [128, 3, 8])
    lbrep_d = dt_in("lbrep", [128, 2, 512])
    snw_d = dt_in("snw", [128, 512])
    hnw_d = dt_in("hnw", [128, 512])
    wout_d = dt_in("w_out", [D, D])
    wr_d = dt_in("w_router", [D, NE])
    if stage >= 7:
        wg_d = dt_in("w_gate", [NE, D, D])
        wu_d = dt_in("w_up", [NE, D, D])
        wd_d = dt_in("w_down", [NE, D, D])
    consts_d = dt_in("consts", [128, NCONST, 128])
    pidx_d = dt_in("pidx", [128, 2 * NH], I32)
    out_d = nc.dram_tensor("out", [HALF, D], F32, kind="ExternalOutput").ap()
    dbg_d = None
    if stage < 9:
        dbg_d = nc.dram_tensor("dbg", [SEQ, D], F32, kind="ExternalOutput").ap()
    ybuf_d = nc.dram_tensor("ybuf", [HALF, D], BF16).ap()
    ysend_t = [nc.dram_tensor(f"ysend{c}", [1024, D], BF16) for c in range(4)]
    ygath_t = [nc.dram_tensor(f"ygath{c}", [2048, D], BF16) for c in range(4)]
    zgbuf_d = nc.dram_tensor("zgbuf", [HALF, D], BF16).ap()
    x1buf_d = nc.dram_tensor("x1buf", [HALF, D], F32).ap()
    h2buf_d = nc.dram_tensor("h2buf", [HALF, ROWW], BF16).ap()
    affs_t = nc.dram_tensor("affsend", [HALF, NE], F32)
    affg_t = nc.dram_tensor("affgath", [SEQ, NE], F32)
    xin_flat = nc.dram_tensor("xin", [NE * CAP + 128, ROWW], BF16).ap()
    xin_d = xin_flat[0:NE * CAP, :].rearrange("(e c) r -> e c r", e=NE)
    acc_d = nc.dram_tensor("moeacc", [HALF + 128, D], F32).ap()
    ybuf_b, ysend_b, ygath_b, zgbuf_b, x1buf_b, h2buf_b = (Buf(n) for n in ("ybuf", "ysend", "ygath", "zgbuf", "x1buf", "h2buf"))
    affs_b, affg_b, xin_b, acc_b = (Buf(n) for n in ("affs", "affg", "xin", "acc"))
    PAIRS = [[0, 1], [2, 3], [4, 5], [6, 7]]

    with contextlib.ExitStack() as es:
        k = K(nc, es)
        S = k.S
        P = [S.buf(f"P{i}", [128, 512], F32, psum=True) for i in range(8)]
        P0b = P[0].t[:, :].bitcast(BF16)

        cst = S.buf("cst", [128, NCONST, 128], F32)
        cstb = S.buf("cstb", [128, NCONST, 128], BF16)
        k.DMA("sp", cst[:, :, :], consts_d, r=[], w=[cst])
        k.CP("dve", cstb[:, :, :], cst[:, :, :], r=[cst], w=[cstb])
        ident, identb = cst[:, 0, :], cstb[:, 0, :]
        trib, negmb = cstb[:, 1, :], cstb[:, 2, :]
        mask01 = cst[:, 3, :]
        tri2i, tri2r, mask2 = cst[:, 4, :], cst[:, 5, :], cst[:, 6, :]
        onesb = cstb[:, 7, :]
        chunkind = cst[:, 9, 0:2]


        cvec = S.buf("cvec", [128, 8, 2], F32)
        k.DMA("sp", cvec[:, :, :], cvec_d, r=[], w=[cvec])
        adabT = S.buf("adabT", [128, 16], F32)
        k.DMA("sp", adabT[:, :], adabT_d, r=[], w=[adabT])
        nw0T = S.buf("nw0T", [128, 8], F32)
        k.DMA("sp", nw0T[:, :], nw0T_d, r=[], w=[nw0T])
        cw = S.buf("cw", [128, 8, 5], F32)
        k.DMA("sp", cw[:, :, :], cw_d, r=[], w=[cw])
        cb = S.buf("cb", [128, 8], F32)
        k.DMA("sp", cb[:, :], cb_d, r=[], w=[cb])
        sp8 = S.buf("sp8", [128, 3, 8], F32)
        k.DMA("sp", sp8[:, :, :], sp8_d, r=[], w=[sp8])
        lbrep = S.buf("lbrep", [128, 2, 512], F32)
        k.DMA("sp", lbrep[:, :, :], lbrep_d, r=[], w=[lbrep])

        if stage >= 7:
            initr = S.buf("initr", [128, ROWW], BF16)
            k.MEMSET("pool", initr[:, :], 0.0, w=[initr])
            k.MEMSET("pool", initr[:, 1026:1028].bitcast(I32), TRASH, w=[initr])
            zt = S.buf("zt", [128, D], F32)
            k.MEMSET("pool", zt[:, :], 0.0, w=[zt])
            xin_e = [Buf(f"xin_e{e_}") for e_ in range(NE)]
            for e_ in range(NE):
                for sc in range(8):
                    k.DMA("sp", xin_d[e_, sc * 128:(sc + 1) * 128, :], initr[:, :], r=[initr], w=[xin_e[e_]])
            for t in range(NH + 1):
                k.DMA("sp", acc_d[t * 128:(t + 1) * 128, :], zt[:, :], r=[zt], w=[acc_b])
        sc = S.buf("sc", [128, 8, 2], F32)
        k.ACT(sc[:, :, :], cvec[:, :, :], AF.Silu, r=[cvec], w=[sc])
        modT = S.buf("modT", [128, 16, 2], F32)
        modrep = S.buf("modrep", [128, 4 * D], F32)
        with contextlib.ExitStack() as es0:
            adap = [S.buf(f"adap{i}", [128, 8, 512], F32, es=es0) for i in range(2)]
            adabrep = S.buf("adabrep", [128, 4 * D], F32, es=es0)
            nwrep = S.buf("nwrep", [128, 3, D], F32, es=es0)
            k.DMA("sp", adabrep[:, :], adabrep_d, r=[], w=[adabrep])
            k.DMA("sp", nwrep[:, :, :], nwrep_d, r=[], w=[nwrep])
            if LITE:
                k.MEMSET("pool", modT[:, :, :], 0.1, w=[modT])
                k.MEMSET("pool", modrep[:, :], 0.1, w=[modrep])
            for j in range(0 if LITE else 12):
                ap_ = adap[j % 2]
                k.DMA("sp", ap_[:, :, :], adaw_d[:, j * 512:(j + 1) * 512].rearrange("(k p) n -> p k n", p=128),
                      r=[], w=[ap_])
                if j < 4:
                    for m in range(4):
                        cc = j * 4 + m
                        for kc in range(8):
                            k.MM(P[6][:, 0:2], ap_[:, kc, m * 128:(m + 1) * 128], sc[:, kc, :], kc == 0, kc == 7,
                                 r=[ap_, sc], w=[P[6]])
                        k.TS("dve", modT[:, cc, :], P[6][:, 0:2], adabT[:, cc:cc + 1], None, ALU.add, None,
                             r=[P[6], adabT], w=[modT])
                else:
                    pb = P[j % 2 + 1]
                    for kc in range(8):
                        k.MM(pb[:, :], sc[:, kc, 0:1].to_broadcast([128, 128]), ap_[:, kc, :], kc == 0, kc == 7,
                             r=[ap_, sc], w=[pb])
                    o = (j - 4) * 512
                    k.TT("dve", modrep[:, o:o + 512], pb[:, :], adabrep[:, o:o + 512], ALU.add,
                         r=[pb, adabrep], w=[modrep])
            k.TT("dve", modrep[:, 0:D], modrep[:, 0:D], nwrep[:, 0, :], ALU.mult, r=[modrep, nwrep], w=[modrep])
            k.STT("dve", modrep[:, 2 * D:3 * D], modrep[:, 2 * D:3 * D], 1.0, nwrep[:, 1, :], ALU.add, ALU.mult,
                  r=[modrep, nwrep], w=[modrep])
            k.TT("dve", modrep[:, 3 * D:4 * D], modrep[:, 3 * D:4 * D], nwrep[:, 2, :], ALU.mult,
                 r=[modrep, nwrep], w=[modrep])
            S.barrier()
        G1, B2, A2, G2 = (modrep[:, i * D:(i + 1) * D] for i in range(4))
        A0 = S.buf("A0", [128, 2, 8], F32)
        B0 = S.buf("B0", [128, 2, 8], F32)
        for i in range(2):
            k.STT("dve", A0[:, i, :], modT[:, 8:16, i], 1.0, nw0T[:, :], ALU.add, ALU.mult, r=[modT, nw0T], w=[A0])
            k.CP("dve", B0[:, i, :], modT[:, 0:8, i], r=[modT], w=[B0])
        aneg = S.buf("aneg", [128, 8], F32)
        k.ACT(aneg[:, :], sp8[:, 1, :], AF.Exp, r=[sp8], w=[aneg])
        k.TS("dve", aneg[:, :], aneg[:, :], -1.0, None, ALU.mult, None, r=[aneg], w=[aneg])
        dsk = S.buf("dsk", [128, 8], F32)
        k.TS("dve", dsk[:, :], sp8[:, 2, :], 0.5, None, ALU.mult, None, r=[sp8], w=[dsk])
        Dm = S.buf("Dm", [128, 8, 128], BF16)
        for j in range(8):
            k.TS("dve", Dm[:, j, :], ident, dsk[:, j:j + 1], None, ALU.mult, None, r=[cst, dsk], w=[Dm])
        c01 = S.buf("c01", [128, 2, 512], F32)
        k.TT("dve", c01[:, 0, :], lbrep[:, 0, :], lbrep[:, 1, :], ALU.subtract, r=[lbrep], w=[c01])
        k.ACT(c01[:, 1, :], c01[:, 0, :], AF.Tanh, r=[c01], w=[c01], scale=0.5)
        k.TS("dve", c01[:, 0, :], c01[:, 1, :], 0.25, 0.75, ALU.mult, ALU.add, r=[c01], w=[c01])
        k.TS("dve", c01[:, 1, :], c01[:, 1, :], -0.25, 0.25, ALU.mult, ALU.add, r=[c01], w=[c01])
        ST = S.buf("ST", [128, 512], F32)
        STb = S.buf("STb", [128, 512], BF16)
        SH = S.buf("SH", [128, 512], F32)
        SHb = S.buf("SHb", [128, 512], BF16)
        k.MEMSET("pool", ST[:, :], 0.0, w=[ST])
        k.MEMSET("pool", STb[:, :], 0.0, w=[STb])
        k.MEMSET("pool", SH[:, :], 0.0, w=[SH])
        k.MEMSET("pool", SHb[:, :], 0.0, w=[SHb])

        with contextlib.ExitStack() as es2:
            def sb(name, shape, dtype):
                return S.buf(name, shape, dtype, es=es2)
            wm = sb("wm", [128, 8, WCOLS], BF16)
            for kc in range(8):
                for (c0, c1) in ((0, 1796), (1796, WCOLS)):
                    k.DMA("pool", wm[:, kc, c0:c1], wmain_d[kc * 128:(kc + 1) * 128, c0:c1], r=[], w=[wm])
            xt = [sb(f"xt{i}", [128, D], F32) for i in range(3)]
            junk = sb("junk", [128, D], BF16)
            ssq = sb("ssq", [128, 2], F32)
            xn = sb("xn", [128, D], BF16)
            hT = [sb(f"hT{i}", [128, 8, 256], BF16) for i in range(2)]
            cacc = sb("cacc", [128, 8, 256], F32)
            xcT = sb("xcT", [128, 8, 256], BF16)
            xs_tm = sb("xs_tm", [128, 512], BF16)
            B_tm = sb("B_tm", [128, 256], BF16)
            dts = sb("dts", [128, 8, 8], F32)
            eatot = sb("eatot", [128, 8], F32)
            a_hi = sb("a_hi", [128, 8], BF16)
            a_lo = sb("a_lo", [128, 8], BF16)
            xdt = sb("xdt", [128, 512], BF16)
            xdtd = sb("xdtd", [128, 512], BF16)
            LT = sb("LT", [128, 8, 128], BF16)
            MT = sb("MT", [128, 8, 128], BF16)
            CBm = sb("CBm", [128, 2, 128], BF16)
            yoff = sb("yoff", [128, 512], F32)
            yo = sb("yo", [128, 512], BF16)
            qs = sb("qs", [128, 512], F32)
            vb = sb("vb", [128, 512], BF16)
            vm = sb("vm", [128, 2, 512], BF16)
            ff = sb("ff", [128, 512], F32)
            lf = sb("lf", [128, 512], F32)
            kk = sb("kk", [128, 512], F32)
            et = [sb(f"et{i}", [128, 512], F32) for i in range(2)]
            qdec = sb("qdec", [128, 512], BF16)
            kinv = sb("kinv", [128, 512], BF16)
            kdec = sb("kdec", [128, 512], BF16)
            qdT = sb("qdT", [128, 4, 128], BF16)
            kiT = sb("kiT", [128, 4, 128], BF16)
            attm = sb("attm", [128, 4, 128], BF16)
            oc = sb("oc", [64, 2, 512], BF16)
            ebt = sb("ebt", [128, 4, 2], F32)
            zg = sb("zg", [128, D], BF16)

            def load_x(ti_all):
                b = xt[ti_all % 3]
                src = ctx_d[ti_all * 128:(ti_all + 1) * 128, :] if ti_all < 2 else \
                    x_d[(ti_all - 2) * 128:(ti_all - 1) * 128, :]
                k.DMA("sp", b[:, :], src, r=[], w=[b])

            def norm_T(ti_all, hbuf, col0, which):
                b = xt[ti_all % 3]
                k.MEMSET("pool", ssq[:, 0:1], 0.0, w=[ssq])
                k.ACT(junk[:, :], b[:, :], AF.Square, r=[b], w=[junk, ssq], accum=ssq[:, 0:1])
                k.ACT(ssq[:, 1:2], ssq[:, 0:1], AF.Ln, r=[ssq], w=[ssq], bias=EPS, scale=1.0 / D)
                k.ACT(ssq[:, 1:2], ssq[:, 1:2], AF.Exp, r=[ssq], w=[ssq], scale=-0.5)
                k.TS("pool", xn[:, :], b[:, :], ssq[:, 1:2], None, ALU.mult, None, r=[b, ssq], w=[xn])
                for kc in range(8):
                    k.TR(P0b[:, kc * 128:(kc + 1) * 128], xn[:, kc * 128:(kc + 1) * 128], identb,
                         r=[xn, cstb], w=[P[0]])
                for kc in range(8):
                    o = hbuf[:, kc, col0:col0 + 128]
                    i_ = P0b[:, kc * 128:(kc + 1) * 128]
                    if kc % 2 == 0:
                        k.ACT(o, i_, AF.Identity, r=[P[0], A0, B0], w=[hbuf],
                              bias=B0[:, which, kc:kc + 1], scale=A0[:, which, kc:kc + 1])
                    else:
                        k.TS("dve", o, i_, A0[:, which, kc:kc + 1], B0[:, which, kc:kc + 1], ALU.mult, ALU.add,
                             r=[P[0], A0, B0], w=[hbuf])

            def conv_stage(hbuf, T, roww, chunks):
                per_bank = 512 // T
                for ci, c in enumerate(chunks):
                    pb = P[1 + ci // per_bank]
                    po = (ci % per_bank) * T
                    for kc in range(8):
                        k.MM(pb[:, po:po + T], wm[:, kc, C_XBC + c * 128:C_XBC + (c + 1) * 128], hbuf[:, kc, 0:T],
                             kc == 0, kc == 7, r=[wm, hbuf], w=[pb])
                for ci, c in enumerate(chunks):
                    pb = P[1 + ci // per_bank]
                    po = (ci % per_bank) * T
                    src = pb[:, po:po + T]
                    acc = cacc[:, c, 0:T]
                    k.ACT(acc, src, AF.Identity, r=[pb, cw, cb], w=[cacc], bias=cb[:, c:c + 1], scale=cw[:, c, 2:3])
                    srcv = src.rearrange("p (r w) -> p r w", w=roww)
                    accv = acc.rearrange("p (r w) -> p r w", w=roww)
                    for kt in (0, 1, 3, 4):
                        s = kt - 2
                        if s > 0:
                            o_, i_ = accv[:, :, 0:roww - s], srcv[:, :, s:roww]
                        else:
                            o_, i_ = accv[:, :, -s:roww], srcv[:, :, 0:roww + s]
                        k.STT("dve", o_, i_, cw[:, c, kt:kt + 1], o_, ALU.mult, ALU.add, r=[pb, cw, cacc], w=[cacc])
                    k.ACT(xcT[:, c, 0:T], acc, AF.Silu, r=[cacc], w=[xcT])

            def scan_tile(hbuf, col0, tcol, lat, ti, zgproj):
                hsl = lambda kc: hbuf[:, kc, col0:col0 + 128]
                if zgproj:
                    for (pb, c0) in ((P[1], C_Z), (P[2], C_G)):
                        for kc in range(8):
                            k.MM(pb[:, :], hsl(kc), wm[:, kc, c0:c0 + 512], kc == 0, kc == 7, r=[hbuf, wm], w=[pb])
                    k.ACT(zg[:, 0:512], P[1][:, :], AF.Silu, r=[P[1]], w=[zg])
                    k.ACT(zg[:, 512:1024], P[2][:, :], AF.Silu, r=[P[2]], w=[zg])
                    k.DMA("sp", zgbuf_d[ti * 128:(ti + 1) * 128, :], zg[:, :], r=[zg], w=[zgbuf_b])
                for (pb, c0) in ((P[3], C_Q), (P[4], C_F), (P[5], C_I)):
                    for kc in range(8):
                        k.MM(pb[:, :], hsl(kc), wm[:, kc, c0:c0 + 512], kc == 0, kc == 7, r=[hbuf, wm], w=[pb])
                for kc in range(8):
                    k.MM(P[6][:, 0:8], hsl(kc), wm[:, kc, C_DT:C_DT + 8], kc == 0, kc == 7, r=[hbuf, wm], w=[P[6]])
                if lat:
                    k.ACT(qs[:, :], P[3][:, :], AF.Silu, r=[P[3]], w=[qs])
                k.ACT(ff[:, :], P[4][:, :], AF.Tanh, r=[P[4]], w=[ff], scale=0.5)
                k.CP("dve", vb[:, :], P[5][:, :], r=[P[5]], w=[vb])
                for c in range(2):
                    k.TS("pool", vm[:, c, :], vb[:, :], chunkind[:, c:c + 1], None, ALU.mult, None, r=[vb, cst], w=[vm])
                if SUBCUT == 'A':
                    return
                for c in range(6):
                    k.TR(P0b[:, c * 128:(c + 1) * 128], xcT[:, c, tcol:tcol + 128], identb, r=[xcT, cstb], w=[P[0]])
                if SUBCUT == 'B1':
                    return
                k.CP("act", xs_tm[:, :], P0b[:, 0:512], r=[P[0]], w=[xs_tm])
                if SUBCUT == 'B2':
                    return
                k.CP("act", B_tm[:, :], P0b[:, 512:768], r=[P[0]], w=[B_tm])
                if SUBCUT == 'B':
                    return
                v_, av_, l_, dt_, a_, nacs, eacs, w2 = (dts[:, i, :] for i in range(8))
                k.TT("dve", v_, P[6][:, 0:8], sp8[:, 0, :], ALU.add, r=[P[6], sp8], w=[dts])
                k.TS("dve", av_, v_, 30.0, None, ALU.min, None, r=[dts], w=[dts])
                k.ACT(av_, av_, AF.Exp, r=[dts], w=[dts])
                k.ACT(l_, av_, AF.Ln, r=[dts], w=[dts], bias=1.0)
                k.TT("dve", dt_, l_, v_, ALU.max, r=[dts], w=[dts])
                k.TT("dve", a_, dt_, aneg[:, :], ALU.mult, r=[dts, aneg], w=[dts])
                k.CP("dve", a_hi[:, :], a_, r=[dts], w=[a_hi])
                k.TT("dve", a_lo[:, :], a_, a_hi[:, :], ALU.subtract, r=[dts, a_hi], w=[a_lo])
                if SUBCUT == 'C1':
                    return
                k.MM(P[6][:, 64:72], trib, a_hi[:, :], True, False, r=[cstb, a_hi], w=[P[6]])
                k.MM(P[6][:, 64:72], trib, a_lo[:, :], False, True, r=[cstb, a_lo], w=[P[6]])
                k.MM(P[6][:, 128:136], onesb, a_hi[:, :], True, False, r=[cstb, a_hi], w=[P[6]])
                k.MM(P[6][:, 128:136], onesb, a_lo[:, :], False, True, r=[cstb, a_lo], w=[P[6]])
                k.TS("dve", nacs, P[6][:, 64:72], -1.0, None, ALU.mult, None, r=[P[6]], w=[dts])
                k.ACT(eacs, P[6][:, 64:72], AF.Exp, r=[P[6]], w=[dts])
                k.TT("dve", w2, P[6][:, 128:136], nacs, ALU.add, r=[P[6], dts], w=[dts])
                k.ACT(w2, w2, AF.Exp, r=[dts], w=[dts])
                k.ACT(eatot[:, :], P[6][:, 128:136], AF.Exp, r=[P[6]], w=[eatot])
                k.TT("dve", w2, w2, dt_, ALU.mult, r=[dts], w=[dts])
                if SUBCUT == 'C2':
                    return
                xs3 = xs_tm[:, :].rearrange("p (j q) -> p j q", q=64)
                k.TT("dve", xdtd[:, :].rearrange("p (j q) -> p j q", q=64), xs3, w2.unsqueeze(2).to_broadcast([128, 8, 64]), ALU.mult,
                     r=[xs_tm, dts], w=[xdtd])
                if lat:
                    k.TT("pool", xdt[:, :].rearrange("p (j q) -> p j q", q=64), xs3, dt_.unsqueeze(2).to_broadcast([128, 8, 64]), ALU.mult,
                         r=[xs_tm, dts], w=[xdt])
                    for j in range(8):
                        pa = P[1 + j // 4]
                        o = pa[:, (j % 4) * 128:(j % 4 + 1) * 128]
                        k.MM(o, a_hi[:, j:j + 1].to_broadcast([128, 128]), trib, True, False, r=[a_hi, cstb], w=[pa])
                        k.MM(o, a_lo[:, j:j + 1].to_broadcast([128, 128]), trib, False, False, r=[a_lo, cstb], w=[pa])
                        k.MM(o, identb, negmb, False, True, r=[cstb], w=[pa])
                        k.ACT(LT[:, j, :], o, AF.Exp, r=[pa, dts], w=[LT], bias=nacs[:, j:j + 1])
                    for g in range(2):
                        k.MM(P[6][:, 128 + g * 128:256 + g * 128], xcT[:, 4 + g, tcol:tcol + 128],
                             xcT[:, 6 + g, tcol:tcol + 128], True, True, r=[xcT], w=[P[6]])
                    k.TT("dve", CBm[:, :, :], P[6][:, 128:384].rearrange("p (g l) -> p g l", g=2),
                         mask01.unsqueeze(1).to_broadcast([128, 2, 128]), ALU.mult, r=[P[6], cst], w=[CBm])
                    for g in range(2):
                        k.TT("pool", MT[:, 4 * g:4 * g + 4, :], LT[:, 4 * g:4 * g + 4, :],
                             CBm[:, g:g + 1, :].to_broadcast([128, 4, 128]), ALU.mult, r=[LT, CBm], w=[MT])
                    for j in range(8):
                        o = P[3][:, j * 64:(j + 1) * 64]
                        k.MM(o, MT[:, j, :], xdt[:, j * 64:(j + 1) * 64], True, False, r=[MT, xdt], w=[P[3]])
                        k.MM(o, Dm[:, j, :], xs_tm[:, j * 64:(j + 1) * 64], False, True, r=[Dm, xs_tm], w=[P[3]])
                    for g in range(2):
                        k.MM(P[7][:, g * 256:(g + 1) * 256], xcT[:, 6 + g, tcol:tcol + 128],
                             STb[:, g * 256:(g + 1) * 256], True, True, r=[xcT, STb], w=[P[7]])
                    k.TT("dve", yoff[:, :].rearrange("p (j q) -> p j q", q=64),
                         P[7][:, :].rearrange("p (j q) -> p j q", q=64),
                         eacs.unsqueeze(2).to_broadcast([128, 8, 64]), ALU.mult, r=[P[7], dts], w=[yoff])
                    k.TT("dve", yo[:, :], P[3][:, :], yoff[:, :], ALU.add, r=[P[3], yoff], w=[yo])
                if SUBCUT == 'C':
                    return
                for g in range(2):
                    k.MM(P[7][:, g * 256:(g + 1) * 256], B_tm[:, g * 128:(g + 1) * 128],
                         xdtd[:, g * 256:(g + 1) * 256], True, True, r=[B_tm, xdtd], w=[P[7]])
                k.TT("dve", ST[:, :].rearrange("p (j q) -> p j q", q=64), ST[:, :].rearrange("p (j q) -> p j q", q=64),
                     eatot[:, :].unsqueeze(2).to_broadcast([128, 8, 64]), ALU.mult, r=[ST, eatot], w=[ST])
                k.TT("dve", ST[:, :], ST[:, :], P[7][:, :], ALU.add, r=[ST, P[7]], w=[ST])
                k.CP("act", STb[:, :], ST[:, :], r=[ST], w=[STb])
                if SUBCUT == 'D':
                    return
                k.TT("dve", ff[:, :], ff[:, :], c01[:, 1, :], ALU.mult, r=[ff, c01], w=[ff])
                k.TT("dve", ff[:, :], ff[:, :], c01[:, 0, :], ALU.add, r=[ff, c01], w=[ff])
                k.ACT(lf[:, :], ff[:, :], AF.Ln, r=[ff], w=[lf])
                k.TS("pool", kk[:, :], ff[:, :], -1.0, 1.0, ALU.mult, ALU.add, r=[ff], w=[kk])
                k.MM(P[4][:, :], tri2i, lf[:, :], True, True, r=[cst, lf], w=[P[4]])
                k.MM(P[5][:, :], tri2r, lf[:, :], True, True, r=[cst, lf], w=[P[5]])
                for h in range(4):
                    k.MM(P[7][:, h * 64:h * 64 + 2], lf[:, h * 128:(h + 1) * 128], chunkind, True, True,
                         r=[lf, cst], w=[P[7]])
                k.ACT(ebt[:, :, :], P[7][:, 0:256].rearrange("p (h c) -> p h c", c=64)[:, :, 0:2], AF.Exp,
                      r=[P[7]], w=[ebt])
                k.ACT(et[0][:, :], P[5][:, :], AF.Exp, r=[P[5]], w=[et[0]])
                k.TT("dve", kdec[:, :], kk[:, :], et[0][:, :], ALU.mult, r=[kk, et[0]], w=[kdec])
                if lat:
                    k.ACT(et[1][:, :], P[4][:, :], AF.Exp, r=[P[4]], w=[et[1]])
                    k.TT("dve", qdec[:, :], qs[:, :], et[1][:, :], ALU.mult, r=[qs, et[1]], w=[qdec])
                    k.ACT(et[0][:, :], P[4][:, :], AF.Exp, r=[P[4]], w=[et[0]], scale=-1.0)
                    k.TT("pool", kinv[:, :], kk[:, :], et[0][:, :], ALU.mult, r=[kk, et[0]], w=[kinv])
                    for h in range(4):
                        k.TR(P0b[:, h * 128:(h + 1) * 128], qdec[:, h * 128:(h + 1) * 128], identb,
                             r=[qdec, cstb], w=[P[0]])
                        k.TR(P0b[:, 512 + h * 128:512 + (h + 1) * 128], kinv[:, h * 128:(h + 1) * 128], identb,
                             r=[kinv, cstb], w=[P[0]])
                    k.CP("act", qdT[:, :, :], P0b[:, 0:512].rearrange("p (h l) -> p h l", h=4), r=[P[0]], w=[qdT])
                    k.CP("act", kiT[:, :, :], P0b[:, 512:1024].rearrange("p (h l) -> p h l", h=4), r=[P[0]], w=[kiT])
                    for h in range(4):
                        k.MM(P[4][:, h * 128:(h + 1) * 128], kiT[:, h, :], qdT[:, h, :], True, True,
                             r=[kiT, qdT], w=[P[4]])
                    k.TT("dve", attm[:, :, :], P[4][:, :].rearrange("p (h l) -> p h l", h=4),
                         mask2.unsqueeze(1).to_broadcast([128, 4, 128]), ALU.mult, r=[P[4], cst], w=[attm])
                if SUBCUT == 'E':
                    return
                for c in range(2):
                    if lat:
                        for h in range(4):
                            o = P[5][0:64, h * 128:(h + 1) * 128]
                            k.MM(o, attm[:, h, c * 64:(c + 1) * 64], vb[:, h * 128:(h + 1) * 128], True, False,
                                 r=[attm, vb], w=[P[5]])
                            k.MM(o, qdT[:, h, c * 64:(c + 1) * 64], SHb[:, h * 128:(h + 1) * 128], False, True,
                                 r=[qdT, SHb], w=[P[5]])
                        k.CP("act", oc[:, c, :], P[5][0:64, :], r=[P[5]], w=[oc])
                    for h in range(4):
                        k.MM(P[7][:, h * 128:(h + 1) * 128], kdec[:, h * 128:(h + 1) * 128],
                             vm[:, c, h * 128:(h + 1) * 128], True, True, r=[kdec, vm], w=[P[7]])
                    for h in range(4):
                        sl = slice(h * 128, (h + 1) * 128)
                        k.STT("dve", SH[:, sl], SH[:, sl], ebt[:, h, c:c + 1], P[7][:, sl], ALU.mult, ALU.add,
                              r=[SH, ebt, P[7]], w=[SH])
                    k.CP("act", SHb[:, :], SH[:, :], r=[SH], w=[SHb])
                if lat:
                    if ti < NH:
                        dst, dstb, rows = ybuf_d, ybuf_b, slice(ti * 128, (ti + 1) * 128)
                    else:
                        dst, dstb = ysend_t[(ti - NH) // 8].ap(), ysend_b
                        rows = slice(((ti - NH) % 8) * 128, ((ti - NH) % 8 + 1) * 128)
                    k.DMA("sp", dst[rows, 0:512], yo[:, :], r=[yo], w=[dstb])
                    k.DMA("sp", dst[rows, 512:1024].rearrange("(c p) v -> p c v", p=64), oc[:, :, :],
                          r=[oc], w=[dstb])

            cut = int(os.environ.get("KCUT", "99"))
            load_x(0)
            load_x(1)
            load_x(2)
            if cut >= 1:
                norm_T(0, hT[0], 0, 1)
                norm_T(1, hT[0], 128, 1)
            if cut >= 2:
                conv_stage(hT[0], 256, 256, [0, 1, 2, 3])
                conv_stage(hT[0], 256, 256, [4, 5, 6, 7])
            if cut >= 3:
                for sub in range(2):
                    scan_tile(hT[0], sub * 128, sub * 128, False, -1, False)
            ntl = NT if stage >= 2 else 4
            ntl = int(os.environ.get("KNT", ntl))
            if cut < 4:
                ntl = 0
            for ti in range(ntl):
                if ti + 1 < ntl:
                    load_x(ti + 3)
                hb = hT[(ti + 1) % 2]
                norm_T(ti + 2, hb, 0, 0)
                conv_stage(hb, 128, 64, list(range(8)))
                scan_tile(hb, 0, 0, True, ti, ti < NH)

            S.barrier()
        if stage < 3:
            with contextlib.ExitStack() as esd:
                tb = S.buf("dbg_b", [128, D], BF16, es=esd)
                tf = S.buf("dbg_f", [128, D], F32, es=esd)
                for ti in range(min(ntl, NH)):
                    rows = slice(ti * 128, (ti + 1) * 128)
                    k.DMA("sp", tb[:, :], ybuf_d[rows, :], r=[ybuf_b], w=[tb])
                    k.CP("dve", tf[:, :], tb[:, :], r=[tb], w=[tf])
                    k.DMA("sp", dbg_d[rows, :], tf[:, :], r=[tf], w=[])
        else:
            for c in range(4):
                if SIM:
                    for hh in range(2):
                        k.DMA("sp", ygath_t[c].ap()[hh * 1024:(hh + 1) * 1024, :], ysend_t[c].ap(), r=[ysend_b], w=[ygath_b])
                else:
                    S.coll((lambda c: lambda e: e.collective_compute(
                        "AllGather", ALU.bypass, replica_groups=PAIRS, ins=[ysend_t[c].ap().opt()],
                        outs=[ygath_t[c].ap().opt()]))(c), r=[ysend_b], w=[ygath_b])
            affall = S.buf("affall", [128, NH, NE], F32)
            pidx = S.buf("pidx", [128, 2 * NH], I32)
            k.DMA("sp", pidx[:, :], pidx_d, r=[], w=[pidx])
            with contextlib.ExitStack() as es4:
                def sb(name, shape, dtype):
                    return S.buf(name, shape, dtype, es=es4)
                wout = sb("wout", [128, 8, D], BF16)
                for kc in range(8):
                    k.DMA("pool", wout[:, kc, :], wout_d[kc * 128:(kc + 1) * 128, :], r=[], w=[wout])
                wr = sb("wr", [128, 8, NE], BF16)
                k.DMA("pool", wr[:, :, :], wr_d.rearrange("(k p) e -> p k e", p=128), r=[], w=[wr])
                snw = sb("snw", [128, 512], F32)
                hnw = sb("hnw", [128, 512], F32)
                k.DMA("sp", snw[:, :], snw_d, r=[], w=[snw])
                k.DMA("sp", hnw[:, :], hnw_d, r=[], w=[hnw])
                yown = [sb(f"yown{i}", [128, D], BF16) for i in range(2)]
                ypar = [sb(f"ypar{i}", [128, D], BF16) for i in range(2)]
                zgt = [sb(f"zgt{i}", [128, D], BF16) for i in range(2)]
                xt2 = [sb(f"xt2{i}", [128, D], F32) for i in range(2)]
                ysum = sb("ysum", [128, D], F32)
                junk4 = sb("junk4", [128, D], BF16)
                ssq6 = sb("ssq6", [128, 8], F32)
                rs6 = sb("rs6", [128, 8], F32)
                tmpo = sb("tmpo", [128, 512], F32)
                ylat = sb("ylat", [128, D], BF16)
                ylT = sb("ylT", [128, 8, 128], BF16)
                ssq2 = sb("ssq2", [128, 4], F32)
                x1 = sb("x1", [128, D], F32)
                h2f = sb("h2f", [128, D], F32)
                h2b = sb("h2b", [128, ROWW], BF16)
                h2T = sb("h2T", [128, 8, 128], BF16)
                smx = sb("smx", [128, 4], F32)
                ex = sb("ex", [128, NE], F32)
                k.MEMSET("pool", h2b[:, 1024:ROWW], 0.0, w=[h2b])

                def p4_load(t):
                    i = t % 2
                    rows = slice(t * 128, (t + 1) * 128)
                    k.DMA("sp", yown[i][:, :], ybuf_d[rows, :], r=[ybuf_b], w=[yown[i]])
                    k.DMA("sp", zgt[i][:, :], zgbuf_d[rows, :], r=[zgbuf_b], w=[zgt[i]])
                    k.DMA("sp", xt2[i][:, :], x_d[rows, :], r=[], w=[xt2[i]])
                    S.dma("pool", lambda e: e.indirect_dma_start(
                        out=ypar[i][:, :], out_offset=None, in_=ygath_t[3 - t // 8].ap(),
                        in_offset=bass.IndirectOffsetOnAxis(ap=pidx[:, t:t + 1], axis=0)),
                        r=[ygath_b, pidx], w=[ypar[i]])

                def rstd_from(ssq_ap, out_ap, n, rbufs):
                    k.ACT(out_ap, ssq_ap, AF.Ln, r=rbufs, w=rbufs, bias=EPS, scale=1.0 / n)
                    k.ACT(out_ap, out_ap, AF.Exp, r=rbufs, w=rbufs, scale=-0.5)

                nt4 = NH if stage >= 4 else 2
                nt4 = int(os.environ.get("KNT4", nt4))
                p4_load(0)
                for t in range(nt4):
                    i = t % 2
                    if t + 1 < nt4:
                        p4_load(t + 1)
                    rows = slice(t * 128, (t + 1) * 128)
                    k.TT("dve", ysum[:, :], yown[i][:, :], ypar[i][:, :], ALU.add, r=[yown[i], ypar[i]], w=[ysum])
                    k.TT("pool", ysum[:, 0:512], ysum[:, 0:512], zgt[i][:, 0:512], ALU.mult, r=[ysum, zgt[i]], w=[ysum])
                    k.MEMSET("pool", ssq6[:, :], 0.0, w=[ssq6])
                    for g in range(2):
                        k.ACT(junk4[:, g * 256:(g + 1) * 256], ysum[:, g * 256:(g + 1) * 256], AF.Square,
                              r=[ysum], w=[junk4, ssq6], accum=ssq6[:, g:g + 1])
                    for h in range(4):
                        sl = slice(512 + h * 128, 512 + (h + 1) * 128)
                        k.ACT(junk4[:, sl], ysum[:, sl], AF.Square, r=[ysum], w=[junk4, ssq6],
                              accum=ssq6[:, 2 + h:3 + h])
                    rstd_from(ssq6[:, 0:2], rs6[:, 0:2], 256, [ssq6, rs6])
                    rstd_from(ssq6[:, 2:6], rs6[:, 2:6], 128, [ssq6, rs6])
                    for g in range(2):
                        sl = slice(g * 256, (g + 1) * 256)
                        k.STT("dve", ylat[:, sl], ysum[:, sl], rs6[:, g:g + 1], snw[:, sl], ALU.mult, ALU.mult,
                              r=[ysum, rs6, snw], w=[ylat])
                    for h in range(4):
                        sl = slice(h * 128, (h + 1) * 128)
                        sl2 = slice(512 + h * 128, 512 + (h + 1) * 128)
                        k.STT("dve", tmpo[:, sl], ysum[:, sl2], rs6[:, 2 + h:3 + h], hnw[:, sl], ALU.mult, ALU.mult,
                              r=[ysum, rs6, hnw], w=[tmpo])
                    k.TT("pool", ylat[:, 512:1024], tmpo[:, :], zgt[i][:, 512:1024], ALU.mult,
                         r=[tmpo, zgt[i]], w=[ylat])
                    for kc in range(8):
                        k.TR(P0b[:, kc * 128:(kc + 1) * 128], ylat[:, kc * 128:(kc + 1) * 128], identb,
                             r=[ylat, cstb], w=[P[0]])
                    k.CP("act", ylT[:, 0:4, :], P0b[:, 0:512].rearrange("p (k l) -> p k l", k=4), r=[P[0]], w=[ylT])
                    k.CP("act", ylT[:, 4:8, :], P0b[:, 512:1024].rearrange("p (k l) -> p k l", k=4), r=[P[0]], w=[ylT])
                    for n in range(2):
                        for kc in range(8):
                            k.MM(P[1 + n][:, :], ylT[:, kc, :], wout[:, kc, n * 512:(n + 1) * 512], kc == 0, kc == 7,
                                 r=[ylT, wout], w=[P[1 + n]])
                    k.MEMSET("pool", ssq2[:, :], 0.0, w=[ssq2])
                    for n in range(2):
                        k.ACT(junk4[:, n * 512:(n + 1) * 512], P[1 + n][:, :], AF.Square, r=[P[1 + n]],
                              w=[junk4, ssq2], accum=ssq2[:, n:n + 1])
                    k.TT("dve", ssq2[:, 2:3], ssq2[:, 0:1], ssq2[:, 1:2], ALU.add, r=[ssq2], w=[ssq2])
                    rstd_from(ssq2[:, 2:3], ssq2[:, 3:4], D, [ssq2])
                    for n in range(2):
                        sl = slice(n * 512, (n + 1) * 512)
                        k.STT("dve", x1[:, sl], P[1 + n][:, :], ssq2[:, 3:4], G1[:, sl], ALU.mult, ALU.mult,
                              r=[P[1 + n], ssq2, modrep], w=[x1])
                    k.TT("pool", x1[:, :], x1[:, :], xt2[i][:, :], ALU.add, r=[x1, xt2[i]], w=[x1])
                    k.DMA("sp", x1buf_d[rows, :], x1[:, :], r=[x1], w=[x1buf_b])
                    k.MEMSET("pool", ssq2[:, 0:1], 0.0, w=[ssq2])
                    k.ACT(junk4[:, :], x1[:, :], AF.Square, r=[x1], w=[junk4, ssq2], accum=ssq2[:, 0:1])
                    rstd_from(ssq2[:, 0:1], ssq2[:, 1:2], D, [ssq2])
                    k.STT("dve", h2f[:, :], x1[:, :], ssq2[:, 1:2], A2, ALU.mult, ALU.mult, r=[x1, ssq2, modrep], w=[h2f])
                    k.TT("dve", h2b[:, 0:D], h2f[:, :], B2, ALU.add, r=[h2f, modrep], w=[h2b])
                    k.CP("dve", h2b[:, 1026:1028].bitcast(I32), pidx[:, NH + t:NH + t + 1], r=[pidx], w=[h2b])
                    k.DMA("sp", h2buf_d[rows, :], h2b[:, :], r=[h2b], w=[h2buf_b])
                    for kc in range(8):
                        k.TR(P0b[:, kc * 128:(kc + 1) * 128], h2b[:, kc * 128:(kc + 1) * 128], identb,
                             r=[h2b, cstb], w=[P[0]])
                    k.CP("act", h2T[:, :, :], P0b[:, :].rearrange("p (k l) -> p k l", k=8), r=[P[0]], w=[h2T])
                    for kc in range(8):
                        k.MM(P[6][:, 0:NE], h2T[:, kc, :], wr[:, kc, :], kc == 0, kc == 7, r=[h2T, wr], w=[P[6]])
                    S.op("dve", lambda e: e.tensor_reduce(out=smx[:, 0:1], in_=P[6][:, 0:NE], axis=AX.X, op=ALU.max),
                         r=[P[6]], w=[smx])
                    k.TS("dve", smx[:, 1:2], smx[:, 0:1], -1.0, None, ALU.mult, None, r=[smx], w=[smx])
                    k.MEMSET("pool", smx[:, 2:3], 0.0, w=[smx])
                    k.ACT(ex[:, :], P[6][:, 0:NE], AF.Exp, r=[P[6], smx], w=[ex, smx], bias=smx[:, 1:2],
                          accum=smx[:, 2:3])
                    S.op("dve", lambda e: e.reciprocal(out=smx[:, 3:4], in_=smx[:, 2:3]), r=[smx], w=[smx])
                    k.TS("dve", affall[:, t, :], ex[:, :], smx[:, 3:4], None, ALU.mult, None, r=[ex, smx], w=[affall])
                S.barrier()
            if stage < 5:
                with contextlib.ExitStack() as esd:
                    tf = S.buf("dbg_f", [128, D], F32, es=esd)
                    for t in range(nt4):
                        rows = slice(t * 128, (t + 1) * 128)
                        k.DMA("sp", tf[:, :], x1buf_d[rows, :], r=[x1buf_b], w=[tf])
                        k.DMA("sp", dbg_d[rows, :], tf[:, :], r=[tf], w=[])
                    k.DMA("sp", dbg_d[HALF:HALF + 128, 0:NH * NE], affall[:, :, :].rearrange("p t e -> p (t e)"),
                          r=[affall], w=[])
        if stage >= 5:
            k.DMA("sp", affs_t.ap().rearrange("(t p) e -> p t e", p=128), affall[:, :, :], r=[affall], w=[affs_b])
            if SIM:
                for hh in range(2):
                    k.DMA("sp", affg_t.ap()[hh * HALF:(hh + 1) * HALF, :], affs_t.ap(), r=[affs_b], w=[affg_b])
            else:
                S.coll(lambda e: e.collective_compute("AllGather", ALU.bypass, replica_groups=PAIRS,
                                                      ins=[affs_t.ap().opt()], outs=[affg_t.ap().opt()]),
                       r=[affs_b], w=[affg_b])
            tau = S.buf("tau", [128, NE], F32)
            gate = S.buf("gate", [128, NH, NE], F32)
            sloti = S.buf("sloti", [128, NH, NE], I32)
            with contextlib.ExitStack() as es6:
                def sb(name, shape, dtype):
                    return S.buf(name, shape, dtype, es=es6)
                affb = sb("affb", [128, NT, NE], F32)
                k.DMA("sp", affb[:, :, :], affg_t.ap().rearrange("(t p) e -> p t e", p=128), r=[affg_b], w=[affb])
                cmpb = sb("cmpb", [128, NT, NE], F32)
                hi = sb("hi", [128, NE], F32)
                d2 = sb("d2", [128, NE], F32)
                mid = sb("mid", [128, NE], F32)
                cntp = sb("cntp", [128, NE], F32)
                ge = sb("ge", [128, NE], F32)
                k.MEMSET("pool", tau[:, :], 0.0, w=[tau])
                k.MEMSET("pool", hi[:, :], 1.0001, w=[hi])
                ones_f = cst[:, 7, :]
                for it in range(32):
                    k.TT("dve", d2[:, :], hi[:, :], tau[:, :], ALU.subtract, r=[hi, tau], w=[d2])
                    k.STT("dve", mid[:, :], d2[:, :], 0.5, tau[:, :], ALU.mult, ALU.add, r=[d2, tau], w=[mid])
                    k.TT("dve", cmpb[:, :, :], affb[:, :, :], mid[:, :].unsqueeze(1).to_broadcast([128, NT, NE]),
                         ALU.is_ge, r=[affb, mid], w=[cmpb])
                    S.op("dve", lambda e: e.tensor_reduce(out=cntp[:, :], in_=cmpb[:, :, :].rearrange("p t e -> p e t"),
                                                          axis=AX.X, op=ALU.add), r=[cmpb], w=[cntp])
                    k.MM(P[6][:, 0:NE], ones_f, cntp[:, :], True, True, r=[cst, cntp], w=[P[6]])
                    k.TS("dve", ge[:, :], P[6][:, 0:NE], float(CAP), None, ALU.is_ge, None, r=[P[6]], w=[ge])
                    k.TT("dve", ge[:, :], ge[:, :], d2[:, :], ALU.mult, r=[ge, d2], w=[ge])
                    k.STT("dve", tau[:, :], ge[:, :], 0.5, tau[:, :], ALU.mult, ALU.add, r=[ge, tau], w=[tau])
                    k.STT("dve", hi[:, :], d2[:, :], -0.5, hi[:, :], ALU.mult, ALU.add, r=[d2, hi], w=[hi])
                    k.STT("dve", hi[:, :], ge[:, :], 0.5, hi[:, :], ALU.mult, ALU.add, r=[ge, hi], w=[hi])
                maskf = sb("maskf", [128, NH, NE], F32)
                maskb = sb("maskb", [128, NH * NE], BF16)
                totE = sb("totE", [128, NE, NH], F32)
                cumE = sb("cumE", [128, NE, NH], F32)
                ones32 = sb("ones32", [128, NH], F32)
                posf = sb("posf", [128, NH, NE], F32)
                k.TT("dve", maskf[:, :, :], affall[:, :, :], tau[:, :].unsqueeze(1).to_broadcast([128, NH, NE]),
                     ALU.is_ge, r=[affall, tau], w=[maskf])
                k.TT("dve", gate[:, :, :], maskf[:, :, :], affall[:, :, :], ALU.mult, r=[maskf, affall], w=[gate])
                k.CP("dve", maskb[:, :], maskf[:, :, :].rearrange("p t e -> p (t e)"), r=[maskf], w=[maskb])
                k.MM(P[1][:, :], cstb[:, 8, :], maskb[:, :], True, True, r=[cstb, maskb], w=[P[1]])
                k.MM(P[2][:, :], onesb, maskb[:, :], True, True, r=[cstb, maskb], w=[P[2]])
                k.CP("dve", totE[:, :, :], P[2][:, :].rearrange("p (t e) -> p e t", e=NE), r=[P[2]], w=[totE])
                k.MEMSET("pool", ones32[:, :], 1.0, w=[ones32])
                for e_ in range(NE):
                    S.op("dve", (lambda e_: lambda eng: eng.tensor_tensor_scan(
                        out=cumE[:, e_, :], data0=ones32[:, :], data1=totE[:, e_, :], initial=0.0,
                        op0=ALU.mult, op1=ALU.add))(e_), r=[ones32, totE], w=[cumE])
                k.TT("dve", cumE[:, :, :], cumE[:, :, :], totE[:, :, :], ALU.subtract, r=[cumE, totE], w=[cumE])
                k.TT("dve", posf[:, :, :], P[1][:, :].rearrange("p (t e) -> p t e", e=NE),
                     cumE[:, :, :].rearrange("p e t -> p t e"), ALU.add, r=[P[1], cumE], w=[posf])
                ltm = sb("ltm", [128, NH, NE], F32)
                k.TS("dve", ltm[:, :, :], posf[:, :, :], float(CAP), None, ALU.is_lt, None, r=[posf], w=[ltm])
                k.TT("dve", maskf[:, :, :], maskf[:, :, :], ltm[:, :, :], ALU.mult, r=[maskf, ltm], w=[maskf])
                k.TT("dve", posf[:, :, :], posf[:, :, :], cst[:, 9, 2:2 + NE].unsqueeze(1).to_broadcast([128, NH, NE]),
                     ALU.add, r=[posf, cst], w=[posf])
                k.TT("dve", posf[:, :, :], posf[:, :, :], maskf[:, :, :], ALU.mult, r=[posf, maskf], w=[posf])
                k.TS("dve", maskf[:, :, :], maskf[:, :, :], -1.0, 1.0, ALU.mult, ALU.add, r=[maskf], w=[maskf])
                k.TS("dve", maskf[:, :, :], maskf[:, :, :], cst[:, 9, 18:19], None, ALU.mult, None, r=[maskf, cst], w=[maskf])
                k.TT("dve", posf[:, :, :], posf[:, :, :], maskf[:, :, :], ALU.add, r=[posf, maskf], w=[posf])
                k.CP("dve", sloti[:, :, :], posf[:, :, :], r=[posf], w=[sloti])
                S.barrier()
            if stage < 7:
                with contextlib.ExitStack() as esd:
                    tf = S.buf("dbg_g", [128, NH * NE], F32, es=esd)
                    k.DMA("sp", dbg_d[HALF + 128:HALF + 256, 0:NE], tau[:, :], r=[tau], w=[])
                    k.DMA("sp", dbg_d[HALF + 256:HALF + 384, 0:NH * NE], gate[:, :, :].rearrange("p t e -> p (t e)"),
                          r=[gate], w=[])
                    k.CP("dve", tf[:, :], sloti[:, :, :].rearrange("p t e -> p (t e)"), r=[sloti], w=[tf])
                    k.DMA("sp", dbg_d[HALF + 384:HALF + 512, 0:NH * NE], tf[:, :], r=[tf], w=[])
        if stage >= 7:
            with contextlib.ExitStack() as es7:
                def sb(name, shape, dtype):
                    return S.buf(name, shape, dtype)
                wgb = sb("wgb", [128, 8, D], BF16)
                wub = sb("wub", [128, 8, D], BF16)
                wdb = sb("wdb", [128, 8, D], BF16)
                hd = [sb(f"hd{i}", [128, ROWW], BF16) for i in range(6)]
                xs_in = [sb(f"xs_in{i}", [128, ROWW], BF16) for i in range(2)]
                xinT = sb("xinT", [128, 8, CAP], BF16)
                hid = sb("hid", [128, 8, CAP], BF16)
                gall = sb("gall", [128, 8, 8], F32)
                iall = sb("iall", [128, 8, 8], I32)
                sg = [sb(f"sg{i}", [128, 512], F32) for i in range(2)]
                yt = [sb(f"yt{i}", [128, D], F32) for i in range(2)]

                def load_w(e_):
                    for (wb, wd_) in ((wgb, wg_d), (wub, wu_d), (wdb, wd_d)):
                        k.DMA("pool", wb[:, :, :], wd_[e_].rearrange("(k p) n -> p k n", p=128), r=[], w=[wb])

                def dispatch(e_):
                    for t in range(NH):
                        hb_ = hd[(e_ * NH + t) % 6]
                        k.DMA("sp", hb_[:, :], h2buf_d[t * 128:(t + 1) * 128, :], r=[h2buf_b], w=[hb_])
                        k.CP("dve", hb_[:, 1024:1026].bitcast(F32), gate[:, t, e_:e_ + 1], r=[gate], w=[hb_])
                        S.dma("pool", (lambda hb_, t, e_: lambda eng: eng.indirect_dma_start(
                            out=xin_flat, out_offset=bass.IndirectOffsetOnAxis(ap=sloti[:, t, e_:e_ + 1], axis=0),
                            in_=hb_[:, :], in_offset=None))(hb_, t, e_),
                            r=[hb_, sloti, xin_e[e_]], w=[])

                nexp = NE if stage >= 8 else 2
                load_w(0)
                dispatch(0)
                pbi = 0
                ybi = 0
                for e_ in range(nexp):
                    for sc in range(8):
                        xb_ = xs_in[sc % 2]
                        if sc == 0:
                            k.DMA("sp", xb_[:, :], xin_d[e_, sc * 128:(sc + 1) * 128, :], r=[], w=[xb_, xin_e[e_]])
                        else:
                            k.DMA("sp", xb_[:, :], xin_d[e_, sc * 128:(sc + 1) * 128, :], r=[xin_e[e_]], w=[xb_])
                        k.CP("dve", gall[:, sc, 0:1], xb_[:, 1024:1026].bitcast(F32), r=[xb_], w=[gall])
                        k.CP("dve", iall[:, sc, 0:1], xb_[:, 1026:1028].bitcast(I32), r=[xb_], w=[iall])
                        for kc in range(8):
                            k.TR(P0b[:, kc * 128:(kc + 1) * 128], xb_[:, kc * 128:(kc + 1) * 128], identb,
                                 r=[xb_, cstb], w=[P[0]])
                        k.CP("act", xinT[:, :, sc * 128:(sc + 1) * 128], P0b[:, :].rearrange("p (k l) -> p k l", k=8),
                             r=[P[0]], w=[xinT])
                    if e_ + 1 < nexp:
                        dispatch(e_ + 1)
                    for fc in range(8):
                        for half in range(2):
                            pg, pu = P[1 + 2 * (pbi % 3)], P[2 + 2 * (pbi % 3)]
                            pbi += 1
                            for kc in range(8):
                                k.MM(pg[:, :], wgb[:, kc, fc * 128:(fc + 1) * 128], xinT[:, kc, half * 512:(half + 1) * 512],
                                     kc == 0, kc == 7, r=[wgb, xinT], w=[pg])
                            for kc in range(8):
                                k.MM(pu[:, :], wub[:, kc, fc * 128:(fc + 1) * 128], xinT[:, kc, half * 512:(half + 1) * 512],
                                     kc == 0, kc == 7, r=[wub, xinT], w=[pu])
                            sgb = sg[(fc * 2 + half) % 2]
                            k.ACT(sgb[:, :], pg[:, :], AF.Silu, r=[pg], w=[sgb])
                            k.TT("dve", hid[:, fc, half * 512:(half + 1) * 512], sgb[:, :], pu[:, :], ALU.mult,
                                 r=[sgb, pu], w=[hid])
                    if e_ + 1 < nexp:
                        for (wb, wd_) in ((wgb, wg_d), (wub, wu_d)):
                            k.DMA("pool", wb[:, :, :], wd_[e_ + 1].rearrange("(k p) n -> p k n", p=128), r=[], w=[wb])
                    for sc in range(8):
                        ytb = yt[sc % 2]
                        for n in range(2):
                            py = P[7] if (ybi % 2 == 0) else P[0]
                            ybi += 1
                            for fc in range(8):
                                k.MM(py[:, :], hid[:, fc, sc * 128:(sc + 1) * 128], wdb[:, fc, n * 512:(n + 1) * 512],
                                     fc == 0, fc == 7, r=[hid, wdb], w=[py])
                            k.ACT(ytb[:, n * 512:(n + 1) * 512], py[:, :], AF.Identity, r=[py, gall], w=[ytb],
                                  scale=gall[:, sc, 0:1])
                        S.dma("pool", (lambda ytb, sc: lambda eng: eng.indirect_dma_start(
                            out=acc_d, out_offset=bass.IndirectOffsetOnAxis(ap=iall[:, sc, 0:1], axis=0),
                            in_=ytb[:, :], in_offset=None, compute_op=ALU.add))(ytb, sc),
                            r=[ytb, iall], w=[acc_b])
                    if e_ + 1 < nexp:
                        k.DMA("pool", wdb[:, :, :], wd_d[e_ + 1].rearrange("(k p) n -> p k n", p=128), r=[], w=[wdb])
                S.barrier()
            with contextlib.ExitStack() as es8:
                def sb(name, shape, dtype):
                    return S.buf(name, shape, dtype)
                at = [sb(f"at{i}", [128, D], F32) for i in range(2)]
                x1t = [sb(f"x1t{i}", [128, D], F32) for i in range(2)]
                ot = [sb(f"ot{i}", [128, D], F32) for i in range(2)]
                junk8 = sb("junk8", [128, D], BF16)
                s8 = sb("s8", [128, 8, 8], F32)
                for t in range(NH):
                    i = t % 2
                    rows = slice(t * 128, (t + 1) * 128)
                    k.DMA("sp", at[i][:, :], acc_d[rows, :], r=[acc_b], w=[at[i]])
                    k.DMA("sp", x1t[i][:, :], x1buf_d[rows, :], r=[x1buf_b], w=[x1t[i]])
                    k.MEMSET("pool", s8[:, 0, 0:1], 0.0, w=[s8])
                    k.ACT(junk8[:, :], at[i][:, :], AF.Square, r=[at[i]], w=[junk8, s8], accum=s8[:, 0, 0:1])
                    k.ACT(s8[:, 1, 0:1], s8[:, 0, 0:1], AF.Ln, r=[s8], w=[s8], bias=EPS, scale=1.0 / D)
                    k.ACT(s8[:, 2, 0:1], s8[:, 1, 0:1], AF.Exp, r=[s8], w=[s8], scale=-0.5)
                    k.STT("dve", ot[i][:, :], at[i][:, :], s8[:, 2, 0:1], G2, ALU.mult, ALU.mult,
                          r=[at[i], s8, modrep], w=[ot[i]])
                    k.TT("pool", ot[i][:, :], ot[i][:, :], x1t[i][:, :], ALU.add, r=[ot[i], x1t[i]], w=[ot[i]])
                    k.DMA("sp", out_d[rows, :], ot[i][:, :], r=[ot[i]], w=[])
        S.wait_all_dma("sp")
        S.emit()
    return nc


def make_consts():
    c = np.zeros((128, NCONST, 128), np.float32)
    i = np.arange(128)
    r, cc = i[:, None], i[None, :]
    same = (r // 64) == (cc // 64)
    c[:, 0] = (r == cc)
    c[:, 1] = (r <= cc)
    c[:, 2] = np.where(r > cc, -30000.0, 0.0)
    c[:, 3] = (r <= cc)
    c[:, 4] = same & (r <= cc)
    c[:, 5] = same & (r > cc)
    c[:, 6] = same & (r <= cc)
    c[:, 7] = 1.0
    c[:, 8] = (r < cc)
    c[:, 9, 0] = (i < 64)
    c[:, 9, 1] = (i >= 64)
    c[:, 9, 2:2 + NE] = np.arange(NE)[None, :] * CAP
    c[:, 9, 18] = NE * CAP + i
    return c


def rep(v, n=128):
    return np.ascontiguousarray(np.broadcast_to(np.asarray(v, np.float32)[None], (n,) + tuple(np.shape(v))))


def fm(v):
    return np.ascontiguousarray(np.asarray(v, np.float32).reshape(-1, 128).T)


def prep_inputs(inp):
    x, c, ctx, c_ctx = inp["x"], inp["c"], inp["ctx"], inp["c_ctx"]
    w_in = inp["w_in"][0]
    consts = make_consts()
    shared = {
        "ada_w": np.ascontiguousarray(inp["ada_w"][0]),
        "ada_bT": np.ascontiguousarray(inp["ada_b"][0][:2048].reshape(16, 128).T),
        "ada_brep": rep(inp["ada_b"][0][2048:]),
        "nw0T": fm(inp["norm_w"][0, 0]),
        "nwrep": rep(inp["norm_w"][0, 1:4]),
        "cb": fm(inp["ssd_conv_b"][0]),
        "snw": rep(inp["ssd_norm_w"][0]),
        "hnw": rep(inp["hgrn_norm_w"][0]),
        "w_out": np.ascontiguousarray(inp["w_out"][0]),
        "w_router": np.ascontiguousarray(inp["w_router"][0]),
        "w_gate": np.ascontiguousarray(inp["w_gate"][0]),
        "w_up": np.ascontiguousarray(inp["w_up"][0]),
        "w_down": np.ascontiguousarray(inp["w_down"][0]),
        "consts": consts,
    }
    maps = []
    for core in range(8):
        b, d = core // 2, core % 2
        m = dict(shared)
        m["xs"] = np.ascontiguousarray(x[b][::-1] if d else x[b])
        m["ctxs"] = np.ascontiguousarray(ctx[b][::-1] if d else ctx[b])
        cv = np.stack([fm(c[b]), fm(c_ctx)], axis=-1)
        m["cvec"] = np.ascontiguousarray(cv)
        cols = [w_in[:, 512:1536], w_in[:, 1552:2064], w_in[:, 2064 + 512 * d:2576 + 512 * d], w_in[:, 3088:3600],
                w_in[:, 1536 + 8 * d:1544 + 8 * d], w_in[:, 0:512], w_in[:, 3600:4112]]
        m["wmain"] = np.ascontiguousarray(np.concatenate(cols, axis=1))
        cwk = inp["ssd_conv_w"][0]
        if d:
            cwk = cwk[::-1]
        m["cw"] = np.ascontiguousarray(cwk.T.reshape(8, 128, 5).transpose(1, 0, 2))
        m["sp8"] = rep(np.stack([inp["ssd_dt_bias"][0, d], inp["ssd_a_log"][0, d], inp["ssd_d"][0]]))
        m["lbrep"] = rep(np.stack([inp["hgrn_lb"][0, d], inp["hgrn_lb"][1, d]]))
        p = np.arange(128)[:, None]
        t = np.arange(NH)[None, :]
        m["pidx"] = np.ascontiguousarray(np.concatenate(
            [(1 - d) * 1024 + ((HALF - 1) - (t * 128 + p)) % 1024, t * 128 + p], axis=1).astype(np.int32))
        maps.append(m)
    return maps


STAGE = int(os.environ.get("KSTAGE", "9"))
LITE = int(os.environ.get("KLITE", "0"))
SIM = int(os.environ.get("KSIM", "0"))
SAME_ENGINE_SYNC = bool(int(os.environ.get("KSES", "1")))
SUBCUT = os.environ.get("KSUB", "")
_CACHE = {}


def kernel(**inputs):
    inp = {k_: np.asarray(v) for k_, v in inputs.items()}
    maps = prep_inputs(inp)
    if LITE:
        for m in maps:
            m["ada_w"] = m["ada_w"][:8]
            m["xs"] = m["xs"][:1024]
    if STAGE < 7:
        for m in maps:
            for kk_ in ("w_gate", "w_up", "w_down"):
                m.pop(kk_)
    if STAGE not in _CACHE:
        _CACHE[STAGE] = build_program(STAGE)
    nc = _CACHE[STAGE]
    res = run_bass_kernel_spmd(nc, maps, core_ids=list(range(8)))
    if STAGE < 9:
        return [r["dbg"] for r in res.results]
    out = np.empty((4, SEQ, D), np.float32)
    for core in range(8):
        b, d = core // 2, core % 2
        o = res.results[core]["out"]
        if d:
            out[b, HALF:] = o[::-1]
        else:
            out[b, :HALF] = o
    return out
```

```python
import contextlib
import os
import numpy as np
import concourse.bass as bass
import concourse.mybir as mybir
from concourse.bass_utils import run_bass_kernel_spmd

F32 = mybir.dt.float32
BF16 = mybir.dt.bfloat16
I32 = mybir.dt.int32
AF = mybir.ActivationFunctionType
ALU = mybir.AluOpType
AX = mybir.AxisListType

D = 1024
SEQ = 8192
CTX = 256
NT = SEQ // 128
NH = NT // 2
HALF = SEQ // 2
NE = 16
CAP = 1024
EPS = 1e-6
WCOLS = 3592
C_XBC, C_Q, C_F, C_I, C_DT, C_Z, C_G = 0, 1024, 1536, 2048, 2560, 2568, 3080
NCONST = 10
ROWW = 1028
TRASH = HALF


class Buf:
    __slots__ = ("name", "t", "last_w", "readers")

    def __init__(self, name, t=None):
        self.name = name
        self.t = t
        self.last_w = None
        self.readers = []

    def __getitem__(self, k):
        return self.t[k]


class Sched:
    COMPUTE = ("pe", "dve", "act", "pool")
    NDSEM = 6

    def __init__(self, nc, es, same_engine_sync=True):
        self.nc = nc
        self.es = es
        self.same_engine_sync = same_engine_sync
        self.prog = {k: [] for k in ("pe", "dve", "act", "pool", "sp")}
        self.ninst = {k: 0 for k in self.COMPUTE}
        self.waited_idx = {}
        self.milestones = {k: set() for k in self.COMPUTE}
        self.csem = {k: es.enter_context(nc.semaphore("cs_" + k)) for k in self.COMPUTE}
        self.dsem, self.dcnt, self.drot = {}, {}, {}
        for q in ("sp", "act", "pool"):
            self.dsem[q] = [es.enter_context(nc.semaphore(f"ds_{q}{j}")) for j in range(self.NDSEM)]
            self.dcnt[q] = [0] * self.NDSEM
            self.drot[q] = 0
        self.ccsem = es.enter_context(nc.semaphore("cc_sem"))
        self.cccnt = 0

    def buf(self, name, shape, dtype, psum=False, es=None):
        es = es or self.es
        name = "s_" + name
        if psum:
            t = es.enter_context(self.nc.psum_tensor(name, list(shape), dtype))
        else:
            t = es.enter_context(self.nc.sbuf_tensor(name, list(shape), dtype))
        return Buf(name, t)

    def _need(self, eng, tok):
        if tok is None:
            return
        semkey, v = tok
        key = (eng, semkey)
        if semkey[0] == "c":
            src = semkey[1]
            if src == eng and (eng == "pe" or not self.same_engine_sync):
                return
            if self.waited_idx.get(key, -1) >= v:
                return
            self.waited_idx[key] = v
            self.milestones[src].add(v)
            self.prog[eng].append(("cwait", src, v))
        elif semkey[0] == "x":
            if self.waited_idx.get(key, -1) >= v:
                return
            self.waited_idx[key] = v
            self.prog[eng].append(("xwait", None, v))
        else:
            if self.waited_idx.get(key, -1) >= v:
                return
            self.waited_idx[key] = v
            _, q, j = semkey
            self.prog[eng].append(("dwait", (q, j), v))

    def _deps(self, eng, r, w):
        for b in r:
            self._need(eng, b.last_w)
        for b in w:
            self._need(eng, b.last_w)
            for t in b.readers:
                self._need(eng, t)

    def _commit(self, tok, r, w):
        for b in w:
            b.last_w = tok
            b.readers = []
        for b in r:
            if b not in w:
                b.readers.append(tok)

    def op(self, eng, fn, r=(), w=()):
        self._deps(eng, r, w)
        idx = self.ninst[eng]
        self.ninst[eng] += 1
        self.prog[eng].append(("op", fn, idx))
        tok = (("c", eng), idx)
        self._commit(tok, r, w)
        return tok

    def dma(self, q, fn, r=(), w=()):
        self._deps(q, r, w)
        j = self.drot[q]
        self.drot[q] = (j + 1) % self.NDSEM
        semkey = ("d", q, j)
        prev = self.dcnt[q][j]
        if prev > 0:
            self._need(q, (semkey, prev))
        self.dcnt[q][j] += 16
        v = self.dcnt[q][j]
        self.prog[q].append(("dma", fn, (q, j)))
        tok = (semkey, v)
        self._commit(tok, r, w)
        return tok

    def coll(self, fn, r=(), w=()):
        q = "pool"
        self._deps(q, r, w)
        self.cccnt += 1
        v = self.cccnt
        self.prog[q].append(("coll", fn, v))
        tok = (("x", "cc"), v)
        self._commit(tok, r, w)
        return tok

    def barrier(self):
        for eng in ("pe", "dve", "act", "pool", "sp"):
            for x in self.COMPUTE:
                if x != eng and self.ninst[x] > 0:
                    self._need(eng, (("c", x), self.ninst[x] - 1))
            self.wait_all_dma(eng)

    def wait_all_dma(self, eng="sp"):
        for q in ("sp", "act", "pool"):
            for j in range(self.NDSEM):
                if self.dcnt[q][j]:
                    self._need(eng, (("d", q, j), self.dcnt[q][j]))
        if self.cccnt:
            self._need(eng, (("x", "cc"), self.cccnt))

    def emit(self):
        nc = self.nc
        rank = {}
        for e in self.COMPUTE:
            ms = sorted(self.milestones[e])
            rank[e] = {idx: i + 1 for i, idx in enumerate(ms)}
        sched = self

        def run(engname, e):
            for ent in sched.prog[engname]:
                kind = ent[0]
                if kind == "op":
                    ins = ent[1](e)
                    if ent[2] in rank[engname]:
                        ins.then_inc(sched.csem[engname], 1)
                elif kind == "cwait":
                    e.wait_ge(sched.csem[ent[1]], rank[ent[1]][ent[2]])
                elif kind == "dwait":
                    q, j = ent[1]
                    e.wait_ge(sched.dsem[q][j], ent[2])
                elif kind == "xwait":
                    e.wait_ge(sched.ccsem, ent[2])
                elif kind == "coll":
                    ent[1](e).then_inc(sched.ccsem, 1)
                elif kind == "dma":
                    q, j = ent[2]
                    ent[1](e).then_inc(sched.dsem[q][j], 16)

        with nc.Block() as block:
            @block.sync
            def _(e):
                run("sp", e)

            @block.scalar
            def _(e):
                run("act", e)

            @block.vector
            def _(e):
                run("dve", e)

            @block.gpsimd
            def _(e):
                run("pool", e)

            @block.tensor
            def _(e):
                run("pe", e)


class K:
    def __init__(self, nc, es):
        self.nc = nc
        self.S = Sched(nc, es, same_engine_sync=SAME_ENGINE_SYNC)

    def MM(self, out, lhsT, rhs, start, stop, r, w):
        self.S.op("pe", lambda e: e.matmul(out, lhsT=lhsT, rhs=rhs, start=start, stop=stop), r=r, w=w)

    def TR(self, out, in_, ident, r, w):
        self.S.op("pe", lambda e: e.transpose(out=out, in_=in_, identity=ident), r=r, w=w)

    def ACT(self, out, in_, func, r, w, bias=None, scale=None, accum=None):
        kw = {}
        if bias is not None:
            kw["bias"] = bias
        if scale is not None:
            kw["scale"] = scale
        if accum is not None:
            kw["accum_out"] = accum
        self.S.op("act", lambda e: e.activation(out=out, in_=in_, func=func, **kw), r=r, w=w)

    def TT(self, eng, out, in0, in1, op, r, w):
        self.S.op(eng, lambda e: e.tensor_tensor(out=out, in0=in0, in1=in1, op=op), r=r, w=w)

    def TS(self, eng, out, in0, s1, s2, op0, op1, r, w, accum=None):
        if op1 is None:
            self.S.op(eng, lambda e: e.tensor_scalar(out=out, in0=in0, scalar1=s1, scalar2=None, op0=op0), r=r, w=w)
        elif accum is not None:
            self.S.op(eng, lambda e: e.tensor_scalar(out=out, in0=in0, scalar1=s1, scalar2=s2, op0=op0, op1=op1,
                                                     accum_out=accum), r=r, w=w)
        else:
            self.S.op(eng, lambda e: e.tensor_scalar(out=out, in0=in0, scalar1=s1, scalar2=s2, op0=op0, op1=op1),
                      r=r, w=w)

    def STT(self, eng, out, in0, scalar, in1, op0, op1, r, w):
        self.S.op(eng, lambda e: e.scalar_tensor_tensor(out=out, in0=in0, scalar=scalar, in1=in1, op0=op0, op1=op1),
                  r=r, w=w)

    def CP(self, eng, out, in_, r, w):
        if eng == "act":
            self.S.op("act", lambda e: e.copy(out=out, in_=in_), r=r, w=w)
        else:
            self.S.op(eng, lambda e: e.tensor_copy(out=out, in_=in_), r=r, w=w)

    def MEMSET(self, eng, ap, val, w):
        self.S.op(eng, lambda e: e.memset(ap, val), w=w)

    def DMA(self, q, out, in_, r, w):
        return self.S.dma(q, lambda e: e.dma_start(out=out, in_=in_), r=r, w=w)


def build_program(stage):
    nc = bass.Bass("TRN2", target_bir_lowering=False)
    dt_in = lambda name, shape, dt=F32: nc.dram_tensor(name, list(shape), dt, kind="ExternalInput").ap()
    x_d = dt_in("xs", [1024 if LITE else SEQ, D])
    ctx_d = dt_in("ctxs", [CTX, D])
    cvec_d = dt_in("cvec", [128, 8, 2])
    adaw_d = dt_in("ada_w", [8 if LITE else D, 6 * D])
    adabT_d = dt_in("ada_bT", [128, 16])
    adabrep_d = dt_in("ada_brep", [128, 4 * D])
    nw0T_d = dt_in("nw0T", [128, 8])
    nwrep_d = dt_in("nwrep", [128, 3, D])
    wmain_d = dt_in("wmain", [D, WCOLS])
    cw_d = dt_in("cw", [128, 8, 5])
    cb_d = dt_in("cb", [128, 8])
    sp8_d = dt_in("sp8", [128, 3, 8])
    lbrep_d = dt_in("lbrep", [128, 2, 512])
    snw_d = dt_in("snw", [128, 512])
    hnw_d = dt_in("hnw", [128, 512])
    wout_d = dt_in("w_out", [D, D])
    wr_d = dt_in("w_router", [D, NE])
    if stage >= 7:
        wg_d = dt_in("w_gate", [NE, D, D])
        wu_d = dt_in("w_up", [NE, D, D])
        wd_d = dt_in("w_down", [NE, D, D])
    consts_d = dt_in("consts", [128, NCONST, 128])
    pidx_d = dt_in("pidx", [128, 2 * NH], I32)
    out_d = nc.dram_tensor("out", [HALF, D], F32, kind="ExternalOutput").ap()
    dbg_d = None
    if stage < 9:
        dbg_d = nc.dram_tensor("dbg", [SEQ, D], F32, kind="ExternalOutput").ap()
    ybuf_d = nc.dram_tensor("ybuf", [HALF, D], BF16).ap()
    ysend_t = [nc.dram_tensor(f"ysend{c}", [1024, D], BF16) for c in range(4)]
    ygath_t = [nc.dram_tensor(f"ygath{c}", [2048, D], BF16) for c in range(4)]
    zgbuf_d = nc.dram_tensor("zgbuf", [HALF, D], BF16).ap()
    x1buf_d = nc.dram_tensor("x1buf", [HALF, D], F32).ap()
    h2buf_d = nc.dram_tensor("h2buf", [HALF, ROWW], BF16).ap()
    affs_t = nc.dram_tensor("affsend", [HALF, NE], F32)
    affg_t = nc.dram_tensor("affgath", [SEQ, NE], F32)
    xin_flat = nc.dram_tensor("xin", [NE * CAP + 128, ROWW], BF16).ap()
    xin_d = xin_flat[0:NE * CAP, :].rearrange("(e c) r -> e c r", e=NE)
    acc_d = nc.dram_tensor("moeacc", [HALF + 128, D], F32).ap()
    ybuf_b, ysend_b, ygath_b, zgbuf_b, x1buf_b, h2buf_b = (Buf(n) for n in ("ybuf", "ysend", "ygath", "zgbuf", "x1buf", "h2buf"))
    affs_b, affg_b, xin_b, acc_b = (Buf(n) for n in ("affs", "affg", "xin", "acc"))
    PAIRS = [[0, 1], [2, 3], [4, 5], [6, 7]]

    with contextlib.ExitStack() as es:
        k = K(nc, es)
        S = k.S
        P = [S.buf(f"P{i}", [128, 512], F32, psum=True) for i in range(8)]
        P0b = P[0].t[:, :].bitcast(BF16)

        cst = S.buf("cst", [128, NCONST, 128], F32)
        cstb = S.buf("cstb", [128, NCONST, 128], BF16)
        k.DMA("sp", cst[:, :, :], consts_d, r=[], w=[cst])
        k.CP("dve", cstb[:, :, :], cst[:, :, :], r=[cst], w=[cstb])
        ident, identb = cst[:, 0, :], cstb[:, 0, :]
        trib, negmb = cstb[:, 1, :], cstb[:, 2, :]
        mask01 = cst[:, 3, :]
        tri2i, tri2r, mask2 = cst[:, 4, :], cst[:, 5, :], cst[:, 6, :]
        onesb = cstb[:, 7, :]
        chunkind = cst[:, 9, 0:2]


        cvec = S.buf("cvec", [128, 8, 2], F32)
        k.DMA("sp", cvec[:, :, :], cvec_d, r=[], w=[cvec])
        adabT = S.buf("adabT", [128, 16], F32)
        k.DMA("sp", adabT[:, :], adabT_d, r=[], w=[adabT])
        nw0T = S.buf("nw0T", [128, 8], F32)
        k.DMA("sp", nw0T[:, :], nw0T_d, r=[], w=[nw0T])
        cw = S.buf("cw", [128, 8, 5], F32)
        k.DMA("sp", cw[:, :, :], cw_d, r=[], w=[cw])
        cb = S.buf("cb", [128, 8], F32)
        k.DMA("sp", cb[:, :], cb_d, r=[], w=[cb])
        sp8 = S.buf("sp8", [128, 3, 8], F32)
        k.DMA("sp", sp8[:, :, :], sp8_d, r=[], w=[sp8])
        lbrep = S.buf("lbrep", [128, 2, 512], F32)
        k.DMA("sp", lbrep[:, :, :], lbrep_d, r=[], w=[lbrep])

        if stage >= 7:
            initr = S.buf("initr", [128, ROWW], BF16)
            k.MEMSET("pool", initr[:, :], 0.0, w=[initr])
            k.MEMSET("pool", initr[:, 1026:1028].bitcast(I32), TRASH, w=[initr])
            zt = S.buf("zt", [128, D], F32)
            k.MEMSET("pool", zt[:, :], 0.0, w=[zt])
            xin_e = [Buf(f"xin_e{e_}") for e_ in range(NE)]
            for e_ in range(NE):
                for sc in range(8):
                    k.DMA("sp", xin_d[e_, sc * 128:(sc + 1) * 128, :], initr[:, :], r=[initr], w=[xin_e[e_]])
            for t in range(NH + 1):
                k.DMA("sp", acc_d[t * 128:(t + 1) * 128, :], zt[:, :], r=[zt], w=[acc_b])
        sc = S.buf("sc", [128, 8, 2], F32)
        k.ACT(sc[:, :, :], cvec[:, :, :], AF.Silu, r=[cvec], w=[sc])
        modT = S.buf("modT", [128, 16, 2], F32)
        modrep = S.buf("modrep", [128, 4 * D], F32)
        with contextlib.ExitStack() as es0:
            adap = [S.buf(f"adap{i}", [128, 8, 512], F32, es=es0) for i in range(2)]
            adabrep = S.buf("adabrep", [128, 4 * D], F32, es=es0)
            nwrep = S.buf("nwrep", [128, 3, D], F32, es=es0)
            k.DMA("sp", adabrep[:, :], adabrep_d, r=[], w=[adabrep])
            k.DMA("sp", nwrep[:, :, :], nwrep_d, r=[], w=[nwrep])
            if LITE:
                k.MEMSET("pool", modT[:, :, :], 0.1, w=[modT])
                k.MEMSET("pool", modrep[:, :], 0.1, w=[modrep])
            for j in range(0 if LITE else 12):
                ap_ = adap[j % 2]
                k.DMA("sp", ap_[:, :, :], adaw_d[:, j * 512:(j + 1) * 512].rearrange("(k p) n -> p k n", p=128),
                      r=[], w=[ap_])
                if j < 4:
                    for m in range(4):
                        cc = j * 4 + m
                        for kc in range(8):
                            k.MM(P[6][:, 0:2], ap_[:, kc, m * 128:(m + 1) * 128], sc[:, kc, :], kc == 0, kc == 7,
                                 r=[ap_, sc], w=[P[6]])
                        k.TS("dve", modT[:, cc, :], P[6][:, 0:2], adabT[:, cc:cc + 1], None, ALU.add, None,
                             r=[P[6], adabT], w=[modT])
                else:
                    pb = P[j % 2 + 1]
                    for kc in range(8):
                        k.MM(pb[:, :], sc[:, kc, 0:1].to_broadcast([128, 128]), ap_[:, kc, :], kc == 0, kc == 7,
                             r=[ap_, sc], w=[pb])
                    o = (j - 4) * 512
                    k.TT("dve", modrep[:, o:o + 512], pb[:, :], adabrep[:, o:o + 512], ALU.add,
                         r=[pb, adabrep], w=[modrep])
            k.TT("dve", modrep[:, 0:D], modrep[:, 0:D], nwrep[:, 0, :], ALU.mult, r=[modrep, nwrep], w=[modrep])
            k.STT("dve", modrep[:, 2 * D:3 * D], modrep[:, 2 * D:3 * D], 1.0, nwrep[:, 1, :], ALU.add, ALU.mult,
                  r=[modrep, nwrep], w=[modrep])
            k.TT("dve", modrep[:, 3 * D:4 * D], modrep[:, 3 * D:4 * D], nwrep[:, 2, :], ALU.mult,
                 r=[modrep, nwrep], w=[modrep])
            S.barrier()
        G1, B2, A2, G2 = (modrep[:, i * D:(i + 1) * D] for i in range(4))
        A0 = S.buf("A0", [128, 2, 8], F32)
        B0 = S.buf("B0", [128, 2, 8], F32)
        for i in range(2):
            k.STT("dve", A0[:, i, :], modT[:, 8:16, i], 1.0, nw0T[:, :], ALU.add, ALU.mult, r=[modT, nw0T], w=[A0])
            k.CP("dve", B0[:, i, :], modT[:, 0:8, i], r=[modT], w=[B0])
        aneg = S.buf("aneg", [128, 8], F32)
        k.ACT(aneg[:, :], sp8[:, 1, :], AF.Exp, r=[sp8], w=[aneg])
        k.TS("dve", aneg[:, :], aneg[:, :], -1.0, None, ALU.mult, None, r=[aneg], w=[aneg])
        dsk = S.buf("dsk", [128, 8], F32)
        k.TS("dve", dsk[:, :], sp8[:, 2, :], 0.5, None, ALU.mult, None, r=[sp8], w=[dsk])
        Dm = S.buf("Dm", [128, 8, 128], BF16)
        for j in range(8):
            k.TS("dve", Dm[:, j, :], ident, dsk[:, j:j + 1], None, ALU.mult, None, r=[cst, dsk], w=[Dm])
        c01 = S.buf("c01", [128, 2, 512], F32)
        k.TT("dve", c01[:, 0, :], lbrep[:, 0, :], lbrep[:, 1, :], ALU.subtract, r=[lbrep], w=[c01])
        k.ACT(c01[:, 1, :], c01[:, 0, :], AF.Tanh, r=[c01], w=[c01], scale=0.5)
        k.TS("dve", c01[:, 0, :], c01[:, 1, :], 0.25, 0.75, ALU.mult, ALU.add, r=[c01], w=[c01])
        k.TS("dve", c01[:, 1, :], c01[:, 1, :], -0.25, 0.25, ALU.mult, ALU.add, r=[c01], w=[c01])
        ST = S.buf("ST", [128, 512], F32)
        STb = S.buf("STb", [128, 512], BF16)
        SH = S.buf("SH", [128, 512], F32)
        SHb = S.buf("SHb", [128, 512], BF16)
        k.MEMSET("pool", ST[:, :], 0.0, w=[ST])
        k.MEMSET("pool", STb[:, :], 0.0, w=[STb])
        k.MEMSET("pool", SH[:, :], 0.0, w=[SH])
        k.MEMSET("pool", SHb[:, :], 0.0, w=[SHb])

        with contextlib.ExitStack() as es2:
            def sb(name, shape, dtype):
                return S.buf(name, shape, dtype, es=es2)
            wm = sb("wm", [128, 8, WCOLS], BF16)
            for kc in range(8):
                for (c0, c1) in ((0, 1796), (1796, WCOLS)):
                    k.DMA("pool", wm[:, kc, c0:c1], wmain_d[kc * 128:(kc + 1) * 128, c0:c1], r=[], w=[wm])
            xt = [sb(f"xt{i}", [128, D], F32) for i in range(4)]
            junk = sb("junk", [128, D], BF16)
            ssq = sb("ssq", [128, 2], F32)
            xn = sb("xn", [128, D], BF16)
            hT = [sb(f"hT{i}", [128, 8, 256], BF16) for i in range(2)]
            cacc = sb("cacc", [128, 8, 256], F32)
            xcT = sb("xcT", [128, 8, 256], BF16)
            xs_tm = sb("xs_tm", [128, 512], BF16)
            B_tm = sb("B_tm", [128, 256], BF16)
            dts = sb("dts", [128, 8, 8], F32)
            eatot = sb("eatot", [128, 8], F32)
            a_hi = sb("a_hi", [128, 8], BF16)
            a_lo = sb("a_lo", [128, 8], BF16)
            xdt = sb("xdt", [128, 512], BF16)
            xdtd = sb("xdtd", [128, 512], BF16)
            LT = sb("LT", [128, 8, 128], BF16)
            MT = sb("MT", [128, 8, 128], BF16)
            CBm = sb("CBm", [128, 2, 128], BF16)
            yoff = sb("yoff", [128, 512], F32)
            yo = sb("yo", [128, 512], BF16)
            qs = sb("qs", [128, 512], F32)
            vb = sb("vb", [128, 512], BF16)
            vm = sb("vm", [128, 2, 512], BF16)
            ff = sb("ff", [128, 512], F32)
            lf = sb("lf", [128, 512], F32)
            kk = sb("kk", [128, 512], F32)
            et = [sb(f"et{i}", [128, 512], F32) for i in range(2)]
            qdec = sb("qdec", [128, 512], BF16)
            kinv = sb("kinv", [128, 512], BF16)
            kdec = sb("kdec", [128, 512], BF16)
            qdT = sb("qdT", [128, 4, 128], BF16)
            kiT = sb("kiT", [128, 4, 128], BF16)
            attm = sb("attm", [128, 4, 128], BF16)
            oc = sb("oc", [64, 2, 512], BF16)
            ebt = sb("ebt", [128, 4, 2], F32)
            zg = sb("zg", [128, D], BF16)

            def load_x(ti_all):
                b = xt[ti_all % 4]
                src = ctx_d[ti_all * 128:(ti_all + 1) * 128, :] if ti_all < 2 else \
                    x_d[(ti_all - 2) * 128:(ti_all - 1) * 128, :]
                k.DMA("sp", b[:, :], src, r=[], w=[b])

            def norm_T(ti_all, hbuf, col0, which):
                b = xt[ti_all % 4]
                k.MEMSET("pool", ssq[:, 0:1], 0.0, w=[ssq])
                k.ACT(junk[:, :], b[:, :], AF.Square, r=[b], w=[junk, ssq], accum=ssq[:, 0:1])
                k.ACT(ssq[:, 1:2], ssq[:, 0:1], AF.Ln, r=[ssq], w=[ssq], bias=EPS, scale=1.0 / D)
                k.ACT(ssq[:, 1:2], ssq[:, 1:2], AF.Exp, r=[ssq], w=[ssq], scale=-0.5)
                k.TS("pool", xn[:, :], b[:, :], ssq[:, 1:2], None, ALU.mult, None, r=[b, ssq], w=[xn])
                for kc in range(8):
                    k.TR(P0b[:, kc * 128:(kc + 1) * 128], xn[:, kc * 128:(kc + 1) * 128], identb,
                         r=[xn, cstb], w=[P[0]])
                for kc in range(8):
                    o = hbuf[:, kc, col0:col0 + 128]
                    i_ = P0b[:, kc * 128:(kc + 1) * 128]
                    if kc % 2 == 0:
                        k.ACT(o, i_, AF.Identity, r=[P[0], A0, B0], w=[hbuf],
                              bias=B0[:, which, kc:kc + 1], scale=A0[:, which, kc:kc + 1])
                    else:
                        k.TS("dve", o, i_, A0[:, which, kc:kc + 1], B0[:, which, kc:kc + 1], ALU.mult, ALU.add,
                             r=[P[0], A0, B0], w=[hbuf])

            def conv_stage(hbuf, T, roww, chunks):
                per_bank = 512 // T
                for ci, c in enumerate(chunks):
                    pb = P[1 + ci // per_bank]
                    po = (ci % per_bank) * T
                    for kc in range(8):
                        k.MM(pb[:, po:po + T], wm[:, kc, C_XBC + c * 128:C_XBC + (c + 1) * 128], hbuf[:, kc, 0:T],
                             kc == 0, kc == 7, r=[wm, hbuf], w=[pb])
                for ci, c in enumerate(chunks):
                    pb = P[1 + ci // per_bank]
                    po = (ci % per_bank) * T
                    src = pb[:, po:po + T]
                    acc = cacc[:, c, 0:T]
                    k.ACT(acc, src, AF.Identity, r=[pb, cw, cb], w=[cacc], bias=cb[:, c:c + 1], scale=cw[:, c, 2:3])
                    srcv = src.rearrange("p (r w) -> p r w", w=roww)
                    accv = acc.rearrange("p (r w) -> p r w", w=roww)
                    for kt in (0, 1, 3, 4):
                        s = kt - 2
                        if s > 0:
                            o_, i_ = accv[:, :, 0:roww - s], srcv[:, :, s:roww]
                        else:
                            o_, i_ = accv[:, :, -s:roww], srcv[:, :, 0:roww + s]
                        k.STT("dve", o_, i_, cw[:, c, kt:kt + 1], o_, ALU.mult, ALU.add, r=[pb, cw, cacc], w=[cacc])
                    k.ACT(xcT[:, c, 0:T], acc, AF.Silu, r=[cacc], w=[xcT])

            def scan_tile(hbuf, col0, tcol, lat, ti, zgproj):
                hsl = lambda kc: hbuf[:, kc, col0:col0 + 128]
                if zgproj:
                    for (pb, c0) in ((P[1], C_Z), (P[2], C_G)):
                        for kc in range(8):
                            k.MM(pb[:, :], hsl(kc), wm[:, kc, c0:c0 + 512], kc == 0, kc == 7, r=[hbuf, wm], w=[pb])
                    k.ACT(zg[:, 0:512], P[1][:, :], AF.Silu, r=[P[1]], w=[zg])
                    k.ACT(zg[:, 512:1024], P[2][:, :], AF.Silu, r=[P[2]], w=[zg])
                    k.DMA("sp", zgbuf_d[ti * 128:(ti + 1) * 128, :], zg[:, :], r=[zg], w=[zgbuf_b])
                for (pb, c0) in ((P[3], C_Q), (P[4], C_F), (P[5], C_I)):
                    for kc in range(8):
                        k.MM(pb[:, :], hsl(kc), wm[:, kc, c0:c0 + 512], kc == 0, kc == 7, r=[hbuf, wm], w=[pb])
                for kc in range(8):
                    k.MM(P[6][:, 0:8], hsl(kc), wm[:, kc, C_DT:C_DT + 8], kc == 0, kc == 7, r=[hbuf, wm], w=[P[6]])
                if lat:
                    k.ACT(qs[:, :], P[3][:, :], AF.Silu, r=[P[3]], w=[qs])
                k.ACT(ff[:, :], P[4][:, :], AF.Tanh, r=[P[4]], w=[ff], scale=0.5)
                k.CP("dve", vb[:, :], P[5][:, :], r=[P[5]], w=[vb])
                for c in range(2):
                    k.TS("pool", vm[:, c, :], vb[:, :], chunkind[:, c:c + 1], None, ALU.mult, None, r=[vb, cst], w=[vm])
                if SUBCUT == 'A':
                    return
                for c in range(6):
                    k.TR(P0b[:, c * 128:(c + 1) * 128], xcT[:, c, tcol:tcol + 128], identb, r=[xcT, cstb], w=[P[0]])
                if SUBCUT == 'B1':
                    return
                k.CP("act", xs_tm[:, :], P0b[:, 0:512], r=[P[0]], w=[xs_tm])
                if SUBCUT == 'B2':
                    return
                k.CP("act", B_tm[:, :], P0b[:, 512:768], r=[P[0]], w=[B_tm])
                if SUBCUT == 'B':
                    return
                v_, av_, l_, dt_, a_, nacs, eacs, w2 = (dts[:, i, :] for i in range(8))
                k.TT("dve", v_, P[6][:, 0:8], sp8[:, 0, :], ALU.add, r=[P[6], sp8], w=[dts])
                k.TS("dve", av_, v_, 30.0, None, ALU.min, None, r=[dts], w=[dts])
                k.ACT(av_, av_, AF.Exp, r=[dts], w=[dts])
                k.ACT(l_, av_, AF.Ln, r=[dts], w=[dts], bias=1.0)
                k.TT("dve", dt_, l_, v_, ALU.max, r=[dts], w=[dts])
                k.TT("dve", a_, dt_, aneg[:, :], ALU.mult, r=[dts, aneg], w=[dts])
                k.CP("dve", a_hi[:, :], a_, r=[dts], w=[a_hi])
                k.TT("dve", a_lo[:, :], a_, a_hi[:, :], ALU.subtract, r=[dts, a_hi], w=[a_lo])
                if SUBCUT == 'C1':
                    return
                k.MM(P[6][:, 64:72], trib, a_hi[:, :], True, False, r=[cstb, a_hi], w=[P[6]])
                k.MM(P[6][:, 64:72], trib, a_lo[:, :], False, True, r=[cstb, a_lo], w=[P[6]])
                k.MM(P[6][:, 128:136], onesb, a_hi[:, :], True, False, r=[cstb, a_hi], w=[P[6]])
                k.MM(P[6][:, 128:136], onesb, a_lo[:, :], False, True, r=[cstb, a_lo], w=[P[6]])
                k.TS("dve", nacs, P[6][:, 64:72], -1.0, None, ALU.mult, None, r=[P[6]], w=[dts])
                k.ACT(eacs, P[6][:, 64:72], AF.Exp, r=[P[6]], w=[dts])
                k.TT("dve", w2, P[6][:, 128:136], nacs, ALU.add, r=[P[6], dts], w=[dts])
                k.ACT(w2, w2, AF.Exp, r=[dts], w=[dts])
                k.ACT(eatot[:, :], P[6][:, 128:136], AF.Exp, r=[P[6]], w=[eatot])
                k.TT("dve", w2, w2, dt_, ALU.mult, r=[dts], w=[dts])
                if SUBCUT == 'C2':
                    return
                xs3 = xs_tm[:, :].rearrange("p (j q) -> p j q", q=64)
                k.TT("dve", xdtd[:, :].rearrange("p (j q) -> p j q", q=64), xs3, w2.unsqueeze(2).to_broadcast([128, 8, 64]), ALU.mult,
                     r=[xs_tm, dts], w=[xdtd])
                if lat:
                    k.TT("pool", xdt[:, :].rearrange("p (j q) -> p j q", q=64), xs3, dt_.unsqueeze(2).to_broadcast([128, 8, 64]), ALU.mult,
                         r=[xs_tm, dts], w=[xdt])
                    for j in range(8):
                        pa = P[1 + j // 4]
                        o = pa[:, (j % 4) * 128:(j % 4 + 1) * 128]
                        k.MM(o, a_hi[:, j:j + 1].to_broadcast([128, 128]), trib, True, False, r=[a_hi, cstb], w=[pa])
                        k.MM(o, a_lo[:, j:j + 1].to_broadcast([128, 128]), trib, False, False, r=[a_lo, cstb], w=[pa])
                        k.MM(o, identb, negmb, False, True, r=[cstb], w=[pa])
                        k.ACT(LT[:, j, :], o, AF.Exp, r=[pa, dts], w=[LT], bias=nacs[:, j:j + 1])
                    for g in range(2):
                        k.MM(P[6][:, 128 + g * 128:256 + g * 128], xcT[:, 4 + g, tcol:tcol + 128],
                             xcT[:, 6 + g, tcol:tcol + 128], True, True, r=[xcT], w=[P[6]])
                    k.TT("dve", CBm[:, :, :], P[6][:, 128:384].rearrange("p (g l) -> p g l", g=2),
                         mask01.unsqueeze(1).to_broadcast([128, 2, 128]), ALU.mult, r=[P[6], cst], w=[CBm])
                    for g in range(2):
                        k.TT("pool", MT[:, 4 * g:4 * g + 4, :], LT[:, 4 * g:4 * g + 4, :],
                             CBm[:, g:g + 1, :].to_broadcast([128, 4, 128]), ALU.mult, r=[LT, CBm], w=[MT])
                    for j in range(8):
                        o = P[3][:, j * 64:(j + 1) * 64]
                        k.MM(o, MT[:, j, :], xdt[:, j * 64:(j + 1) * 64], True, False, r=[MT, xdt], w=[P[3]])
                        k.MM(o, Dm[:, j, :], xs_tm[:, j * 64:(j + 1) * 64], False, True, r=[Dm, xs_tm], w=[P[3]])
                    for g in range(2):
                        k.MM(P[7][:, g * 256:(g + 1) * 256], xcT[:, 6 + g, tcol:tcol + 128],
                             STb[:, g * 256:(g + 1) * 256], True, True, r=[xcT, STb], w=[P[7]])
                    k.TT("dve", yoff[:, :].rearrange("p (j q) -> p j q", q=64),
                         P[7][:, :].rearrange("p (j q) -> p j q", q=64),
                         eacs.unsqueeze(2).to_broadcast([128, 8, 64]), ALU.mult, r=[P[7], dts], w=[yoff])
                    k.TT("dve", yo[:, :], P[3][:, :], yoff[:, :], ALU.add, r=[P[3], yoff], w=[yo])
                if SUBCUT == 'C':
                    return
                for g in range(2):
                    k.MM(P[7][:, g * 256:(g + 1) * 256], B_tm[:, g * 128:(g + 1) * 128],
                         xdtd[:, g * 256:(g + 1) * 256], True, True, r=[B_tm, xdtd], w=[P[7]])
                k.TT("dve", ST[:, :].rearrange("p (j q) -> p j q", q=64), ST[:, :].rearrange("p (j q) -> p j q", q=64),
                     eatot[:, :].unsqueeze(2).to_broadcast([128, 8, 64]), ALU.mult, r=[ST, eatot], w=[ST])
                k.TT("dve", ST[:, :], ST[:, :], P[7][:, :], ALU.add, r=[ST, P[7]], w=[ST])
                k.CP("act", STb[:, :], ST[:, :], r=[ST], w=[STb])
                if SUBCUT == 'D':
                    return
                k.TT("dve", ff[:, :], ff[:, :], c01[:, 1, :], ALU.mult, r=[ff, c01], w=[ff])
                k.TT("dve", ff[:, :], ff[:, :], c01[:, 0, :], ALU.add, r=[ff, c01], w=[ff])
                k.ACT(lf[:, :], ff[:, :], AF.Ln, r=[ff], w=[lf])
                k.TS("pool", kk[:, :], ff[:, :], -1.0, 1.0, ALU.mult, ALU.add, r=[ff], w=[kk])
                k.MM(P[4][:, :], tri2i, lf[:, :], True, True, r=[cst, lf], w=[P[4]])
                k.MM(P[5][:, :], tri2r, lf[:, :], True, True, r=[cst, lf], w=[P[5]])
                for h in range(4):
                    k.MM(P[7][:, h * 64:h * 64 + 2], lf[:, h * 128:(h + 1) * 128], chunkind, True, True,
                         r=[lf, cst], w=[P[7]])
                k.ACT(ebt[:, :, :], P[7][:, 0:256].rearrange("p (h c) -> p h c", c=64)[:, :, 0:2], AF.Exp,
                      r=[P[7]], w=[ebt])
                k.ACT(et[0][:, :], P[5][:, :], AF.Exp, r=[P[5]], w=[et[0]])
                k.TT("dve", kdec[:, :], kk[:, :], et[0][:, :], ALU.mult, r=[kk, et[0]], w=[kdec])
                if lat:
                    k.ACT(et[1][:, :], P[4][:, :], AF.Exp, r=[P[4]], w=[et[1]])
                    k.TT("dve", qdec[:, :], qs[:, :], et[1][:, :], ALU.mult, r=[qs, et[1]], w=[qdec])
                    k.ACT(et[0][:, :], P[4][:, :], AF.Exp, r=[P[4]], w=[et[0]], scale=-1.0)
                    k.TT("pool", kinv[:, :], kk[:, :], et[0][:, :], ALU.mult, r=[kk, et[0]], w=[kinv])
                    for h in range(4):
                        k.TR(P0b[:, h * 128:(h + 1) * 128], qdec[:, h * 128:(h + 1) * 128], identb,
                             r=[qdec, cstb], w=[P[0]])
                        k.TR(P0b[:, 512 + h * 128:512 + (h + 1) * 128], kinv[:, h * 128:(h + 1) * 128], identb,
                             r=[kinv, cstb], w=[P[0]])
                    k.CP("act", qdT[:, :, :], P0b[:, 0:512].rearrange("p (h l) -> p h l", h=4), r=[P[0]], w=[qdT])
                    k.CP("act", kiT[:, :, :], P0b[:, 512:1024].rearrange("p (h l) -> p h l", h=4), r=[P[0]], w=[kiT])
                    for h in range(4):
                        k.MM(P[4][:, h * 128:(h + 1) * 128], kiT[:, h, :], qdT[:, h, :], True, True,
                             r=[kiT, qdT], w=[P[4]])
                    k.TT("dve", attm[:, :, :], P[4][:, :].rearrange("p (h l) -> p h l", h=4),
                         mask2.unsqueeze(1).to_broadcast([128, 4, 128]), ALU.mult, r=[P[4], cst], w=[attm])
                if SUBCUT == 'E':
                    return
                for c in range(2):
                    if lat:
                        for h in range(4):
                            o = P[5][0:64, h * 128:(h + 1) * 128]
                            k.MM(o, attm[:, h, c * 64:(c + 1) * 64], vb[:, h * 128:(h + 1) * 128], True, False,
                                 r=[attm, vb], w=[P[5]])
                            k.MM(o, qdT[:, h, c * 64:(c + 1) * 64], SHb[:, h * 128:(h + 1) * 128], False, True,
                                 r=[qdT, SHb], w=[P[5]])
                        k.CP("act", oc[:, c, :], P[5][0:64, :], r=[P[5]], w=[oc])
                    for h in range(4):
                        k.MM(P[7][:, h * 128:(h + 1) * 128], kdec[:, h * 128:(h + 1) * 128],
                             vm[:, c, h * 128:(h + 1) * 128], True, True, r=[kdec, vm], w=[P[7]])
                    for h in range(4):
                        sl = slice(h * 128, (h + 1) * 128)
                        k.STT("dve", SH[:, sl], SH[:, sl], ebt[:, h, c:c + 1], P[7][:, sl], ALU.mult, ALU.add,
                              r=[SH, ebt, P[7]], w=[SH])
                    k.CP("act", SHb[:, :], SH[:, :], r=[SH], w=[SHb])
                if lat:
                    if ti < NH:
                        dst, dstb, rows = ybuf_d, ybuf_b, slice(ti * 128, (ti + 1) * 128)
                    else:
                        dst, dstb = ysend_t[(ti - NH) // 8].ap(), ysend_b
                        rows = slice(((ti - NH) % 8) * 128, ((ti - NH) % 8 + 1) * 128)
                    k.DMA("sp", dst[rows, 0:512], yo[:, :], r=[yo], w=[dstb])
                    k.DMA("sp", dst[rows, 512:1024].rearrange("(c p) v -> p c v", p=64), oc[:, :, :],
                          r=[oc], w=[dstb])

            cut = int(os.environ.get("KCUT", "99"))
            load_x(0)
            load_x(1)
            load_x(2)
            if cut >= 1:
                norm_T(0, hT[0], 0, 1)
                norm_T(1, hT[0], 128, 1)
            if cut >= 2:
                conv_stage(hT[0], 256, 256, [0, 1, 2, 3])
                conv_stage(hT[0], 256, 256, [4, 5, 6, 7])
            if cut >= 3:
                for sub in range(2):
                    scan_tile(hT[0], sub * 128, sub * 128, False, -1, False)
            ntl = NT if stage >= 2 else 4
            ntl = int(os.environ.get("KNT", ntl))
            if cut < 4:
                ntl = 0
            if cut >= 4 and ntl > 1:
                load_x(3)
            for j in range(ntl // 2):
                a_, b_ = 2 * j, 2 * j + 1
                for nx in (a_ + 4, b_ + 4):
                    if nx < ntl + 2:
                        load_x(nx)
                hb = hT[(j + 1) % 2]
                norm_T(a_ + 2, hb, 0, 0)
                norm_T(b_ + 2, hb, 128, 0)
                conv_stage(hb, 256, 64, [0, 1, 2, 3])
                conv_stage(hb, 256, 64, [4, 5, 6, 7])
                scan_tile(hb, 0, 0, True, a_, a_ < NH)
                scan_tile(hb, 128, 128, True, b_, b_ < NH)

            S.barrier()
        if stage < 3:
            with contextlib.ExitStack() as esd:
                tb = S.buf("dbg_b", [128, D], BF16, es=esd)
                tf = S.buf("dbg_f", [128, D], F32, es=esd)
                for ti in range(min(ntl, NH)):
                    rows = slice(ti * 128, (ti + 1) * 128)
                    k.DMA("sp", tb[:, :], ybuf_d[rows, :], r=[ybuf_b], w=[tb])
                    k.CP("dve", tf[:, :], tb[:, :], r=[tb], w=[tf])
                    k.DMA("sp", dbg_d[rows, :], tf[:, :], r=[tf], w=[])
        else:
            for c in range(4):
                if SIM:
                    for hh in range(2):
                        k.DMA("sp", ygath_t[c].ap()[hh * 1024:(hh + 1) * 1024, :], ysend_t[c].ap(), r=[ysend_b], w=[ygath_b])
                else:
                    S.coll((lambda c: lambda e: e.collective_compute(
                        "AllGather", ALU.bypass, replica_groups=PAIRS, ins=[ysend_t[c].ap().opt()],
                        outs=[ygath_t[c].ap().opt()]))(c), r=[ysend_b], w=[ygath_b])
            affall = S.buf("affall", [128, NH, NE], F32)
            pidx = S.buf("pidx", [128, 2 * NH], I32)
            k.DMA("sp", pidx[:, :], pidx_d, r=[], w=[pidx])
            with contextlib.ExitStack() as es4:
                def sb(name, shape, dtype):
                    return S.buf(name, shape, dtype, es=es4)
                wout = sb("wout", [128, 8, D], BF16)
                for kc in range(8):
                    k.DMA("pool", wout[:, kc, :], wout_d[kc * 128:(kc + 1) * 128, :], r=[], w=[wout])
                wr = sb("wr", [128, 8, NE], BF16)
                k.DMA("pool", wr[:, :, :], wr_d.rearrange("(k p) e -> p k e", p=128), r=[], w=[wr])
                snw = sb("snw", [128, 512], F32)
                hnw = sb("hnw", [128, 512], F32)
                k.DMA("sp", snw[:, :], snw_d, r=[], w=[snw])
                k.DMA("sp", hnw[:, :], hnw_d, r=[], w=[hnw])
                yown = [sb(f"yown{i}", [128, D], BF16) for i in range(2)]
                ypar = [sb(f"ypar{i}", [128, D], BF16) for i in range(2)]
                zgt = [sb(f"zgt{i}", [128, D], BF16) for i in range(2)]
                xt2 = [sb(f"xt2{i}", [128, D], F32) for i in range(2)]
                ysum = sb("ysum", [128, D], F32)
                junk4 = sb("junk4", [128, D], BF16)
                ssq6 = sb("ssq6", [128, 8], F32)
                rs6 = sb("rs6", [128, 8], F32)
                tmpo = sb("tmpo", [128, 512], F32)
                ylat = sb("ylat", [128, D], BF16)
                ylT = sb("ylT", [128, 8, 128], BF16)
                ssq2 = sb("ssq2", [128, 4], F32)
                x1 = sb("x1", [128, D], F32)
                h2f = sb("h2f", [128, D], F32)
                h2b = sb("h2b", [128, ROWW], BF16)
                h2T = sb("h2T", [128, 8, 128], BF16)
                smx = sb("smx", [128, 4], F32)
                ex = sb("ex", [128, NE], F32)
                k.MEMSET("pool", h2b[:, 1024:ROWW], 0.0, w=[h2b])

                def p4_load(t):
                    i = t % 2
                    rows = slice(t * 128, (t + 1) * 128)
                    k.DMA("sp", yown[i][:, :], ybuf_d[rows, :], r=[ybuf_b], w=[yown[i]])
                    k.DMA("sp", zgt[i][:, :], zgbuf_d[rows, :], r=[zgbuf_b], w=[zgt[i]])
                    k.DMA("sp", xt2[i][:, :], x_d[rows, :], r=[], w=[xt2[i]])
                    S.dma("pool", lambda e: e.indirect_dma_start(
                        out=ypar[i][:, :], out_offset=None, in_=ygath_t[3 - t // 8].ap(),
                        in_offset=bass.IndirectOffsetOnAxis(ap=pidx[:, t:t + 1], axis=0)),
                        r=[ygath_b, pidx], w=[ypar[i]])

                def rstd_from(ssq_ap, out_ap, n, rbufs):
                    k.ACT(out_ap, ssq_ap, AF.Ln, r=rbufs, w=rbufs, bias=EPS, scale=1.0 / n)
                    k.ACT(out_ap, out_ap, AF.Exp, r=rbufs, w=rbufs, scale=-0.5)

                nt4 = NH if stage >= 4 else 2
                nt4 = int(os.environ.get("KNT4", nt4))
                p4_load(0)
                for t in range(nt4):
                    i = t % 2
                    if t + 1 < nt4:
                        p4_load(t + 1)
                    rows = slice(t * 128, (t + 1) * 128)
                    k.TT("dve", ysum[:, :], yown[i][:, :], ypar[i][:, :], ALU.add, r=[yown[i], ypar[i]], w=[ysum])
                    k.TT("pool", ysum[:, 0:512], ysum[:, 0:512], zgt[i][:, 0:512], ALU.mult, r=[ysum, zgt[i]], w=[ysum])
                    k.MEMSET("pool", ssq6[:, :], 0.0, w=[ssq6])
                    for g in range(2):
                        k.ACT(junk4[:, g * 256:(g + 1) * 256], ysum[:, g * 256:(g + 1) * 256], AF.Square,
                              r=[ysum], w=[junk4, ssq6], accum=ssq6[:, g:g + 1])
                    for h in range(4):
                        sl = slice(512 + h * 128, 512 + (h + 1) * 128)
                        k.ACT(junk4[:, sl], ysum[:, sl], AF.Square, r=[ysum], w=[junk4, ssq6],
                              accum=ssq6[:, 2 + h:3 + h])
                    rstd_from(ssq6[:, 0:2], rs6[:, 0:2], 256, [ssq6, rs6])
                    rstd_from(ssq6[:, 2:6], rs6[:, 2:6], 128, [ssq6, rs6])
                    for g in range(2):
                        sl = slice(g * 256, (g + 1) * 256)
                        k.STT("dve", ylat[:, sl], ysum[:, sl], rs6[:, g:g + 1], snw[:, sl], ALU.mult, ALU.mult,
                              r=[ysum, rs6, snw], w=[ylat])
                    for h in range(4):
                        sl = slice(h * 128, (h + 1) * 128)
                        sl2 = slice(512 + h * 128, 512 + (h + 1) * 128)
                        k.STT("dve", tmpo[:, sl], ysum[:, sl2], rs6[:, 2 + h:3 + h], hnw[:, sl], ALU.mult, ALU.mult,
                              r=[ysum, rs6, hnw], w=[tmpo])
                    k.TT("pool", ylat[:, 512:1024], tmpo[:, :], zgt[i][:, 512:1024], ALU.mult,
                         r=[tmpo, zgt[i]], w=[ylat])
                    for kc in range(8):
                        k.TR(P0b[:, kc * 128:(kc + 1) * 128], ylat[:, kc * 128:(kc + 1) * 128], identb,
                             r=[ylat, cstb], w=[P[0]])
                    k.CP("act", ylT[:, 0:4, :], P0b[:, 0:512].rearrange("p (k l) -> p k l", k=4), r=[P[0]], w=[ylT])
                    k.CP("act", ylT[:, 4:8, :], P0b[:, 512:1024].rearrange("p (k l) -> p k l", k=4), r=[P[0]], w=[ylT])
                    for n in range(2):
                        for kc in range(8):
                            k.MM(P[1 + n][:, :], ylT[:, kc, :], wout[:, kc, n * 512:(n + 1) * 512], kc == 0, kc == 7,
                                 r=[ylT, wout], w=[P[1 + n]])
                    k.MEMSET("pool", ssq2[:, :], 0.0, w=[ssq2])
                    for n in range(2):
                        k.ACT(junk4[:, n * 512:(n + 1) * 512], P[1 + n][:, :], AF.Square, r=[P[1 + n]],
                              w=[junk4, ssq2], accum=ssq2[:, n:n + 1])
                    k.TT("dve", ssq2[:, 2:3], ssq2[:, 0:1], ssq2[:, 1:2], ALU.add, r=[ssq2], w=[ssq2])
                    rstd_from(ssq2[:, 2:3], ssq2[:, 3:4], D, [ssq2])
                    for n in range(2):
                        sl = slice(n * 512, (n + 1) * 512)
                        k.STT("dve", x1[:, sl], P[1 + n][:, :], ssq2[:, 3:4], G1[:, sl], ALU.mult, ALU.mult,
                              r=[P[1 + n], ssq2, modrep], w=[x1])
                    k.TT("pool", x1[:, :], x1[:, :], xt2[i][:, :], ALU.add, r=[x1, xt2[i]], w=[x1])
                    k.DMA("sp", x1buf_d[rows, :], x1[:, :], r=[x1], w=[x1buf_b])
                    k.MEMSET("pool", ssq2[:, 0:1], 0.0, w=[ssq2])
                    k.ACT(junk4[:, :], x1[:, :], AF.Square, r=[x1], w=[junk4, ssq2], accum=ssq2[:, 0:1])
                    rstd_from(ssq2[:, 0:1], ssq2[:, 1:2], D, [ssq2])
                    k.STT("dve", h2f[:, :], x1[:, :], ssq2[:, 1:2], A2, ALU.mult, ALU.mult, r=[x1, ssq2, modrep], w=[h2f])
                    k.TT("dve", h2b[:, 0:D], h2f[:, :], B2, ALU.add, r=[h2f, modrep], w=[h2b])
                    k.CP("dve", h2b[:, 1026:1028].bitcast(I32), pidx[:, NH + t:NH + t + 1], r=[pidx], w=[h2b])
                    k.DMA("sp", h2buf_d[rows, :], h2b[:, :], r=[h2b], w=[h2buf_b])
                    for kc in range(8):
                        k.TR(P0b[:, kc * 128:(kc + 1) * 128], h2b[:, kc * 128:(kc + 1) * 128], identb,
                             r=[h2b, cstb], w=[P[0]])
                    k.CP("act", h2T[:, :, :], P0b[:, :].rearrange("p (k l) -> p k l", k=8), r=[P[0]], w=[h2T])
                    for kc in range(8):
                        k.MM(P[6][:, 0:NE], h2T[:, kc, :], wr[:, kc, :], kc == 0, kc == 7, r=[h2T, wr], w=[P[6]])
                    S.op("dve", lambda e: e.tensor_reduce(out=smx[:, 0:1], in_=P[6][:, 0:NE], axis=AX.X, op=ALU.max),
                         r=[P[6]], w=[smx])
                    k.TS("dve", smx[:, 1:2], smx[:, 0:1], -1.0, None, ALU.mult, None, r=[smx], w=[smx])
                    k.MEMSET("pool", smx[:, 2:3], 0.0, w=[smx])
                    k.ACT(ex[:, :], P[6][:, 0:NE], AF.Exp, r=[P[6], smx], w=[ex, smx], bias=smx[:, 1:2],
                          accum=smx[:, 2:3])
                    S.op("dve", lambda e: e.reciprocal(out=smx[:, 3:4], in_=smx[:, 2:3]), r=[smx], w=[smx])
                    k.TS("dve", affall[:, t, :], ex[:, :], smx[:, 3:4], None, ALU.mult, None, r=[ex, smx], w=[affall])
                S.barrier()
            if stage < 5:
                with contextlib.ExitStack() as esd:
                    tf = S.buf("dbg_f", [128, D], F32, es=esd)
                    for t in range(nt4):
                        rows = slice(t * 128, (t + 1) * 128)
                        k.DMA("sp", tf[:, :], x1buf_d[rows, :], r=[x1buf_b], w=[tf])
                        k.DMA("sp", dbg_d[rows, :], tf[:, :], r=[tf], w=[])
                    k.DMA("sp", dbg_d[HALF:HALF + 128, 0:NH * NE], affall[:, :, :].rearrange("p t e -> p (t e)"),
                          r=[affall], w=[])
        if stage >= 5:
            k.DMA("sp", affs_t.ap().rearrange("(t p) e -> p t e", p=128), affall[:, :, :], r=[affall], w=[affs_b])
            if SIM:
                for hh in range(2):
                    k.DMA("sp", affg_t.ap()[hh * HALF:(hh + 1) * HALF, :], affs_t.ap(), r=[affs_b], w=[affg_b])
            else:
                S.coll(lambda e: e.collective_compute("AllGather", ALU.bypass, replica_groups=PAIRS,
                                                      ins=[affs_t.ap().opt()], outs=[affg_t.ap().opt()]),
                       r=[affs_b], w=[affg_b])
            tau = S.buf("tau", [128, NE], F32)
            gate = S.buf("gate", [128, NH, NE], F32)
            sloti = S.buf("sloti", [128, NH, NE], I32)
            with contextlib.ExitStack() as es6:
                def sb(name, shape, dtype):
                    return S.buf(name, shape, dtype, es=es6)
                affb = sb("affb", [128, NT, NE], F32)
                k.DMA("sp", affb[:, :, :], affg_t.ap().rearrange("(t p) e -> p t e", p=128), r=[affg_b], w=[affb])
                cmpb = sb("cmpb", [128, NT, NE], F32)
                hi = sb("hi", [128, NE], F32)
                d2 = sb("d2", [128, NE], F32)
                mid = sb("mid", [128, NE], F32)
                cntp = sb("cntp", [128, NE], F32)
                ge = sb("ge", [128, NE], F32)
                k.MEMSET("pool", tau[:, :], 0.0, w=[tau])
                k.MEMSET("pool", hi[:, :], 1.0001, w=[hi])
                ones_f = cst[:, 7, :]
                for it in range(32):
                    k.TT("dve", d2[:, :], hi[:, :], tau[:, :], ALU.subtract, r=[hi, tau], w=[d2])
                    k.STT("dve", mid[:, :], d2[:, :], 0.5, tau[:, :], ALU.mult, ALU.add, r=[d2, tau], w=[mid])
                    k.TT("dve", cmpb[:, :, :], affb[:, :, :], mid[:, :].unsqueeze(1).to_broadcast([128, NT, NE]),
                         ALU.is_ge, r=[affb, mid], w=[cmpb])
                    S.op("dve", lambda e: e.tensor_reduce(out=cntp[:, :], in_=cmpb[:, :, :].rearrange("p t e -> p e t"),
                                                          axis=AX.X, op=ALU.add), r=[cmpb], w=[cntp])
                    k.MM(P[6][:, 0:NE], ones_f, cntp[:, :], True, True, r=[cst, cntp], w=[P[6]])
                    k.TS("dve", ge[:, :], P[6][:, 0:NE], float(CAP), None, ALU.is_ge, None, r=[P[6]], w=[ge])
                    k.TT("dve", ge[:, :], ge[:, :], d2[:, :], ALU.mult, r=[ge, d2], w=[ge])
                    k.STT("dve", tau[:, :], ge[:, :], 0.5, tau[:, :], ALU.mult, ALU.add, r=[ge, tau], w=[tau])
                    k.STT("dve", hi[:, :], d2[:, :], -0.5, hi[:, :], ALU.mult, ALU.add, r=[d2, hi], w=[hi])
                    k.STT("dve", hi[:, :], ge[:, :], 0.5, hi[:, :], ALU.mult, ALU.add, r=[ge, hi], w=[hi])
                maskf = sb("maskf", [128, NH, NE], F32)
                maskb = sb("maskb", [128, NH * NE], BF16)
                totE = sb("totE", [128, NE, NH], F32)
                cumE = sb("cumE", [128, NE, NH], F32)
                ones32 = sb("ones32", [128, NH], F32)
                posf = sb("posf", [128, NH, NE], F32)
                k.TT("dve", maskf[:, :, :], affall[:, :, :], tau[:, :].unsqueeze(1).to_broadcast([128, NH, NE]),
                     ALU.is_ge, r=[affall, tau], w=[maskf])
                k.TT("dve", gate[:, :, :], maskf[:, :, :], affall[:, :, :], ALU.mult, r=[maskf, affall], w=[gate])
                k.CP("dve", maskb[:, :], maskf[:, :, :].rearrange("p t e -> p (t e)"), r=[maskf], w=[maskb])
                k.MM(P[1][:, :], cstb[:, 8, :], maskb[:, :], True, True, r=[cstb, maskb], w=[P[1]])
                k.MM(P[2][:, :], onesb, maskb[:, :], True, True, r=[cstb, maskb], w=[P[2]])
                k.CP("dve", totE[:, :, :], P[2][:, :].rearrange("p (t e) -> p e t", e=NE), r=[P[2]], w=[totE])
                k.MEMSET("pool", ones32[:, :], 1.0, w=[ones32])
                for e_ in range(NE):
                    S.op("dve", (lambda e_: lambda eng: eng.tensor_tensor_scan(
                        out=cumE[:, e_, :], data0=ones32[:, :], data1=totE[:, e_, :], initial=0.0,
                        op0=ALU.mult, op1=ALU.add))(e_), r=[ones32, totE], w=[cumE])
                k.TT("dve", cumE[:, :, :], cumE[:, :, :], totE[:, :, :], ALU.subtract, r=[cumE, totE], w=[cumE])
                k.TT("dve", posf[:, :, :], P[1][:, :].rearrange("p (t e) -> p t e", e=NE),
                     cumE[:, :, :].rearrange("p e t -> p t e"), ALU.add, r=[P[1], cumE], w=[posf])
                ltm = sb("ltm", [128, NH, NE], F32)
                k.TS("dve", ltm[:, :, :], posf[:, :, :], float(CAP), None, ALU.is_lt, None, r=[posf], w=[ltm])
                k.TT("dve", maskf[:, :, :], maskf[:, :, :], ltm[:, :, :], ALU.mult, r=[maskf, ltm], w=[maskf])
                k.TT("dve", posf[:, :, :], posf[:, :, :], cst[:, 9, 2:2 + NE].unsqueeze(1).to_broadcast([128, NH, NE]),
                     ALU.add, r=[posf, cst], w=[posf])
                k.TT("dve", posf[:, :, :], posf[:, :, :], maskf[:, :, :], ALU.mult, r=[posf, maskf], w=[posf])
                k.TS("dve", maskf[:, :, :], maskf[:, :, :], -1.0, 1.0, ALU.mult, ALU.add, r=[maskf], w=[maskf])
                k.TS("dve", maskf[:, :, :], maskf[:, :, :], cst[:, 9, 18:19], None, ALU.mult, None, r=[maskf, cst], w=[maskf])
                k.TT("dve", posf[:, :, :], posf[:, :, :], maskf[:, :, :], ALU.add, r=[posf, maskf], w=[posf])
                k.CP("dve", sloti[:, :, :], posf[:, :, :], r=[posf], w=[sloti])
                S.barrier()
            if stage < 7:
                with contextlib.ExitStack() as esd:
                    tf = S.buf("dbg_g", [128, NH * NE], F32, es=esd)
                    k.DMA("sp", dbg_d[HALF + 128:HALF + 256, 0:NE], tau[:, :], r=[tau], w=[])
                    k.DMA("sp", dbg_d[HALF + 256:HALF + 384, 0:NH * NE], gate[:, :, :].rearrange("p t e -> p (t e)"),
                          r=[gate], w=[])
                    k.CP("dve", tf[:, :], sloti[:, :, :].rearrange("p t e -> p (t e)"), r=[sloti], w=[tf])
                    k.DMA("sp", dbg_d[HALF + 384:HALF + 512, 0:NH * NE], tf[:, :], r=[tf], w=[])
        if stage >= 7:
            with contextlib.ExitStack() as es7:
                def sb(name, shape, dtype):
                    return S.buf(name, shape, dtype)
                wgb = sb("wgb", [128, 8, D], BF16)
                wub = sb("wub", [128, 8, D], BF16)
                wdb = sb("wdb", [128, 8, D], BF16)
                hd = [sb(f"hd{i}", [128, ROWW], BF16) for i in range(6)]
                xs_in = [sb(f"xs_in{i}", [128, ROWW], BF16) for i in range(2)]
                xinT = sb("xinT", [128, 8, CAP], BF16)
                hid = sb("hid", [128, 8, CAP], BF16)
                gall = sb("gall", [128, 8, 8], F32)
                iall = sb("iall", [128, 8, 8], I32)
                sg = [sb(f"sg{i}", [128, 512], F32) for i in range(2)]
                yt = [sb(f"yt{i}", [128, D], F32) for i in range(2)]

                def load_w(e_):
                    for (wb, wd_) in ((wgb, wg_d), (wub, wu_d), (wdb, wd_d)):
                        k.DMA("pool", wb[:, :, :], wd_[e_].rearrange("(k p) n -> p k n", p=128), r=[], w=[wb])

                def dispatch(e_):
                    for t in range(NH):
                        hb_ = hd[(e_ * NH + t) % 6]
                        k.DMA("sp", hb_[:, :], h2buf_d[t * 128:(t + 1) * 128, :], r=[h2buf_b], w=[hb_])
                        k.CP("dve", hb_[:, 1024:1026].bitcast(F32), gate[:, t, e_:e_ + 1], r=[gate], w=[hb_])
                        S.dma("pool", (lambda hb_, t, e_: lambda eng: eng.indirect_dma_start(
                            out=xin_flat, out_offset=bass.IndirectOffsetOnAxis(ap=sloti[:, t, e_:e_ + 1], axis=0),
                            in_=hb_[:, :], in_offset=None))(hb_, t, e_),
                            r=[hb_, sloti, xin_e[e_]], w=[])

                nexp = NE if stage >= 8 else 2
                load_w(0)
                dispatch(0)
                pbi = 0
                ybi = 0
                for e_ in range(nexp):
                    for sc in range(8):
                        xb_ = xs_in[sc % 2]
                        if sc == 0:
                            k.DMA("sp", xb_[:, :], xin_d[e_, sc * 128:(sc + 1) * 128, :], r=[], w=[xb_, xin_e[e_]])
                        else:
                            k.DMA("sp", xb_[:, :], xin_d[e_, sc * 128:(sc + 1) * 128, :], r=[xin_e[e_]], w=[xb_])
                        k.CP("dve", gall[:, sc, 0:1], xb_[:, 1024:1026].bitcast(F32), r=[xb_], w=[gall])
                        k.CP("dve", iall[:, sc, 0:1], xb_[:, 1026:1028].bitcast(I32), r=[xb_], w=[iall])
                        for kc in range(8):
                            k.TR(P0b[:, kc * 128:(kc + 1) * 128], xb_[:, kc * 128:(kc + 1) * 128], identb,
                                 r=[xb_, cstb], w=[P[0]])
                        k.CP("act", xinT[:, :, sc * 128:(sc + 1) * 128], P0b[:, :].rearrange("p (k l) -> p k l", k=8),
                             r=[P[0]], w=[xinT])
                    if e_ + 1 < nexp:
                        dispatch(e_ + 1)
                    for fc in range(8):
                        for half in range(2):
                            pg, pu = P[1 + 2 * (pbi % 3)], P[2 + 2 * (pbi % 3)]
                            pbi += 1
                            for kc in range(8):
                                k.MM(pg[:, :], wgb[:, kc, fc * 128:(fc + 1) * 128], xinT[:, kc, half * 512:(half + 1) * 512],
                                     kc == 0, kc == 7, r=[wgb, xinT], w=[pg])
                            for kc in range(8):
                                k.MM(pu[:, :], wub[:, kc, fc * 128:(fc + 1) * 128], xinT[:, kc, half * 512:(half + 1) * 512],
                                     kc == 0, kc == 7, r=[wub, xinT], w=[pu])
                            sgb = sg[(fc * 2 + half) % 2]
                            k.ACT(sgb[:, :], pg[:, :], AF.Silu, r=[pg], w=[sgb])
                            k.TT("dve", hid[:, fc, half * 512:(half + 1) * 512], sgb[:, :], pu[:, :], ALU.mult,
                                 r=[sgb, pu], w=[hid])
                    if e_ + 1 < nexp:
                        for (wb, wd_) in ((wgb, wg_d), (wub, wu_d)):
                            k.DMA("pool", wb[:, :, :], wd_[e_ + 1].rearrange("(k p) n -> p k n", p=128), r=[], w=[wb])
                    for sc in range(8):
                        ytb = yt[sc % 2]
                        for n in range(2):
                            py = P[7] if (ybi % 2 == 0) else P[0]
                            ybi += 1
                            for fc in range(8):
                                k.MM(py[:, :], hid[:, fc, sc * 128:(sc + 1) * 128], wdb[:, fc, n * 512:(n + 1) * 512],
                                     fc == 0, fc == 7, r=[hid, wdb], w=[py])
                            k.ACT(ytb[:, n * 512:(n + 1) * 512], py[:, :], AF.Identity, r=[py, gall], w=[ytb],
                                  scale=gall[:, sc, 0:1])
                        S.dma("pool", (lambda ytb, sc: lambda eng: eng.indirect_dma_start(
                            out=acc_d, out_offset=bass.IndirectOffsetOnAxis(ap=iall[:, sc, 0:1], axis=0),
                            in_=ytb[:, :], in_offset=None, compute_op=ALU.add))(ytb, sc),
                            r=[ytb, iall], w=[acc_b])
                    if e_ + 1 < nexp:
                        k.DMA("pool", wdb[:, :, :], wd_d[e_ + 1].rearrange("(k p) n -> p k n", p=128), r=[], w=[wdb])
                S.barrier()
            with contextlib.ExitStack() as es8:
                def sb(name, shape, dtype):
                    return S.buf(name, shape, dtype)
                at = [sb(f"at{i}", [128, D], F32) for i in range(2)]
                x1t = [sb(f"x1t{i}", [128, D], F32) for i in range(2)]
                ot = [sb(f"ot{i}", [128, D], F32) for i in range(2)]
                junk8 = sb("junk8", [128, D], BF16)
                s8 = sb("s8", [128, 8, 8], F32)
                for t in range(NH):
                    i = t % 2
                    rows = slice(t * 128, (t + 1) * 128)
                    k.DMA("sp", at[i][:, :], acc_d[rows, :], r=[acc_b], w=[at[i]])
                    k.DMA("sp", x1t[i][:, :], x1buf_d[rows, :], r=[x1buf_b], w=[x1t[i]])
                    k.MEMSET("pool", s8[:, 0, 0:1], 0.0, w=[s8])
                    k.ACT(junk8[:, :], at[i][:, :], AF.Square, r=[at[i]], w=[junk8, s8], accum=s8[:, 0, 0:1])
                    k.ACT(s8[:, 1, 0:1], s8[:, 0, 0:1], AF.Ln, r=[s8], w=[s8], bias=EPS, scale=1.0 / D)
                    k.ACT(s8[:, 2, 0:1], s8[:, 1, 0:1], AF.Exp, r=[s8], w=[s8], scale=-0.5)
                    k.STT("dve", ot[i][:, :], at[i][:, :], s8[:, 2, 0:1], G2, ALU.mult, ALU.mult,
                          r=[at[i], s8, modrep], w=[ot[i]])
                    k.TT("pool", ot[i][:, :], ot[i][:, :], x1t[i][:, :], ALU.add, r=[ot[i], x1t[i]], w=[ot[i]])
                    k.DMA("sp", out_d[rows, :], ot[i][:, :], r=[ot[i]], w=[])
        S.wait_all_dma("sp")
        S.emit()
    return nc


def make_consts():
    c = np.zeros((128, NCONST, 128), np.float32)
    i = np.arange(128)
    r, cc = i[:, None], i[None, :]
    same = (r // 64) == (cc // 64)
    c[:, 0] = (r == cc)
    c[:, 1] = (r <= cc)
    c[:, 2] = np.where(r > cc, -30000.0, 0.0)
    c[:, 3] = (r <= cc)
    c[:, 4] = same & (r <= cc)
    c[:, 5] = same & (r > cc)
    c[:, 6] = same & (r <= cc)
    c[:, 7] = 1.0
    c[:, 8] = (r < cc)
    c[:, 9, 0] = (i < 64)
    c[:, 9, 1] = (i >= 64)
    c[:, 9, 2:2 + NE] = np.arange(NE)[None, :] * CAP
    c[:, 9, 18] = NE * CAP + i
    return c


def rep(v, n=128):
    return np.ascontiguousarray(np.broadcast_to(np.asarray(v, np.float32)[None], (n,) + tuple(np.shape(v))))


def fm(v):
    return np.ascontiguousarray(np.asarray(v, np.float32).reshape(-1, 128).T)


def prep_inputs(inp):
    x, c, ctx, c_ctx = inp["x"], inp["c"], inp["ctx"], inp["c_ctx"]
    w_in = inp["w_in"][0]
    consts = make_consts()
    shared = {
        "ada_w": np.ascontiguousarray(inp["ada_w"][0]),
        "ada_bT": np.ascontiguousarray(inp["ada_b"][0][:2048].reshape(16, 128).T),
        "ada_brep": rep(inp["ada_b"][0][2048:]),
        "nw0T": fm(inp["norm_w"][0, 0]),
        "nwrep": rep(inp["norm_w"][0, 1:4]),
        "cb": fm(inp["ssd_conv_b"][0]),
        "snw": rep(inp["ssd_norm_w"][0]),
        "hnw": rep(inp["hgrn_norm_w"][0]),
        "w_out": np.ascontiguousarray(inp["w_out"][0]),
        "w_router": np.ascontiguousarray(inp["w_router"][0]),
        "w_gate": np.ascontiguousarray(inp["w_gate"][0]),
        "w_up": np.ascontiguousarray(inp["w_up"][0]),
        "w_down": np.ascontiguousarray(inp["w_down"][0]),
        "consts": consts,
    }
    maps = []
    for core in range(8):
        b, d = core // 2, core % 2
        m = dict(shared)
        m["xs"] = np.ascontiguousarray(x[b][::-1] if d else x[b])
        m["ctxs"] = np.ascontiguousarray(ctx[b][::-1] if d else ctx[b])
        cv = np.stack([fm(c[b]), fm(c_ctx)], axis=-1)
        m["cvec"] = np.ascontiguousarray(cv)
        cols = [w_in[:, 512:1536], w_in[:, 1552:2064], w_in[:, 2064 + 512 * d:2576 + 512 * d], w_in[:, 3088:3600],
                w_in[:, 1536 + 8 * d:1544 + 8 * d], w_in[:, 0:512], w_in[:, 3600:4112]]
        m["wmain"] = np.ascontiguousarray(np.concatenate(cols, axis=1))
        cwk = inp["ssd_conv_w"][0]
        if d:
            cwk = cwk[::-1]
        m["cw"] = np.ascontiguousarray(cwk.T.reshape(8, 128, 5).transpose(1, 0, 2))
        m["sp8"] = rep(np.stack([inp["ssd_dt_bias"][0, d], inp["ssd_a_log"][0, d], inp["ssd_d"][0]]))
        m["lbrep"] = rep(np.stack([inp["hgrn_lb"][0, d], inp["hgrn_lb"][1, d]]))
        p = np.arange(128)[:, None]
        t = np.arange(NH)[None, :]
        m["pidx"] = np.ascontiguousarray(np.concatenate(
            [(1 - d) * 1024 + ((HALF - 1) - (t * 128 + p)) % 1024, t * 128 + p], axis=1).astype(np.int32))
        maps.append(m)
    return maps


STAGE = int(os.environ.get("KSTAGE", "9"))
LITE = int(os.environ.get("KLITE", "0"))
SIM = int(os.environ.get("KSIM", "0"))
SAME_ENGINE_SYNC = bool(int(os.environ.get("KSES", "1")))
SUBCUT = os.environ.get("KSUB", "")
_CACHE = {}


def kernel(**inputs):
    inp = {k_: np.asarray(v) for k_, v in inputs.items()}
    maps = prep_inputs(inp)
    if LITE:
        for m in maps:
            m["ada_w"] = m["ada_w"][:8]
            m["xs"] = m["xs"][:1024]
    if STAGE < 7:
        for m in maps:
            for kk_ in ("w_gate", "w_up", "w_down"):
                m.pop(kk_)
    if STAGE not in _CACHE:
        _CACHE[STAGE] = build_program(STAGE)
    nc = _CACHE[STAGE]
    res = run_bass_kernel_spmd(nc, maps, core_ids=list(range(8)))
    if STAGE < 9:
        return [r["dbg"] for r in res.results]
    out = np.empty((4, SEQ, D), np.float32)
    for core in range(8):
        b, d = core // 2, core % 2
        o = res.results[core]["out"]
        if d:
            out[b, HALF:] = o[::-1]
        else:
            out[b, :HALF] = o
    return out
```

```python
import contextlib
import os
import numpy as np
import concourse.bass as bass
import concourse.mybir as mybir
from concourse.bass_utils import run_bass_kernel_spmd

F32 = mybir.dt.float32
BF16 = mybir.dt.bfloat16
I32 = mybir.dt.int32
AF = mybir.ActivationFunctionType
ALU = mybir.AluOpType
AX = mybir.AxisListType

D = 1024
SEQ = 8192
CTX = 256
NT = SEQ // 128
NH = NT // 2
HALF = SEQ // 2
NE = 16
CAP = 1024
EPS = 1e-6
WCOLS = 3592
C_XBC, C_Q, C_F, C_I, C_DT, C_Z, C_G = 0, 1024, 1536, 2048, 2560, 2568, 3080
NCONST = 10
ROWW = 1028
TRASH = HALF


class Buf:
    __slots__ = ("name", "t", "last_w", "readers")

    def __init__(self, name, t=None):
        self.name = name
        self.t = t
        self.last_w = None
        self.readers = []

    def __getitem__(self, k):
        return self.t[k]


class Sched:
    COMPUTE = ("pe", "dve", "act", "pool")
    NDSEM = 6

    def __init__(self, nc, es, same_engine_sync=True):
        self.nc = nc
        self.es = es
        self.same_engine_sync = same_engine_sync
        self.prog = {k: [] for k in ("pe", "dve", "act", "pool", "sp")}
        self.ninst = {k: 0 for k in self.COMPUTE}
        self.waited_idx = {}
        self.milestones = {k: set() for k in self.COMPUTE}
        self.csem = {k: es.enter_context(nc.semaphore("cs_" + k)) for k in self.COMPUTE}
        self.dsem, self.dcnt, self.drot = {}, {}, {}
        for q in ("sp", "act", "pool"):
            self.dsem[q] = [es.enter_context(nc.semaphore(f"ds_{q}{j}")) for j in range(self.NDSEM)]
            self.dcnt[q] = [0] * self.NDSEM
            self.drot[q] = 0
        self.ccsem = es.enter_context(nc.semaphore("cc_sem"))
        self.cccnt = 0

    def buf(self, name, shape, dtype, psum=False, es=None):
        es = es or self.es
        name = "s_" + name
        if psum:
            t = es.enter_context(self.nc.psum_tensor(name, list(shape), dtype))
        else:
            t = es.enter_context(self.nc.sbuf_tensor(name, list(shape), dtype))
        return Buf(name, t)

    def _need(self, eng, tok):
        if tok is None:
            return
        semkey, v = tok
        key = (eng, semkey)
        if semkey[0] == "c":
            src = semkey[1]
            if src == eng and (eng == "pe" or not self.same_engine_sync):
                return
            if self.waited_idx.get(key, -1) >= v:
                return
            self.waited_idx[key] = v
            self.milestones[src].add(v)
            self.prog[eng].append(("cwait", src, v))
        elif semkey[0] == "x":
            if self.waited_idx.get(key, -1) >= v:
                return
            self.waited_idx[key] = v
            self.prog[eng].append(("xwait", None, v))
        else:
            if self.waited_idx.get(key, -1) >= v:
                return
            self.waited_idx[key] = v
            _, q, j = semkey
            self.prog[eng].append(("dwait", (q, j), v))

    def _deps(self, eng, r, w):
        for b in r:
            self._need(eng, b.last_w)
        for b in w:
            self._need(eng, b.last_w)
            for t in b.readers:
                self._need(eng, t)

    def _commit(self, tok, r, w):
        for b in w:
            b.last_w = tok
            b.readers = []
        for b in r:
            if b not in w:
                b.readers.append(tok)

    def op(self, eng, fn, r=(), w=()):
        self._deps(eng, r, w)
        idx = self.ninst[eng]
        self.ninst[eng] += 1
        self.prog[eng].append(("op", fn, idx))
        tok = (("c", eng), idx)
        self._commit(tok, r, w)
        return tok

    def dma(self, q, fn, r=(), w=()):
        self._deps(q, r, w)
        j = self.drot[q]
        self.drot[q] = (j + 1) % self.NDSEM
        semkey = ("d", q, j)
        prev = self.dcnt[q][j]
        if prev > 0:
            self._need(q, (semkey, prev))
        self.dcnt[q][j] += 16
        v = self.dcnt[q][j]
        self.prog[q].append(("dma", fn, (q, j)))
        tok = (semkey, v)
        self._commit(tok, r, w)
        return tok

    def coll(self, fn, r=(), w=()):
        q = "pool"
        self._deps(q, r, w)
        self.cccnt += 1
        v = self.cccnt
        self.prog[q].append(("coll", fn, v))
        tok = (("x", "cc"), v)
        self._commit(tok, r, w)
        return tok

    def barrier(self):
        for eng in ("pe", "dve", "act", "pool", "sp"):
            for x in self.COMPUTE:
                if x != eng and self.ninst[x] > 0:
                    self._need(eng, (("c", x), self.ninst[x] - 1))
            self.wait_all_dma(eng)

    def wait_all_dma(self, eng="sp"):
        for q in ("sp", "act", "pool"):
            for j in range(self.NDSEM):
                if self.dcnt[q][j]:
                    self._need(eng, (("d", q, j), self.dcnt[q][j]))
        if self.cccnt:
            self._need(eng, (("x", "cc"), self.cccnt))

    def emit(self):
        nc = self.nc
        rank = {}
        for e in self.COMPUTE:
            ms = sorted(self.milestones[e])
            rank[e] = {idx: i + 1 for i, idx in enumerate(ms)}
        sched = self

        def run(engname, e):
            for ent in sched.prog[engname]:
                kind = ent[0]
                if kind == "op":
                    ins = ent[1](e)
                    if ent[2] in rank[engname]:
                        ins.then_inc(sched.csem[engname], 1)
                elif kind == "cwait":
                    e.wait_ge(sched.csem[ent[1]], rank[ent[1]][ent[2]])
                elif kind == "dwait":
                    q, j = ent[1]
                    e.wait_ge(sched.dsem[q][j], ent[2])
                elif kind == "xwait":
                    e.wait_ge(sched.ccsem, ent[2])
                elif kind == "coll":
                    ent[1](e).then_inc(sched.ccsem, 1)
                elif kind == "dma":
                    q, j = ent[2]
                    ent[1](e).then_inc(sched.dsem[q][j], 16)

        with nc.Block() as block:
            @block.sync
            def _(e):
                run("sp", e)

            @block.scalar
            def _(e):
                run("act", e)

            @block.vector
            def _(e):
                run("dve", e)

            @block.gpsimd
            def _(e):
                run("pool", e)

            @block.tensor
            def _(e):
                run("pe", e)


class K:
    def __init__(self, nc, es):
        self.nc = nc
        self.S = Sched(nc, es, same_engine_sync=SAME_ENGINE_SYNC)

    def MM(self, out, lhsT, rhs, start, stop, r, w):
        self.S.op("pe", lambda e: e.matmul(out, lhsT=lhsT, rhs=rhs, start=start, stop=stop), r=r, w=w)

    def TR(self, out, in_, ident, r, w):
        self.S.op("pe", lambda e: e.transpose(out=out, in_=in_, identity=ident), r=r, w=w)

    def ACT(self, out, in_, func, r, w, bias=None, scale=None, accum=None):
        kw = {}
        if bias is not None:
            kw["bias"] = bias
        if scale is not None:
            kw["scale"] = scale
        if accum is not None:
            kw["accum_out"] = accum
        self.S.op("act", lambda e: e.activation(out=out, in_=in_, func=func, **kw), r=r, w=w)

    def TT(self, eng, out, in0, in1, op, r, w):
        self.S.op(eng, lambda e: e.tensor_tensor(out=out, in0=in0, in1=in1, op=op), r=r, w=w)

    def TS(self, eng, out, in0, s1, s2, op0, op1, r, w, accum=None):
        if op1 is None:
            self.S.op(eng, lambda e: e.tensor_scalar(out=out, in0=in0, scalar1=s1, scalar2=None, op0=op0), r=r, w=w)
        elif accum is not None:
            self.S.op(eng, lambda e: e.tensor_scalar(out=out, in0=in0, scalar1=s1, scalar2=s2, op0=op0, op1=op1,
                                                     accum_out=accum), r=r, w=w)
        else:
            self.S.op(eng, lambda e: e.tensor_scalar(out=out, in0=in0, scalar1=s1, scalar2=s2, op0=op0, op1=op1),
                      r=r, w=w)

    def STT(self, eng, out, in0, scalar, in1, op0, op1, r, w):
        self.S.op(eng, lambda e: e.scalar_tensor_tensor(out=out, in0=in0, scalar=scalar, in1=in1, op0=op0, op1=op1),
                  r=r, w=w)

    def CP(self, eng, out, in_, r, w):
        if eng == "act":
            self.S.op("act", lambda e: e.copy(out=out, in_=in_), r=r, w=w)
        else:
            self.S.op(eng, lambda e: e.tensor_copy(out=out, in_=in_), r=r, w=w)

    def MEMSET(self, eng, ap, val, w):
        self.S.op(eng, lambda e: e.memset(ap, val), w=w)

    def DMA(self, q, out, in_, r, w):
        return self.S.dma(q, lambda e: e.dma_start(out=out, in_=in_), r=r, w=w)


def build_program(stage):
    nc = bass.Bass("TRN2", target_bir_lowering=False)
    dt_in = lambda name, shape, dt=F32: nc.dram_tensor(name, list(shape), dt, kind="ExternalInput").ap()
    x_d = dt_in("xs", [1024 if LITE else SEQ, D])
    ctx_d = dt_in("ctxs", [CTX, D])
    cvec_d = dt_in("cvec", [128, 8, 2])
    adaw_d = dt_in("ada_w", [8 if LITE else D, 6 * D])
    adabT_d = dt_in("ada_bT", [128, 16])
    adabrep_d = dt_in("ada_brep", [128, 4 * D])
    nw0T_d = dt_in("nw0T", [128, 8])
    nwrep_d = dt_in("nwrep", [128, 3, D])
    wmain_d = dt_in("wmain", [D, WCOLS])
    cw_d = dt_in("cw", [128, 8, 5])
    cb_d = dt_in("cb", [128, 8])
    sp8_d = dt_in("sp8", [128, 3, 8])
    lbrep_d = dt_in("lbrep", [128, 2, 512])
    snw_d = dt_in("snw", [128, 512])
    hnw_d = dt_in("hnw", [128, 512])
    wout_d = dt_in("w_out", [D, D])
    wr_d = dt_in("w_router", [D, NE])
    if stage >= 7:
        wg_d = dt_in("w_gate", [NE, D, D])
        wu_d = dt_in("w_up", [NE, D, D])
        wd_d = dt_in("w_down", [NE, D, D])
    consts_d = dt_in("consts", [128, NCONST, 128])
    pidx_d = dt_in("pidx", [128, 2 * NH], I32)
    out_d = nc.dram_tensor("out", [HALF, D], F32, kind="ExternalOutput").ap()
    dbg_d = None
    if stage < 9:
        dbg_d = nc.dram_tensor("dbg", [SEQ, D], F32, kind="ExternalOutput").ap()
    ybuf_d = nc.dram_tensor("ybuf", [HALF, D], BF16).ap()
    ysend_t = [nc.dram_tensor(f"ysend{c}", [1024, D], BF16) for c in range(4)]
    ygath_t = [nc.dram_tensor(f"ygath{c}", [2048, D], BF16) for c in range(4)]
    zgbuf_d = nc.dram_tensor("zgbuf", [HALF, D], BF16).ap()
    x1buf_d = nc.dram_tensor("x1buf", [HALF, D], F32).ap()
    h2buf_d = nc.dram_tensor("h2buf", [HALF, ROWW], BF16).ap()
    affs_t = nc.dram_tensor("affsend", [HALF, NE], F32)
    affg_t = nc.dram_tensor("affgath", [SEQ, NE], F32)
    xin_flat = nc.dram_tensor("xin", [NE * CAP + 128, ROWW], BF16).ap()
    xin_d = xin_flat[0:NE * CAP, :].rearrange("(e c) r -> e c r", e=NE)
    acc_d = nc.dram_tensor("moeacc", [HALF + 128, D], F32).ap()
    ybuf_b, ysend_b, ygath_b, zgbuf_b, x1buf_b, h2buf_b = (Buf(n) for n in ("ybuf", "ysend", "ygath", "zgbuf", "x1buf", "h2buf"))
    affs_b, affg_b, xin_b, acc_b = (Buf(n) for n in ("affs", "affg", "xin", "acc"))
    PAIRS = [[0, 1], [2, 3], [4, 5], [6, 7]]

    with contextlib.ExitStack() as es:
        k = K(nc, es)
        S = k.S
        P = [S.buf(f"P{i}", [128, 512], F32, psum=True) for i in range(8)]
        P0b = P[0].t[:, :].bitcast(BF16)

        cst = S.buf("cst", [128, NCONST, 128], F32)
        cstb = S.buf("cstb", [128, NCONST, 128], BF16)
        k.DMA("sp", cst[:, :, :], consts_d, r=[], w=[cst])
        k.CP("dve", cstb[:, :, :], cst[:, :, :], r=[cst], w=[cstb])
        ident, identb = cst[:, 0, :], cstb[:, 0, :]
        trib, negmb = cstb[:, 1, :], cstb[:, 2, :]
        mask01 = cst[:, 3, :]
        tri2i, tri2r, mask2 = cst[:, 4, :], cst[:, 5, :], cst[:, 6, :]
        onesb = cstb[:, 7, :]
        chunkind = cst[:, 9, 0:2]


        cvec = S.buf("cvec", [128, 8, 2], F32)
        k.DMA("sp", cvec[:, :, :], cvec_d, r=[], w=[cvec])
        adabT = S.buf("adabT", [128, 16], F32)
        k.DMA("sp", adabT[:, :], adabT_d, r=[], w=[adabT])
        nw0T = S.buf("nw0T", [128, 8], F32)
        k.DMA("sp", nw0T[:, :], nw0T_d, r=[], w=[nw0T])
        cw = S.buf("cw", [128, 8, 5], F32)
        k.DMA("sp", cw[:, :, :], cw_d, r=[], w=[cw])
        cb = S.buf("cb", [128, 8], F32)
        k.DMA("sp", cb[:, :], cb_d, r=[], w=[cb])
        sp8 = S.buf("sp8", [128, 3, 8], F32)
        k.DMA("sp", sp8[:, :, :], sp8_d, r=[], w=[sp8])
        lbrep = S.buf("lbrep", [128, 2, 512], F32)
        k.DMA("sp", lbrep[:, :, :], lbrep_d, r=[], w=[lbrep])

        if stage >= 7:
            initr = S.buf("initr", [128, ROWW], BF16)
            k.MEMSET("pool", initr[:, :], 0.0, w=[initr])
            k.MEMSET("pool", initr[:, 1026:1028].bitcast(I32), TRASH, w=[initr])
            zt = S.buf("zt", [128, D], F32)
            k.MEMSET("pool", zt[:, :], 0.0, w=[zt])
            xin_e = [Buf(f"xin_e{e_}") for e_ in range(NE)]
            for e_ in range(NE):
                for sc in range(8):
                    k.DMA("sp", xin_d[e_, sc * 128:(sc + 1) * 128, :], initr[:, :], r=[initr], w=[xin_e[e_]])
            for t in range(NH + 1):
                k.DMA("sp", acc_d[t * 128:(t + 1) * 128, :], zt[:, :], r=[zt], w=[acc_b])
        sc = S.buf("sc", [128, 8, 2], F32)
        k.ACT(sc[:, :, :], cvec[:, :, :], AF.Silu, r=[cvec], w=[sc])
        modT = S.buf("modT", [128, 16, 2], F32)
        modrep = S.buf("modrep", [128, 4 * D], F32)
        with contextlib.ExitStack() as es0:
            adap = [S.buf(f"adap{i}", [128, 8, 512], F32, es=es0) for i in range(2)]
            adabrep = S.buf("adabrep", [128, 4 * D], F32, es=es0)
            nwrep = S.buf("nwrep", [128, 3, D], F32, es=es0)
            k.DMA("sp", adabrep[:, :], adabrep_d, r=[], w=[adabrep])
            k.DMA("sp", nwrep[:, :, :], nwrep_d, r=[], w=[nwrep])
            if LITE:
                k.MEMSET("pool", modT[:, :, :], 0.1, w=[modT])
                k.MEMSET("pool", modrep[:, :], 0.1, w=[modrep])
            for j in range(0 if LITE else 12):
                ap_ = adap[j % 2]
                k.DMA("sp", ap_[:, :, :], adaw_d[:, j * 512:(j + 1) * 512].rearrange("(k p) n -> p k n", p=128),
                      r=[], w=[ap_])
                if j < 4:
                    for m in range(4):
                        cc = j * 4 + m
                        for kc in range(8):
                            k.MM(P[6][:, 0:2], ap_[:, kc, m * 128:(m + 1) * 128], sc[:, kc, :], kc == 0, kc == 7,
                                 r=[ap_, sc], w=[P[6]])
                        k.TS("dve", modT[:, cc, :], P[6][:, 0:2], adabT[:, cc:cc + 1], None, ALU.add, None,
                             r=[P[6], adabT], w=[modT])
                else:
                    pb = P[j % 2 + 1]
                    for kc in range(8):
                        k.MM(pb[:, :], sc[:, kc, 0:1].to_broadcast([128, 128]), ap_[:, kc, :], kc == 0, kc == 7,
                             r=[ap_, sc], w=[pb])
                    o = (j - 4) * 512
                    k.TT("dve", modrep[:, o:o + 512], pb[:, :], adabrep[:, o:o + 512], ALU.add,
                         r=[pb, adabrep], w=[modrep])
            k.TT("dve", modrep[:, 0:D], modrep[:, 0:D], nwrep[:, 0, :], ALU.mult, r=[modrep, nwrep], w=[modrep])
            k.STT("dve", modrep[:, 2 * D:3 * D], modrep[:, 2 * D:3 * D], 1.0, nwrep[:, 1, :], ALU.add, ALU.mult,
                  r=[modrep, nwrep], w=[modrep])
            k.TT("dve", modrep[:, 3 * D:4 * D], modrep[:, 3 * D:4 * D], nwrep[:, 2, :], ALU.mult,
                 r=[modrep, nwrep], w=[modrep])
            S.barrier()
        G1, B2, A2, G2 = (modrep[:, i * D:(i + 1) * D] for i in range(4))
        A0 = S.buf("A0", [128, 2, 8], F32)
        B0 = S.buf("B0", [128, 2, 8], F32)
        for i in range(2):
            k.STT("dve", A0[:, i, :], modT[:, 8:16, i], 1.0, nw0T[:, :], ALU.add, ALU.mult, r=[modT, nw0T], w=[A0])
            k.CP("dve", B0[:, i, :], modT[:, 0:8, i], r=[modT], w=[B0])
        aneg = S.buf("aneg", [128, 8], F32)
        k.ACT(aneg[:, :], sp8[:, 1, :], AF.Exp, r=[sp8], w=[aneg])
        k.TS("dve", aneg[:, :], aneg[:, :], -1.0, None, ALU.mult, None, r=[aneg], w=[aneg])
        dsk = S.buf("dsk", [128, 8], F32)
        k.TS("dve", dsk[:, :], sp8[:, 2, :], 0.5, None, ALU.mult, None, r=[sp8], w=[dsk])
        Dm = S.buf("Dm", [128, 8, 128], BF16)
        for j in range(8):
            k.TS("dve", Dm[:, j, :], ident, dsk[:, j:j + 1], None, ALU.mult, None, r=[cst, dsk], w=[Dm])
        c01 = S.buf("c01", [128, 2, 512], F32)
        k.TT("dve", c01[:, 0, :], lbrep[:, 0, :], lbrep[:, 1, :], ALU.subtract, r=[lbrep], w=[c01])
        k.ACT(c01[:, 1, :], c01[:, 0, :], AF.Tanh, r=[c01], w=[c01], scale=0.5)
        k.TS("dve", c01[:, 0, :], c01[:, 1, :], 0.25, 0.75, ALU.mult, ALU.add, r=[c01], w=[c01])
        k.TS("dve", c01[:, 1, :], c01[:, 1, :], -0.25, 0.25, ALU.mult, ALU.add, r=[c01], w=[c01])
        ST = S.buf("ST", [128, 512], F32)
        STb = S.buf("STb", [128, 512], BF16)
        SH = S.buf("SH", [128, 512], F32)
        SHb = S.buf("SHb", [128, 512], BF16)
        k.MEMSET("pool", ST[:, :], 0.0, w=[ST])
        k.MEMSET("pool", STb[:, :], 0.0, w=[STb])
        k.MEMSET("pool", SH[:, :], 0.0, w=[SH])
        k.MEMSET("pool", SHb[:, :], 0.0, w=[SHb])

        with contextlib.ExitStack() as es2:
            def sb(name, shape, dtype):
                return S.buf(name, shape, dtype, es=es2)
            wm = sb("wm", [128, 8, WCOLS], BF16)
            for kc in range(8):
                for (c0, c1) in ((0, 1796), (1796, WCOLS)):
                    k.DMA("pool", wm[:, kc, c0:c1], wmain_d[kc * 128:(kc + 1) * 128, c0:c1], r=[], w=[wm])
            xt = [sb(f"xt{i}", [128, D], F32) for i in range(4)]
            junk = sb("junk", [128, D], BF16)
            ssq = sb("ssq", [128, 2], F32)
            xn = sb("xn", [128, D], BF16)
            hT = [sb(f"hT{i}", [128, 8, 256], BF16) for i in range(2)]
            cacc2 = [sb(f"cacc{i}", [128, 8, 256], F32) for i in range(2)]
            xcT2 = [sb(f"xcT{i}", [128, 8, 256], BF16) for i in range(2)]
            xs_tm = sb("xs_tm", [128, 512], BF16)
            B_tm = sb("B_tm", [128, 256], BF16)
            dts = sb("dts", [128, 8, 8], F32)
            eatot = sb("eatot", [128, 8], F32)
            a_hi = sb("a_hi", [128, 8], BF16)
            a_lo = sb("a_lo", [128, 8], BF16)
            xdt = sb("xdt", [128, 512], BF16)
            xdtd = sb("xdtd", [128, 512], BF16)
            LT = sb("LT", [128, 8, 128], BF16)
            MT = sb("MT", [128, 8, 128], BF16)
            CBm = sb("CBm", [128, 2, 128], BF16)
            yoff = sb("yoff", [128, 512], F32)
            yo = sb("yo", [128, 512], BF16)
            qs = sb("qs", [128, 512], F32)
            vb = sb("vb", [128, 512], BF16)
            vm = sb("vm", [128, 2, 512], BF16)
            ff = sb("ff", [128, 512], F32)
            lf = sb("lf", [128, 512], F32)
            kk = sb("kk", [128, 512], F32)
            et = [sb(f"et{i}", [128, 512], F32) for i in range(2)]
            qdec = sb("qdec", [128, 512], BF16)
            kinv = sb("kinv", [128, 512], BF16)
            kdec = sb("kdec", [128, 512], BF16)
            qdT = sb("qdT", [128, 4, 128], BF16)
            kiT = sb("kiT", [128, 4, 128], BF16)
            attm = sb("attm", [128, 4, 128], BF16)
            oc = sb("oc", [64, 2, 512], BF16)
            ebt = sb("ebt", [128, 4, 2], F32)
            zg = sb("zg", [128, D], BF16)

            def load_x(ti_all):
                b = xt[ti_all % 4]
                src = ctx_d[ti_all * 128:(ti_all + 1) * 128, :] if ti_all < 2 else \
                    x_d[(ti_all - 2) * 128:(ti_all - 1) * 128, :]
                k.DMA("sp", b[:, :], src, r=[], w=[b])

            def norm_T(ti_all, hbuf, col0, which):
                b = xt[ti_all % 4]
                k.MEMSET("pool", ssq[:, 0:1], 0.0, w=[ssq])
                k.ACT(junk[:, :], b[:, :], AF.Square, r=[b], w=[junk, ssq], accum=ssq[:, 0:1])
                k.ACT(ssq[:, 1:2], ssq[:, 0:1], AF.Ln, r=[ssq], w=[ssq], bias=EPS, scale=1.0 / D)
                k.ACT(ssq[:, 1:2], ssq[:, 1:2], AF.Exp, r=[ssq], w=[ssq], scale=-0.5)
                k.TS("pool", xn[:, :], b[:, :], ssq[:, 1:2], None, ALU.mult, None, r=[b, ssq], w=[xn])
                for kc in range(8):
                    k.TR(P0b[:, kc * 128:(kc + 1) * 128], xn[:, kc * 128:(kc + 1) * 128], identb,
                         r=[xn, cstb], w=[P[0]])
                for kc in range(8):
                    o = hbuf[:, kc, col0:col0 + 128]
                    i_ = P0b[:, kc * 128:(kc + 1) * 128]
                    if kc % 2 == 0:
                        k.ACT(o, i_, AF.Identity, r=[P[0], A0, B0], w=[hbuf],
                              bias=B0[:, which, kc:kc + 1], scale=A0[:, which, kc:kc + 1])
                    else:
                        k.TS("dve", o, i_, A0[:, which, kc:kc + 1], B0[:, which, kc:kc + 1], ALU.mult, ALU.add,
                             r=[P[0], A0, B0], w=[hbuf])

            def conv_stage(hbuf, T, roww, chunks, cb_i=0):
                cacc, xcT = cacc2[cb_i], xcT2[cb_i]
                per_bank = 512 // T
                for ci, c in enumerate(chunks):
                    pb = P[1 + ci // per_bank]
                    po = (ci % per_bank) * T
                    for kc in range(8):
                        k.MM(pb[:, po:po + T], wm[:, kc, C_XBC + c * 128:C_XBC + (c + 1) * 128], hbuf[:, kc, 0:T],
                             kc == 0, kc == 7, r=[wm, hbuf], w=[pb])
                for ci, c in enumerate(chunks):
                    pb = P[1 + ci // per_bank]
                    po = (ci % per_bank) * T
                    src = pb[:, po:po + T]
                    acc = cacc[:, c, 0:T]
                    k.ACT(acc, src, AF.Identity, r=[pb, cw, cb], w=[cacc], bias=cb[:, c:c + 1], scale=cw[:, c, 2:3])
                    srcv = src.rearrange("p (r w) -> p r w", w=roww)
                    accv = acc.rearrange("p (r w) -> p r w", w=roww)
                    for kt in (0, 1, 3, 4):
                        s = kt - 2
                        if s > 0:
                            o_, i_ = accv[:, :, 0:roww - s], srcv[:, :, s:roww]
                        else:
                            o_, i_ = accv[:, :, -s:roww], srcv[:, :, 0:roww + s]
                        k.STT("dve", o_, i_, cw[:, c, kt:kt + 1], o_, ALU.mult, ALU.add, r=[pb, cw, cacc], w=[cacc])
                    k.ACT(xcT[:, c, 0:T], acc, AF.Silu, r=[cacc], w=[xcT])

            def scan_tile(hbuf, col0, tcol, lat, ti, zgproj, cb_i=0):
                xcT = xcT2[cb_i]
                hsl = lambda kc: hbuf[:, kc, col0:col0 + 128]
                if zgproj:
                    for (pb, c0) in ((P[1], C_Z), (P[2], C_G)):
                        for kc in range(8):
                            k.MM(pb[:, :], hsl(kc), wm[:, kc, c0:c0 + 512], kc == 0, kc == 7, r=[hbuf, wm], w=[pb])
                    k.ACT(zg[:, 0:512], P[1][:, :], AF.Silu, r=[P[1]], w=[zg])
                    k.ACT(zg[:, 512:1024], P[2][:, :], AF.Silu, r=[P[2]], w=[zg])
                    k.DMA("sp", zgbuf_d[ti * 128:(ti + 1) * 128, :], zg[:, :], r=[zg], w=[zgbuf_b])
                for (pb, c0) in ((P[3], C_Q), (P[4], C_F), (P[5], C_I)):
                    for kc in range(8):
                        k.MM(pb[:, :], hsl(kc), wm[:, kc, c0:c0 + 512], kc == 0, kc == 7, r=[hbuf, wm], w=[pb])
                for kc in range(8):
                    k.MM(P[6][:, 0:8], hsl(kc), wm[:, kc, C_DT:C_DT + 8], kc == 0, kc == 7, r=[hbuf, wm], w=[P[6]])
                if lat:
                    k.ACT(qs[:, :], P[3][:, :], AF.Silu, r=[P[3]], w=[qs])
                k.ACT(ff[:, :], P[4][:, :], AF.Tanh, r=[P[4]], w=[ff], scale=0.5)
                k.CP("dve", vb[:, :], P[5][:, :], r=[P[5]], w=[vb])
                for c in range(2):
                    k.TS("pool", vm[:, c, :], vb[:, :], chunkind[:, c:c + 1], None, ALU.mult, None, r=[vb, cst], w=[vm])
                if SUBCUT == 'A':
                    return
                for c in range(6):
                    k.TR(P0b[:, c * 128:(c + 1) * 128], xcT[:, c, tcol:tcol + 128], identb, r=[xcT, cstb], w=[P[0]])
                if SUBCUT == 'B1':
                    return
                k.CP("act", xs_tm[:, :], P0b[:, 0:512], r=[P[0]], w=[xs_tm])
                if SUBCUT == 'B2':
                    return
                k.CP("act", B_tm[:, :], P0b[:, 512:768], r=[P[0]], w=[B_tm])
                if SUBCUT == 'B':
                    return
                v_, av_, l_, dt_, a_, nacs, eacs, w2 = (dts[:, i, :] for i in range(8))
                k.TT("dve", v_, P[6][:, 0:8], sp8[:, 0, :], ALU.add, r=[P[6], sp8], w=[dts])
                k.TS("dve", av_, v_, 30.0, None, ALU.min, None, r=[dts], w=[dts])
                k.ACT(av_, av_, AF.Exp, r=[dts], w=[dts])
                k.ACT(l_, av_, AF.Ln, r=[dts], w=[dts], bias=1.0)
                k.TT("dve", dt_, l_, v_, ALU.max, r=[dts], w=[dts])
                k.TT("dve", a_, dt_, aneg[:, :], ALU.mult, r=[dts, aneg], w=[dts])
                k.CP("dve", a_hi[:, :], a_, r=[dts], w=[a_hi])
                k.TT("dve", a_lo[:, :], a_, a_hi[:, :], ALU.subtract, r=[dts, a_hi], w=[a_lo])
                if SUBCUT == 'C1':
                    return
                k.MM(P[6][:, 64:72], trib, a_hi[:, :], True, False, r=[cstb, a_hi], w=[P[6]])
                k.MM(P[6][:, 64:72], trib, a_lo[:, :], False, True, r=[cstb, a_lo], w=[P[6]])
                k.MM(P[6][:, 128:136], onesb, a_hi[:, :], True, False, r=[cstb, a_hi], w=[P[6]])
                k.MM(P[6][:, 128:136], onesb, a_lo[:, :], False, True, r=[cstb, a_lo], w=[P[6]])
                k.TS("dve", nacs, P[6][:, 64:72], -1.0, None, ALU.mult, None, r=[P[6]], w=[dts])
                k.ACT(eacs, P[6][:, 64:72], AF.Exp, r=[P[6]], w=[dts])
                k.TT("dve", w2, P[6][:, 128:136], nacs, ALU.add, r=[P[6], dts], w=[dts])
                k.ACT(w2, w2, AF.Exp, r=[dts], w=[dts])
                k.ACT(eatot[:, :], P[6][:, 128:136], AF.Exp, r=[P[6]], w=[eatot])
                k.TT("dve", w2, w2, dt_, ALU.mult, r=[dts], w=[dts])
                if SUBCUT == 'C2':
                    return
                xs3 = xs_tm[:, :].rearrange("p (j q) -> p j q", q=64)
                k.TT("dve", xdtd[:, :].rearrange("p (j q) -> p j q", q=64), xs3, w2.unsqueeze(2).to_broadcast([128, 8, 64]), ALU.mult,
                     r=[xs_tm, dts], w=[xdtd])
                if lat:
                    k.TT("pool", xdt[:, :].rearrange("p (j q) -> p j q", q=64), xs3, dt_.unsqueeze(2).to_broadcast([128, 8, 64]), ALU.mult,
                         r=[xs_tm, dts], w=[xdt])
                    for j in range(8):
                        pa = P[1 + j // 4]
                        o = pa[:, (j % 4) * 128:(j % 4 + 1) * 128]
                        k.MM(o, a_hi[:, j:j + 1].to_broadcast([128, 128]), trib, True, False, r=[a_hi, cstb], w=[pa])
                        k.MM(o, a_lo[:, j:j + 1].to_broadcast([128, 128]), trib, False, False, r=[a_lo, cstb], w=[pa])
                        k.MM(o, identb, negmb, False, True, r=[cstb], w=[pa])
                        k.ACT(LT[:, j, :], o, AF.Exp, r=[pa, dts], w=[LT], bias=nacs[:, j:j + 1])
                    for g in range(2):
                        k.MM(P[6][:, 128 + g * 128:256 + g * 128], xcT[:, 4 + g, tcol:tcol + 128],
                             xcT[:, 6 + g, tcol:tcol + 128], True, True, r=[xcT], w=[P[6]])
                    k.TT("dve", CBm[:, :, :], P[6][:, 128:384].rearrange("p (g l) -> p g l", g=2),
                         mask01.unsqueeze(1).to_broadcast([128, 2, 128]), ALU.mult, r=[P[6], cst], w=[CBm])
                    for g in range(2):
                        k.TT("pool", MT[:, 4 * g:4 * g + 4, :], LT[:, 4 * g:4 * g + 4, :],
                             CBm[:, g:g + 1, :].to_broadcast([128, 4, 128]), ALU.mult, r=[LT, CBm], w=[MT])
                    for j in range(8):
                        o = P[3][:, j * 64:(j + 1) * 64]
                        k.MM(o, MT[:, j, :], xdt[:, j * 64:(j + 1) * 64], True, False, r=[MT, xdt], w=[P[3]])
                        k.MM(o, Dm[:, j, :], xs_tm[:, j * 64:(j + 1) * 64], False, True, r=[Dm, xs_tm], w=[P[3]])
                    for g in range(2):
                        k.MM(P[7][:, g * 256:(g + 1) * 256], xcT[:, 6 + g, tcol:tcol + 128],
                             STb[:, g * 256:(g + 1) * 256], True, True, r=[xcT, STb], w=[P[7]])
                    k.TT("dve", yoff[:, :].rearrange("p (j q) -> p j q", q=64),
                         P[7][:, :].rearrange("p (j q) -> p j q", q=64),
                         eacs.unsqueeze(2).to_broadcast([128, 8, 64]), ALU.mult, r=[P[7], dts], w=[yoff])
                    k.TT("dve", yo[:, :], P[3][:, :], yoff[:, :], ALU.add, r=[P[3], yoff], w=[yo])
                if SUBCUT == 'C':
                    return
                for g in range(2):
                    k.MM(P[7][:, g * 256:(g + 1) * 256], B_tm[:, g * 128:(g + 1) * 128],
                         xdtd[:, g * 256:(g + 1) * 256], True, True, r=[B_tm, xdtd], w=[P[7]])
                k.TT("dve", ST[:, :].rearrange("p (j q) -> p j q", q=64), ST[:, :].rearrange("p (j q) -> p j q", q=64),
                     eatot[:, :].unsqueeze(2).to_broadcast([128, 8, 64]), ALU.mult, r=[ST, eatot], w=[ST])
                k.TT("dve", ST[:, :], ST[:, :], P[7][:, :], ALU.add, r=[ST, P[7]], w=[ST])
                k.CP("act", STb[:, :], ST[:, :], r=[ST], w=[STb])
                if SUBCUT == 'D':
                    return
                k.TT("dve", ff[:, :], ff[:, :], c01[:, 1, :], ALU.mult, r=[ff, c01], w=[ff])
                k.TT("dve", ff[:, :], ff[:, :], c01[:, 0, :], ALU.add, r=[ff, c01], w=[ff])
                k.ACT(lf[:, :], ff[:, :], AF.Ln, r=[ff], w=[lf])
                k.TS("pool", kk[:, :], ff[:, :], -1.0, 1.0, ALU.mult, ALU.add, r=[ff], w=[kk])
                k.MM(P[4][:, :], tri2i, lf[:, :], True, True, r=[cst, lf], w=[P[4]])
                k.MM(P[5][:, :], tri2r, lf[:, :], True, True, r=[cst, lf], w=[P[5]])
                for h in range(4):
                    k.MM(P[7][:, h * 64:h * 64 + 2], lf[:, h * 128:(h + 1) * 128], chunkind, True, True,
                         r=[lf, cst], w=[P[7]])
                k.ACT(ebt[:, :, :], P[7][:, 0:256].rearrange("p (h c) -> p h c", c=64)[:, :, 0:2], AF.Exp,
                      r=[P[7]], w=[ebt])
                k.ACT(et[0][:, :], P[5][:, :], AF.Exp, r=[P[5]], w=[et[0]])
                k.TT("dve", kdec[:, :], kk[:, :], et[0][:, :], ALU.mult, r=[kk, et[0]], w=[kdec])
                if lat:
                    k.ACT(et[1][:, :], P[4][:, :], AF.Exp, r=[P[4]], w=[et[1]])
                    k.TT("dve", qdec[:, :], qs[:, :], et[1][:, :], ALU.mult, r=[qs, et[1]], w=[qdec])
                    k.ACT(et[0][:, :], P[4][:, :], AF.Exp, r=[P[4]], w=[et[0]], scale=-1.0)
                    k.TT("pool", kinv[:, :], kk[:, :], et[0][:, :], ALU.mult, r=[kk, et[0]], w=[kinv])
                    for h in range(4):
                        k.TR(P0b[:, h * 128:(h + 1) * 128], qdec[:, h * 128:(h + 1) * 128], identb,
                             r=[qdec, cstb], w=[P[0]])
                        k.TR(P0b[:, 512 + h * 128:512 + (h + 1) * 128], kinv[:, h * 128:(h + 1) * 128], identb,
                             r=[kinv, cstb], w=[P[0]])
                    k.CP("act", qdT[:, :, :], P0b[:, 0:512].rearrange("p (h l) -> p h l", h=4), r=[P[0]], w=[qdT])
                    k.CP("act", kiT[:, :, :], P0b[:, 512:1024].rearrange("p (h l) -> p h l", h=4), r=[P[0]], w=[kiT])
                    for h in range(4):
                        k.MM(P[4][:, h * 128:(h + 1) * 128], kiT[:, h, :], qdT[:, h, :], True, True,
                             r=[kiT, qdT], w=[P[4]])
                    k.TT("dve", attm[:, :, :], P[4][:, :].rearrange("p (h l) -> p h l", h=4),
                         mask2.unsqueeze(1).to_broadcast([128, 4, 128]), ALU.mult, r=[P[4], cst], w=[attm])
                if SUBCUT == 'E':
                    return
                for c in range(2):
                    if lat:
                        for h in range(4):
                            o = P[5][0:64, h * 128:(h + 1) * 128]
                            k.MM(o, attm[:, h, c * 64:(c + 1) * 64], vb[:, h * 128:(h + 1) * 128], True, False,
                                 r=[attm, vb], w=[P[5]])
                            k.MM(o, qdT[:, h, c * 64:(c + 1) * 64], SHb[:, h * 128:(h + 1) * 128], False, True,
                                 r=[qdT, SHb], w=[P[5]])
                        k.CP("act", oc[:, c, :], P[5][0:64, :], r=[P[5]], w=[oc])
                    for h in range(4):
                        k.MM(P[7][:, h * 128:(h + 1) * 128], kdec[:, h * 128:(h + 1) * 128],
                             vm[:, c, h * 128:(h + 1) * 128], True, True, r=[kdec, vm], w=[P[7]])
                    for h in range(4):
                        sl = slice(h * 128, (h + 1) * 128)
                        k.STT("dve", SH[:, sl], SH[:, sl], ebt[:, h, c:c + 1], P[7][:, sl], ALU.mult, ALU.add,
                              r=[SH, ebt, P[7]], w=[SH])
                    k.CP("act", SHb[:, :], SH[:, :], r=[SH], w=[SHb])
                if lat:
                    if ti < NH:
                        dst, dstb, rows = ybuf_d, ybuf_b, slice(ti * 128, (ti + 1) * 128)
                    else:
                        dst, dstb = ysend_t[(ti - NH) // 8].ap(), ysend_b
                        rows = slice(((ti - NH) % 8) * 128, ((ti - NH) % 8 + 1) * 128)
                    k.DMA("sp", dst[rows, 0:512], yo[:, :], r=[yo], w=[dstb])
                    k.DMA("sp", dst[rows, 512:1024].rearrange("(c p) v -> p c v", p=64), oc[:, :, :],
                          r=[oc], w=[dstb])

            cut = int(os.environ.get("KCUT", "99"))
            load_x(0)
            load_x(1)
            load_x(2)
            if cut >= 1:
                norm_T(0, hT[0], 0, 1)
                norm_T(1, hT[0], 128, 1)
            if cut >= 2:
                conv_stage(hT[0], 256, 256, [0, 1, 2, 3])
                conv_stage(hT[0], 256, 256, [4, 5, 6, 7])
            if cut >= 3:
                for sub in range(2):
                    scan_tile(hT[0], sub * 128, sub * 128, False, -1, False)
            ntl = NT if stage >= 2 else 4
            ntl = int(os.environ.get("KNT", ntl))
            if cut < 4:
                ntl = 0
            if cut >= 4 and ntl > 1:
                load_x(3)
            npair = ntl // 2

            def front(j):
                a_, b_ = 2 * j, 2 * j + 1
                for nx in (a_ + 4, b_ + 4):
                    if nx < ntl + 2:
                        load_x(nx)
                hb = hT[(j + 1) % 2]
                norm_T(a_ + 2, hb, 0, 0)
                norm_T(b_ + 2, hb, 128, 0)
                conv_stage(hb, 256, 64, [0, 1, 2, 3], (j + 1) % 2)
                conv_stage(hb, 256, 64, [4, 5, 6, 7], (j + 1) % 2)

            if npair > 0:
                front(0)
            for j in range(npair):
                a_, b_ = 2 * j, 2 * j + 1
                hb = hT[(j + 1) % 2]
                scan_tile(hb, 0, 0, True, a_, a_ < NH, (j + 1) % 2)
                if j + 1 < npair:
                    front(j + 1)
                scan_tile(hb, 128, 128, True, b_, b_ < NH, (j + 1) % 2)

            S.barrier()
        if stage < 3:
            with contextlib.ExitStack() as esd:
                tb = S.buf("dbg_b", [128, D], BF16, es=esd)
                tf = S.buf("dbg_f", [128, D], F32, es=esd)
                for ti in range(min(ntl, NH)):
                    rows = slice(ti * 128, (ti + 1) * 128)
                    k.DMA("sp", tb[:, :], ybuf_d[rows, :], r=[ybuf_b], w=[tb])
                    k.CP("dve", tf[:, :], tb[:, :], r=[tb], w=[tf])
                    k.DMA("sp", dbg_d[rows, :], tf[:, :], r=[tf], w=[])
        else:
            for c in range(4):
                if SIM:
                    for hh in range(2):
                        k.DMA("sp", ygath_t[c].ap()[hh * 1024:(hh + 1) * 1024, :], ysend_t[c].ap(), r=[ysend_b], w=[ygath_b])
                else:
                    S.coll((lambda c: lambda e: e.collective_compute(
                        "AllGather", ALU.bypass, replica_groups=PAIRS, ins=[ysend_t[c].ap().opt()],
                        outs=[ygath_t[c].ap().opt()]))(c), r=[ysend_b], w=[ygath_b])
            affall = S.buf("affall", [128, NH, NE], F32)
            pidx = S.buf("pidx", [128, 2 * NH], I32)
            k.DMA("sp", pidx[:, :], pidx_d, r=[], w=[pidx])
            with contextlib.ExitStack() as es4:
                def sb(name, shape, dtype):
                    return S.buf(name, shape, dtype, es=es4)
                wout = sb("wout", [128, 8, D], BF16)
                for kc in range(8):
                    k.DMA("pool", wout[:, kc, :], wout_d[kc * 128:(kc + 1) * 128, :], r=[], w=[wout])
                wr = sb("wr", [128, 8, NE], BF16)
                k.DMA("pool", wr[:, :, :], wr_d.rearrange("(k p) e -> p k e", p=128), r=[], w=[wr])
                snw = sb("snw", [128, 512], F32)
                hnw = sb("hnw", [128, 512], F32)
                k.DMA("sp", snw[:, :], snw_d, r=[], w=[snw])
                k.DMA("sp", hnw[:, :], hnw_d, r=[], w=[hnw])
                yown = [sb(f"yown{i}", [128, D], BF16) for i in range(2)]
                ypar = [sb(f"ypar{i}", [128, D], BF16) for i in range(2)]
                zgt = [sb(f"zgt{i}", [128, D], BF16) for i in range(2)]
                xt2 = [sb(f"xt2{i}", [128, D], F32) for i in range(2)]
                ysum = sb("ysum", [128, D], F32)
                junk4 = sb("junk4", [128, D], BF16)
                ssq6 = sb("ssq6", [128, 8], F32)
                rs6 = sb("rs6", [128, 8], F32)
                tmpo = sb("tmpo", [128, 512], F32)
                ylat = sb("ylat", [128, D], BF16)
                ylT = sb("ylT", [128, 8, 128], BF16)
                ssq2 = sb("ssq2", [128, 4], F32)
                x1 = sb("x1", [128, D], F32)
                h2f = sb("h2f", [128, D], F32)
                h2b = sb("h2b", [128, ROWW], BF16)
                h2T = sb("h2T", [128, 8, 128], BF16)
                smx = sb("smx", [128, 4], F32)
                ex = sb("ex", [128, NE], F32)
                k.MEMSET("pool", h2b[:, 1024:ROWW], 0.0, w=[h2b])

                def p4_load(t):
                    i = t % 2
                    rows = slice(t * 128, (t + 1) * 128)
                    k.DMA("sp", yown[i][:, :], ybuf_d[rows, :], r=[ybuf_b], w=[yown[i]])
                    k.DMA("sp", zgt[i][:, :], zgbuf_d[rows, :], r=[zgbuf_b], w=[zgt[i]])
                    k.DMA("sp", xt2[i][:, :], x_d[rows, :], r=[], w=[xt2[i]])
                    S.dma("pool", lambda e: e.indirect_dma_start(
                        out=ypar[i][:, :], out_offset=None, in_=ygath_t[3 - t // 8].ap(),
                        in_offset=bass.IndirectOffsetOnAxis(ap=pidx[:, t:t + 1], axis=0)),
                        r=[ygath_b, pidx], w=[ypar[i]])

                def rstd_from(ssq_ap, out_ap, n, rbufs):
                    k.ACT(out_ap, ssq_ap, AF.Ln, r=rbufs, w=rbufs, bias=EPS, scale=1.0 / n)
                    k.ACT(out_ap, out_ap, AF.Exp, r=rbufs, w=rbufs, scale=-0.5)

                nt4 = NH if stage >= 4 else 2
                nt4 = int(os.environ.get("KNT4", nt4))
                p4_load(0)
                for t in range(nt4):
                    i = t % 2
                    if t + 1 < nt4:
                        p4_load(t + 1)
                    rows = slice(t * 128, (t + 1) * 128)
                    k.TT("dve", ysum[:, :], yown[i][:, :], ypar[i][:, :], ALU.add, r=[yown[i], ypar[i]], w=[ysum])
                    k.TT("pool", ysum[:, 0:512], ysum[:, 0:512], zgt[i][:, 0:512], ALU.mult, r=[ysum, zgt[i]], w=[ysum])
                    k.MEMSET("pool", ssq6[:, :], 0.0, w=[ssq6])
                    for g in range(2):
                        k.ACT(junk4[:, g * 256:(g + 1) * 256], ysum[:, g * 256:(g + 1) * 256], AF.Square,
                              r=[ysum], w=[junk4, ssq6], accum=ssq6[:, g:g + 1])
                    for h in range(4):
                        sl = slice(512 + h * 128, 512 + (h + 1) * 128)
                        k.ACT(junk4[:, sl], ysum[:, sl], AF.Square, r=[ysum], w=[junk4, ssq6],
                              accum=ssq6[:, 2 + h:3 + h])
                    rstd_from(ssq6[:, 0:2], rs6[:, 0:2], 256, [ssq6, rs6])
                    rstd_from(ssq6[:, 2:6], rs6[:, 2:6], 128, [ssq6, rs6])
                    for g in range(2):
                        sl = slice(g * 256, (g + 1) * 256)
                        k.STT("dve", ylat[:, sl], ysum[:, sl], rs6[:, g:g + 1], snw[:, sl], ALU.mult, ALU.mult,
                              r=[ysum, rs6, snw], w=[ylat])
                    for h in range(4):
                        sl = slice(h * 128, (h + 1) * 128)
                        sl2 = slice(512 + h * 128, 512 + (h + 1) * 128)
                        k.STT("dve", tmpo[:, sl], ysum[:, sl2], rs6[:, 2 + h:3 + h], hnw[:, sl], ALU.mult, ALU.mult,
                              r=[ysum, rs6, hnw], w=[tmpo])
                    k.TT("pool", ylat[:, 512:1024], tmpo[:, :], zgt[i][:, 512:1024], ALU.mult,
                         r=[tmpo, zgt[i]], w=[ylat])
                    for kc in range(8):
                        k.TR(P0b[:, kc * 128:(kc + 1) * 128], ylat[:, kc * 128:(kc + 1) * 128], identb,
                             r=[ylat, cstb], w=[P[0]])
                    k.CP("act", ylT[:, 0:4, :], P0b[:, 0:512].rearrange("p (k l) -> p k l", k=4), r=[P[0]], w=[ylT])
                    k.CP("act", ylT[:, 4:8, :], P0b[:, 512:1024].rearrange("p (k l) -> p k l", k=4), r=[P[0]], w=[ylT])
                    for n in range(2):
                        for kc in range(8):
                            k.MM(P[1 + n][:, :], ylT[:, kc, :], wout[:, kc, n * 512:(n + 1) * 512], kc == 0, kc == 7,
                                 r=[ylT, wout], w=[P[1 + n]])
                    k.MEMSET("pool", ssq2[:, :], 0.0, w=[ssq2])
                    for n in range(2):
                        k.ACT(junk4[:, n * 512:(n + 1) * 512], P[1 + n][:, :], AF.Square, r=[P[1 + n]],
                              w=[junk4, ssq2], accum=ssq2[:, n:n + 1])
                    k.TT("dve", ssq2[:, 2:3], ssq2[:, 0:1], ssq2[:, 1:2], ALU.add, r=[ssq2], w=[ssq2])
                    rstd_from(ssq2[:, 2:3], ssq2[:, 3:4], D, [ssq2])
                    for n in range(2):
                        sl = slice(n * 512, (n + 1) * 512)
                        k.STT("dve", x1[:, sl], P[1 + n][:, :], ssq2[:, 3:4], G1[:, sl], ALU.mult, ALU.mult,
                              r=[P[1 + n], ssq2, modrep], w=[x1])
                    k.TT("pool", x1[:, :], x1[:, :], xt2[i][:, :], ALU.add, r=[x1, xt2[i]], w=[x1])
                    k.DMA("sp", x1buf_d[rows, :], x1[:, :], r=[x1], w=[x1buf_b])
                    k.MEMSET("pool", ssq2[:, 0:1], 0.0, w=[ssq2])
                    k.ACT(junk4[:, :], x1[:, :], AF.Square, r=[x1], w=[junk4, ssq2], accum=ssq2[:, 0:1])
                    rstd_from(ssq2[:, 0:1], ssq2[:, 1:2], D, [ssq2])
                    k.STT("dve", h2f[:, :], x1[:, :], ssq2[:, 1:2], A2, ALU.mult, ALU.mult, r=[x1, ssq2, modrep], w=[h2f])
                    k.TT("dve", h2b[:, 0:D], h2f[:, :], B2, ALU.add, r=[h2f, modrep], w=[h2b])
                    k.CP("dve", h2b[:, 1026:1028].bitcast(I32), pidx[:, NH + t:NH + t + 1], r=[pidx], w=[h2b])
                    k.DMA("sp", h2buf_d[rows, :], h2b[:, :], r=[h2b], w=[h2buf_b])
                    for kc in range(8):
                        k.TR(P0b[:, kc * 128:(kc + 1) * 128], h2b[:, kc * 128:(kc + 1) * 128], identb,
                             r=[h2b, cstb], w=[P[0]])
                    k.CP("act", h2T[:, :, :], P0b[:, :].rearrange("p (k l) -> p k l", k=8), r=[P[0]], w=[h2T])
                    for kc in range(8):
                        k.MM(P[6][:, 0:NE], h2T[:, kc, :], wr[:, kc, :], kc == 0, kc == 7, r=[h2T, wr], w=[P[6]])
                    S.op("dve", lambda e: e.tensor_reduce(out=smx[:, 0:1], in_=P[6][:, 0:NE], axis=AX.X, op=ALU.max),
                         r=[P[6]], w=[smx])
                    k.TS("dve", smx[:, 1:2], smx[:, 0:1], -1.0, None, ALU.mult, None, r=[smx], w=[smx])
                    k.MEMSET("pool", smx[:, 2:3], 0.0, w=[smx])
                    k.ACT(ex[:, :], P[6][:, 0:NE], AF.Exp, r=[P[6], smx], w=[ex, smx], bias=smx[:, 1:2],
                          accum=smx[:, 2:3])
                    S.op("dve", lambda e: e.reciprocal(out=smx[:, 3:4], in_=smx[:, 2:3]), r=[smx], w=[smx])
                    k.TS("dve", affall[:, t, :], ex[:, :], smx[:, 3:4], None, ALU.mult, None, r=[ex, smx], w=[affall])
                S.barrier()
            if stage < 5:
                with contextlib.ExitStack() as esd:
                    tf = S.buf("dbg_f", [128, D], F32, es=esd)
                    for t in range(nt4):
                        rows = slice(t * 128, (t + 1) * 128)
                        k.DMA("sp", tf[:, :], x1buf_d[rows, :], r=[x1buf_b], w=[tf])
                        k.DMA("sp", dbg_d[rows, :], tf[:, :], r=[tf], w=[])
                    k.DMA("sp", dbg_d[HALF:HALF + 128, 0:NH * NE], affall[:, :, :].rearrange("p t e -> p (t e)"),
                          r=[affall], w=[])
        if stage >= 5:
            k.DMA("sp", affs_t.ap().rearrange("(t p) e -> p t e", p=128), affall[:, :, :], r=[affall], w=[affs_b])
            if SIM:
                for hh in range(2):
                    k.DMA("sp", affg_t.ap()[hh * HALF:(hh + 1) * HALF, :], affs_t.ap(), r=[affs_b], w=[affg_b])
            else:
                S.coll(lambda e: e.collective_compute("AllGather", ALU.bypass, replica_groups=PAIRS,
                                                      ins=[affs_t.ap().opt()], outs=[affg_t.ap().opt()]),
                       r=[affs_b], w=[affg_b])
            tau = S.buf("tau", [128, NE], F32)
            gate = S.buf("gate", [128, NH, NE], F32)
            sloti = S.buf("sloti", [128, NH, NE], I32)
            with contextlib.ExitStack() as es6:
                def sb(name, shape, dtype):
                    return S.buf(name, shape, dtype, es=es6)
                affb = sb("affb", [128, NT, NE], F32)
                k.DMA("sp", affb[:, :, :], affg_t.ap().rearrange("(t p) e -> p t e", p=128), r=[affg_b], w=[affb])
                cmpb = sb("cmpb", [128, NT, NE], F32)
                hi = sb("hi", [128, NE], F32)
                d2 = sb("d2", [128, NE], F32)
                mid = sb("mid", [128, NE], F32)
                cntp = sb("cntp", [128, NE], F32)
                ge = sb("ge", [128, NE], F32)
                k.MEMSET("pool", tau[:, :], 0.0, w=[tau])
                k.MEMSET("pool", hi[:, :], 1.0001, w=[hi])
                ones_f = cst[:, 7, :]
                for it in range(32):
                    k.TT("dve", d2[:, :], hi[:, :], tau[:, :], ALU.subtract, r=[hi, tau], w=[d2])
                    k.STT("dve", mid[:, :], d2[:, :], 0.5, tau[:, :], ALU.mult, ALU.add, r=[d2, tau], w=[mid])
                    k.TT("dve", cmpb[:, :, :], affb[:, :, :], mid[:, :].unsqueeze(1).to_broadcast([128, NT, NE]),
                         ALU.is_ge, r=[affb, mid], w=[cmpb])
                    S.op("dve", lambda e: e.tensor_reduce(out=cntp[:, :], in_=cmpb[:, :, :].rearrange("p t e -> p e t"),
                                                          axis=AX.X, op=ALU.add), r=[cmpb], w=[cntp])
                    k.MM(P[6][:, 0:NE], ones_f, cntp[:, :], True, True, r=[cst, cntp], w=[P[6]])
                    k.TS("dve", ge[:, :], P[6][:, 0:NE], float(CAP), None, ALU.is_ge, None, r=[P[6]], w=[ge])
                    k.TT("dve", ge[:, :], ge[:, :], d2[:, :], ALU.mult, r=[ge, d2], w=[ge])
                    k.STT("dve", tau[:, :], ge[:, :], 0.5, tau[:, :], ALU.mult, ALU.add, r=[ge, tau], w=[tau])
                    k.STT("dve", hi[:, :], d2[:, :], -0.5, hi[:, :], ALU.mult, ALU.add, r=[d2, hi], w=[hi])
                    k.STT("dve", hi[:, :], ge[:, :], 0.5, hi[:, :], ALU.mult, ALU.add, r=[ge, hi], w=[hi])
                maskf = sb("maskf", [128, NH, NE], F32)
                maskb = sb("maskb", [128, NH * NE], BF16)
                totE = sb("totE", [128, NE, NH], F32)
                cumE = sb("cumE", [128, NE, NH], F32)
                ones32 = sb("ones32", [128, NH], F32)
                posf = sb("posf", [128, NH, NE], F32)
                k.TT("dve", maskf[:, :, :], affall[:, :, :], tau[:, :].unsqueeze(1).to_broadcast([128, NH, NE]),
                     ALU.is_ge, r=[affall, tau], w=[maskf])
                k.TT("dve", gate[:, :, :], maskf[:, :, :], affall[:, :, :], ALU.mult, r=[maskf, affall], w=[gate])
                k.CP("dve", maskb[:, :], maskf[:, :, :].rearrange("p t e -> p (t e)"), r=[maskf], w=[maskb])
                k.MM(P[1][:, :], cstb[:, 8, :], maskb[:, :], True, True, r=[cstb, maskb], w=[P[1]])
                k.MM(P[2][:, :], onesb, maskb[:, :], True, True, r=[cstb, maskb], w=[P[2]])
                k.CP("dve", totE[:, :, :], P[2][:, :].rearrange("p (t e) -> p e t", e=NE), r=[P[2]], w=[totE])
                k.MEMSET("pool", ones32[:, :], 1.0, w=[ones32])
                for e_ in range(NE):
                    S.op("dve", (lambda e_: lambda eng: eng.tensor_tensor_scan(
                        out=cumE[:, e_, :], data0=ones32[:, :], data1=totE[:, e_, :], initial=0.0,
                        op0=ALU.mult, op1=ALU.add))(e_), r=[ones32, totE], w=[cumE])
                k.TT("dve", cumE[:, :, :], cumE[:, :, :], totE[:, :, :], ALU.subtract, r=[cumE, totE], w=[cumE])
                k.TT("dve", posf[:, :, :], P[1][:, :].rearrange("p (t e) -> p t e", e=NE),
                     cumE[:, :, :].rearrange("p e t -> p t e"), ALU.add, r=[P[1], cumE], w=[posf])
                ltm = sb("ltm", [128, NH, NE], F32)
                k.TS("dve", ltm[:, :, :], posf[:, :, :], float(CAP), None, ALU.is_lt, None, r=[posf], w=[ltm])
                k.TT("dve", maskf[:, :, :], maskf[:, :, :], ltm[:, :, :], ALU.mult, r=[maskf, ltm], w=[maskf])
                k.TT("dve", posf[:, :, :], posf[:, :, :], cst[:, 9, 2:2 + NE].unsqueeze(1).to_broadcast([128, NH, NE]),
                     ALU.add, r=[posf, cst], w=[posf])
                k.TT("dve", posf[:, :, :], posf[:, :, :], maskf[:, :, :], ALU.mult, r=[posf, maskf], w=[posf])
                k.TS("dve", maskf[:, :, :], maskf[:, :, :], -1.0, 1.0, ALU.mult, ALU.add, r=[maskf], w=[maskf])
                k.TS("dve", maskf[:, :, :], maskf[:, :, :], cst[:, 9, 18:19], None, ALU.mult, None, r=[maskf, cst], w=[maskf])
                k.TT("dve", posf[:, :, :], posf[:, :, :], maskf[:, :, :], ALU.add, r=[posf, maskf], w=[posf])
                k.CP("dve", sloti[:, :, :], posf[:, :, :], r=[posf], w=[sloti])
                S.barrier()
            if stage < 7:
                with contextlib.ExitStack() as esd:
                    tf = S.buf("dbg_g", [128, NH * NE], F32, es=esd)
                    k.DMA("sp", dbg_d[HALF + 128:HALF + 256, 0:NE], tau[:, :], r=[tau], w=[])
                    k.DMA("sp", dbg_d[HALF + 256:HALF + 384, 0:NH * NE], gate[:, :, :].rearrange("p t e -> p (t e)"),
                          r=[gate], w=[])
                    k.CP("dve", tf[:, :], sloti[:, :, :].rearrange("p t e -> p (t e)"), r=[sloti], w=[tf])
                    k.DMA("sp", dbg_d[HALF + 384:HALF + 512, 0:NH * NE], tf[:, :], r=[tf], w=[])
        if stage >= 7:
            with contextlib.ExitStack() as es7:
                def sb(name, shape, dtype):
                    return S.buf(name, shape, dtype)
                wgb = sb("wgb", [128, 8, D], BF16)
                wub = sb("wub", [128, 8, D], BF16)
                wdb = sb("wdb", [128, 8, D], BF16)
                hd = [sb(f"hd{i}", [128, ROWW], BF16) for i in range(6)]
                xs_in = [sb(f"xs_in{i}", [128, ROWW], BF16) for i in range(2)]
                xinT = sb("xinT", [128, 8, CAP], BF16)
                hid = sb("hid", [128, 8, CAP], BF16)
                gall = sb("gall", [128, 8, 8], F32)
                iall = sb("iall", [128, 8, 8], I32)
                sg = [sb(f"sg{i}", [128, 512], F32) for i in range(2)]
                yt = [sb(f"yt{i}", [128, D], F32) for i in range(2)]

                def load_w(e_):
                    for (wb, wd_) in ((wgb, wg_d), (wub, wu_d), (wdb, wd_d)):
                        k.DMA("pool", wb[:, :, :], wd_[e_].rearrange("(k p) n -> p k n", p=128), r=[], w=[wb])

                def dispatch(e_):
                    for t in range(NH):
                        hb_ = hd[(e_ * NH + t) % 6]
                        k.DMA("sp", hb_[:, :], h2buf_d[t * 128:(t + 1) * 128, :], r=[h2buf_b], w=[hb_])
                        k.CP("dve", hb_[:, 1024:1026].bitcast(F32), gate[:, t, e_:e_ + 1], r=[gate], w=[hb_])
                        S.dma("pool", (lambda hb_, t, e_: lambda eng: eng.indirect_dma_start(
                            out=xin_flat, out_offset=bass.IndirectOffsetOnAxis(ap=sloti[:, t, e_:e_ + 1], axis=0),
                            in_=hb_[:, :], in_offset=None))(hb_, t, e_),
                            r=[hb_, sloti, xin_e[e_]], w=[])

                nexp = NE if stage >= 8 else 2
                load_w(0)
                dispatch(0)
                pbi = 0
                ybi = 0
                for e_ in range(nexp):
                    for sc in range(8):
                        xb_ = xs_in[sc % 2]
                        if sc == 0:
                            k.DMA("sp", xb_[:, :], xin_d[e_, sc * 128:(sc + 1) * 128, :], r=[], w=[xb_, xin_e[e_]])
                        else:
                            k.DMA("sp", xb_[:, :], xin_d[e_, sc * 128:(sc + 1) * 128, :], r=[xin_e[e_]], w=[xb_])
                        k.CP("dve", gall[:, sc, 0:1], xb_[:, 1024:1026].bitcast(F32), r=[xb_], w=[gall])
                        k.CP("dve", iall[:, sc, 0:1], xb_[:, 1026:1028].bitcast(I32), r=[xb_], w=[iall])
                        for kc in range(8):
                            k.TR(P0b[:, kc * 128:(kc + 1) * 128], xb_[:, kc * 128:(kc + 1) * 128], identb,
                                 r=[xb_, cstb], w=[P[0]])
                        k.CP("act", xinT[:, :, sc * 128:(sc + 1) * 128], P0b[:, :].rearrange("p (k l) -> p k l", k=8),
                             r=[P[0]], w=[xinT])
                    if e_ + 1 < nexp:
                        dispatch(e_ + 1)
                    for fc in range(8):
                        for half in range(2):
                            pg, pu = P[1 + 2 * (pbi % 3)], P[2 + 2 * (pbi % 3)]
                            pbi += 1
                            for kc in range(8):
                                k.MM(pg[:, :], wgb[:, kc, fc * 128:(fc + 1) * 128], xinT[:, kc, half * 512:(half + 1) * 512],
                                     kc == 0, kc == 7, r=[wgb, xinT], w=[pg])
                            for kc in range(8):
                                k.MM(pu[:, :], wub[:, kc, fc * 128:(fc + 1) * 128], xinT[:, kc, half * 512:(half + 1) * 512],
                                     kc == 0, kc == 7, r=[wub, xinT], w=[pu])
                            sgb = sg[(fc * 2 + half) % 2]
                            k.ACT(sgb[:, :], pg[:, :], AF.Silu, r=[pg], w=[sgb])
                            k.TT("dve", hid[:, fc, half * 512:(half + 1) * 512], sgb[:, :], pu[:, :], ALU.mult,
                                 r=[sgb, pu], w=[hid])
                    if e_ + 1 < nexp:
                        for (wb, wd_) in ((wgb, wg_d), (wub, wu_d)):
                            k.DMA("pool", wb[:, :, :], wd_[e_ + 1].rearrange("(k p) n -> p k n", p=128), r=[], w=[wb])
                    for sc in range(8):
                        ytb = yt[sc % 2]
                        for n in range(2):
                            py = P[7] if (ybi % 2 == 0) else P[0]
                            ybi += 1
                            for fc in range(8):
                                k.MM(py[:, :], hid[:, fc, sc * 128:(sc + 1) * 128], wdb[:, fc, n * 512:(n + 1) * 512],
                                     fc == 0, fc == 7, r=[hid, wdb], w=[py])
                            k.ACT(ytb[:, n * 512:(n + 1) * 512], py[:, :], AF.Identity, r=[py, gall], w=[ytb],
                                  scale=gall[:, sc, 0:1])
                        S.dma("pool", (lambda ytb, sc: lambda eng: eng.indirect_dma_start(
                            out=acc_d, out_offset=bass.IndirectOffsetOnAxis(ap=iall[:, sc, 0:1], axis=0),
                            in_=ytb[:, :], in_offset=None, compute_op=ALU.add))(ytb, sc),
                            r=[ytb, iall], w=[acc_b])
                    if e_ + 1 < nexp:
                        k.DMA("pool", wdb[:, :, :], wd_d[e_ + 1].rearrange("(k p) n -> p k n", p=128), r=[], w=[wdb])
                S.barrier()
            with contextlib.ExitStack() as es8:
                def sb(name, shape, dtype):
                    return S.buf(name, shape, dtype)
                at = [sb(f"at{i}", [128, D], F32) for i in range(2)]
                x1t = [sb(f"x1t{i}", [128, D], F32) for i in range(2)]
                ot = [sb(f"ot{i}", [128, D], F32) for i in range(2)]
                junk8 = sb("junk8", [128, D], BF16)
                s8 = sb("s8", [128, 8, 8], F32)
                for t in range(NH):
                    i = t % 2
                    rows = slice(t * 128, (t + 1) * 128)
                    k.DMA("sp", at[i][:, :], acc_d[rows, :], r=[acc_b], w=[at[i]])
                    k.DMA("sp", x1t[i][:, :], x1buf_d[rows, :], r=[x1buf_b], w=[x1t[i]])
                    k.MEMSET("pool", s8[:, 0, 0:1], 0.0, w=[s8])
                    k.ACT(junk8[:, :], at[i][:, :], AF.Square, r=[at[i]], w=[junk8, s8], accum=s8[:, 0, 0:1])
                    k.ACT(s8[:, 1, 0:1], s8[:, 0, 0:1], AF.Ln, r=[s8], w=[s8], bias=EPS, scale=1.0 / D)
                    k.ACT(s8[:, 2, 0:1], s8[:, 1, 0:1], AF.Exp, r=[s8], w=[s8], scale=-0.5)
                    k.STT("dve", ot[i][:, :], at[i][:, :], s8[:, 2, 0:1], G2, ALU.mult, ALU.mult,
                          r=[at[i], s8, modrep], w=[ot[i]])
                    k.TT("pool", ot[i][:, :], ot[i][:, :], x1t[i][:, :], ALU.add, r=[ot[i], x1t[i]], w=[ot[i]])
                    k.DMA("sp", out_d[rows, :], ot[i][:, :], r=[ot[i]], w=[])
        S.wait_all_dma("sp")
        S.emit()
    return nc


def make_consts():
    c = np.zeros((128, NCONST, 128), np.float32)
    i = np.arange(128)
    r, cc = i[:, None], i[None, :]
    same = (r // 64) == (cc // 64)
    c[:, 0] = (r == cc)
    c[:, 1] = (r <= cc)
    c[:, 2] = np.where(r > cc, -30000.0, 0.0)
    c[:, 3] = (r <= cc)
    c[:, 4] = same & (r <= cc)
    c[:, 5] = same & (r > cc)
    c[:, 6] = same & (r <= cc)
    c[:, 7] = 1.0
    c[:, 8] = (r < cc)
    c[:, 9, 0] = (i < 64)
    c[:, 9, 1] = (i >= 64)
    c[:, 9, 2:2 + NE] = np.arange(NE)[None, :] * CAP
    c[:, 9, 18] = NE * CAP + i
    return c


def rep(v, n=128):
    return np.ascontiguousarray(np.broadcast_to(np.asarray(v, np.float32)[None], (n,) + tuple(np.shape(v))))


def fm(v):
    return np.ascontiguousarray(np.asarray(v, np.float32).reshape(-1, 128).T)


def prep_inputs(inp):
    x, c, ctx, c_ctx = inp["x"], inp["c"], inp["ctx"], inp["c_ctx"]
    w_in = inp["w_in"][0]
    consts = make_consts()
    shared = {
        "ada_w": np.ascontiguousarray(inp["ada_w"][0]),
        "ada_bT": np.ascontiguousarray(inp["ada_b"][0][:2048].reshape(16, 128).T),
        "ada_brep": rep(inp["ada_b"][0][2048:]),
        "nw0T": fm(inp["norm_w"][0, 0]),
        "nwrep": rep(inp["norm_w"][0, 1:4]),
        "cb": fm(inp["ssd_conv_b"][0]),
        "snw": rep(inp["ssd_norm_w"][0]),
        "hnw": rep(inp["hgrn_norm_w"][0]),
        "w_out": np.ascontiguousarray(inp["w_out"][0]),
        "w_router": np.ascontiguousarray(inp["w_router"][0]),
        "w_gate": np.ascontiguousarray(inp["w_gate"][0]),
        "w_up": np.ascontiguousarray(inp["w_up"][0]),
        "w_down": np.ascontiguousarray(inp["w_down"][0]),
        "consts": consts,
    }
    maps = []
    for core in range(8):
        b, d = core // 2, core % 2
        m = dict(shared)
        m["xs"] = np.ascontiguousarray(x[b][::-1] if d else x[b])
        m["ctxs"] = np.ascontiguousarray(ctx[b][::-1] if d else ctx[b])
        cv = np.stack([fm(c[b]), fm(c_ctx)], axis=-1)
        m["cvec"] = np.ascontiguousarray(cv)
        cols = [w_in[:, 512:1536], w_in[:, 1552:2064], w_in[:, 2064 + 512 * d:2576 + 512 * d], w_in[:, 3088:3600],
                w_in[:, 1536 + 8 * d:1544 + 8 * d], w_in[:, 0:512], w_in[:, 3600:4112]]
        m["wmain"] = np.ascontiguousarray(np.concatenate(cols, axis=1))
        cwk = inp["ssd_conv_w"][0]
        if d:
            cwk = cwk[::-1]
        m["cw"] = np.ascontiguousarray(cwk.T.reshape(8, 128, 5).transpose(1, 0, 2))
        m["sp8"] = rep(np.stack([inp["ssd_dt_bias"][0, d], inp["ssd_a_log"][0, d], inp["ssd_d"][0]]))
        m["lbrep"] = rep(np.stack([inp["hgrn_lb"][0, d], inp["hgrn_lb"][1, d]]))
        p = np.arange(128)[:, None]
        t = np.arange(NH)[None, :]
        m["pidx"] = np.ascontiguousarray(np.concatenate(
            [(1 - d) * 1024 + ((HALF - 1) - (t * 128 + p)) % 1024, t * 128 + p], axis=1).astype(np.int32))
        maps.append(m)
    return maps


STAGE = int(os.environ.get("KSTAGE", "9"))
LITE = int(os.environ.get("KLITE", "0"))
SIM = int(os.environ.get("KSIM", "0"))
SAME_ENGINE_SYNC = bool(int(os.environ.get("KSES", "1")))
SUBCUT = os.environ.get("KSUB", "")
_CACHE = {}


def kernel(**inputs):
    inp = {k_: np.asarray(v) for k_, v in inputs.items()}
    maps = prep_inputs(inp)
    if LITE:
        for m in maps:
            m["ada_w"] = m["ada_w"][:8]
            m["xs"] = m["xs"][:1024]
    if STAGE < 7:
        for m in maps:
            for kk_ in ("w_gate", "w_up", "w_down"):
                m.pop(kk_)
    if STAGE not in _CACHE:
        _CACHE[STAGE] = build_program(STAGE)
    nc = _CACHE[STAGE]
    res = run_bass_kernel_spmd(nc, maps, core_ids=list(range(8)))
    if STAGE < 9:
        return [r["dbg"] for r in res.results]
    out = np.empty((4, SEQ, D), np.float32)
    for core in range(8):
        b, d = core // 2, core % 2
        o = res.results[core]["out"]
        if d:
            out[b, HALF:] = o[::-1]
        else:
            out[b, :HALF] = o
    return out
```

```python
import contextlib
import os
import numpy as np
import concourse.bass as bass
import concourse.mybir as mybir
from concourse.bass_utils import run_bass_kernel_spmd

F32 = mybir.dt.float32
BF16 = mybir.dt.bfloat16
I32 = mybir.dt.int32
AF = mybir.ActivationFunctionType
ALU = mybir.AluOpType
AX = mybir.AxisListType

D = 1024
SEQ = 8192
CTX = 256
NT = SEQ // 128
NH = NT // 2
HALF = SEQ // 2
NE = 16
CAP = 1024
EPS = 1e-6
WCOLS = 3592
C_XBC, C_Q, C_F, C_I, C_DT, C_Z, C_G = 0, 1024, 1536, 2048, 2560, 2568, 3080
NCONST = 10
ROWW = 1028
TRASH = HALF


class Buf:
    __slots__ = ("name", "t", "last_w", "readers")

    def __init__(self, name, t=None):
        self.name = name
        self.t = t
        self.last_w = None
        self.readers = []

    def __getitem__(self, k):
        return self.t[k]


class Sched:
    COMPUTE = ("pe", "dve", "act", "pool")
    NDSEM = 6

    def __init__(self, nc, es, same_engine_sync=True):
        self.nc = nc
        self.es = es
        self.same_engine_sync = same_engine_sync
        self.prog = {k: [] for k in ("pe", "dve", "act", "pool", "sp")}
        self.ninst = {k: 0 for k in self.COMPUTE}
        self.waited_idx = {}
        self.milestones = {k: set() for k in self.COMPUTE}
        self.csem = {k: es.enter_context(nc.semaphore("cs_" + k)) for k in self.COMPUTE}
        self.dsem, self.dcnt, self.drot = {}, {}, {}
        for q in ("sp", "act", "pool"):
            self.dsem[q] = [es.enter_context(nc.semaphore(f"ds_{q}{j}")) for j in range(self.NDSEM)]
            self.dcnt[q] = [0] * self.NDSEM
            self.drot[q] = 0
        self.ccsem = es.enter_context(nc.semaphore("cc_sem"))
        self.cccnt = 0

    def buf(self, name, shape, dtype, psum=False, es=None):
        es = es or self.es
        name = "s_" + name
        if psum:
            t = es.enter_context(self.nc.psum_tensor(name, list(shape), dtype))
        else:
            t = es.enter_context(self.nc.sbuf_tensor(name, list(shape), dtype))
        return Buf(name, t)

    def _need(self, eng, tok):
        if tok is None:
            return
        semkey, v = tok
        key = (eng, semkey)
        if semkey[0] == "c":
            src = semkey[1]
            if src == eng and (eng == "pe" or not self.same_engine_sync):
                return
            if self.waited_idx.get(key, -1) >= v:
                return
            self.waited_idx[key] = v
            self.milestones[src].add(v)
            self.prog[eng].append(("cwait", src, v))
        elif semkey[0] == "x":
            if self.waited_idx.get(key, -1) >= v:
                return
            self.waited_idx[key] = v
            self.prog[eng].append(("xwait", None, v))
        else:
            if self.waited_idx.get(key, -1) >= v:
                return
            self.waited_idx[key] = v
            _, q, j = semkey
            self.prog[eng].append(("dwait", (q, j), v))

    def _deps(self, eng, r, w):
        for b in r:
            self._need(eng, b.last_w)
        for b in w:
            self._need(eng, b.last_w)
            for t in b.readers:
                self._need(eng, t)

    def _commit(self, tok, r, w):
        for b in w:
            b.last_w = tok
            b.readers = []
        for b in r:
            if b not in w:
                b.readers.append(tok)

    def op(self, eng, fn, r=(), w=()):
        self._deps(eng, r, w)
        idx = self.ninst[eng]
        self.ninst[eng] += 1
        self.prog[eng].append(("op", fn, idx))
        tok = (("c", eng), idx)
        self._commit(tok, r, w)
        return tok

    def dma(self, q, fn, r=(), w=()):
        self._deps(q, r, w)
        j = self.drot[q]
        self.drot[q] = (j + 1) % self.NDSEM
        semkey = ("d", q, j)
        prev = self.dcnt[q][j]
        if prev > 0:
            self._need(q, (semkey, prev))
        self.dcnt[q][j] += 16
        v = self.dcnt[q][j]
        self.prog[q].append(("dma", fn, (q, j)))
        tok = (semkey, v)
        self._commit(tok, r, w)
        return tok

    def coll(self, fn, r=(), w=()):
        q = "pool"
        self._deps(q, r, w)
        self.cccnt += 1
        v = self.cccnt
        self.prog[q].append(("coll", fn, v))
        tok = (("x", "cc"), v)
        self._commit(tok, r, w)
        return tok

    def barrier(self):
        for eng in ("pe", "dve", "act", "pool", "sp"):
            for x in self.COMPUTE:
                if x != eng and self.ninst[x] > 0:
                    self._need(eng, (("c", x), self.ninst[x] - 1))
            self.wait_all_dma(eng)

    def wait_all_dma(self, eng="sp"):
        for q in ("sp", "act", "pool"):
            for j in range(self.NDSEM):
                if self.dcnt[q][j]:
                    self._need(eng, (("d", q, j), self.dcnt[q][j]))
        if self.cccnt:
            self._need(eng, (("x", "cc"), self.cccnt))

    def emit(self):
        nc = self.nc
        rank = {}
        for e in self.COMPUTE:
            ms = sorted(self.milestones[e])
            rank[e] = {idx: i + 1 for i, idx in enumerate(ms)}
        sched = self

        def run(engname, e):
            for ent in sched.prog[engname]:
                kind = ent[0]
                if kind == "op":
                    ins = ent[1](e)
                    if ent[2] in rank[engname]:
                        ins.then_inc(sched.csem[engname], 1)
                elif kind == "cwait":
                    e.wait_ge(sched.csem[ent[1]], rank[ent[1]][ent[2]])
                elif kind == "dwait":
                    q, j = ent[1]
                    e.wait_ge(sched.dsem[q][j], ent[2])
                elif kind == "xwait":
                    e.wait_ge(sched.ccsem, ent[2])
                elif kind == "coll":
                    ent[1](e).then_inc(sched.ccsem, 1)
                elif kind == "dma":
                    q, j = ent[2]
                    ent[1](e).then_inc(sched.dsem[q][j], 16)

        with nc.Block() as block:
            @block.sync
            def _(e):
                run("sp", e)

            @block.scalar
            def _(e):
                run("act", e)

            @block.vector
            def _(e):
                run("dve", e)

            @block.gpsimd
            def _(e):
                run("pool", e)

            @block.tensor
            def _(e):
                run("pe", e)


class K:
    def __init__(self, nc, es):
        self.nc = nc
        self.S = Sched(nc, es, same_engine_sync=SAME_ENGINE_SYNC)

    def MM(self, out, lhsT, rhs, start, stop, r, w):
        self.S.op("pe", lambda e: e.matmul(out, lhsT=lhsT, rhs=rhs, start=start, stop=stop), r=r, w=w)

    def TR(self, out, in_, ident, r, w):
        self.S.op("pe", lambda e: e.transpose(out=out, in_=in_, identity=ident), r=r, w=w)

    def ACT(self, out, in_, func, r, w, bias=None, scale=None, accum=None):
        kw = {}
        if bias is not None:
            kw["bias"] = bias
        if scale is not None:
            kw["scale"] = scale
        if accum is not None:
            kw["accum_out"] = accum
        self.S.op("act", lambda e: e.activation(out=out, in_=in_, func=func, **kw), r=r, w=w)

    def TT(self, eng, out, in0, in1, op, r, w):
        self.S.op(eng, lambda e: e.tensor_tensor(out=out, in0=in0, in1=in1, op=op), r=r, w=w)

    def TS(self, eng, out, in0, s1, s2, op0, op1, r, w, accum=None):
        if op1 is None:
            self.S.op(eng, lambda e: e.tensor_scalar(out=out, in0=in0, scalar1=s1, scalar2=None, op0=op0), r=r, w=w)
        elif accum is not None:
            self.S.op(eng, lambda e: e.tensor_scalar(out=out, in0=in0, scalar1=s1, scalar2=s2, op0=op0, op1=op1,
                                                     accum_out=accum), r=r, w=w)
        else:
            self.S.op(eng, lambda e: e.tensor_scalar(out=out, in0=in0, scalar1=s1, scalar2=s2, op0=op0, op1=op1),
                      r=r, w=w)

    def STT(self, eng, out, in0, scalar, in1, op0, op1, r, w):
        self.S.op(eng, lambda e: e.scalar_tensor_tensor(out=out, in0=in0, scalar=scalar, in1=in1, op0=op0, op1=op1),
                  r=r, w=w)

    def CP(self, eng, out, in_, r, w):
        if eng == "act":
            self.S.op("act", lambda e: e.copy(out=out, in_=in_), r=r, w=w)
        else:
            self.S.op(eng, lambda e: e.tensor_copy(out=out, in_=in_), r=r, w=w)

    def MEMSET(self, eng, ap, val, w):
        self.S.op(eng, lambda e: e.memset(ap, val), w=w)

    def DMA(self, q, out, in_, r, w):
        return self.S.dma(q, lambda e: e.dma_start(out=out, in_=in_), r=r, w=w)


def build_program(stage):
    nc = bass.Bass("TRN2", target_bir_lowering=False)
    dt_in = lambda name, shape, dt=F32: nc.dram_tensor(name, list(shape), dt, kind="ExternalInput").ap()
    x_d = dt_in("xs", [1024 if LITE else SEQ, D])
    ctx_d = dt_in("ctxs", [CTX, D])
    cvec_d = dt_in("cvec", [128, 8, 2])
    adaw_d = dt_in("ada_w", [8 if LITE else D, 6 * D])
    adabT_d = dt_in("ada_bT", [128, 16])
    adabrep_d = dt_in("ada_brep", [128, 4 * D])
    nw0T_d = dt_in("nw0T", [128, 8])
    nwrep_d = dt_in("nwrep", [128, 3, D])
    wmain_d = dt_in("wmain", [D, WCOLS])
    cw_d = dt_in("cw", [128, 8, 5])
    cb_d = dt_in("cb", [128, 8])
    sp8_d = dt_in("sp8", [128, 3, 8])
    lbrep_d = dt_in("lbrep", [128, 2, 512])
    snw_d = dt_in("snw", [128, 512])
    hnw_d = dt_in("hnw", [128, 512])
    wout_d = dt_in("w_out", [D, D])
    wr_d = dt_in("w_router", [D, NE])
    if stage >= 7:
        wg_d = dt_in("w_gate", [NE, D, D])
        wu_d = dt_in("w_up", [NE, D, D])
        wd_d = dt_in("w_down", [NE, D, D])
    consts_d = dt_in("consts", [128, NCONST, 128])
    pidx_d = dt_in("pidx", [128, 2 * NH], I32)
    out_d = nc.dram_tensor("out", [HALF, D], F32, kind="ExternalOutput").ap()
    dbg_d = None
    if stage < 9:
        dbg_d = nc.dram_tensor("dbg", [SEQ, D], F32, kind="ExternalOutput").ap()
    ybuf_d = nc.dram_tensor("ybuf", [HALF, D], BF16).ap()
    ysend_t = [nc.dram_tensor(f"ysend{c}", [1024, D], BF16) for c in range(4)]
    ygath_t = [nc.dram_tensor(f"ygath{c}", [2048, D], BF16) for c in range(4)]
    zgbuf_d = nc.dram_tensor("zgbuf", [HALF, D], BF16).ap()
    x1buf_d = nc.dram_tensor("x1buf", [HALF, D], F32).ap()
    h2buf_d = nc.dram_tensor("h2buf", [HALF, ROWW], BF16).ap()
    affs_t = nc.dram_tensor("affsend", [HALF, NE], F32)
    affg_t = nc.dram_tensor("affgath", [SEQ, NE], F32)
    xin_flat = nc.dram_tensor("xin", [NE * CAP + 128, ROWW], BF16).ap()
    xin_d = xin_flat[0:NE * CAP, :].rearrange("(e c) r -> e c r", e=NE)
    acc_d = nc.dram_tensor("moeacc", [HALF + 128, D], F32).ap()
    ybuf_b, ysend_b, ygath_b, zgbuf_b, x1buf_b, h2buf_b = (Buf(n) for n in ("ybuf", "ysend", "ygath", "zgbuf", "x1buf", "h2buf"))
    affs_b, affg_b, xin_b, acc_b = (Buf(n) for n in ("affs", "affg", "xin", "acc"))
    PAIRS = [[0, 1], [2, 3], [4, 5], [6, 7]]

    with contextlib.ExitStack() as es:
        k = K(nc, es)
        S = k.S
        P = [S.buf(f"P{i}", [128, 512], F32, psum=True) for i in range(8)]
        P0b = P[0].t[:, :].bitcast(BF16)

        cst = S.buf("cst", [128, NCONST, 128], F32)
        cstb = S.buf("cstb", [128, NCONST, 128], BF16)
        k.DMA("sp", cst[:, :, :], consts_d, r=[], w=[cst])
        k.CP("dve", cstb[:, :, :], cst[:, :, :], r=[cst], w=[cstb])
        ident, identb = cst[:, 0, :], cstb[:, 0, :]
        trib, negmb = cstb[:, 1, :], cstb[:, 2, :]
        mask01 = cst[:, 3, :]
        tri2i, tri2r, mask2 = cst[:, 4, :], cst[:, 5, :], cst[:, 6, :]
        onesb = cstb[:, 7, :]
        chunkind = cst[:, 9, 0:2]


        cvec = S.buf("cvec", [128, 8, 2], F32)
        k.DMA("sp", cvec[:, :, :], cvec_d, r=[], w=[cvec])
        adabT = S.buf("adabT", [128, 16], F32)
        k.DMA("sp", adabT[:, :], adabT_d, r=[], w=[adabT])
        nw0T = S.buf("nw0T", [128, 8], F32)
        k.DMA("sp", nw0T[:, :], nw0T_d, r=[], w=[nw0T])
        cw = S.buf("cw", [128, 8, 5], F32)
        k.DMA("sp", cw[:, :, :], cw_d, r=[], w=[cw])
        cb = S.buf("cb", [128, 8], F32)
        k.DMA("sp", cb[:, :], cb_d, r=[], w=[cb])
        sp8 = S.buf("sp8", [128, 3, 8], F32)
        k.DMA("sp", sp8[:, :, :], sp8_d, r=[], w=[sp8])
        lbrep = S.buf("lbrep", [128, 2, 512], F32)
        k.DMA("sp", lbrep[:, :, :], lbrep_d, r=[], w=[lbrep])

        if stage >= 7:
            initr = S.buf("initr", [128, ROWW], BF16)
            k.MEMSET("pool", initr[:, :], 0.0, w=[initr])
            k.MEMSET("pool", initr[:, 1026:1028].bitcast(I32), TRASH, w=[initr])
            zt = S.buf("zt", [128, D], F32)
            k.MEMSET("pool", zt[:, :], 0.0, w=[zt])
            xin_e = [Buf(f"xin_e{e_}") for e_ in range(NE)]
            for e_ in range(NE):
                for sc in range(8):
                    k.DMA("sp", xin_d[e_, sc * 128:(sc + 1) * 128, :], initr[:, :], r=[initr], w=[xin_e[e_]])
            for t in range(NH + 1):
                k.DMA("sp", acc_d[t * 128:(t + 1) * 128, :], zt[:, :], r=[zt], w=[acc_b])
        sc = S.buf("sc", [128, 8, 2], F32)
        k.ACT(sc[:, :, :], cvec[:, :, :], AF.Silu, r=[cvec], w=[sc])
        modT = S.buf("modT", [128, 16, 2], F32)
        modrep = S.buf("modrep", [128, 4 * D], F32)
        with contextlib.ExitStack() as es0:
            adap = [S.buf(f"adap{i}", [128, 8, 512], F32, es=es0) for i in range(2)]
            adabrep = S.buf("adabrep", [128, 4 * D], F32, es=es0)
            nwrep = S.buf("nwrep", [128, 3, D], F32, es=es0)
            k.DMA("sp", adabrep[:, :], adabrep_d, r=[], w=[adabrep])
            k.DMA("sp", nwrep[:, :, :], nwrep_d, r=[], w=[nwrep])
            if LITE:
                k.MEMSET("pool", modT[:, :, :], 0.1, w=[modT])
                k.MEMSET("pool", modrep[:, :], 0.1, w=[modrep])
            for j in range(0 if LITE else 12):
                ap_ = adap[j % 2]
                k.DMA("sp", ap_[:, :, :], adaw_d[:, j * 512:(j + 1) * 512].rearrange("(k p) n -> p k n", p=128),
                      r=[], w=[ap_])
                if j < 4:
                    for m in range(4):
                        cc = j * 4 + m
                        for kc in range(8):
                            k.MM(P[6][:, 0:2], ap_[:, kc, m * 128:(m + 1) * 128], sc[:, kc, :], kc == 0, kc == 7,
                                 r=[ap_, sc], w=[P[6]])
                        k.TS("dve", modT[:, cc, :], P[6][:, 0:2], adabT[:, cc:cc + 1], None, ALU.add, None,
                             r=[P[6], adabT], w=[modT])
                else:
                    pb = P[j % 2 + 1]
                    for kc in range(8):
                        k.MM(pb[:, :], sc[:, kc, 0:1].to_broadcast([128, 128]), ap_[:, kc, :], kc == 0, kc == 7,
                             r=[ap_, sc], w=[pb])
                    o = (j - 4) * 512
                    k.TT("dve", modrep[:, o:o + 512], pb[:, :], adabrep[:, o:o + 512], ALU.add,
                         r=[pb, adabrep], w=[modrep])
            k.TT("dve", modrep[:, 0:D], modrep[:, 0:D], nwrep[:, 0, :], ALU.mult, r=[modrep, nwrep], w=[modrep])
            k.STT("dve", modrep[:, 2 * D:3 * D], modrep[:, 2 * D:3 * D], 1.0, nwrep[:, 1, :], ALU.add, ALU.mult,
                  r=[modrep, nwrep], w=[modrep])
            k.TT("dve", modrep[:, 3 * D:4 * D], modrep[:, 3 * D:4 * D], nwrep[:, 2, :], ALU.mult,
                 r=[modrep, nwrep], w=[modrep])
            S.barrier()
        G1, B2, A2, G2 = (modrep[:, i * D:(i + 1) * D] for i in range(4))
        A0 = S.buf("A0", [128, 2, 8], F32)
        B0 = S.buf("B0", [128, 2, 8], F32)
        for i in range(2):
            k.STT("dve", A0[:, i, :], modT[:, 8:16, i], 1.0, nw0T[:, :], ALU.add, ALU.mult, r=[modT, nw0T], w=[A0])
            k.CP("dve", B0[:, i, :], modT[:, 0:8, i], r=[modT], w=[B0])
        aneg = S.buf("aneg", [128, 8], F32)
        k.ACT(aneg[:, :], sp8[:, 1, :], AF.Exp, r=[sp8], w=[aneg])
        k.TS("dve", aneg[:, :], aneg[:, :], -1.0, None, ALU.mult, None, r=[aneg], w=[aneg])
        dsk = S.buf("dsk", [128, 8], F32)
        k.TS("dve", dsk[:, :], sp8[:, 2, :], 0.5, None, ALU.mult, None, r=[sp8], w=[dsk])
        Dm = S.buf("Dm", [128, 8, 128], BF16)
        for j in range(8):
            k.TS("dve", Dm[:, j, :], ident, dsk[:, j:j + 1], None, ALU.mult, None, r=[cst, dsk], w=[Dm])
        c01 = S.buf("c01", [128, 2, 512], F32)
        k.TT("dve", c01[:, 0, :], lbrep[:, 0, :], lbrep[:, 1, :], ALU.subtract, r=[lbrep], w=[c01])
        k.ACT(c01[:, 1, :], c01[:, 0, :], AF.Tanh, r=[c01], w=[c01], scale=0.5)
        k.TS("dve", c01[:, 0, :], c01[:, 1, :], 0.25, 0.75, ALU.mult, ALU.add, r=[c01], w=[c01])
        k.TS("dve", c01[:, 1, :], c01[:, 1, :], -0.25, 0.25, ALU.mult, ALU.add, r=[c01], w=[c01])
        ST = S.buf("ST", [128, 512], F32)
        STb = S.buf("STb", [128, 512], BF16)
        SH = S.buf("SH", [128, 512], F32)
        SHb = S.buf("SHb", [128, 512], BF16)
        k.MEMSET("pool", ST[:, :], 0.0, w=[ST])
        k.MEMSET("pool", STb[:, :], 0.0, w=[STb])
        k.MEMSET("pool", SH[:, :], 0.0, w=[SH])
        k.MEMSET("pool", SHb[:, :], 0.0, w=[SHb])

        with contextlib.ExitStack() as es2:
            def sb(name, shape, dtype):
                return S.buf(name, shape, dtype, es=es2)
            wm = sb("wm", [128, 8, WCOLS], BF16)
            for kc in range(8):
                for (c0, c1) in ((0, 1796), (1796, WCOLS)):
                    k.DMA("pool", wm[:, kc, c0:c1], wmain_d[kc * 128:(kc + 1) * 128, c0:c1], r=[], w=[wm])
            xt = [sb(f"xt{i}", [128, D], F32) for i in range(4)]
            junk = sb("junk", [128, D], BF16)
            ssq = sb("ssq", [128, 2], F32)
            xn = sb("xn", [128, D], BF16)
            hT = [sb(f"hT{i}", [128, 8, 256], BF16) for i in range(2)]
            cacc = sb("cacc", [128, 8, 256], F32)
            xcT = sb("xcT", [128, 8, 256], BF16)
            xs_tm = sb("xs_tm", [128, 512], BF16)
            B_tm = sb("B_tm", [128, 256], BF16)
            dts = sb("dts", [128, 8, 8], F32)
            eatot = sb("eatot", [128, 8], F32)
            a_hi = sb("a_hi", [128, 8], BF16)
            a_lo = sb("a_lo", [128, 8], BF16)
            xdt = sb("xdt", [128, 512], BF16)
            xdtd = sb("xdtd", [128, 512], BF16)
            LT = sb("LT", [128, 8, 128], BF16)
            MT = sb("MT", [128, 8, 128], BF16)
            CBm = sb("CBm", [128, 2, 128], BF16)
            yoff = sb("yoff", [128, 512], F32)
            yo = sb("yo", [128, 512], BF16)
            qs = sb("qs", [128, 512], F32)
            vb = sb("vb", [128, 512], BF16)
            vm = sb("vm", [128, 2, 512], BF16)
            ff = sb("ff", [128, 512], F32)
            lf = sb("lf", [128, 512], F32)
            kk = sb("kk", [128, 512], F32)
            et = [sb(f"et{i}", [128, 512], F32) for i in range(2)]
            qdec = sb("qdec", [128, 512], BF16)
            kinv = sb("kinv", [128, 512], BF16)
            kdec = sb("kdec", [128, 512], BF16)
            qdT = sb("qdT", [128, 4, 128], BF16)
            kiT = sb("kiT", [128, 4, 128], BF16)
            attm = sb("attm", [128, 4, 128], BF16)
            oc = sb("oc", [64, 2, 512], BF16)
            ebt = sb("ebt", [128, 4, 2], F32)
            zg = sb("zg", [128, D], BF16)

            def load_x(ti_all):
                b = xt[ti_all % 4]
                src = ctx_d[ti_all * 128:(ti_all + 1) * 128, :] if ti_all < 2 else \
                    x_d[(ti_all - 2) * 128:(ti_all - 1) * 128, :]
                k.DMA("sp", b[:, :], src, r=[], w=[b])

            def norm_T(ti_all, hbuf, col0, which):
                b = xt[ti_all % 4]
                k.MEMSET("pool", ssq[:, 0:1], 0.0, w=[ssq])
                k.ACT(junk[:, :], b[:, :], AF.Square, r=[b], w=[junk, ssq], accum=ssq[:, 0:1])
                k.ACT(ssq[:, 1:2], ssq[:, 0:1], AF.Ln, r=[ssq], w=[ssq], bias=EPS, scale=1.0 / D)
                k.ACT(ssq[:, 1:2], ssq[:, 1:2], AF.Exp, r=[ssq], w=[ssq], scale=-0.5)
                k.ACT(xn[:, :], b[:, :], AF.Identity, r=[b, ssq], w=[xn], scale=ssq[:, 1:2])
                for kc in range(8):
                    k.TR(P0b[:, kc * 128:(kc + 1) * 128], xn[:, kc * 128:(kc + 1) * 128], identb,
                         r=[xn, cstb], w=[P[0]])
                for kc in range(8):
                    o = hbuf[:, kc, col0:col0 + 128]
                    i_ = P0b[:, kc * 128:(kc + 1) * 128]
                    if kc % 2 == 0:
                        k.ACT(o, i_, AF.Identity, r=[P[0], A0, B0], w=[hbuf],
                              bias=B0[:, which, kc:kc + 1], scale=A0[:, which, kc:kc + 1])
                    else:
                        k.TS("dve", o, i_, A0[:, which, kc:kc + 1], B0[:, which, kc:kc + 1], ALU.mult, ALU.add,
                             r=[P[0], A0, B0], w=[hbuf])

            def conv_stage(hbuf, T, roww, chunks):
                per_bank = 512 // T
                for ci, c in enumerate(chunks):
                    pb = P[1 + ci // per_bank]
                    po = (ci % per_bank) * T
                    for kc in range(8):
                        k.MM(pb[:, po:po + T], wm[:, kc, C_XBC + c * 128:C_XBC + (c + 1) * 128], hbuf[:, kc, 0:T],
                             kc == 0, kc == 7, r=[wm, hbuf], w=[pb])
                for ci, c in enumerate(chunks):
                    pb = P[1 + ci // per_bank]
                    po = (ci % per_bank) * T
                    src = pb[:, po:po + T]
                    acc = cacc[:, c, 0:T]
                    k.ACT(acc, src, AF.Identity, r=[pb, cw, cb], w=[cacc], bias=cb[:, c:c + 1], scale=cw[:, c, 2:3])
                    srcv = src.rearrange("p (r w) -> p r w", w=roww)
                    accv = acc.rearrange("p (r w) -> p r w", w=roww)
                    for kt in (0, 1, 3, 4):
                        s = kt - 2
                        if s > 0:
                            o_, i_ = accv[:, :, 0:roww - s], srcv[:, :, s:roww]
                        else:
                            o_, i_ = accv[:, :, -s:roww], srcv[:, :, 0:roww + s]
                        k.STT("dve", o_, i_, cw[:, c, kt:kt + 1], o_, ALU.mult, ALU.add, r=[pb, cw, cacc], w=[cacc])
                    k.ACT(xcT[:, c, 0:T], acc, AF.Silu, r=[cacc], w=[xcT])

            def scan_tile(hbuf, col0, tcol, lat, ti, zgproj):
                hsl = lambda kc: hbuf[:, kc, col0:col0 + 128]
                if zgproj:
                    for (pb, c0) in ((P[1], C_Z), (P[2], C_G)):
                        for kc in range(8):
                            k.MM(pb[:, :], hsl(kc), wm[:, kc, c0:c0 + 512], kc == 0, kc == 7, r=[hbuf, wm], w=[pb])
                    k.ACT(zg[:, 0:512], P[1][:, :], AF.Silu, r=[P[1]], w=[zg])
                    k.ACT(zg[:, 512:1024], P[2][:, :], AF.Silu, r=[P[2]], w=[zg])
                    k.DMA("sp", zgbuf_d[ti * 128:(ti + 1) * 128, :], zg[:, :], r=[zg], w=[zgbuf_b])
                for (pb, c0) in ((P[3], C_Q), (P[4], C_F), (P[5], C_I)):
                    for kc in range(8):
                        k.MM(pb[:, :], hsl(kc), wm[:, kc, c0:c0 + 512], kc == 0, kc == 7, r=[hbuf, wm], w=[pb])
                for kc in range(8):
                    k.MM(P[6][:, 0:8], hsl(kc), wm[:, kc, C_DT:C_DT + 8], kc == 0, kc == 7, r=[hbuf, wm], w=[P[6]])
                if lat:
                    k.ACT(qs[:, :], P[3][:, :], AF.Silu, r=[P[3]], w=[qs])
                k.ACT(ff[:, :], P[4][:, :], AF.Tanh, r=[P[4]], w=[ff], scale=0.5)
                k.CP("dve", vb[:, :], P[5][:, :], r=[P[5]], w=[vb])
                for c in range(2):
                    k.TS("pool", vm[:, c, :], vb[:, :], chunkind[:, c:c + 1], None, ALU.mult, None, r=[vb, cst], w=[vm])
                if SUBCUT == 'A':
                    return
                for c in range(6):
                    k.TR(P0b[:, c * 128:(c + 1) * 128], xcT[:, c, tcol:tcol + 128], identb, r=[xcT, cstb], w=[P[0]])
                if SUBCUT == 'B1':
                    return
                k.CP("act", xs_tm[:, :], P0b[:, 0:512], r=[P[0]], w=[xs_tm])
                if SUBCUT == 'B2':
                    return
                k.CP("act", B_tm[:, :], P0b[:, 512:768], r=[P[0]], w=[B_tm])
                if SUBCUT == 'B':
                    return
                v_, av_, l_, dt_, a_, nacs, eacs, w2 = (dts[:, i, :] for i in range(8))
                k.TT("dve", v_, P[6][:, 0:8], sp8[:, 0, :], ALU.add, r=[P[6], sp8], w=[dts])
                k.TS("dve", av_, v_, 30.0, None, ALU.min, None, r=[dts], w=[dts])
                k.ACT(av_, av_, AF.Exp, r=[dts], w=[dts])
                k.ACT(l_, av_, AF.Ln, r=[dts], w=[dts], bias=1.0)
                k.TT("dve", dt_, l_, v_, ALU.max, r=[dts], w=[dts])
                k.TT("dve", a_, dt_, aneg[:, :], ALU.mult, r=[dts, aneg], w=[dts])
                k.CP("dve", a_hi[:, :], a_, r=[dts], w=[a_hi])
                k.TT("dve", a_lo[:, :], a_, a_hi[:, :], ALU.subtract, r=[dts, a_hi], w=[a_lo])
                if SUBCUT == 'C1':
                    return
                k.MM(P[6][:, 64:72], trib, a_hi[:, :], True, False, r=[cstb, a_hi], w=[P[6]])
                k.MM(P[6][:, 64:72], trib, a_lo[:, :], False, True, r=[cstb, a_lo], w=[P[6]])
                k.MM(P[6][:, 128:136], onesb, a_hi[:, :], True, False, r=[cstb, a_hi], w=[P[6]])
                k.MM(P[6][:, 128:136], onesb, a_lo[:, :], False, True, r=[cstb, a_lo], w=[P[6]])
                k.TS("dve", nacs, P[6][:, 64:72], -1.0, None, ALU.mult, None, r=[P[6]], w=[dts])
                k.ACT(eacs, P[6][:, 64:72], AF.Exp, r=[P[6]], w=[dts])
                k.TT("dve", w2, P[6][:, 128:136], nacs, ALU.add, r=[P[6], dts], w=[dts])
                k.ACT(w2, w2, AF.Exp, r=[dts], w=[dts])
                k.ACT(eatot[:, :], P[6][:, 128:136], AF.Exp, r=[P[6]], w=[eatot])
                k.TT("dve", w2, w2, dt_, ALU.mult, r=[dts], w=[dts])
                if SUBCUT == 'C2':
                    return
                xs3 = xs_tm[:, :].rearrange("p (j q) -> p j q", q=64)
                k.TT("dve", xdtd[:, :].rearrange("p (j q) -> p j q", q=64), xs3, w2.unsqueeze(2).to_broadcast([128, 8, 64]), ALU.mult,
                     r=[xs_tm, dts], w=[xdtd])
                if lat:
                    k.TT("dve", xdt[:, :].rearrange("p (j q) -> p j q", q=64), xs3, dt_.unsqueeze(2).to_broadcast([128, 8, 64]), ALU.mult,
                         r=[xs_tm, dts], w=[xdt])
                    for j in range(8):
                        pa = P[1 + j // 4]
                        o = pa[:, (j % 4) * 128:(j % 4 + 1) * 128]
                        k.MM(o, a_hi[:, j:j + 1].to_broadcast([128, 128]), trib, True, False, r=[a_hi, cstb], w=[pa])
                        k.MM(o, a_lo[:, j:j + 1].to_broadcast([128, 128]), trib, False, False, r=[a_lo, cstb], w=[pa])
                        k.MM(o, identb, negmb, False, True, r=[cstb], w=[pa])
                        k.ACT(LT[:, j, :], o, AF.Exp, r=[pa, dts], w=[LT], bias=nacs[:, j:j + 1])
                    for g in range(2):
                        k.MM(P[6][:, 128 + g * 128:256 + g * 128], xcT[:, 4 + g, tcol:tcol + 128],
                             xcT[:, 6 + g, tcol:tcol + 128], True, True, r=[xcT], w=[P[6]])
                    k.TT("dve", CBm[:, :, :], P[6][:, 128:384].rearrange("p (g l) -> p g l", g=2),
                         mask01.unsqueeze(1).to_broadcast([128, 2, 128]), ALU.mult, r=[P[6], cst], w=[CBm])
                    for g in range(2):
                        k.TT("dve", MT[:, 4 * g:4 * g + 4, :], LT[:, 4 * g:4 * g + 4, :],
                             CBm[:, g:g + 1, :].to_broadcast([128, 4, 128]), ALU.mult, r=[LT, CBm], w=[MT])
                    for j in range(8):
                        o = P[3][:, j * 64:(j + 1) * 64]
                        k.MM(o, MT[:, j, :], xdt[:, j * 64:(j + 1) * 64], True, False, r=[MT, xdt], w=[P[3]])
                        k.MM(o, Dm[:, j, :], xs_tm[:, j * 64:(j + 1) * 64], False, True, r=[Dm, xs_tm], w=[P[3]])
                    for g in range(2):
                        k.MM(P[7][:, g * 256:(g + 1) * 256], xcT[:, 6 + g, tcol:tcol + 128],
                             STb[:, g * 256:(g + 1) * 256], True, True, r=[xcT, STb], w=[P[7]])
                    k.TT("dve", yoff[:, :].rearrange("p (j q) -> p j q", q=64),
                         P[7][:, :].rearrange("p (j q) -> p j q", q=64),
                         eacs.unsqueeze(2).to_broadcast([128, 8, 64]), ALU.mult, r=[P[7], dts], w=[yoff])
                    k.TT("dve", yo[:, :], P[3][:, :], yoff[:, :], ALU.add, r=[P[3], yoff], w=[yo])
                if SUBCUT == 'C':
                    return
                for g in range(2):
                    k.MM(P[7][:, g * 256:(g + 1) * 256], B_tm[:, g * 128:(g + 1) * 128],
                         xdtd[:, g * 256:(g + 1) * 256], True, True, r=[B_tm, xdtd], w=[P[7]])
                k.TT("dve", ST[:, :].rearrange("p (j q) -> p j q", q=64), ST[:, :].rearrange("p (j q) -> p j q", q=64),
                     eatot[:, :].unsqueeze(2).to_broadcast([128, 8, 64]), ALU.mult, r=[ST, eatot], w=[ST])
                k.TT("dve", ST[:, :], ST[:, :], P[7][:, :], ALU.add, r=[ST, P[7]], w=[ST])
                k.CP("act", STb[:, :], ST[:, :], r=[ST], w=[STb])
                if SUBCUT == 'D':
                    return
                k.TT("dve", ff[:, :], ff[:, :], c01[:, 1, :], ALU.mult, r=[ff, c01], w=[ff])
                k.TT("dve", ff[:, :], ff[:, :], c01[:, 0, :], ALU.add, r=[ff, c01], w=[ff])
                k.ACT(lf[:, :], ff[:, :], AF.Ln, r=[ff], w=[lf])
                k.ACT(kk[:, :], ff[:, :], AF.Identity, r=[ff], w=[kk], bias=1.0, scale=-1.0)
                k.MM(P[4][:, :], tri2i, lf[:, :], True, True, r=[cst, lf], w=[P[4]])
                k.MM(P[5][:, :], tri2r, lf[:, :], True, True, r=[cst, lf], w=[P[5]])
                for h in range(4):
                    k.MM(P[7][:, h * 64:h * 64 + 2], lf[:, h * 128:(h + 1) * 128], chunkind, True, True,
                         r=[lf, cst], w=[P[7]])
                k.ACT(ebt[:, :, :], P[7][:, 0:256].rearrange("p (h c) -> p h c", c=64)[:, :, 0:2], AF.Exp,
                      r=[P[7]], w=[ebt])
                k.ACT(et[0][:, :], P[5][:, :], AF.Exp, r=[P[5]], w=[et[0]])
                k.TT("dve", kdec[:, :], kk[:, :], et[0][:, :], ALU.mult, r=[kk, et[0]], w=[kdec])
                if lat:
                    k.ACT(et[1][:, :], P[4][:, :], AF.Exp, r=[P[4]], w=[et[1]])
                    k.TT("dve", qdec[:, :], qs[:, :], et[1][:, :], ALU.mult, r=[qs, et[1]], w=[qdec])
                    k.ACT(et[0][:, :], P[4][:, :], AF.Exp, r=[P[4]], w=[et[0]], scale=-1.0)
                    k.TT("dve", kinv[:, :], kk[:, :], et[0][:, :], ALU.mult, r=[kk, et[0]], w=[kinv])
                    for h in range(4):
                        k.TR(P0b[:, h * 128:(h + 1) * 128], qdec[:, h * 128:(h + 1) * 128], identb,
                             r=[qdec, cstb], w=[P[0]])
                        k.TR(P0b[:, 512 + h * 128:512 + (h + 1) * 128], kinv[:, h * 128:(h + 1) * 128], identb,
                             r=[kinv, cstb], w=[P[0]])
                    k.CP("act", qdT[:, :, :], P0b[:, 0:512].rearrange("p (h l) -> p h l", h=4), r=[P[0]], w=[qdT])
                    k.CP("act", kiT[:, :, :], P0b[:, 512:1024].rearrange("p (h l) -> p h l", h=4), r=[P[0]], w=[kiT])
                    for h in range(4):
                        k.MM(P[4][:, h * 128:(h + 1) * 128], kiT[:, h, :], qdT[:, h, :], True, True,
                             r=[kiT, qdT], w=[P[4]])
                    k.TT("dve", attm[:, :, :], P[4][:, :].rearrange("p (h l) -> p h l", h=4),
                         mask2.unsqueeze(1).to_broadcast([128, 4, 128]), ALU.mult, r=[P[4], cst], w=[attm])
                if SUBCUT == 'E':
                    return
                for c in range(2):
                    if lat:
                        for h in range(4):
                            o = P[5][0:64, h * 128:(h + 1) * 128]
                            k.MM(o, attm[:, h, c * 64:(c + 1) * 64], vb[:, h * 128:(h + 1) * 128], True, False,
                                 r=[attm, vb], w=[P[5]])
                            k.MM(o, qdT[:, h, c * 64:(c + 1) * 64], SHb[:, h * 128:(h + 1) * 128], False, True,
                                 r=[qdT, SHb], w=[P[5]])
                        k.CP("act", oc[:, c, :], P[5][0:64, :], r=[P[5]], w=[oc])
                    for h in range(4):
                        k.MM(P[7][:, h * 128:(h + 1) * 128], kdec[:, h * 128:(h + 1) * 128],
                             vm[:, c, h * 128:(h + 1) * 128], True, True, r=[kdec, vm], w=[P[7]])
                    for h in range(4):
                        sl = slice(h * 128, (h + 1) * 128)
                        k.STT("dve", SH[:, sl], SH[:, sl], ebt[:, h, c:c + 1], P[7][:, sl], ALU.mult, ALU.add,
                              r=[SH, ebt, P[7]], w=[SH])
                    k.CP("act", SHb[:, :], SH[:, :], r=[SH], w=[SHb])
                if lat:
                    if ti < NH:
                        dst, dstb, rows = ybuf_d, ybuf_b, slice(ti * 128, (ti + 1) * 128)
                    else:
                        dst, dstb = ysend_t[(ti - NH) // 8].ap(), ysend_b
                        rows = slice(((ti - NH) % 8) * 128, ((ti - NH) % 8 + 1) * 128)
                    k.DMA("sp", dst[rows, 0:512], yo[:, :], r=[yo], w=[dstb])
                    k.DMA("sp", dst[rows, 512:1024].rearrange("(c p) v -> p c v", p=64), oc[:, :, :],
                          r=[oc], w=[dstb])

            cut = int(os.environ.get("KCUT", "99"))
            load_x(0)
            load_x(1)
            load_x(2)
            if cut >= 1:
                norm_T(0, hT[0], 0, 1)
                norm_T(1, hT[0], 128, 1)
            if cut >= 2:
                conv_stage(hT[0], 256, 256, [0, 1, 2, 3])
                conv_stage(hT[0], 256, 256, [4, 5, 6, 7])
            if cut >= 3:
                for sub in range(2):
                    scan_tile(hT[0], sub * 128, sub * 128, False, -1, False)
            ntl = NT if stage >= 2 else 4
            ntl = int(os.environ.get("KNT", ntl))
            if cut < 4:
                ntl = 0
            if cut >= 4 and ntl > 1:
                load_x(3)
            for j in range(ntl // 2):
                a_, b_ = 2 * j, 2 * j + 1
                for nx in (a_ + 4, b_ + 4):
                    if nx < ntl + 2:
                        load_x(nx)
                hb = hT[(j + 1) % 2]
                norm_T(a_ + 2, hb, 0, 0)
                norm_T(b_ + 2, hb, 128, 0)
                conv_stage(hb, 256, 64, [0, 1, 2, 3])
                conv_stage(hb, 256, 64, [4, 5, 6, 7])
                scan_tile(hb, 0, 0, True, a_, a_ < NH)
                scan_tile(hb, 128, 128, True, b_, b_ < NH)

            S.barrier()
        if stage < 3:
            with contextlib.ExitStack() as esd:
                tb = S.buf("dbg_b", [128, D], BF16, es=esd)
                tf = S.buf("dbg_f", [128, D], F32, es=esd)
                for ti in range(min(ntl, NH)):
                    rows = slice(ti * 128, (ti + 1) * 128)
                    k.DMA("sp", tb[:, :], ybuf_d[rows, :], r=[ybuf_b], w=[tb])
                    k.CP("dve", tf[:, :], tb[:, :], r=[tb], w=[tf])
                    k.DMA("sp", dbg_d[rows, :], tf[:, :], r=[tf], w=[])
        else:
            for c in range(4):
                if SIM:
                    for hh in range(2):
                        k.DMA("sp", ygath_t[c].ap()[hh * 1024:(hh + 1) * 1024, :], ysend_t[c].ap(), r=[ysend_b], w=[ygath_b])
                else:
                    S.coll((lambda c: lambda e: e.collective_compute(
                        "AllGather", ALU.bypass, replica_groups=PAIRS, ins=[ysend_t[c].ap().opt()],
                        outs=[ygath_t[c].ap().opt()]))(c), r=[ysend_b], w=[ygath_b])
            affall = S.buf("affall", [128, NH, NE], F32)
            pidx = S.buf("pidx", [128, 2 * NH], I32)
            k.DMA("sp", pidx[:, :], pidx_d, r=[], w=[pidx])
            with contextlib.ExitStack() as es4:
                def sb(name, shape, dtype):
                    return S.buf(name, shape, dtype, es=es4)
                wout = sb("wout", [128, 8, D], BF16)
                for kc in range(8):
                    k.DMA("pool", wout[:, kc, :], wout_d[kc * 128:(kc + 1) * 128, :], r=[], w=[wout])
                wr = sb("wr", [128, 8, NE], BF16)
                k.DMA("pool", wr[:, :, :], wr_d.rearrange("(k p) e -> p k e", p=128), r=[], w=[wr])
                snw = sb("snw", [128, 512], F32)
                hnw = sb("hnw", [128, 512], F32)
                k.DMA("sp", snw[:, :], snw_d, r=[], w=[snw])
                k.DMA("sp", hnw[:, :], hnw_d, r=[], w=[hnw])
                yown = [sb(f"yown{i}", [128, D], BF16) for i in range(2)]
                ypar = [sb(f"ypar{i}", [128, D], BF16) for i in range(2)]
                zgt = [sb(f"zgt{i}", [128, D], BF16) for i in range(2)]
                xt2 = [sb(f"xt2{i}", [128, D], F32) for i in range(2)]
                ysum = sb("ysum", [128, D], F32)
                junk4 = sb("junk4", [128, D], BF16)
                ssq6 = sb("ssq6", [128, 8], F32)
                rs6 = sb("rs6", [128, 8], F32)
                tmpo = sb("tmpo", [128, 512], F32)
                ylat = sb("ylat", [128, D], BF16)
                ylT = sb("ylT", [128, 8, 128], BF16)
                ssq2 = sb("ssq2", [128, 4], F32)
                x1 = sb("x1", [128, D], F32)
                h2f = sb("h2f", [128, D], F32)
                h2b = sb("h2b", [128, ROWW], BF16)
                h2T = sb("h2T", [128, 8, 128], BF16)
                smx = sb("smx", [128, 4], F32)
                ex = sb("ex", [128, NE], F32)
                k.MEMSET("pool", h2b[:, 1024:ROWW], 0.0, w=[h2b])

                def p4_load(t):
                    i = t % 2
                    rows = slice(t * 128, (t + 1) * 128)
                    k.DMA("sp", yown[i][:, :], ybuf_d[rows, :], r=[ybuf_b], w=[yown[i]])
                    k.DMA("sp", zgt[i][:, :], zgbuf_d[rows, :], r=[zgbuf_b], w=[zgt[i]])
                    k.DMA("sp", xt2[i][:, :], x_d[rows, :], r=[], w=[xt2[i]])
                    S.dma("pool", lambda e: e.indirect_dma_start(
                        out=ypar[i][:, :], out_offset=None, in_=ygath_t[3 - t // 8].ap(),
                        in_offset=bass.IndirectOffsetOnAxis(ap=pidx[:, t:t + 1], axis=0)),
                        r=[ygath_b, pidx], w=[ypar[i]])

                def rstd_from(ssq_ap, out_ap, n, rbufs):
                    k.ACT(out_ap, ssq_ap, AF.Ln, r=rbufs, w=rbufs, bias=EPS, scale=1.0 / n)
                    k.ACT(out_ap, out_ap, AF.Exp, r=rbufs, w=rbufs, scale=-0.5)

                nt4 = NH if stage >= 4 else 2
                nt4 = int(os.environ.get("KNT4", nt4))
                p4_load(0)
                for t in range(nt4):
                    i = t % 2
                    if t + 1 < nt4:
                        p4_load(t + 1)
                    rows = slice(t * 128, (t + 1) * 128)
                    k.TT("dve", ysum[:, :], yown[i][:, :], ypar[i][:, :], ALU.add, r=[yown[i], ypar[i]], w=[ysum])
                    k.TT("pool", ysum[:, 0:512], ysum[:, 0:512], zgt[i][:, 0:512], ALU.mult, r=[ysum, zgt[i]], w=[ysum])
                    k.MEMSET("pool", ssq6[:, :], 0.0, w=[ssq6])
                    for g in range(2):
                        k.ACT(junk4[:, g * 256:(g + 1) * 256], ysum[:, g * 256:(g + 1) * 256], AF.Square,
                              r=[ysum], w=[junk4, ssq6], accum=ssq6[:, g:g + 1])
                    for h in range(4):
                        sl = slice(512 + h * 128, 512 + (h + 1) * 128)
                        k.ACT(junk4[:, sl], ysum[:, sl], AF.Square, r=[ysum], w=[junk4, ssq6],
                              accum=ssq6[:, 2 + h:3 + h])
                    rstd_from(ssq6[:, 0:2], rs6[:, 0:2], 256, [ssq6, rs6])
                    rstd_from(ssq6[:, 2:6], rs6[:, 2:6], 128, [ssq6, rs6])
                    for g in range(2):
                        sl = slice(g * 256, (g + 1) * 256)
                        k.STT("dve", ylat[:, sl], ysum[:, sl], rs6[:, g:g + 1], snw[:, sl], ALU.mult, ALU.mult,
                              r=[ysum, rs6, snw], w=[ylat])
                    for h in range(4):
                        sl = slice(h * 128, (h + 1) * 128)
                        sl2 = slice(512 + h * 128, 512 + (h + 1) * 128)
                        k.STT("dve", tmpo[:, sl], ysum[:, sl2], rs6[:, 2 + h:3 + h], hnw[:, sl], ALU.mult, ALU.mult,
                              r=[ysum, rs6, hnw], w=[tmpo])
                    k.TT("pool", ylat[:, 512:1024], tmpo[:, :], zgt[i][:, 512:1024], ALU.mult,
                         r=[tmpo, zgt[i]], w=[ylat])
                    for kc in range(8):
                        k.TR(P0b[:, kc * 128:(kc + 1) * 128], ylat[:, kc * 128:(kc + 1) * 128], identb,
                             r=[ylat, cstb], w=[P[0]])
                    k.CP("act", ylT[:, 0:4, :], P0b[:, 0:512].rearrange("p (k l) -> p k l", k=4), r=[P[0]], w=[ylT])
                    k.CP("act", ylT[:, 4:8, :], P0b[:, 512:1024].rearrange("p (k l) -> p k l", k=4), r=[P[0]], w=[ylT])
                    for n in range(2):
                        for kc in range(8):
                            k.MM(P[1 + n][:, :], ylT[:, kc, :], wout[:, kc, n * 512:(n + 1) * 512], kc == 0, kc == 7,
                                 r=[ylT, wout], w=[P[1 + n]])
                    k.MEMSET("pool", ssq2[:, :], 0.0, w=[ssq2])
                    for n in range(2):
                        k.ACT(junk4[:, n * 512:(n + 1) * 512], P[1 + n][:, :], AF.Square, r=[P[1 + n]],
                              w=[junk4, ssq2], accum=ssq2[:, n:n + 1])
                    k.TT("dve", ssq2[:, 2:3], ssq2[:, 0:1], ssq2[:, 1:2], ALU.add, r=[ssq2], w=[ssq2])
                    rstd_from(ssq2[:, 2:3], ssq2[:, 3:4], D, [ssq2])
                    for n in range(2):
                        sl = slice(n * 512, (n + 1) * 512)
                        k.STT("dve", x1[:, sl], P[1 + n][:, :], ssq2[:, 3:4], G1[:, sl], ALU.mult, ALU.mult,
                              r=[P[1 + n], ssq2, modrep], w=[x1])
                    k.TT("pool", x1[:, :], x1[:, :], xt2[i][:, :], ALU.add, r=[x1, xt2[i]], w=[x1])
                    k.DMA("sp", x1buf_d[rows, :], x1[:, :], r=[x1], w=[x1buf_b])
                    k.MEMSET("pool", ssq2[:, 0:1], 0.0, w=[ssq2])
                    k.ACT(junk4[:, :], x1[:, :], AF.Square, r=[x1], w=[junk4, ssq2], accum=ssq2[:, 0:1])
                    rstd_from(ssq2[:, 0:1], ssq2[:, 1:2], D, [ssq2])
                    k.STT("dve", h2f[:, :], x1[:, :], ssq2[:, 1:2], A2, ALU.mult, ALU.mult, r=[x1, ssq2, modrep], w=[h2f])
                    k.TT("dve", h2b[:, 0:D], h2f[:, :], B2, ALU.add, r=[h2f, modrep], w=[h2b])
                    k.CP("dve", h2b[:, 1026:1028].bitcast(I32), pidx[:, NH + t:NH + t + 1], r=[pidx], w=[h2b])
                    k.DMA("sp", h2buf_d[rows, :], h2b[:, :], r=[h2b], w=[h2buf_b])
                    for kc in range(8):
                        k.TR(P0b[:, kc * 128:(kc + 1) * 128], h2b[:, kc * 128:(kc + 1) * 128], identb,
                             r=[h2b, cstb], w=[P[0]])
                    k.CP("act", h2T[:, :, :], P0b[:, :].rearrange("p (k l) -> p k l", k=8), r=[P[0]], w=[h2T])
                    for kc in range(8):
                        k.MM(P[6][:, 0:NE], h2T[:, kc, :], wr[:, kc, :], kc == 0, kc == 7, r=[h2T, wr], w=[P[6]])
                    S.op("dve", lambda e: e.tensor_reduce(out=smx[:, 0:1], in_=P[6][:, 0:NE], axis=AX.X, op=ALU.max),
                         r=[P[6]], w=[smx])
                    k.TS("dve", smx[:, 1:2], smx[:, 0:1], -1.0, None, ALU.mult, None, r=[smx], w=[smx])
                    k.MEMSET("pool", smx[:, 2:3], 0.0, w=[smx])
                    k.ACT(ex[:, :], P[6][:, 0:NE], AF.Exp, r=[P[6], smx], w=[ex, smx], bias=smx[:, 1:2],
                          accum=smx[:, 2:3])
                    S.op("dve", lambda e: e.reciprocal(out=smx[:, 3:4], in_=smx[:, 2:3]), r=[smx], w=[smx])
                    k.TS("dve", affall[:, t, :], ex[:, :], smx[:, 3:4], None, ALU.mult, None, r=[ex, smx], w=[affall])
                S.barrier()
            if stage < 5:
                with contextlib.ExitStack() as esd:
                    tf = S.buf("dbg_f", [128, D], F32, es=esd)
                    for t in range(nt4):
                        rows = slice(t * 128, (t + 1) * 128)
                        k.DMA("sp", tf[:, :], x1buf_d[rows, :], r=[x1buf_b], w=[tf])
                        k.DMA("sp", dbg_d[rows, :], tf[:, :], r=[tf], w=[])
                    k.DMA("sp", dbg_d[HALF:HALF + 128, 0:NH * NE], affall[:, :, :].rearrange("p t e -> p (t e)"),
                          r=[affall], w=[])
        if stage >= 5:
            k.DMA("sp", affs_t.ap().rearrange("(t p) e -> p t e", p=128), affall[:, :, :], r=[affall], w=[affs_b])
            if SIM:
                for hh in range(2):
                    k.DMA("sp", affg_t.ap()[hh * HALF:(hh + 1) * HALF, :], affs_t.ap(), r=[affs_b], w=[affg_b])
            else:
                S.coll(lambda e: e.collective_compute("AllGather", ALU.bypass, replica_groups=PAIRS,
                                                      ins=[affs_t.ap().opt()], outs=[affg_t.ap().opt()]),
                       r=[affs_b], w=[affg_b])
            tau = S.buf("tau", [128, NE], F32)
            gate = S.buf("gate", [128, NH, NE], F32)
            sloti = S.buf("sloti", [128, NH, NE], I32)
            with contextlib.ExitStack() as es6:
                def sb(name, shape, dtype):
                    return S.buf(name, shape, dtype, es=es6)
                affb = sb("affb", [128, NT, NE], F32)
                k.DMA("sp", affb[:, :, :], affg_t.ap().rearrange("(t p) e -> p t e", p=128), r=[affg_b], w=[affb])
                cmpb = sb("cmpb", [128, NT, NE], F32)
                hi = sb("hi", [128, NE], F32)
                d2 = sb("d2", [128, NE], F32)
                mid = sb("mid", [128, NE], F32)
                cntp = sb("cntp", [128, NE], F32)
                ge = sb("ge", [128, NE], F32)
                k.MEMSET("pool", tau[:, :], 0.0, w=[tau])
                k.MEMSET("pool", hi[:, :], 1.0001, w=[hi])
                ones_f = cst[:, 7, :]
                for it in range(32):
                    k.TT("dve", d2[:, :], hi[:, :], tau[:, :], ALU.subtract, r=[hi, tau], w=[d2])
                    k.STT("dve", mid[:, :], d2[:, :], 0.5, tau[:, :], ALU.mult, ALU.add, r=[d2, tau], w=[mid])
                    k.TT("dve", cmpb[:, :, :], affb[:, :, :], mid[:, :].unsqueeze(1).to_broadcast([128, NT, NE]),
                         ALU.is_ge, r=[affb, mid], w=[cmpb])
                    S.op("dve", lambda e: e.tensor_reduce(out=cntp[:, :], in_=cmpb[:, :, :].rearrange("p t e -> p e t"),
                                                          axis=AX.X, op=ALU.add), r=[cmpb], w=[cntp])
                    k.MM(P[6][:, 0:NE], ones_f, cntp[:, :], True, True, r=[cst, cntp], w=[P[6]])
                    k.TS("dve", ge[:, :], P[6][:, 0:NE], float(CAP), None, ALU.is_ge, None, r=[P[6]], w=[ge])
                    k.TT("dve", ge[:, :], ge[:, :], d2[:, :], ALU.mult, r=[ge, d2], w=[ge])
                    k.STT("dve", tau[:, :], ge[:, :], 0.5, tau[:, :], ALU.mult, ALU.add, r=[ge, tau], w=[tau])
                    k.STT("dve", hi[:, :], d2[:, :], -0.5, hi[:, :], ALU.mult, ALU.add, r=[d2, hi], w=[hi])
                    k.STT("dve", hi[:, :], ge[:, :], 0.5, hi[:, :], ALU.mult, ALU.add, r=[ge, hi], w=[hi])
                maskf = sb("maskf", [128, NH, NE], F32)
                maskb = sb("maskb", [128, NH * NE], BF16)
                totE = sb("totE", [128, NE, NH], F32)
                cumE = sb("cumE", [128, NE, NH], F32)
                ones32 = sb("ones32", [128, NH], F32)
                posf = sb("posf", [128, NH, NE], F32)
                k.TT("dve", maskf[:, :, :], affall[:, :, :], tau[:, :].unsqueeze(1).to_broadcast([128, NH, NE]),
                     ALU.is_ge, r=[affall, tau], w=[maskf])
                k.TT("dve", gate[:, :, :], maskf[:, :, :], affall[:, :, :], ALU.mult, r=[maskf, affall], w=[gate])
                k.CP("dve", maskb[:, :], maskf[:, :, :].rearrange("p t e -> p (t e)"), r=[maskf], w=[maskb])
                k.MM(P[1][:, :], cstb[:, 8, :], maskb[:, :], True, True, r=[cstb, maskb], w=[P[1]])
                k.MM(P[2][:, :], onesb, maskb[:, :], True, True, r=[cstb, maskb], w=[P[2]])
                k.CP("dve", totE[:, :, :], P[2][:, :].rearrange("p (t e) -> p e t", e=NE), r=[P[2]], w=[totE])
                k.MEMSET("pool", ones32[:, :], 1.0, w=[ones32])
                for e_ in range(NE):
                    S.op("dve", (lambda e_: lambda eng: eng.tensor_tensor_scan(
                        out=cumE[:, e_, :], data0=ones32[:, :], data1=totE[:, e_, :], initial=0.0,
                        op0=ALU.mult, op1=ALU.add))(e_), r=[ones32, totE], w=[cumE])
                k.TT("dve", cumE[:, :, :], cumE[:, :, :], totE[:, :, :], ALU.subtract, r=[cumE, totE], w=[cumE])
                k.TT("dve", posf[:, :, :], P[1][:, :].rearrange("p (t e) -> p t e", e=NE),
                     cumE[:, :, :].rearrange("p e t -> p t e"), ALU.add, r=[P[1], cumE], w=[posf])
                ltm = sb("ltm", [128, NH, NE], F32)
                k.TS("dve", ltm[:, :, :], posf[:, :, :], float(CAP), None, ALU.is_lt, None, r=[posf], w=[ltm])
                k.TT("dve", maskf[:, :, :], maskf[:, :, :], ltm[:, :, :], ALU.mult, r=[maskf, ltm], w=[maskf])
                k.TT("dve", posf[:, :, :], posf[:, :, :], cst[:, 9, 2:2 + NE].unsqueeze(1).to_broadcast([128, NH, NE]),
                     ALU.add, r=[posf, cst], w=[posf])
                k.TT("dve", posf[:, :, :], posf[:, :, :], maskf[:, :, :], ALU.mult, r=[posf, maskf], w=[posf])
                k.TS("dve", maskf[:, :, :], maskf[:, :, :], -1.0, 1.0, ALU.mult, ALU.add, r=[maskf], w=[maskf])
                k.TS("dve", maskf[:, :, :], maskf[:, :, :], cst[:, 9, 18:19], None, ALU.mult, None, r=[maskf, cst], w=[maskf])
                k.TT("dve", posf[:, :, :], posf[:, :, :], maskf[:, :, :], ALU.add, r=[posf, maskf], w=[posf])
                k.CP("dve", sloti[:, :, :], posf[:, :, :], r=[posf], w=[sloti])
                S.barrier()
            if stage < 7:
                with contextlib.ExitStack() as esd:
                    tf = S.buf("dbg_g", [128, NH * NE], F32, es=esd)
                    k.DMA("sp", dbg_d[HALF + 128:HALF + 256, 0:NE], tau[:, :], r=[tau], w=[])
                    k.DMA("sp", dbg_d[HALF + 256:HALF + 384, 0:NH * NE], gate[:, :, :].rearrange("p t e -> p (t e)"),
                          r=[gate], w=[])
                    k.CP("dve", tf[:, :], sloti[:, :, :].rearrange("p t e -> p (t e)"), r=[sloti], w=[tf])
                    k.DMA("sp", dbg_d[HALF + 384:HALF + 512, 0:NH * NE], tf[:, :], r=[tf], w=[])
        if stage >= 7:
            with contextlib.ExitStack() as es7:
                def sb(name, shape, dtype):
                    return S.buf(name, shape, dtype)
                wgb = sb("wgb", [128, 8, D], BF16)
                wub = sb("wub", [128, 8, D], BF16)
                wdb = sb("wdb", [128, 8, D], BF16)
                hd = [sb(f"hd{i}", [128, ROWW], BF16) for i in range(6)]
                xs_in = [sb(f"xs_in{i}", [128, ROWW], BF16) for i in range(2)]
                xinT = sb("xinT", [128, 8, CAP], BF16)
                hid = sb("hid", [128, 8, CAP], BF16)
                gall = sb("gall", [128, 8, 8], F32)
                iall = sb("iall", [128, 8, 8], I32)
                sg = [sb(f"sg{i}", [128, 512], F32) for i in range(2)]
                yt = [sb(f"yt{i}", [128, D], F32) for i in range(2)]

                def load_w(e_):
                    for (wb, wd_) in ((wgb, wg_d), (wub, wu_d), (wdb, wd_d)):
                        k.DMA("pool", wb[:, :, :], wd_[e_].rearrange("(k p) n -> p k n", p=128), r=[], w=[wb])

                def dispatch(e_):
                    for t in range(NH):
                        hb_ = hd[(e_ * NH + t) % 6]
                        k.DMA("sp", hb_[:, :], h2buf_d[t * 128:(t + 1) * 128, :], r=[h2buf_b], w=[hb_])
                        k.CP("dve", hb_[:, 1024:1026].bitcast(F32), gate[:, t, e_:e_ + 1], r=[gate], w=[hb_])
                        S.dma("pool", (lambda hb_, t, e_: lambda eng: eng.indirect_dma_start(
                            out=xin_flat, out_offset=bass.IndirectOffsetOnAxis(ap=sloti[:, t, e_:e_ + 1], axis=0),
                            in_=hb_[:, :], in_offset=None))(hb_, t, e_),
                            r=[hb_, sloti, xin_e[e_]], w=[])

                nexp = NE if stage >= 8 else 2
                load_w(0)
                dispatch(0)
                pbi = 0
                ybi = 0
                for e_ in range(nexp):
                    for sc in range(8):
                        xb_ = xs_in[sc % 2]
                        if sc == 0:
                            k.DMA("sp", xb_[:, :], xin_d[e_, sc * 128:(sc + 1) * 128, :], r=[], w=[xb_, xin_e[e_]])
                        else:
                            k.DMA("sp", xb_[:, :], xin_d[e_, sc * 128:(sc + 1) * 128, :], r=[xin_e[e_]], w=[xb_])
                        k.CP("dve", gall[:, sc, 0:1], xb_[:, 1024:1026].bitcast(F32), r=[xb_], w=[gall])
                        k.CP("dve", iall[:, sc, 0:1], xb_[:, 1026:1028].bitcast(I32), r=[xb_], w=[iall])
                        for kc in range(8):
                            k.TR(P0b[:, kc * 128:(kc + 1) * 128], xb_[:, kc * 128:(kc + 1) * 128], identb,
                                 r=[xb_, cstb], w=[P[0]])
                        k.CP("act", xinT[:, :, sc * 128:(sc + 1) * 128], P0b[:, :].rearrange("p (k l) -> p k l", k=8),
                             r=[P[0]], w=[xinT])
                    if e_ + 1 < nexp:
                        dispatch(e_ + 1)
                    for fc in range(8):
                        for half in range(2):
                            pg, pu = P[1 + 2 * (pbi % 3)], P[2 + 2 * (pbi % 3)]
                            pbi += 1
                            for kc in range(8):
                                k.MM(pg[:, :], wgb[:, kc, fc * 128:(fc + 1) * 128], xinT[:, kc, half * 512:(half + 1) * 512],
                                     kc == 0, kc == 7, r=[wgb, xinT], w=[pg])
                            for kc in range(8):
                                k.MM(pu[:, :], wub[:, kc, fc * 128:(fc + 1) * 128], xinT[:, kc, half * 512:(half + 1) * 512],
                                     kc == 0, kc == 7, r=[wub, xinT], w=[pu])
                            sgb = sg[(fc * 2 + half) % 2]
                            k.ACT(sgb[:, :], pg[:, :], AF.Silu, r=[pg], w=[sgb])
                            k.TT("dve", hid[:, fc, half * 512:(half + 1) * 512], sgb[:, :], pu[:, :], ALU.mult,
                                 r=[sgb, pu], w=[hid])
                    if e_ + 1 < nexp:
                        for (wb, wd_) in ((wgb, wg_d), (wub, wu_d)):
                            k.DMA("pool", wb[:, :, :], wd_[e_ + 1].rearrange("(k p) n -> p k n", p=128), r=[], w=[wb])
                    for sc in range(8):
                        ytb = yt[sc % 2]
                        for n in range(2):
                            py = P[7] if (ybi % 2 == 0) else P[0]
                            ybi += 1
                            for fc in range(8):
                                k.MM(py[:, :], hid[:, fc, sc * 128:(sc + 1) * 128], wdb[:, fc, n * 512:(n + 1) * 512],
                                     fc == 0, fc == 7, r=[hid, wdb], w=[py])
                            k.ACT(ytb[:, n * 512:(n + 1) * 512], py[:, :], AF.Identity, r=[py, gall], w=[ytb],
                                  scale=gall[:, sc, 0:1])
                        S.dma("pool", (lambda ytb, sc: lambda eng: eng.indirect_dma_start(
                            out=acc_d, out_offset=bass.IndirectOffsetOnAxis(ap=iall[:, sc, 0:1], axis=0),
                            in_=ytb[:, :], in_offset=None, compute_op=ALU.add))(ytb, sc),
                            r=[ytb, iall], w=[acc_b])
                    if e_ + 1 < nexp:
                        k.DMA("pool", wdb[:, :, :], wd_d[e_ + 1].rearrange("(k p) n -> p k n", p=128), r=[], w=[wdb])
                S.barrier()
            with contextlib.ExitStack() as es8:
                def sb(name, shape, dtype):
                    return S.buf(name, shape, dtype)
                at = [sb(f"at{i}", [128, D], F32) for i in range(2)]
                x1t = [sb(f"x1t{i}", [128, D], F32) for i in range(2)]
                ot = [sb(f"ot{i}", [128, D], F32) for i in range(2)]
                junk8 = sb("junk8", [128, D], BF16)
                s8 = sb("s8", [128, 8, 8], F32)
                for t in range(NH):
                    i = t % 2
                    rows = slice(t * 128, (t + 1) * 128)
                    k.DMA("sp", at[i][:, :], acc_d[rows, :], r=[acc_b], w=[at[i]])
                    k.DMA("sp", x1t[i][:, :], x1buf_d[rows, :], r=[x1buf_b], w=[x1t[i]])
                    k.MEMSET("pool", s8[:, 0, 0:1], 0.0, w=[s8])
                    k.ACT(junk8[:, :], at[i][:, :], AF.Square, r=[at[i]], w=[junk8, s8], accum=s8[:, 0, 0:1])
                    k.ACT(s8[:, 1, 0:1], s8[:, 0, 0:1], AF.Ln, r=[s8], w=[s8], bias=EPS, scale=1.0 / D)
                    k.ACT(s8[:, 2, 0:1], s8[:, 1, 0:1], AF.Exp, r=[s8], w=[s8], scale=-0.5)
                    k.STT("dve", ot[i][:, :], at[i][:, :], s8[:, 2, 0:1], G2, ALU.mult, ALU.mult,
                          r=[at[i], s8, modrep], w=[ot[i]])
                    k.TT("pool", ot[i][:, :], ot[i][:, :], x1t[i][:, :], ALU.add, r=[ot[i], x1t[i]], w=[ot[i]])
                    k.DMA("sp", out_d[rows, :], ot[i][:, :], r=[ot[i]], w=[])
        S.wait_all_dma("sp")
        S.emit()
    return nc


def make_consts():
    c = np.zeros((128, NCONST, 128), np.float32)
    i = np.arange(128)
    r, cc = i[:, None], i[None, :]
    same = (r // 64) == (cc // 64)
    c[:, 0] = (r == cc)
    c[:, 1] = (r <= cc)
    c[:, 2] = np.where(r > cc, -30000.0, 0.0)
    c[:, 3] = (r <= cc)
    c[:, 4] = same & (r <= cc)
    c[:, 5] = same & (r > cc)
    c[:, 6] = same & (r <= cc)
    c[:, 7] = 1.0
    c[:, 8] = (r < cc)
    c[:, 9, 0] = (i < 64)
    c[:, 9, 1] = (i >= 64)
    c[:, 9, 2:2 + NE] = np.arange(NE)[None, :] * CAP
    c[:, 9, 18] = NE * CAP + i
    return c


def rep(v, n=128):
    return np.ascontiguousarray(np.broadcast_to(np.asarray(v, np.float32)[None], (n,) + tuple(np.shape(v))))


def fm(v):
    return np.ascontiguousarray(np.asarray(v, np.float32).reshape(-1, 128).T)


def prep_inputs(inp):
    x, c, ctx, c_ctx = inp["x"], inp["c"], inp["ctx"], inp["c_ctx"]
    w_in = inp["w_in"][0]
    consts = make_consts()
    shared = {
        "ada_w": np.ascontiguousarray(inp["ada_w"][0]),
        "ada_bT": np.ascontiguousarray(inp["ada_b"][0][:2048].reshape(16, 128).T),
        "ada_brep": rep(inp["ada_b"][0][2048:]),
        "nw0T": fm(inp["norm_w"][0, 0]),
        "nwrep": rep(inp["norm_w"][0, 1:4]),
        "cb": fm(inp["ssd_conv_b"][0]),
        "snw": rep(inp["ssd_norm_w"][0]),
        "hnw": rep(inp["hgrn_norm_w"][0]),
        "w_out": np.ascontiguousarray(inp["w_out"][0]),
        "w_router": np.ascontiguousarray(inp["w_router"][0]),
        "w_gate": np.ascontiguousarray(inp["w_gate"][0]),
        "w_up": np.ascontiguousarray(inp["w_up"][0]),
        "w_down": np.ascontiguousarray(inp["w_down"][0]),
        "consts": consts,
    }
    maps = []
    for core in range(8):
        b, d = core // 2, core % 2
        m = dict(shared)
        m["xs"] = np.ascontiguousarray(x[b][::-1] if d else x[b])
        m["ctxs"] = np.ascontiguousarray(ctx[b][::-1] if d else ctx[b])
        cv = np.stack([fm(c[b]), fm(c_ctx)], axis=-1)
        m["cvec"] = np.ascontiguousarray(cv)
        cols = [w_in[:, 512:1536], w_in[:, 1552:2064], w_in[:, 2064 + 512 * d:2576 + 512 * d], w_in[:, 3088:3600],
                w_in[:, 1536 + 8 * d:1544 + 8 * d], w_in[:, 0:512], w_in[:, 3600:4112]]
        m["wmain"] = np.ascontiguousarray(np.concatenate(cols, axis=1))
        cwk = inp["ssd_conv_w"][0]
        if d:
            cwk = cwk[::-1]
        m["cw"] = np.ascontiguousarray(cwk.T.reshape(8, 128, 5).transpose(1, 0, 2))
        m["sp8"] = rep(np.stack([inp["ssd_dt_bias"][0, d], inp["ssd_a_log"][0, d], inp["ssd_d"][0]]))
        m["lbrep"] = rep(np.stack([inp["hgrn_lb"][0, d], inp["hgrn_lb"][1, d]]))
        p = np.arange(128)[:, None]
        t = np.arange(NH)[None, :]
        m["pidx"] = np.ascontiguousarray(np.concatenate(
            [(1 - d) * 1024 + ((HALF - 1) - (t * 128 + p)) % 1024, t * 128 + p], axis=1).astype(np.int32))
        maps.append(m)
    return maps


STAGE = int(os.environ.get("KSTAGE", "9"))
LITE = int(os.environ.get("KLITE", "0"))
SIM = int(os.environ.get("KSIM", "0"))
SAME_ENGINE_SYNC = bool(int(os.environ.get("KSES", "1")))
SUBCUT = os.environ.get("KSUB", "")
_CACHE = {}


def kernel(**inputs):
    inp = {k_: np.asarray(v) for k_, v in inputs.items()}
    maps = prep_inputs(inp)
    if LITE:
        for m in maps:
            m["ada_w"] = m["ada_w"][:8]
            m["xs"] = m["xs"][:1024]
    if STAGE < 7:
        for m in maps:
            for kk_ in ("w_gate", "w_up", "w_down"):
                m.pop(kk_)
    if STAGE not in _CACHE:
        _CACHE[STAGE] = build_program(STAGE)
    nc = _CACHE[STAGE]
    res = run_bass_kernel_spmd(nc, maps, core_ids=list(range(8)))
    if STAGE < 9:
        return [r["dbg"] for r in res.results]
    out = np.empty((4, SEQ, D), np.float32)
    for core in range(8):
        b, d = core // 2, core % 2
        o = res.results[core]["out"]
        if d:
            out[b, HALF:] = o[::-1]
        else:
            out[b, :HALF] = o
    return out
```

```python
import contextlib
import os
import numpy as np
import concourse.bass as bass
import concourse.mybir as mybir
from concourse.bass_utils import run_bass_kernel_spmd

F32 = mybir.dt.float32
BF16 = mybir.dt.bfloat16
I32 = mybir.dt.int32
AF = mybir.ActivationFunctionType
ALU = mybir.AluOpType
AX = mybir.AxisListType

D = 1024
SEQ = 8192
CTX = 256
NT = SEQ // 128
NH = NT // 2
HALF = SEQ // 2
NE = 16
CAP = 1024
EPS = 1e-6
WCOLS = 3592
C_XBC, C_Q, C_F, C_I, C_DT, C_Z, C_G = 0, 1024, 1536, 2048, 2560, 2568, 3080
NCONST = 10
ROWW = 1028
TRASH = HALF


class Buf:
    __slots__ = ("name", "t", "last_w", "readers")

    def __init__(self, name, t=None):
        self.name = name
        self.t = t
        self.last_w = None
        self.readers = []

    def __getitem__(self, k):
        return self.t[k]


class Sched:
    COMPUTE = ("pe", "dve", "act", "pool")
    NDSEM = 6

    def __init__(self, nc, es, same_engine_sync=True):
        self.nc = nc
        self.es = es
        self.same_engine_sync = same_engine_sync
        self.prog = {k: [] for k in ("pe", "dve", "act", "pool", "sp")}
        self.ninst = {k: 0 for k in self.COMPUTE}
        self.waited_idx = {}
        self.milestones = {k: set() for k in self.COMPUTE}
        self.csem = {k: es.enter_context(nc.semaphore("cs_" + k)) for k in self.COMPUTE}
        self.dsem, self.dcnt, self.drot = {}, {}, {}
        for q in ("sp", "act", "pool"):
            self.dsem[q] = [es.enter_context(nc.semaphore(f"ds_{q}{j}")) for j in range(self.NDSEM)]
            self.dcnt[q] = [0] * self.NDSEM
            self.drot[q] = 0
        self.ccsem = es.enter_context(nc.semaphore("cc_sem"))
        self.cccnt = 0

    def buf(self, name, shape, dtype, psum=False, es=None):
        es = es or self.es
        name = "s_" + name
        if psum:
            t = es.enter_context(self.nc.psum_tensor(name, list(shape), dtype))
        else:
            t = es.enter_context(self.nc.sbuf_tensor(name, list(shape), dtype))
        return Buf(name, t)

    def _need(self, eng, tok):
        if tok is None:
            return
        semkey, v = tok
        key = (eng, semkey)
        if semkey[0] == "c":
            src = semkey[1]
            if src == eng and (eng == "pe" or not self.same_engine_sync):
                return
            if self.waited_idx.get(key, -1) >= v:
                return
            self.waited_idx[key] = v
            self.milestones[src].add(v)
            self.prog[eng].append(("cwait", src, v))
        elif semkey[0] == "x":
            if self.waited_idx.get(key, -1) >= v:
                return
            self.waited_idx[key] = v
            self.prog[eng].append(("xwait", None, v))
        else:
            if self.waited_idx.get(key, -1) >= v:
                return
            self.waited_idx[key] = v
            _, q, j = semkey
            self.prog[eng].append(("dwait", (q, j), v))

    def _deps(self, eng, r, w):
        for b in r:
            self._need(eng, b.last_w)
        for b in w:
            self._need(eng, b.last_w)
            for t in b.readers:
                self._need(eng, t)

    def _commit(self, tok, r, w):
        for b in w:
            b.last_w = tok
            b.readers = []
        for b in r:
            if b not in w:
                b.readers.append(tok)

    def op(self, eng, fn, r=(), w=()):
        self._deps(eng, r, w)
        idx = self.ninst[eng]
        self.ninst[eng] += 1
        self.prog[eng].append(("op", fn, idx))
        tok = (("c", eng), idx)
        self._commit(tok, r, w)
        return tok

    def dma(self, q, fn, r=(), w=()):
        self._deps(q, r, w)
        j = self.drot[q]
        self.drot[q] = (j + 1) % self.NDSEM
        semkey = ("d", q, j)
        prev = self.dcnt[q][j]
        if prev > 0:
            self._need(q, (semkey, prev))
        self.dcnt[q][j] += 16
        v = self.dcnt[q][j]
        self.prog[q].append(("dma", fn, (q, j)))
        tok = (semkey, v)
        self._commit(tok, r, w)
        return tok

    def coll(self, fn, r=(), w=()):
        q = "pool"
        self._deps(q, r, w)
        self.cccnt += 1
        v = self.cccnt
        self.prog[q].append(("coll", fn, v))
        tok = (("x", "cc"), v)
        self._commit(tok, r, w)
        return tok

    def barrier(self):
        for eng in ("pe", "dve", "act", "pool", "sp"):
            for x in self.COMPUTE:
                if x != eng and self.ninst[x] > 0:
                    self._need(eng, (("c", x), self.ninst[x] - 1))
            self.wait_all_dma(eng)

    def wait_all_dma(self, eng="sp"):
        for q in ("sp", "act", "pool"):
            for j in range(self.NDSEM):
                if self.dcnt[q][j]:
                    self._need(eng, (("d", q, j), self.dcnt[q][j]))
        if self.cccnt:
            self._need(eng, (("x", "cc"), self.cccnt))

    def emit(self):
        nc = self.nc
        rank = {}
        for e in self.COMPUTE:
            ms = sorted(self.milestones[e])
            rank[e] = {idx: i + 1 for i, idx in enumerate(ms)}
        sched = self

        def run(engname, e):
            for ent in sched.prog[engname]:
                kind = ent[0]
                if kind == "op":
                    ins = ent[1](e)
                    if ent[2] in rank[engname]:
                        ins.then_inc(sched.csem[engname], 1)
                elif kind == "cwait":
                    e.wait_ge(sched.csem[ent[1]], rank[ent[1]][ent[2]])
                elif kind == "dwait":
                    q, j = ent[1]
                    e.wait_ge(sched.dsem[q][j], ent[2])
                elif kind == "xwait":
                    e.wait_ge(sched.ccsem, ent[2])
                elif kind == "coll":
                    ent[1](e).then_inc(sched.ccsem, 1)
                elif kind == "dma":
                    q, j = ent[2]
                    ent[1](e).then_inc(sched.dsem[q][j], 16)

        with nc.Block() as block:
            @block.sync
            def _(e):
                run("sp", e)

            @block.scalar
            def _(e):
                run("act", e)

            @block.vector
            def _(e):
                run("dve", e)

            @block.gpsimd
            def _(e):
                run("pool", e)

            @block.tensor
            def _(e):
                run("pe", e)


class K:
    def __init__(self, nc, es):
        self.nc = nc
        self.S = Sched(nc, es, same_engine_sync=SAME_ENGINE_SYNC)

    def MM(self, out, lhsT, rhs, start, stop, r, w):
        self.S.op("pe", lambda e: e.matmul(out, lhsT=lhsT, rhs=rhs, start=start, stop=stop), r=r, w=w)

    def TR(self, out, in_, ident, r, w):
        self.S.op("pe", lambda e: e.transpose(out=out, in_=in_, identity=ident), r=r, w=w)

    def ACT(self, out, in_, func, r, w, bias=None, scale=None, accum=None):
        kw = {}
        if bias is not None:
            kw["bias"] = bias
        if scale is not None:
            kw["scale"] = scale
        if accum is not None:
            kw["accum_out"] = accum
        self.S.op("act", lambda e: e.activation(out=out, in_=in_, func=func, **kw), r=r, w=w)

    def TT(self, eng, out, in0, in1, op, r, w):
        self.S.op(eng, lambda e: e.tensor_tensor(out=out, in0=in0, in1=in1, op=op), r=r, w=w)

    def TS(self, eng, out, in0, s1, s2, op0, op1, r, w, accum=None):
        if op1 is None:
            self.S.op(eng, lambda e: e.tensor_scalar(out=out, in0=in0, scalar1=s1, scalar2=None, op0=op0), r=r, w=w)
        elif accum is not None:
            self.S.op(eng, lambda e: e.tensor_scalar(out=out, in0=in0, scalar1=s1, scalar2=s2, op0=op0, op1=op1,
                                                     accum_out=accum), r=r, w=w)
        else:
            self.S.op(eng, lambda e: e.tensor_scalar(out=out, in0=in0, scalar1=s1, scalar2=s2, op0=op0, op1=op1),
                      r=r, w=w)

    def STT(self, eng, out, in0, scalar, in1, op0, op1, r, w):
        self.S.op(eng, lambda e: e.scalar_tensor_tensor(out=out, in0=in0, scalar=scalar, in1=in1, op0=op0, op1=op1),
                  r=r, w=w)

    def CP(self, eng, out, in_, r, w):
        if eng == "act":
            self.S.op("act", lambda e: e.copy(out=out, in_=in_), r=r, w=w)
        else:
            self.S.op(eng, lambda e: e.tensor_copy(out=out, in_=in_), r=r, w=w)

    def MEMSET(self, eng, ap, val, w):
        self.S.op(eng, lambda e: e.memset(ap, val), w=w)

    def DMA(self, q, out, in_, r, w):
        return self.S.dma(q, lambda e: e.dma_start(out=out, in_=in_), r=r, w=w)


def build_program(stage):
    nc = bass.Bass("TRN2", target_bir_lowering=False)
    dt_in = lambda name, shape, dt=F32: nc.dram_tensor(name, list(shape), dt, kind="ExternalInput").ap()
    x_d = dt_in("xs", [1024 if LITE else SEQ, D])
    ctx_d = dt_in("ctxs", [CTX, D])
    cvec_d = dt_in("cvec", [128, 8, 2])
    adaw_d = dt_in("ada_w", [8 if LITE else D, 6 * D])
    adabT_d = dt_in("ada_bT", [128, 16])
    adabrep_d = dt_in("ada_brep", [128, 4 * D])
    nw0T_d = dt_in("nw0T", [128, 8])
    nwrep_d = dt_in("nwrep", [128, 3, D])
    wmain_d = dt_in("wmain", [D, WCOLS])
    cw_d = dt_in("cw", [128, 8, 5])
    cb_d = dt_in("cb", [128, 8])
    sp8_d = dt_in("sp8", [128, 3, 8])
    lbrep_d = dt_in("lbrep", [128, 2, 512])
    snw_d = dt_in("snw", [128, 512])
    hnw_d = dt_in("hnw", [128, 512])
    wout_d = dt_in("w_out", [D, D])
    wr_d = dt_in("w_router", [D, NE])
    if stage >= 7:
        wg_d = dt_in("w_gate", [NE, D, D])
        wu_d = dt_in("w_up", [NE, D, D])
        wd_d = dt_in("w_down", [NE, D, D])
    consts_d = dt_in("consts", [128, NCONST, 128])
    pidx_d = dt_in("pidx", [128, 2 * NH], I32)
    out_d = nc.dram_tensor("out", [HALF, D], F32, kind="ExternalOutput").ap()
    dbg_d = None
    if stage < 9:
        dbg_d = nc.dram_tensor("dbg", [SEQ, D], F32, kind="ExternalOutput").ap()
    ybuf_d = nc.dram_tensor("ybuf", [HALF, D], BF16).ap()
    ysend_t = [nc.dram_tensor(f"ysend{c}", [1024, D], BF16) for c in range(4)]
    ygath_t = [nc.dram_tensor(f"ygath{c}", [2048, D], BF16) for c in range(4)]
    zgbuf_d = nc.dram_tensor("zgbuf", [HALF, D], BF16).ap()
    x1buf_d = nc.dram_tensor("x1buf", [HALF, D], F32).ap()
    h2buf_d = nc.dram_tensor("h2buf", [HALF, ROWW], BF16).ap()
    affs_t = nc.dram_tensor("affsend", [HALF, NE], F32)
    affg_t = nc.dram_tensor("affgath", [SEQ, NE], F32)
    xin_flat = nc.dram_tensor("xin", [NE * CAP + 128, ROWW], BF16).ap()
    xin_d = xin_flat[0:NE * CAP, :].rearrange("(e c) r -> e c r", e=NE)
    acc_d = nc.dram_tensor("moeacc", [HALF + 128, D], F32).ap()
    ybuf_b, ysend_b, ygath_b, zgbuf_b, x1buf_b, h2buf_b = (Buf(n) for n in ("ybuf", "ysend", "ygath", "zgbuf", "x1buf", "h2buf"))
    affs_b, affg_b, xin_b, acc_b = (Buf(n) for n in ("affs", "affg", "xin", "acc"))
    PAIRS = [[0, 1], [2, 3], [4, 5], [6, 7]]

    with contextlib.ExitStack() as es:
        k = K(nc, es)
        S = k.S
        P = [S.buf(f"P{i}", [128, 512], F32, psum=True) for i in range(8)]
        P0b = P[0].t[:, :].bitcast(BF16)

        cst = S.buf("cst", [128, NCONST, 128], F32)
        cstb = S.buf("cstb", [128, NCONST, 128], BF16)
        k.DMA("sp", cst[:, :, :], consts_d, r=[], w=[cst])
        k.CP("dve", cstb[:, :, :], cst[:, :, :], r=[cst], w=[cstb])
        ident, identb = cst[:, 0, :], cstb[:, 0, :]
        trib, negmb = cstb[:, 1, :], cstb[:, 2, :]
        mask01 = cst[:, 3, :]
        tri2i, tri2r, mask2 = cst[:, 4, :], cst[:, 5, :], cst[:, 6, :]
        onesb = cstb[:, 7, :]
        chunkind = cst[:, 9, 0:2]


        cvec = S.buf("cvec", [128, 8, 2], F32)
        k.DMA("sp", cvec[:, :, :], cvec_d, r=[], w=[cvec])
        adabT = S.buf("adabT", [128, 16], F32)
        k.DMA("sp", adabT[:, :], adabT_d, r=[], w=[adabT])
        nw0T = S.buf("nw0T", [128, 8], F32)
        k.DMA("sp", nw0T[:, :], nw0T_d, r=[], w=[nw0T])
        cw = S.buf("cw", [128, 8, 5], F32)
        k.DMA("sp", cw[:, :, :], cw_d, r=[], w=[cw])
        cb = S.buf("cb", [128, 8], F32)
        k.DMA("sp", cb[:, :], cb_d, r=[], w=[cb])
        sp8 = S.buf("sp8", [128, 3, 8], F32)
        k.DMA("sp", sp8[:, :, :], sp8_d, r=[], w=[sp8])
        lbrep = S.buf("lbrep", [128, 2, 512], F32)
        k.DMA("sp", lbrep[:, :, :], lbrep_d, r=[], w=[lbrep])

        if stage >= 7:
            initr = S.buf("initr", [128, ROWW], BF16)
            k.MEMSET("pool", initr[:, :], 0.0, w=[initr])
            k.MEMSET("pool", initr[:, 1026:1028].bitcast(I32), TRASH, w=[initr])
            zt = S.buf("zt", [128, D], F32)
            k.MEMSET("pool", zt[:, :], 0.0, w=[zt])
            xin_e = [Buf(f"xin_e{e_}") for e_ in range(NE)]
            for e_ in range(NE):
                for sc in range(8):
                    k.DMA("sp", xin_d[e_, sc * 128:(sc + 1) * 128, :], initr[:, :], r=[initr], w=[xin_e[e_]])
            for t in range(NH + 1):
                k.DMA("sp", acc_d[t * 128:(t + 1) * 128, :], zt[:, :], r=[zt], w=[acc_b])
        sc = S.buf("sc", [128, 8, 2], F32)
        k.ACT(sc[:, :, :], cvec[:, :, :], AF.Silu, r=[cvec], w=[sc])
        modT = S.buf("modT", [128, 16, 2], F32)
        modrep = S.buf("modrep", [128, 4 * D], F32)
        with contextlib.ExitStack() as es0:
            adap = [S.buf(f"adap{i}", [128, 8, 512], F32, es=es0) for i in range(2)]
            adabrep = S.buf("adabrep", [128, 4 * D], F32, es=es0)
            nwrep = S.buf("nwrep", [128, 3, D], F32, es=es0)
            k.DMA("sp", adabrep[:, :], adabrep_d, r=[], w=[adabrep])
            k.DMA("sp", nwrep[:, :, :], nwrep_d, r=[], w=[nwrep])
            if LITE:
                k.MEMSET("pool", modT[:, :, :], 0.1, w=[modT])
                k.MEMSET("pool", modrep[:, :], 0.1, w=[modrep])
            for j in range(0 if LITE else 12):
                ap_ = adap[j % 2]
                k.DMA("sp", ap_[:, :, :], adaw_d[:, j * 512:(j + 1) * 512].rearrange("(k p) n -> p k n", p=128),
                      r=[], w=[ap_])
                if j < 4:
                    for m in range(4):
                        cc = j * 4 + m
                        for kc in range(8):
                            k.MM(P[6][:, 0:2], ap_[:, kc, m * 128:(m + 1) * 128], sc[:, kc, :], kc == 0, kc == 7,
                                 r=[ap_, sc], w=[P[6]])
                        k.TS("dve", modT[:, cc, :], P[6][:, 0:2], adabT[:, cc:cc + 1], None, ALU.add, None,
                             r=[P[6], adabT], w=[modT])
                else:
                    pb = P[j % 2 + 1]
                    for kc in range(8):
                        k.MM(pb[:, :], sc[:, kc, 0:1].to_broadcast([128, 128]), ap_[:, kc, :], kc == 0, kc == 7,
                             r=[ap_, sc], w=[pb])
                    o = (j - 4) * 512
                    k.TT("dve", modrep[:, o:o + 512], pb[:, :], adabrep[:, o:o + 512], ALU.add,
                         r=[pb, adabrep], w=[modrep])
            k.TT("dve", modrep[:, 0:D], modrep[:, 0:D], nwrep[:, 0, :], ALU.mult, r=[modrep, nwrep], w=[modrep])
            k.STT("dve", modrep[:, 2 * D:3 * D], modrep[:, 2 * D:3 * D], 1.0, nwrep[:, 1, :], ALU.add, ALU.mult,
                  r=[modrep, nwrep], w=[modrep])
            k.TT("dve", modrep[:, 3 * D:4 * D], modrep[:, 3 * D:4 * D], nwrep[:, 2, :], ALU.mult,
                 r=[modrep, nwrep], w=[modrep])
            S.barrier()
        G1, B2, A2, G2 = (modrep[:, i * D:(i + 1) * D] for i in range(4))
        A0 = S.buf("A0", [128, 2, 8], F32)
        B0 = S.buf("B0", [128, 2, 8], F32)
        for i in range(2):
            k.STT("dve", A0[:, i, :], modT[:, 8:16, i], 1.0, nw0T[:, :], ALU.add, ALU.mult, r=[modT, nw0T], w=[A0])
            k.CP("dve", B0[:, i, :], modT[:, 0:8, i], r=[modT], w=[B0])
        aneg = S.buf("aneg", [128, 8], F32)
        k.ACT(aneg[:, :], sp8[:, 1, :], AF.Exp, r=[sp8], w=[aneg])
        k.TS("dve", aneg[:, :], aneg[:, :], -1.0, None, ALU.mult, None, r=[aneg], w=[aneg])
        dsk = S.buf("dsk", [128, 8], F32)
        k.TS("dve", dsk[:, :], sp8[:, 2, :], 0.5, None, ALU.mult, None, r=[sp8], w=[dsk])
        Dm = S.buf("Dm", [128, 8, 128], BF16)
        for j in range(8):
            k.TS("dve", Dm[:, j, :], ident, dsk[:, j:j + 1], None, ALU.mult, None, r=[cst, dsk], w=[Dm])
        c01 = S.buf("c01", [128, 2, 512], F32)
        k.TT("dve", c01[:, 0, :], lbrep[:, 0, :], lbrep[:, 1, :], ALU.subtract, r=[lbrep], w=[c01])
        k.ACT(c01[:, 1, :], c01[:, 0, :], AF.Tanh, r=[c01], w=[c01], scale=0.5)
        k.TS("dve", c01[:, 0, :], c01[:, 1, :], 0.25, 0.75, ALU.mult, ALU.add, r=[c01], w=[c01])
        k.TS("dve", c01[:, 1, :], c01[:, 1, :], -0.25, 0.25, ALU.mult, ALU.add, r=[c01], w=[c01])
        ST = S.buf("ST", [128, 512], F32)
        STb = S.buf("STb", [128, 512], BF16)
        SH = S.buf("SH", [128, 512], F32)
        SHb = S.buf("SHb", [128, 512], BF16)
        k.MEMSET("pool", ST[:, :], 0.0, w=[ST])
        k.MEMSET("pool", STb[:, :], 0.0, w=[STb])
        k.MEMSET("pool", SH[:, :], 0.0, w=[SH])
        k.MEMSET("pool", SHb[:, :], 0.0, w=[SHb])

        with contextlib.ExitStack() as es2:
            def sb(name, shape, dtype):
                return S.buf(name, shape, dtype, es=es2)
            wm = sb("wm", [128, 8, WCOLS], BF16)
            for kc in range(8):
                for (c0, c1) in ((0, 1796), (1796, WCOLS)):
                    k.DMA("pool", wm[:, kc, c0:c1], wmain_d[kc * 128:(kc + 1) * 128, c0:c1], r=[], w=[wm])
            xt = [sb(f"xt{i}", [128, D], F32) for i in range(4)]
            junk = sb("junk", [128, D], BF16)
            ssq = sb("ssq", [128, 2], F32)
            xn = sb("xn", [128, D], BF16)
            hT = [sb(f"hT{i}", [128, 8, 256], BF16) for i in range(2)]
            cacc = sb("cacc", [128, 8, 256], F32)
            xcT = sb("xcT", [128, 8, 256], BF16)
            xs_tm = sb("xs_tm", [128, 512], BF16)
            B_tm = sb("B_tm", [128, 256], BF16)
            dts = sb("dts", [128, 8, 8], F32)
            eatot = sb("eatot", [128, 8], F32)
            a_hi = sb("a_hi", [128, 8], BF16)
            a_lo = sb("a_lo", [128, 8], BF16)
            xdt = sb("xdt", [128, 512], BF16)
            xdtd = sb("xdtd", [128, 512], BF16)
            LT = sb("LT", [128, 8, 128], BF16)
            MT = sb("MT", [128, 8, 128], BF16)
            CBm = sb("CBm", [128, 2, 128], BF16)
            yoff = sb("yoff", [128, 512], F32)
            yo = sb("yo", [128, 512], BF16)
            qs = sb("qs", [128, 512], F32)
            vb = sb("vb", [128, 512], BF16)
            vm = sb("vm", [128, 2, 512], BF16)
            ff = sb("ff", [128, 512], F32)
            lf = sb("lf", [128, 512], F32)
            kk = sb("kk", [128, 512], F32)
            et = [sb(f"et{i}", [128, 512], F32) for i in range(2)]
            qdec = sb("qdec", [128, 512], BF16)
            kinv = sb("kinv", [128, 512], BF16)
            kdec = sb("kdec", [128, 512], BF16)
            qdT = sb("qdT", [128, 4, 128], BF16)
            kiT = sb("kiT", [128, 4, 128], BF16)
            attm = sb("attm", [128, 4, 128], BF16)
            oc = sb("oc", [64, 2, 512], BF16)
            ebt = sb("ebt", [128, 4, 2], F32)
            zg = sb("zg", [128, D], BF16)

            def load_x(ti_all):
                b = xt[ti_all % 4]
                src = ctx_d[ti_all * 128:(ti_all + 1) * 128, :] if ti_all < 2 else \
                    x_d[(ti_all - 2) * 128:(ti_all - 1) * 128, :]
                k.DMA("sp", b[:, :], src, r=[], w=[b])

            def norm_T(ti_all, hbuf, col0, which):
                b = xt[ti_all % 4]
                k.MEMSET("dve", ssq[:, 0:1], 0.0, w=[ssq])
                k.ACT(junk[:, :], b[:, :], AF.Square, r=[b], w=[junk, ssq], accum=ssq[:, 0:1])
                k.ACT(ssq[:, 1:2], ssq[:, 0:1], AF.Ln, r=[ssq], w=[ssq], bias=EPS, scale=1.0 / D)
                k.ACT(ssq[:, 1:2], ssq[:, 1:2], AF.Exp, r=[ssq], w=[ssq], scale=-0.5)
                k.ACT(xn[:, :], b[:, :], AF.Identity, r=[b, ssq], w=[xn], scale=ssq[:, 1:2])
                for kc in range(8):
                    k.TR(P0b[:, kc * 128:(kc + 1) * 128], xn[:, kc * 128:(kc + 1) * 128], identb,
                         r=[xn, cstb], w=[P[0]])
                for kc in range(8):
                    o = hbuf[:, kc, col0:col0 + 128]
                    i_ = P0b[:, kc * 128:(kc + 1) * 128]
                    if kc % 2 == 0:
                        k.ACT(o, i_, AF.Identity, r=[P[0], A0, B0], w=[hbuf],
                              bias=B0[:, which, kc:kc + 1], scale=A0[:, which, kc:kc + 1])
                    else:
                        k.TS("dve", o, i_, A0[:, which, kc:kc + 1], B0[:, which, kc:kc + 1], ALU.mult, ALU.add,
                             r=[P[0], A0, B0], w=[hbuf])

            def conv_stage(hbuf, T, roww, chunks):
                per_bank = 512 // T
                for ci, c in enumerate(chunks):
                    pb = P[1 + ci // per_bank]
                    po = (ci % per_bank) * T
                    for kc in range(8):
                        k.MM(pb[:, po:po + T], wm[:, kc, C_XBC + c * 128:C_XBC + (c + 1) * 128], hbuf[:, kc, 0:T],
                             kc == 0, kc == 7, r=[wm, hbuf], w=[pb])
                for ci, c in enumerate(chunks):
                    pb = P[1 + ci // per_bank]
                    po = (ci % per_bank) * T
                    src = pb[:, po:po + T]
                    acc = cacc[:, c, 0:T]
                    k.ACT(acc, src, AF.Identity, r=[pb, cw, cb], w=[cacc], bias=cb[:, c:c + 1], scale=cw[:, c, 2:3])
                    srcv = src.rearrange("p (r w) -> p r w", w=roww)
                    accv = acc.rearrange("p (r w) -> p r w", w=roww)
                    for kt in (0, 1, 3, 4):
                        s = kt - 2
                        if s > 0:
                            o_, i_ = accv[:, :, 0:roww - s], srcv[:, :, s:roww]
                        else:
                            o_, i_ = accv[:, :, -s:roww], srcv[:, :, 0:roww + s]
                        k.STT("dve", o_, i_, cw[:, c, kt:kt + 1], o_, ALU.mult, ALU.add, r=[pb, cw, cacc], w=[cacc])
                    k.ACT(xcT[:, c, 0:T], acc, AF.Silu, r=[cacc], w=[xcT])

            def scan_tile(hbuf, col0, tcol, lat, ti, zgproj):
                hsl = lambda kc: hbuf[:, kc, col0:col0 + 128]
                if zgproj:
                    for (pb, c0) in ((P[1], C_Z), (P[2], C_G)):
                        for kc in range(8):
                            k.MM(pb[:, :], hsl(kc), wm[:, kc, c0:c0 + 512], kc == 0, kc == 7, r=[hbuf, wm], w=[pb])
                    k.ACT(zg[:, 0:512], P[1][:, :], AF.Silu, r=[P[1]], w=[zg])
                    k.ACT(zg[:, 512:1024], P[2][:, :], AF.Silu, r=[P[2]], w=[zg])
                    k.DMA("sp", zgbuf_d[ti * 128:(ti + 1) * 128, :], zg[:, :], r=[zg], w=[zgbuf_b])
                for (pb, c0) in ((P[3], C_Q), (P[4], C_F), (P[5], C_I)):
                    for kc in range(8):
                        k.MM(pb[:, :], hsl(kc), wm[:, kc, c0:c0 + 512], kc == 0, kc == 7, r=[hbuf, wm], w=[pb])
                for kc in range(8):
                    k.MM(P[6][:, 0:8], hsl(kc), wm[:, kc, C_DT:C_DT + 8], kc == 0, kc == 7, r=[hbuf, wm], w=[P[6]])
                if lat:
                    k.ACT(qs[:, :], P[3][:, :], AF.Silu, r=[P[3]], w=[qs])
                k.ACT(ff[:, :], P[4][:, :], AF.Tanh, r=[P[4]], w=[ff], scale=0.5)
                k.CP("dve", vb[:, :], P[5][:, :], r=[P[5]], w=[vb])
                for c in range(2):
                    k.TS("dve", vm[:, c, :], vb[:, :], chunkind[:, c:c + 1], None, ALU.mult, None, r=[vb, cst], w=[vm])
                if SUBCUT == 'A':
                    return
                for c in range(6):
                    k.TR(P0b[:, c * 128:(c + 1) * 128], xcT[:, c, tcol:tcol + 128], identb, r=[xcT, cstb], w=[P[0]])
                if SUBCUT == 'B1':
                    return
                k.CP("act", xs_tm[:, :], P0b[:, 0:512], r=[P[0]], w=[xs_tm])
                if SUBCUT == 'B2':
                    return
                k.CP("act", B_tm[:, :], P0b[:, 512:768], r=[P[0]], w=[B_tm])
                if SUBCUT == 'B':
                    return
                v_, av_, l_, dt_, a_, nacs, eacs, w2 = (dts[:, i, :] for i in range(8))
                k.TT("dve", v_, P[6][:, 0:8], sp8[:, 0, :], ALU.add, r=[P[6], sp8], w=[dts])
                k.TS("dve", av_, v_, 30.0, None, ALU.min, None, r=[dts], w=[dts])
                k.ACT(av_, av_, AF.Exp, r=[dts], w=[dts])
                k.ACT(l_, av_, AF.Ln, r=[dts], w=[dts], bias=1.0)
                k.TT("dve", dt_, l_, v_, ALU.max, r=[dts], w=[dts])
                k.TT("dve", a_, dt_, aneg[:, :], ALU.mult, r=[dts, aneg], w=[dts])
                k.CP("dve", a_hi[:, :], a_, r=[dts], w=[a_hi])
                k.TT("dve", a_lo[:, :], a_, a_hi[:, :], ALU.subtract, r=[dts, a_hi], w=[a_lo])
                if SUBCUT == 'C1':
                    return
                k.MM(P[6][:, 64:72], trib, a_hi[:, :], True, False, r=[cstb, a_hi], w=[P[6]])
                k.MM(P[6][:, 64:72], trib, a_lo[:, :], False, True, r=[cstb, a_lo], w=[P[6]])
                k.MM(P[6][:, 128:136], onesb, a_hi[:, :], True, False, r=[cstb, a_hi], w=[P[6]])
                k.MM(P[6][:, 128:136], onesb, a_lo[:, :], False, True, r=[cstb, a_lo], w=[P[6]])
                k.TS("dve", nacs, P[6][:, 64:72], -1.0, None, ALU.mult, None, r=[P[6]], w=[dts])
                k.ACT(eacs, P[6][:, 64:72], AF.Exp, r=[P[6]], w=[dts])
                k.TT("dve", w2, P[6][:, 128:136], nacs, ALU.add, r=[P[6], dts], w=[dts])
                k.ACT(w2, w2, AF.Exp, r=[dts], w=[dts])
                k.ACT(eatot[:, :], P[6][:, 128:136], AF.Exp, r=[P[6]], w=[eatot])
                k.TT("dve", w2, w2, dt_, ALU.mult, r=[dts], w=[dts])
                if SUBCUT == 'C2':
                    return
                xs3 = xs_tm[:, :].rearrange("p (j q) -> p j q", q=64)
                k.TT("dve", xdtd[:, :].rearrange("p (j q) -> p j q", q=64), xs3, w2.unsqueeze(2).to_broadcast([128, 8, 64]), ALU.mult,
                     r=[xs_tm, dts], w=[xdtd])
                if lat:
                    k.TT("dve", xdt[:, :].rearrange("p (j q) -> p j q", q=64), xs3, dt_.unsqueeze(2).to_broadcast([128, 8, 64]), ALU.mult,
                         r=[xs_tm, dts], w=[xdt])
                    for j in range(8):
                        pa = P[1 + j // 4]
                        o = pa[:, (j % 4) * 128:(j % 4 + 1) * 128]
                        k.MM(o, a_hi[:, j:j + 1].to_broadcast([128, 128]), trib, True, False, r=[a_hi, cstb], w=[pa])
                        k.MM(o, a_lo[:, j:j + 1].to_broadcast([128, 128]), trib, False, False, r=[a_lo, cstb], w=[pa])
                        k.MM(o, identb, negmb, False, True, r=[cstb], w=[pa])
                        k.ACT(LT[:, j, :], o, AF.Exp, r=[pa, dts], w=[LT], bias=nacs[:, j:j + 1])
                    for g in range(2):
                        k.MM(P[6][:, 128 + g * 128:256 + g * 128], xcT[:, 4 + g, tcol:tcol + 128],
                             xcT[:, 6 + g, tcol:tcol + 128], True, True, r=[xcT], w=[P[6]])
                    k.TT("dve", CBm[:, :, :], P[6][:, 128:384].rearrange("p (g l) -> p g l", g=2),
                         mask01.unsqueeze(1).to_broadcast([128, 2, 128]), ALU.mult, r=[P[6], cst], w=[CBm])
                    for g in range(2):
                        k.TT("dve", MT[:, 4 * g:4 * g + 4, :], LT[:, 4 * g:4 * g + 4, :],
                             CBm[:, g:g + 1, :].to_broadcast([128, 4, 128]), ALU.mult, r=[LT, CBm], w=[MT])
                    for j in range(8):
                        o = P[3][:, j * 64:(j + 1) * 64]
                        k.MM(o, MT[:, j, :], xdt[:, j * 64:(j + 1) * 64], True, False, r=[MT, xdt], w=[P[3]])
                        k.MM(o, Dm[:, j, :], xs_tm[:, j * 64:(j + 1) * 64], False, True, r=[Dm, xs_tm], w=[P[3]])
                    for g in range(2):
                        k.MM(P[7][:, g * 256:(g + 1) * 256], xcT[:, 6 + g, tcol:tcol + 128],
                             STb[:, g * 256:(g + 1) * 256], True, True, r=[xcT, STb], w=[P[7]])
                    k.TT("dve", yoff[:, :].rearrange("p (j q) -> p j q", q=64),
                         P[7][:, :].rearrange("p (j q) -> p j q", q=64),
                         eacs.unsqueeze(2).to_broadcast([128, 8, 64]), ALU.mult, r=[P[7], dts], w=[yoff])
                    k.TT("dve", yo[:, :], P[3][:, :], yoff[:, :], ALU.add, r=[P[3], yoff], w=[yo])
                if SUBCUT == 'C':
                    return
                for g in range(2):
                    k.MM(P[7][:, g * 256:(g + 1) * 256], B_tm[:, g * 128:(g + 1) * 128],
                         xdtd[:, g * 256:(g + 1) * 256], True, True, r=[B_tm, xdtd], w=[P[7]])
                k.TT("dve", ST[:, :].rearrange("p (j q) -> p j q", q=64), ST[:, :].rearrange("p (j q) -> p j q", q=64),
                     eatot[:, :].unsqueeze(2).to_broadcast([128, 8, 64]), ALU.mult, r=[ST, eatot], w=[ST])
                k.TT("dve", ST[:, :], ST[:, :], P[7][:, :], ALU.add, r=[ST, P[7]], w=[ST])
                k.CP("act", STb[:, :], ST[:, :], r=[ST], w=[STb])
                if SUBCUT == 'D':
                    return
                k.TT("dve", ff[:, :], ff[:, :], c01[:, 1, :], ALU.mult, r=[ff, c01], w=[ff])
                k.TT("dve", ff[:, :], ff[:, :], c01[:, 0, :], ALU.add, r=[ff, c01], w=[ff])
                k.ACT(lf[:, :], ff[:, :], AF.Ln, r=[ff], w=[lf])
                k.ACT(kk[:, :], ff[:, :], AF.Identity, r=[ff], w=[kk], bias=1.0, scale=-1.0)
                k.MM(P[4][:, :], tri2i, lf[:, :], True, True, r=[cst, lf], w=[P[4]])
                k.MM(P[5][:, :], tri2r, lf[:, :], True, True, r=[cst, lf], w=[P[5]])
                for h in range(4):
                    k.MM(P[7][:, h * 64:h * 64 + 2], lf[:, h * 128:(h + 1) * 128], chunkind, True, True,
                         r=[lf, cst], w=[P[7]])
                k.ACT(ebt[:, :, :], P[7][:, 0:256].rearrange("p (h c) -> p h c", c=64)[:, :, 0:2], AF.Exp,
                      r=[P[7]], w=[ebt])
                k.ACT(et[0][:, :], P[5][:, :], AF.Exp, r=[P[5]], w=[et[0]])
                k.TT("dve", kdec[:, :], kk[:, :], et[0][:, :], ALU.mult, r=[kk, et[0]], w=[kdec])
                if lat:
                    k.ACT(et[1][:, :], P[4][:, :], AF.Exp, r=[P[4]], w=[et[1]])
                    k.TT("dve", qdec[:, :], qs[:, :], et[1][:, :], ALU.mult, r=[qs, et[1]], w=[qdec])
                    k.ACT(et[0][:, :], P[4][:, :], AF.Exp, r=[P[4]], w=[et[0]], scale=-1.0)
                    k.TT("dve", kinv[:, :], kk[:, :], et[0][:, :], ALU.mult, r=[kk, et[0]], w=[kinv])
                    for h in range(4):
                        k.TR(P0b[:, h * 128:(h + 1) * 128], qdec[:, h * 128:(h + 1) * 128], identb,
                             r=[qdec, cstb], w=[P[0]])
                        k.TR(P0b[:, 512 + h * 128:512 + (h + 1) * 128], kinv[:, h * 128:(h + 1) * 128], identb,
                             r=[kinv, cstb], w=[P[0]])
                    k.CP("act", qdT[:, :, :], P0b[:, 0:512].rearrange("p (h l) -> p h l", h=4), r=[P[0]], w=[qdT])
                    k.CP("act", kiT[:, :, :], P0b[:, 512:1024].rearrange("p (h l) -> p h l", h=4), r=[P[0]], w=[kiT])
                    for h in range(4):
                        k.MM(P[4][:, h * 128:(h + 1) * 128], kiT[:, h, :], qdT[:, h, :], True, True,
                             r=[kiT, qdT], w=[P[4]])
                    k.TT("dve", attm[:, :, :], P[4][:, :].rearrange("p (h l) -> p h l", h=4),
                         mask2.unsqueeze(1).to_broadcast([128, 4, 128]), ALU.mult, r=[P[4], cst], w=[attm])
                if SUBCUT == 'E':
                    return
                for c in range(2):
                    if lat:
                        for h in range(4):
                            o = P[5][0:64, h * 128:(h + 1) * 128]
                            k.MM(o, attm[:, h, c * 64:(c + 1) * 64], vb[:, h * 128:(h + 1) * 128], True, False,
                                 r=[attm, vb], w=[P[5]])
                            k.MM(o, qdT[:, h, c * 64:(c + 1) * 64], SHb[:, h * 128:(h + 1) * 128], False, True,
                                 r=[qdT, SHb], w=[P[5]])
                        k.CP("act", oc[:, c, :], P[5][0:64, :], r=[P[5]], w=[oc])
                    for h in range(4):
                        k.MM(P[7][:, h * 128:(h + 1) * 128], kdec[:, h * 128:(h + 1) * 128],
                             vm[:, c, h * 128:(h + 1) * 128], True, True, r=[kdec, vm], w=[P[7]])
                    for h in range(4):
                        sl = slice(h * 128, (h + 1) * 128)
                        k.STT("dve", SH[:, sl], SH[:, sl], ebt[:, h, c:c + 1], P[7][:, sl], ALU.mult, ALU.add,
                              r=[SH, ebt, P[7]], w=[SH])
                    k.CP("act", SHb[:, :], SH[:, :], r=[SH], w=[SHb])
                if lat:
                    if ti < NH:
                        dst, dstb, rows = ybuf_d, ybuf_b, slice(ti * 128, (ti + 1) * 128)
                    else:
                        dst, dstb = ysend_t[(ti - NH) // 8].ap(), ysend_b
                        rows = slice(((ti - NH) % 8) * 128, ((ti - NH) % 8 + 1) * 128)
                    k.DMA("sp", dst[rows, 0:512], yo[:, :], r=[yo], w=[dstb])
                    k.DMA("sp", dst[rows, 512:1024].rearrange("(c p) v -> p c v", p=64), oc[:, :, :],
                          r=[oc], w=[dstb])

            cut = int(os.environ.get("KCUT", "99"))
            load_x(0)
            load_x(1)
            load_x(2)
            if cut >= 1:
                norm_T(0, hT[0], 0, 1)
                norm_T(1, hT[0], 128, 1)
            if cut >= 2:
                conv_stage(hT[0], 256, 256, [0, 1, 2, 3])
                conv_stage(hT[0], 256, 256, [4, 5, 6, 7])
            if cut >= 3:
                for sub in range(2):
                    scan_tile(hT[0], sub * 128, sub * 128, False, -1, False)
            ntl = NT if stage >= 2 else 4
            ntl = int(os.environ.get("KNT", ntl))
            if cut < 4:
                ntl = 0
            if cut >= 4 and ntl > 1:
                load_x(3)
            for j in range(ntl // 2):
                a_, b_ = 2 * j, 2 * j + 1
                for nx in (a_ + 4, b_ + 4):
                    if nx < ntl + 2:
                        load_x(nx)
                hb = hT[(j + 1) % 2]
                norm_T(a_ + 2, hb, 0, 0)
                norm_T(b_ + 2, hb, 128, 0)
                conv_stage(hb, 256, 64, [0, 1, 2, 3])
                conv_stage(hb, 256, 64, [4, 5, 6, 7])
                scan_tile(hb, 0, 0, True, a_, a_ < NH)
                scan_tile(hb, 128, 128, True, b_, b_ < NH)

            S.barrier()
        if stage < 3:
            with contextlib.ExitStack() as esd:
                tb = S.buf("dbg_b", [128, D], BF16, es=esd)
                tf = S.buf("dbg_f", [128, D], F32, es=esd)
                for ti in range(min(ntl, NH)):
                    rows = slice(ti * 128, (ti + 1) * 128)
                    k.DMA("sp", tb[:, :], ybuf_d[rows, :], r=[ybuf_b], w=[tb])
                    k.CP("dve", tf[:, :], tb[:, :], r=[tb], w=[tf])
                    k.DMA("sp", dbg_d[rows, :], tf[:, :], r=[tf], w=[])
        else:
            for c in range(4):
                if SIM:
                    for hh in range(2):
                        k.DMA("sp", ygath_t[c].ap()[hh * 1024:(hh + 1) * 1024, :], ysend_t[c].ap(), r=[ysend_b], w=[ygath_b])
                else:
                    S.coll((lambda c: lambda e: e.collective_compute(
                        "AllGather", ALU.bypass, replica_groups=PAIRS, ins=[ysend_t[c].ap().opt()],
                        outs=[ygath_t[c].ap().opt()]))(c), r=[ysend_b], w=[ygath_b])
            affall = S.buf("affall", [128, NH, NE], F32)
            pidx = S.buf("pidx", [128, 2 * NH], I32)
            k.DMA("sp", pidx[:, :], pidx_d, r=[], w=[pidx])
            with contextlib.ExitStack() as es4:
                def sb(name, shape, dtype):
                    return S.buf(name, shape, dtype, es=es4)
                wout = sb("wout", [128, 8, D], BF16)
                for kc in range(8):
                    k.DMA("pool", wout[:, kc, :], wout_d[kc * 128:(kc + 1) * 128, :], r=[], w=[wout])
                wr = sb("wr", [128, 8, NE], BF16)
                k.DMA("pool", wr[:, :, :], wr_d.rearrange("(k p) e -> p k e", p=128), r=[], w=[wr])
                snw = sb("snw", [128, 512], F32)
                hnw = sb("hnw", [128, 512], F32)
                k.DMA("sp", snw[:, :], snw_d, r=[], w=[snw])
                k.DMA("sp", hnw[:, :], hnw_d, r=[], w=[hnw])
                yown = [sb(f"yown{i}", [128, D], BF16) for i in range(2)]
                ypar = [sb(f"ypar{i}", [128, D], BF16) for i in range(2)]
                zgt = [sb(f"zgt{i}", [128, D], BF16) for i in range(2)]
                xt2 = [sb(f"xt2{i}", [128, D], F32) for i in range(2)]
                ysum = sb("ysum", [128, D], F32)
                junk4 = sb("junk4", [128, D], BF16)
                ssq6 = sb("ssq6", [128, 8], F32)
                rs6 = sb("rs6", [128, 8], F32)
                tmpo = sb("tmpo", [128, 512], F32)
                ylat = sb("ylat", [128, D], BF16)
                ylT = sb("ylT", [128, 8, 128], BF16)
                ssq2 = sb("ssq2", [128, 4], F32)
                x1 = sb("x1", [128, D], F32)
                h2f = sb("h2f", [128, D], F32)
                h2b = sb("h2b", [128, ROWW], BF16)
                h2T = sb("h2T", [128, 8, 128], BF16)
                smx = sb("smx", [128, 4], F32)
                ex = sb("ex", [128, NE], F32)
                k.MEMSET("pool", h2b[:, 1024:ROWW], 0.0, w=[h2b])

                def p4_load(t):
                    i = t % 2
                    rows = slice(t * 128, (t + 1) * 128)
                    k.DMA("sp", yown[i][:, :], ybuf_d[rows, :], r=[ybuf_b], w=[yown[i]])
                    k.DMA("sp", zgt[i][:, :], zgbuf_d[rows, :], r=[zgbuf_b], w=[zgt[i]])
                    k.DMA("sp", xt2[i][:, :], x_d[rows, :], r=[], w=[xt2[i]])
                    S.dma("pool", lambda e: e.indirect_dma_start(
                        out=ypar[i][:, :], out_offset=None, in_=ygath_t[3 - t // 8].ap(),
                        in_offset=bass.IndirectOffsetOnAxis(ap=pidx[:, t:t + 1], axis=0)),
                        r=[ygath_b, pidx], w=[ypar[i]])

                def rstd_from(ssq_ap, out_ap, n, rbufs):
                    k.ACT(out_ap, ssq_ap, AF.Ln, r=rbufs, w=rbufs, bias=EPS, scale=1.0 / n)
                    k.ACT(out_ap, out_ap, AF.Exp, r=rbufs, w=rbufs, scale=-0.5)

                nt4 = NH if stage >= 4 else 2
                nt4 = int(os.environ.get("KNT4", nt4))
                p4_load(0)
                for t in range(nt4):
                    i = t % 2
                    if t + 1 < nt4:
                        p4_load(t + 1)
                    rows = slice(t * 128, (t + 1) * 128)
                    k.TT("dve", ysum[:, :], yown[i][:, :], ypar[i][:, :], ALU.add, r=[yown[i], ypar[i]], w=[ysum])
                    k.TT("dve", ysum[:, 0:512], ysum[:, 0:512], zgt[i][:, 0:512], ALU.mult, r=[ysum, zgt[i]], w=[ysum])
                    k.MEMSET("dve", ssq6[:, :], 0.0, w=[ssq6])
                    for g in range(2):
                        k.ACT(junk4[:, g * 256:(g + 1) * 256], ysum[:, g * 256:(g + 1) * 256], AF.Square,
                              r=[ysum], w=[junk4, ssq6], accum=ssq6[:, g:g + 1])
                    for h in range(4):
                        sl = slice(512 + h * 128, 512 + (h + 1) * 128)
                        k.ACT(junk4[:, sl], ysum[:, sl], AF.Square, r=[ysum], w=[junk4, ssq6],
                              accum=ssq6[:, 2 + h:3 + h])
                    rstd_from(ssq6[:, 0:2], rs6[:, 0:2], 256, [ssq6, rs6])
                    rstd_from(ssq6[:, 2:6], rs6[:, 2:6], 128, [ssq6, rs6])
                    for g in range(2):
                        sl = slice(g * 256, (g + 1) * 256)
                        k.STT("dve", ylat[:, sl], ysum[:, sl], rs6[:, g:g + 1], snw[:, sl], ALU.mult, ALU.mult,
                              r=[ysum, rs6, snw], w=[ylat])
                    for h in range(4):
                        sl = slice(h * 128, (h + 1) * 128)
                        sl2 = slice(512 + h * 128, 512 + (h + 1) * 128)
                        k.STT("dve", tmpo[:, sl], ysum[:, sl2], rs6[:, 2 + h:3 + h], hnw[:, sl], ALU.mult, ALU.mult,
                              r=[ysum, rs6, hnw], w=[tmpo])
                    k.TT("dve", ylat[:, 512:1024], tmpo[:, :], zgt[i][:, 512:1024], ALU.mult,
                         r=[tmpo, zgt[i]], w=[ylat])
                    for kc in range(8):
                        k.TR(P0b[:, kc * 128:(kc + 1) * 128], ylat[:, kc * 128:(kc + 1) * 128], identb,
                             r=[ylat, cstb], w=[P[0]])
                    k.CP("act", ylT[:, 0:4, :], P0b[:, 0:512].rearrange("p (k l) -> p k l", k=4), r=[P[0]], w=[ylT])
                    k.CP("act", ylT[:, 4:8, :], P0b[:, 512:1024].rearrange("p (k l) -> p k l", k=4), r=[P[0]], w=[ylT])
                    for n in range(2):
                        for kc in range(8):
                            k.MM(P[1 + n][:, :], ylT[:, kc, :], wout[:, kc, n * 512:(n + 1) * 512], kc == 0, kc == 7,
                                 r=[ylT, wout], w=[P[1 + n]])
                    k.MEMSET("dve", ssq2[:, :], 0.0, w=[ssq2])
                    for n in range(2):
                        k.ACT(junk4[:, n * 512:(n + 1) * 512], P[1 + n][:, :], AF.Square, r=[P[1 + n]],
                              w=[junk4, ssq2], accum=ssq2[:, n:n + 1])
                    k.TT("dve", ssq2[:, 2:3], ssq2[:, 0:1], ssq2[:, 1:2], ALU.add, r=[ssq2], w=[ssq2])
                    rstd_from(ssq2[:, 2:3], ssq2[:, 3:4], D, [ssq2])
                    for n in range(2):
                        sl = slice(n * 512, (n + 1) * 512)
                        k.STT("dve", x1[:, sl], P[1 + n][:, :], ssq2[:, 3:4], G1[:, sl], ALU.mult, ALU.mult,
                              r=[P[1 + n], ssq2, modrep], w=[x1])
                    k.TT("dve", x1[:, :], x1[:, :], xt2[i][:, :], ALU.add, r=[x1, xt2[i]], w=[x1])
                    k.DMA("sp", x1buf_d[rows, :], x1[:, :], r=[x1], w=[x1buf_b])
                    k.MEMSET("dve", ssq2[:, 0:1], 0.0, w=[ssq2])
                    k.ACT(junk4[:, :], x1[:, :], AF.Square, r=[x1], w=[junk4, ssq2], accum=ssq2[:, 0:1])
                    rstd_from(ssq2[:, 0:1], ssq2[:, 1:2], D, [ssq2])
                    k.STT("dve", h2f[:, :], x1[:, :], ssq2[:, 1:2], A2, ALU.mult, ALU.mult, r=[x1, ssq2, modrep], w=[h2f])
                    k.TT("dve", h2b[:, 0:D], h2f[:, :], B2, ALU.add, r=[h2f, modrep], w=[h2b])
                    k.CP("dve", h2b[:, 1026:1028].bitcast(I32), pidx[:, NH + t:NH + t + 1], r=[pidx], w=[h2b])
                    k.DMA("sp", h2buf_d[rows, :], h2b[:, :], r=[h2b], w=[h2buf_b])
                    for kc in range(8):
                        k.TR(P0b[:, kc * 128:(kc + 1) * 128], h2b[:, kc * 128:(kc + 1) * 128], identb,
                             r=[h2b, cstb], w=[P[0]])
                    k.CP("act", h2T[:, :, :], P0b[:, :].rearrange("p (k l) -> p k l", k=8), r=[P[0]], w=[h2T])
                    for kc in range(8):
                        k.MM(P[6][:, 0:NE], h2T[:, kc, :], wr[:, kc, :], kc == 0, kc == 7, r=[h2T, wr], w=[P[6]])
                    S.op("dve", lambda e: e.tensor_reduce(out=smx[:, 0:1], in_=P[6][:, 0:NE], axis=AX.X, op=ALU.max),
                         r=[P[6]], w=[smx])
                    k.TS("dve", smx[:, 1:2], smx[:, 0:1], -1.0, None, ALU.mult, None, r=[smx], w=[smx])
                    k.MEMSET("dve", smx[:, 2:3], 0.0, w=[smx])
                    k.ACT(ex[:, :], P[6][:, 0:NE], AF.Exp, r=[P[6], smx], w=[ex, smx], bias=smx[:, 1:2],
                          accum=smx[:, 2:3])
                    S.op("dve", lambda e: e.reciprocal(out=smx[:, 3:4], in_=smx[:, 2:3]), r=[smx], w=[smx])
                    k.TS("dve", affall[:, t, :], ex[:, :], smx[:, 3:4], None, ALU.mult, None, r=[ex, smx], w=[affall])
                S.barrier()
            if stage < 5:
                with contextlib.ExitStack() as esd:
                    tf = S.buf("dbg_f", [128, D], F32, es=esd)
                    for t in range(nt4):
                        rows = slice(t * 128, (t + 1) * 128)
                        k.DMA("sp", tf[:, :], x1buf_d[rows, :], r=[x1buf_b], w=[tf])
                        k.DMA("sp", dbg_d[rows, :], tf[:, :], r=[tf], w=[])
                    k.DMA("sp", dbg_d[HALF:HALF + 128, 0:NH * NE], affall[:, :, :].rearrange("p t e -> p (t e)"),
                          r=[affall], w=[])
        if stage >= 5:
            k.DMA("sp", affs_t.ap().rearrange("(t p) e -> p t e", p=128), affall[:, :, :], r=[affall], w=[affs_b])
            if SIM:
                for hh in range(2):
                    k.DMA("sp", affg_t.ap()[hh * HALF:(hh + 1) * HALF, :], affs_t.ap(), r=[affs_b], w=[affg_b])
            else:
                S.coll(lambda e: e.collective_compute("AllGather", ALU.bypass, replica_groups=PAIRS,
                                                      ins=[affs_t.ap().opt()], outs=[affg_t.ap().opt()]),
                       r=[affs_b], w=[affg_b])
            tau = S.buf("tau", [128, NE], F32)
            gate = S.buf("gate", [128, NH, NE], F32)
            sloti = S.buf("sloti", [128, NH, NE], I32)
            with contextlib.ExitStack() as es6:
                def sb(name, shape, dtype):
                    return S.buf(name, shape, dtype, es=es6)
                affb = sb("affb", [128, NT, NE], F32)
                k.DMA("sp", affb[:, :, :], affg_t.ap().rearrange("(t p) e -> p t e", p=128), r=[affg_b], w=[affb])
                cmpb = sb("cmpb", [128, NT, NE], F32)
                hi = sb("hi", [128, NE], F32)
                d2 = sb("d2", [128, NE], F32)
                mid = sb("mid", [128, NE], F32)
                cntp = sb("cntp", [128, NE], F32)
                ge = sb("ge", [128, NE], F32)
                k.MEMSET("pool", tau[:, :], 0.0, w=[tau])
                k.MEMSET("pool", hi[:, :], 1.0001, w=[hi])
                ones_f = cst[:, 7, :]
                for it in range(32):
                    k.TT("dve", d2[:, :], hi[:, :], tau[:, :], ALU.subtract, r=[hi, tau], w=[d2])
                    k.STT("dve", mid[:, :], d2[:, :], 0.5, tau[:, :], ALU.mult, ALU.add, r=[d2, tau], w=[mid])
                    k.TT("dve", cmpb[:, :, :], affb[:, :, :], mid[:, :].unsqueeze(1).to_broadcast([128, NT, NE]),
                         ALU.is_ge, r=[affb, mid], w=[cmpb])
                    S.op("dve", lambda e: e.tensor_reduce(out=cntp[:, :], in_=cmpb[:, :, :].rearrange("p t e -> p e t"),
                                                          axis=AX.X, op=ALU.add), r=[cmpb], w=[cntp])
                    k.MM(P[6][:, 0:NE], ones_f, cntp[:, :], True, True, r=[cst, cntp], w=[P[6]])
                    k.TS("dve", ge[:, :], P[6][:, 0:NE], float(CAP), None, ALU.is_ge, None, r=[P[6]], w=[ge])
                    k.TT("dve", ge[:, :], ge[:, :], d2[:, :], ALU.mult, r=[ge, d2], w=[ge])
                    k.STT("dve", tau[:, :], ge[:, :], 0.5, tau[:, :], ALU.mult, ALU.add, r=[ge, tau], w=[tau])
                    k.STT("dve", hi[:, :], d2[:, :], -0.5, hi[:, :], ALU.mult, ALU.add, r=[d2, hi], w=[hi])
                    k.STT("dve", hi[:, :], ge[:, :], 0.5, hi[:, :], ALU.mult, ALU.add, r=[ge, hi], w=[hi])
                maskf = sb("maskf", [128, NH, NE], F32)
                maskb = sb("maskb", [128, NH * NE], BF16)
                totE = sb("totE", [128, NE, NH], F32)
                cumE = sb("cumE", [128, NE, NH], F32)
                ones32 = sb("ones32", [128, NH], F32)
                posf = sb("posf", [128, NH, NE], F32)
                k.TT("dve", maskf[:, :, :], affall[:, :, :], tau[:, :].unsqueeze(1).to_broadcast([128, NH, NE]),
                     ALU.is_ge, r=[affall, tau], w=[maskf])
                k.TT("dve", gate[:, :, :], maskf[:, :, :], affall[:, :, :], ALU.mult, r=[maskf, affall], w=[gate])
                k.CP("dve", maskb[:, :], maskf[:, :, :].rearrange("p t e -> p (t e)"), r=[maskf], w=[maskb])
                k.MM(P[1][:, :], cstb[:, 8, :], maskb[:, :], True, True, r=[cstb, maskb], w=[P[1]])
                k.MM(P[2][:, :], onesb, maskb[:, :], True, True, r=[cstb, maskb], w=[P[2]])
                k.CP("dve", totE[:, :, :], P[2][:, :].rearrange("p (t e) -> p e t", e=NE), r=[P[2]], w=[totE])
                k.MEMSET("pool", ones32[:, :], 1.0, w=[ones32])
                for e_ in range(NE):
                    S.op("dve", (lambda e_: lambda eng: eng.tensor_tensor_scan(
                        out=cumE[:, e_, :], data0=ones32[:, :], data1=totE[:, e_, :], initial=0.0,
                        op0=ALU.mult, op1=ALU.add))(e_), r=[ones32, totE], w=[cumE])
                k.TT("dve", cumE[:, :, :], cumE[:, :, :], totE[:, :, :], ALU.subtract, r=[cumE, totE], w=[cumE])
                k.TT("dve", posf[:, :, :], P[1][:, :].rearrange("p (t e) -> p t e", e=NE),
                     cumE[:, :, :].rearrange("p e t -> p t e"), ALU.add, r=[P[1], cumE], w=[posf])
                ltm = sb("ltm", [128, NH, NE], F32)
                k.TS("dve", ltm[:, :, :], posf[:, :, :], float(CAP), None, ALU.is_lt, None, r=[posf], w=[ltm])
                k.TT("dve", maskf[:, :, :], maskf[:, :, :], ltm[:, :, :], ALU.mult, r=[maskf, ltm], w=[maskf])
                k.TT("dve", posf[:, :, :], posf[:, :, :], cst[:, 9, 2:2 + NE].unsqueeze(1).to_broadcast([128, NH, NE]),
                     ALU.add, r=[posf, cst], w=[posf])
                k.TT("dve", posf[:, :, :], posf[:, :, :], maskf[:, :, :], ALU.mult, r=[posf, maskf], w=[posf])
                k.TS("dve", maskf[:, :, :], maskf[:, :, :], -1.0, 1.0, ALU.mult, ALU.add, r=[maskf], w=[maskf])
                k.TS("dve", maskf[:, :, :], maskf[:, :, :], cst[:, 9, 18:19], None, ALU.mult, None, r=[maskf, cst], w=[maskf])
                k.TT("dve", posf[:, :, :], posf[:, :, :], maskf[:, :, :], ALU.add, r=[posf, maskf], w=[posf])
                k.CP("dve", sloti[:, :, :], posf[:, :, :], r=[posf], w=[sloti])
                S.barrier()
            if stage < 7:
                with contextlib.ExitStack() as esd:
                    tf = S.buf("dbg_g", [128, NH * NE], F32, es=esd)
                    k.DMA("sp", dbg_d[HALF + 128:HALF + 256, 0:NE], tau[:, :], r=[tau], w=[])
                    k.DMA("sp", dbg_d[HALF + 256:HALF + 384, 0:NH * NE], gate[:, :, :].rearrange("p t e -> p (t e)"),
                          r=[gate], w=[])
                    k.CP("dve", tf[:, :], sloti[:, :, :].rearrange("p t e -> p (t e)"), r=[sloti], w=[tf])
                    k.DMA("sp", dbg_d[HALF + 384:HALF + 512, 0:NH * NE], tf[:, :], r=[tf], w=[])
        if stage >= 7:
            with contextlib.ExitStack() as es7:
                def sb(name, shape, dtype):
                    return S.buf(name, shape, dtype)
                wgb = sb("wgb", [128, 8, D], BF16)
                wub = sb("wub", [128, 8, D], BF16)
                wdb = sb("wdb", [128, 8, D], BF16)
                hd = [sb(f"hd{i}", [128, ROWW], BF16) for i in range(6)]
                xs_in = [sb(f"xs_in{i}", [128, ROWW], BF16) for i in range(2)]
                xinT = sb("xinT", [128, 8, CAP], BF16)
                hid = sb("hid", [128, 8, CAP], BF16)
                gall = sb("gall", [128, 8, 8], F32)
                iall = sb("iall", [128, 8, 8], I32)
                sg = [sb(f"sg{i}", [128, 512], F32) for i in range(2)]
                yt = [sb(f"yt{i}", [128, D], F32) for i in range(2)]

                def load_w(e_):
                    for (wb, wd_) in ((wgb, wg_d), (wub, wu_d), (wdb, wd_d)):
                        k.DMA("pool", wb[:, :, :], wd_[e_].rearrange("(k p) n -> p k n", p=128), r=[], w=[wb])

                def dispatch(e_):
                    for t in range(NH):
                        hb_ = hd[(e_ * NH + t) % 6]
                        k.DMA("sp", hb_[:, :], h2buf_d[t * 128:(t + 1) * 128, :], r=[h2buf_b], w=[hb_])
                        k.CP("dve", hb_[:, 1024:1026].bitcast(F32), gate[:, t, e_:e_ + 1], r=[gate], w=[hb_])
                        S.dma("pool", (lambda hb_, t, e_: lambda eng: eng.indirect_dma_start(
                            out=xin_flat, out_offset=bass.IndirectOffsetOnAxis(ap=sloti[:, t, e_:e_ + 1], axis=0),
                            in_=hb_[:, :], in_offset=None))(hb_, t, e_),
                            r=[hb_, sloti, xin_e[e_]], w=[])

                nexp = NE if stage >= 8 else 2
                load_w(0)
                dispatch(0)
                pbi = 0
                ybi = 0
                for e_ in range(nexp):
                    for sc in range(8):
                        xb_ = xs_in[sc % 2]
                        if sc == 0:
                            k.DMA("sp", xb_[:, :], xin_d[e_, sc * 128:(sc + 1) * 128, :], r=[], w=[xb_, xin_e[e_]])
                        else:
                            k.DMA("sp", xb_[:, :], xin_d[e_, sc * 128:(sc + 1) * 128, :], r=[xin_e[e_]], w=[xb_])
                        k.CP("dve", gall[:, sc, 0:1], xb_[:, 1024:1026].bitcast(F32), r=[xb_], w=[gall])
                        k.CP("dve", iall[:, sc, 0:1], xb_[:, 1026:1028].bitcast(I32), r=[xb_], w=[iall])
                        for kc in range(8):
                            k.TR(P0b[:, kc * 128:(kc + 1) * 128], xb_[:, kc * 128:(kc + 1) * 128], identb,
                                 r=[xb_, cstb], w=[P[0]])
                        k.CP("act", xinT[:, :, sc * 128:(sc + 1) * 128], P0b[:, :].rearrange("p (k l) -> p k l", k=8),
                             r=[P[0]], w=[xinT])
                    if e_ + 1 < nexp:
                        dispatch(e_ + 1)
                    for fc in range(8):
                        for half in range(2):
                            pg, pu = P[1 + 2 * (pbi % 3)], P[2 + 2 * (pbi % 3)]
                            pbi += 1
                            for kc in range(8):
                                k.MM(pg[:, :], wgb[:, kc, fc * 128:(fc + 1) * 128], xinT[:, kc, half * 512:(half + 1) * 512],
                                     kc == 0, kc == 7, r=[wgb, xinT], w=[pg])
                            for kc in range(8):
                                k.MM(pu[:, :], wub[:, kc, fc * 128:(fc + 1) * 128], xinT[:, kc, half * 512:(half + 1) * 512],
                                     kc == 0, kc == 7, r=[wub, xinT], w=[pu])
                            sgb = sg[(fc * 2 + half) % 2]
                            k.ACT(sgb[:, :], pg[:, :], AF.Silu, r=[pg], w=[sgb])
                            k.TT("dve", hid[:, fc, half * 512:(half + 1) * 512], sgb[:, :], pu[:, :], ALU.mult,
                                 r=[sgb, pu], w=[hid])
                    if e_ + 1 < nexp:
                        for (wb, wd_) in ((wgb, wg_d), (wub, wu_d)):
                            k.DMA("pool", wb[:, :, :], wd_[e_ + 1].rearrange("(k p) n -> p k n", p=128), r=[], w=[wb])
                    for sc in range(8):
                        ytb = yt[sc % 2]
                        for n in range(2):
                            py = P[7] if (ybi % 2 == 0) else P[0]
                            ybi += 1
                            for fc in range(8):
                                k.MM(py[:, :], hid[:, fc, sc * 128:(sc + 1) * 128], wdb[:, fc, n * 512:(n + 1) * 512],
                                     fc == 0, fc == 7, r=[hid, wdb], w=[py])
                            k.ACT(ytb[:, n * 512:(n + 1) * 512], py[:, :], AF.Identity, r=[py, gall], w=[ytb],
                                  scale=gall[:, sc, 0:1])
                        S.dma("pool", (lambda ytb, sc: lambda eng: eng.indirect_dma_start(
                            out=acc_d, out_offset=bass.IndirectOffsetOnAxis(ap=iall[:, sc, 0:1], axis=0),
                            in_=ytb[:, :], in_offset=None, compute_op=ALU.add))(ytb, sc),
                            r=[ytb, iall], w=[acc_b])
                    if e_ + 1 < nexp:
                        k.DMA("pool", wdb[:, :, :], wd_d[e_ + 1].rearrange("(k p) n -> p k n", p=128), r=[], w=[wdb])
                S.barrier()
            with contextlib.ExitStack() as es8:
                def sb(name, shape, dtype):
                    return S.buf(name, shape, dtype)
                at = [sb(f"at{i}", [128, D], F32) for i in range(2)]
                x1t = [sb(f"x1t{i}", [128, D], F32) for i in range(2)]
                ot = [sb(f"ot{i}", [128, D], F32) for i in range(2)]
                junk8 = sb("junk8", [128, D], BF16)
                s8 = sb("s8", [128, 8, 8], F32)
                for t in range(NH):
                    i = t % 2
                    rows = slice(t * 128, (t + 1) * 128)
                    k.DMA("sp", at[i][:, :], acc_d[rows, :], r=[acc_b], w=[at[i]])
                    k.DMA("sp", x1t[i][:, :], x1buf_d[rows, :], r=[x1buf_b], w=[x1t[i]])
                    k.MEMSET("dve", s8[:, 0, 0:1], 0.0, w=[s8])
                    k.ACT(junk8[:, :], at[i][:, :], AF.Square, r=[at[i]], w=[junk8, s8], accum=s8[:, 0, 0:1])
                    k.ACT(s8[:, 1, 0:1], s8[:, 0, 0:1], AF.Ln, r=[s8], w=[s8], bias=EPS, scale=1.0 / D)
                    k.ACT(s8[:, 2, 0:1], s8[:, 1, 0:1], AF.Exp, r=[s8], w=[s8], scale=-0.5)
                    k.STT("dve", ot[i][:, :], at[i][:, :], s8[:, 2, 0:1], G2, ALU.mult, ALU.mult,
                          r=[at[i], s8, modrep], w=[ot[i]])
                    k.TT("dve", ot[i][:, :], ot[i][:, :], x1t[i][:, :], ALU.add, r=[ot[i], x1t[i]], w=[ot[i]])
                    k.DMA("sp", out_d[rows, :], ot[i][:, :], r=[ot[i]], w=[])
        S.wait_all_dma("sp")
        S.emit()
    return nc


def make_consts():
    c = np.zeros((128, NCONST, 128), np.float32)
    i = np.arange(128)
    r, cc = i[:, None], i[None, :]
    same = (r // 64) == (cc // 64)
    c[:, 0] = (r == cc)
    c[:, 1] = (r <= cc)
    c[:, 2] = np.where(r > cc, -30000.0, 0.0)
    c[:, 3] = (r <= cc)
    c[:, 4] = same & (r <= cc)
    c[:, 5] = same & (r > cc)
    c[:, 6] = same & (r <= cc)
    c[:, 7] = 1.0
    c[:, 8] = (r < cc)
    c[:, 9, 0] = (i < 64)
    c[:, 9, 1] = (i >= 64)
    c[:, 9, 2:2 + NE] = np.arange(NE)[None, :] * CAP
    c[:, 9, 18] = NE * CAP + i
    return c


def rep(v, n=128):
    return np.ascontiguousarray(np.broadcast_to(np.asarray(v, np.float32)[None], (n,) + tuple(np.shape(v))))


def fm(v):
    return np.ascontiguousarray(np.asarray(v, np.float32).reshape(-1, 128).T)


def prep_inputs(inp):
    x, c, ctx, c_ctx = inp["x"], inp["c"], inp["ctx"], inp["c_ctx"]
    w_in = inp["w_in"][0]
    consts = make_consts()
    shared = {
        "ada_w": np.ascontiguousarray(inp["ada_w"][0]),
        "ada_bT": np.ascontiguousarray(inp["ada_b"][0][:2048].reshape(16, 128).T),
        "ada_brep": rep(inp["ada_b"][0][2048:]),
        "nw0T": fm(inp["norm_w"][0, 0]),
        "nwrep": rep(inp["norm_w"][0, 1:4]),
        "cb": fm(inp["ssd_conv_b"][0]),
        "snw": rep(inp["ssd_norm_w"][0]),
        "hnw": rep(inp["hgrn_norm_w"][0]),
        "w_out": np.ascontiguousarray(inp["w_out"][0]),
        "w_router": np.ascontiguousarray(inp["w_router"][0]),
        "w_gate": np.ascontiguousarray(inp["w_gate"][0]),
        "w_up": np.ascontiguousarray(inp["w_up"][0]),
        "w_down": np.ascontiguousarray(inp["w_down"][0]),
        "consts": consts,
    }
    maps = []
    for core in range(8):
        b, d = core // 2, core % 2
        m = dict(shared)
        m["xs"] = np.ascontiguousarray(x[b][::-1] if d else x[b])
        m["ctxs"] = np.ascontiguousarray(ctx[b][::-1] if d else ctx[b])
        cv = np.stack([fm(c[b]), fm(c_ctx)], axis=-1)
        m["cvec"] = np.ascontiguousarray(cv)
        cols = [w_in[:, 512:1536], w_in[:, 1552:2064], w_in[:, 2064 + 512 * d:2576 + 512 * d], w_in[:, 3088:3600],
                w_in[:, 1536 + 8 * d:1544 + 8 * d], w_in[:, 0:512], w_in[:, 3600:4112]]
        m["wmain"] = np.ascontiguousarray(np.concatenate(cols, axis=1))
        cwk = inp["ssd_conv_w"][0]
        if d:
            cwk = cwk[::-1]
        m["cw"] = np.ascontiguousarray(cwk.T.reshape(8, 128, 5).transpose(1, 0, 2))
        m["sp8"] = rep(np.stack([inp["ssd_dt_bias"][0, d], inp["ssd_a_log"][0, d], inp["ssd_d"][0]]))
        m["lbrep"] = rep(np.stack([inp["hgrn_lb"][0, d], inp["hgrn_lb"][1, d]]))
        p = np.arange(128)[:, None]
        t = np.arange(NH)[None, :]
        m["pidx"] = np.ascontiguousarray(np.concatenate(
            [(1 - d) * 1024 + ((HALF - 1) - (t * 128 + p)) % 1024, t * 128 + p], axis=1).astype(np.int32))
        maps.append(m)
    return maps


STAGE = int(os.environ.get("KSTAGE", "9"))
LITE = int(os.environ.get("KLITE", "0"))
SIM = int(os.environ.get("KSIM", "0"))
SAME_ENGINE_SYNC = bool(int(os.environ.get("KSES", "1")))
SUBCUT = os.environ.get("KSUB", "")
_CACHE = {}


def kernel(**inputs):
    inp = {k_: np.asarray(v) for k_, v in inputs.items()}
    maps = prep_inputs(inp)
    if LITE:
        for m in maps:
            m["ada_w"] = m["ada_w"][:8]
            m["xs"] = m["xs"][:1024]
    if STAGE < 7:
        for m in maps:
            for kk_ in ("w_gate", "w_up", "w_down"):
                m.pop(kk_)
    if STAGE not in _CACHE:
        _CACHE[STAGE] = build_program(STAGE)
    nc = _CACHE[STAGE]
    res = run_bass_kernel_spmd(nc, maps, core_ids=list(range(8)))
    if STAGE < 9:
        return [r["dbg"] for r in res.results]
    out = np.empty((4, SEQ, D), np.float32)
    for core in range(8):
        b, d = core // 2, core % 2
        o = res.results[core]["out"]
        if d:
            out[b, HALF:] = o[::-1]
        else:
            out[b, :HALF] = o
    return out
```
